# Optimizing a Trainium2 kernel written in Bass

```python
import math
import numpy as np
import jax
import jax.numpy as jnp
from jax import lax

D_MODEL = 1024
BATCH = 8
SEQ = 4096
DEPTH = 1

MLA_HEADS = 8
MLA_NOPE = 64
MLA_ROPE = 32
MLA_V = 64
MLA_Q_LORA = 384
MLA_KV_LORA = 256
ROPE_THETA = 10000.0
Q_BLOCK = 128

DIL_HEADS = 8
DIL_HEAD_DIM = 64
DIL_PATTERNS = ((128, 1), (512, 4), (2048, 16))

REL_BUCKETS = 32
REL_MAX_EXACT = 8
REL_MAX_DIST = 1024

N_EXPERTS = 16
EC_CAPACITY_FACTOR = 2
D_FF_EXPERT = 1536

MLA_WIDTH = MLA_HEADS * MLA_V
DIL_WIDTH = DIL_HEADS * DIL_HEAD_DIM
MIX_WIDTH = MLA_WIDTH + DIL_WIDTH
IN_WIDTH = MLA_Q_LORA + MLA_KV_LORA + MLA_ROPE + 3 * DIL_WIDTH
DEEPNORM_ALPHA = (2 * DEPTH) ** 0.25
DEEPNORM_BETA = (8 * DEPTH) ** -0.25
NORM_EPS = 1e-6
NEG_INF = -1e30

kernel_name = "hybrid_mla_dilated_ec_moe_deepnorm_adaln"


def layer_norm(x, g, b):
    xf = x.astype(jnp.float32)
    mu = jnp.mean(xf, axis=-1, keepdims=True)
    var = jnp.mean(jnp.square(xf - mu), axis=-1, keepdims=True)
    y = (xf - mu) * lax.rsqrt(var + NORM_EPS) * g.astype(jnp.float32) + b.astype(jnp.float32)
    return y.astype(x.dtype)


def rms_norm(x, g):
    xf = x.astype(jnp.float32)
    y = xf * lax.rsqrt(jnp.mean(jnp.square(xf), axis=-1, keepdims=True) + NORM_EPS)
    return (y * g.astype(jnp.float32)).astype(x.dtype)


def rope(t, pos):
    r = t.shape[-1]
    inv = ROPE_THETA ** (-jnp.arange(0, r, 2, dtype=jnp.float32) / r)
    ang = pos.astype(jnp.float32)[:, None] * inv[None, :]
    cos = jnp.cos(ang)[None, :, None, :]
    sin = jnp.sin(ang)[None, :, None, :]
    tf = t.astype(jnp.float32)
    t1, t2 = tf[..., : r // 2], tf[..., r // 2:]
    return jnp.concatenate([t1 * cos - t2 * sin, t1 * sin + t2 * cos], axis=-1).astype(t.dtype)


def t5_bucket(rel):
    half = REL_BUCKETS // 2
    ret = (rel > 0).astype(np.int32) * half
    n = np.abs(rel)
    large = REL_MAX_EXACT + (np.log(np.maximum(n, 1) / REL_MAX_EXACT)
                             / np.log(REL_MAX_DIST / REL_MAX_EXACT)
                             * (half - REL_MAX_EXACT)).astype(np.int32)
    large = np.minimum(large, half - 1)
    return ret + np.where(n < REL_MAX_EXACT, n, large).astype(np.int32)


def mla_mixer(c_q, c_kv, k_rope, q_norm_g, w_uq, kv_norm_g, w_ukv):
    b, s, _ = c_q.shape
    dq = MLA_NOPE + MLA_ROPE
    pos = jnp.arange(s)
    q = (rms_norm(c_q, q_norm_g) @ w_uq).reshape(b, s, MLA_HEADS, dq)
    q = jnp.concatenate([q[..., :MLA_NOPE], rope(q[..., MLA_NOPE:], pos)], axis=-1)
    kv = (rms_norm(c_kv, kv_norm_g) @ w_ukv).reshape(b, s, MLA_HEADS, MLA_NOPE + MLA_V)
    kr = jnp.broadcast_to(rope(k_rope[:, :, None, :], pos), (b, s, MLA_HEADS, MLA_ROPE))
    k = jnp.concatenate([kv[..., :MLA_NOPE], kr], axis=-1)
    v = kv[..., MLA_NOPE:]
    scale = dq ** -0.5
    nq = s // Q_BLOCK
    qb = jnp.moveaxis(q.reshape(b, nq, Q_BLOCK, MLA_HEADS, dq), 1, 0)

    def block(q_blk):
        sc = jnp.einsum('bqhd,bkhd->bhqk', q_blk, k).astype(jnp.float32) * scale
        p = jax.nn.softmax(sc, axis=-1).astype(v.dtype)
        return jnp.einsum('bhqk,bkhd->bqhd', p, v)

    o = lax.map(block, qb)
    return jnp.moveaxis(o, 0, 1).reshape(b, s, MLA_WIDTH)


def banded_attention(q, k, v, bias, half):
    b, g, l, h, dh = q.shape
    nb = -(-l // half)
    lp = nb * half
    qp = jnp.pad(q, ((0, 0), (0, 0), (0, lp - l), (0, 0), (0, 0))).reshape(b, g, nb, half, h, dh)
    pad_kv = ((0, 0), (0, 0), (half, lp - l + half), (0, 0), (0, 0))
    kp = jnp.pad(k, pad_kv)
    vp = jnp.pad(v, pad_kv)

    def band(t):
        return jnp.concatenate(
            [t[:, :, i * half: i * half + lp].reshape(b, g, nb, half, h, dh) for i in range(3)], axis=3)

    kb, vb = band(kp), band(vp)
    rel = np.arange(3 * half)[None, :] - half - np.arange(half)[:, None]
    key_pos = np.arange(nb)[:, None] * half + np.arange(3 * half)[None, :] - half
    mask = (np.abs(rel) <= half)[None] & ((key_pos >= 0) & (key_pos < l))[:, None, :]
    sc = jnp.einsum('bgnqhd,bgnkhd->bgnhqk', qp, kb).astype(jnp.float32) * (dh ** -0.5)
    sc = sc + jnp.transpose(bias, (2, 0, 1))[None, None, None]
    sc = jnp.where(mask[None, None, :, None], sc, NEG_INF)
    m = jnp.max(sc, axis=-1, keepdims=True)
    e = jnp.exp(sc - m)
    den = jnp.sum(e, axis=-1)
    o = jnp.einsum('bgnhqk,bgnkhd->bgnqhd', e, vb.astype(jnp.float32))
    o = o / jnp.swapaxes(den, -1, -2)[..., None]
    lse = jnp.swapaxes(m[..., 0] + jnp.log(den), -1, -2)
    o = o.reshape(b, g, lp, h, dh)[:, :, :l]
    lse = lse.reshape(b, g, lp, h)[:, :, :l]
    return o, lse


def dilated_mixer(q, k, v, rel_bias):
    b, s, h, dh = q.shape
    outs, lses = [], []
    for window, dil in DIL_PATTERNS:
        half = window // 2 // dil
        l = s // dil

        def perm(t):
            return t.reshape(b, l, dil, h, dh).transpose(0, 2, 1, 3, 4)

        rel = np.arange(3 * half)[None, :] - half - np.arange(half)[:, None]
        bias = rel_bias[jnp.asarray(t5_bucket(rel * dil))].astype(jnp.float32)
        o, lse = banded_attention(perm(q), perm(k), perm(v), bias, half)
        outs.append(o.transpose(0, 2, 1, 3, 4).reshape(b, s, h, dh))
        lses.append(lse.transpose(0, 2, 1, 3).reshape(b, s, h))
    w = jax.nn.softmax(jnp.stack(lses, axis=0), axis=0)
    o = jnp.sum(w[..., None] * jnp.stack(outs, axis=0), axis=0)
    return o.reshape(b, s, h * dh).astype(q.dtype)


def expert_choice_ffn(h, w_router, w_gate, w_up, w_down):
    b, s, d = h.shape
    cap = max(1, EC_CAPACITY_FACTOR * s // N_EXPERTS)
    aff = jax.nn.softmax((h @ w_router).astype(jnp.float32), axis=-1)
    vals, idx = lax.top_k(jnp.swapaxes(aff, 1, 2), cap)
    bidx = jnp.arange(b)[:, None, None]
    xin = h[bidx, idx]
    gt = jnp.einsum('becd,edf->becf', xin, w_gate)
    up = jnp.einsum('becd,edf->becf', xin, w_up)
    y = jnp.einsum('becf,efd->becd', jax.nn.silu(gt) * up, w_down)
    y = y * vals[..., None].astype(h.dtype)
    return jnp.zeros_like(h).at[bidx, idx].add(y)


def setup_inputs(seed: int = 0) -> dict:
    key = jax.random.key(seed)
    ks = jax.random.split(key, 20)
    f32 = jnp.float32
    L, D = DEPTH, D_MODEL

    def nrm(k, shape, scale):
        return jax.random.normal(k, shape, f32) * scale

    return {
        "x": nrm(ks[0], (BATCH, SEQ, D), 1.0),
        "c": nrm(ks[1], (BATCH, D), 1.0),
        "w_ada": nrm(ks[2], (L, D, 6 * D), 0.5 * D ** -0.5),
        "b_ada": nrm(ks[3], (L, 6 * D), 0.02),
        "w_in": nrm(ks[4], (L, D, IN_WIDTH), D ** -0.5),
        "q_norm_g": 1.0 + nrm(ks[5], (L, MLA_Q_LORA), 0.02),
        "w_uq": nrm(ks[6], (L, MLA_Q_LORA, MLA_HEADS * (MLA_NOPE + MLA_ROPE)), MLA_Q_LORA ** -0.5),
        "kv_norm_g": 1.0 + nrm(ks[7], (L, MLA_KV_LORA), 0.02),
        "w_ukv": nrm(ks[8], (L, MLA_KV_LORA, MLA_HEADS * (MLA_NOPE + MLA_V)), MLA_KV_LORA ** -0.5),
        "rel_bias": nrm(ks[9], (REL_BUCKETS, DIL_HEADS), 0.1),
        "w_out": nrm(ks[10], (L, MIX_WIDTH, D), MIX_WIDTH ** -0.5 * DEEPNORM_BETA),
        "ln1_g": 1.0 + nrm(ks[11], (L, D), 0.02),
        "ln1_b": nrm(ks[12], (L, D), 0.02),
        "w_router": nrm(ks[13], (L, D, N_EXPERTS), D ** -0.5),
        "w_gate": nrm(ks[14], (L, N_EXPERTS, D, D_FF_EXPERT), D ** -0.5),
        "w_up": nrm(ks[15], (L, N_EXPERTS, D, D_FF_EXPERT), D ** -0.5),
        "w_down": nrm(ks[16], (L, N_EXPERTS, D_FF_EXPERT, D), D_FF_EXPERT ** -0.5 * DEEPNORM_BETA),
        "ln2_g": 1.0 + nrm(ks[17], (L, D), 0.02),
        "ln2_b": nrm(ks[18], (L, D), 0.02),
    }


def reference(x, c, w_ada, b_ada, w_in, q_norm_g, w_uq, kv_norm_g, w_ukv, rel_bias,
              w_out, ln1_g, ln1_b, w_router, w_gate, w_up, w_down, ln2_g, ln2_b):
    b, s, _ = x.shape
    splits = np.cumsum([MLA_Q_LORA, MLA_KV_LORA, MLA_ROPE, DIL_WIDTH, DIL_WIDTH])
    cond = jax.nn.silu(c)
    for l in range(DEPTH):
        mod = cond @ w_ada[l] + b_ada[l]
        sh1, sc1, g1, sh2, sc2, g2 = [m[:, None, :] for m in jnp.split(mod, 6, axis=-1)]

        h = x * (1.0 + sc1) + sh1
        proj = h @ w_in[l]
        c_q, c_kv, k_rope, dq, dk, dv = jnp.split(proj, splits, axis=-1)
        mla_out = mla_mixer(c_q, c_kv, k_rope, q_norm_g[l], w_uq[l], kv_norm_g[l], w_ukv[l])
        hs = (b, s, DIL_HEADS, DIL_HEAD_DIM)
        dil_out = dilated_mixer(dq.reshape(hs), dk.reshape(hs), dv.reshape(hs), rel_bias)
        mix = jnp.concatenate([mla_out, dil_out], axis=-1) @ w_out[l]
        x = layer_norm(DEEPNORM_ALPHA * x + g1 * mix, ln1_g[l], ln1_b[l])

        h = x * (1.0 + sc2) + sh2
        moe = expert_choice_ffn(h, w_router[l], w_gate[l], w_up[l], w_down[l])
        x = layer_norm(DEEPNORM_ALPHA * x + g2 * moe, ln2_g[l], ln2_b[l])
    return x
```

```python
import math
import os
from contextlib import ExitStack

import numpy as np
import concourse.bass as bass
import concourse.mybir as mybir
from concourse.bass_utils import run_bass_kernel_spmd

F32 = mybir.dt.float32
BF16 = mybir.dt.bfloat16
I32 = mybir.dt.int32
AF = mybir.ActivationFunctionType
ALU = mybir.AluOpType
AX = mybir.AxisListType

D = 1024
S = 4096
NT = 32
NG = 8
ALPHA = 2.0 ** 0.25
EPS = 1e-6
NE = 16
CAP = 512
DFF = 1536
C0 = 1408
MW = 2944
NBIS = 26


class Buf:
    __slots__ = ("w", "r", "name", "excl")

    def __init__(self, name="", excl=False):
        self.excl = excl
        self.w = []
        self.r = []
        self.name = name


def _compact(toks):
    best = {}
    for t in toks:
        if t[0] not in best or best[t[0]][2] < t[2]:
            best[t[0]] = t
    return list(best.values())


class KB:
    COMPUTE = ("pe", "act", "dve", "pool")
    NDS = 12

    def __init__(self, nc, es):
        self.nc = nc
        self.E = {"pe": nc.tensor, "act": nc.scalar, "dve": nc.vector, "pool": nc.gpsimd, "sp": nc.sync}
        self.csem = {e: es.enter_context(nc.semaphore("c_" + e)) for e in self.COMPUTE}
        self.ccnt = {e: 0 for e in self.COMPUTE}
        self.dsem = {q: [es.enter_context(nc.semaphore("d_%s%d" % (q, i))) for i in range(self.NDS)]
                     for q in ("sp", "pool", "act")}
        self.dcnt = {q: 0 for q in self.dsem}
        self.dtok = {q: [None] * self.NDS for q in self.dsem}
        self.seen = {e: {} for e in self.E}
        self.nwait = 0

    def _wait(self, eng, toks):
        need = {}
        for t in toks:
            if t is None:
                continue
            key, sem, val = t
            if self.seen[eng].get(key, 0) >= val:
                continue
            if need.get(key, (None, 0))[1] < val:
                need[key] = (sem, val)
        for key, (sem, val) in need.items():
            self.E[eng].wait_ge(sem, val)
            self.seen[eng][key] = val
            self.nwait += 1

    def _deps(self, eng, reads, writes, pwrites, is_dma):
        own = None if is_dma else "c_" + eng
        toks = []
        for b in reads:
            for t in b.w:
                if not (eng == "pe" and t[0] == own):
                    toks.append(t)
            if b.excl:
                toks.extend(t for t in b.r if t[0] != own)
        for b in writes:
            toks.extend(t for t in b.w if t[0] != own)
            toks.extend(t for t in b.r if t[0] != own)
        for b in pwrites:
            toks.extend(t for t in b.r if t[0] != own)
        return toks

    def _commit(self, tok, reads, writes, pwrites):
        for b in reads:
            b.r.append(tok)
            if len(b.r) > 16:
                b.r = _compact(b.r)
        for b in writes:
            b.w = [tok]
            b.r = []
        for b in pwrites:
            b.w.append(tok)
            if len(b.w) > 16:
                b.w = _compact(b.w)

    def op(self, eng, reads, writes, fn, pwrites=()):
        self._wait(eng, self._deps(eng, reads, writes, pwrites, False))
        inst = fn(self.E[eng])
        self.ccnt[eng] += 1
        tok = ("c_" + eng, self.csem[eng], self.ccnt[eng])
        inst.then_inc(self.csem[eng], 1)
        self._commit(tok, reads, writes, pwrites)
        return tok

    def dma(self, q, reads, writes, out, in_, pwrites=(), indirect=None, **kw):
        i = self.dcnt[q]
        slot = i % self.NDS
        self._wait(q, self._deps(q, reads, writes, pwrites, True) + [self.dtok[q][slot]])
        if indirect is None:
            inst = self.E[q].dma_start(out=out, in_=in_, **kw)
        else:
            inst = self.E[q].indirect_dma_start(out=out, in_=in_, **indirect)
        val = 16 * (i // self.NDS + 1)
        sem = self.dsem[q][slot]
        inst.then_inc(sem, 16)
        tok = ("d_%s%d" % (q, slot), sem, val)
        self.dtok[q][slot] = tok
        self.dcnt[q] += 1
        self._commit(tok, reads, writes, pwrites)
        return tok

    def barrier(self, engines=None):
        toks = []
        for e in self.COMPUTE:
            if self.ccnt[e]:
                toks.append(("c_" + e, self.csem[e], self.ccnt[e]))
        for q in self.dsem:
            toks.extend(t for t in self.dtok[q] if t is not None)
        for e in (engines or self.E):
            self._wait(e, toks)


def AP(t, off, dims):
    return bass.AP(t, off, [list(d) for d in dims])


def pdim(ap):
    return list(ap.ap[0])


def build_program(stages=99, dbg=()):
    nc = bass.Bass("TRN2", target_bir_lowering=False)
    es = ExitStack()
    with es:
        _build(nc, es, stages, dbg)
    return nc


def _build(nc, es, stages, dbg):
    def din(name, shape, dt=F32):
        return nc.dram_tensor(name, list(shape), dt, kind="ExternalInput").ap()

    def dscr(name, shape, dt):
        kind = "ExternalOutput" if name in dbg else "Internal"
        return nc.dram_tensor(name, list(shape), dt, kind=kind).ap()

    x_d = din("x", [S, D])
    cfm_d = din("c_fm", [128, 8])
    wada_d = din("w_ada", [D, 6 * D])
    bada_d = din("b_ada", [1, 6 * D])
    win_d = din("w_in", [D, 2208])
    wkr_d = din("w_kr", [D, 192])
    gq_d = din("g_q", [128, 3])
    gkv_d = din("g_kv", [128, 2])
    wuq_d = din("w_uq2", [384, 2, 768])
    wukvk_d = din("w_ukv_k", [256, 512])
    wukvv_d = din("w_ukv_v", [256, 512])
    cos_d = din("rope_cos", [32, S])
    sin_d = din("rope_sin", [32, S])
    toep_d = din("relb_toep", [8, 128, MW])
    mult_d = din("toep_mult", [128, MW])
    wout_d = din("w_out", [D, D])
    ln1g_d = din("ln1_g", [1, D])
    ln1b_d = din("ln1_b", [1, D])
    wr_d = din("w_router", [D, NE])
    wg_d = din("w_gate", [NE, D, DFF])
    wu_d = din("w_up", [NE, D, DFF])
    wd_d = din("w_down", [NE, DFF, D])
    ln2g_d = din("ln2_g", [1, D])
    ln2b_d = din("ln2_b", [1, D])
    ident_d = din("ident", [128, 128])
    triu_d = din("triu", [128, 128])
    iota_d = din("iota512", [128, 512])
    tokhl_d = din("tokhl", [128, NT, 2])
    out_d = nc.dram_tensor("out", [S, D], F32, kind="ExternalOutput").ap()

    mod_d = dscr("mod_s", [1, 6 * D], F32)
    qT_d = dscr("qT_s", [8, 96, S], BF16)
    kT_d = dscr("kT_s", [8, 96, S], BF16)
    Vm_d = dscr("Vm_s", [NT, 128, 768], BF16)
    dqT_d = dscr("dqT_s", [4, 128, S], BF16)
    dkT_d = dscr("dkT_s", [4, 128, S], BF16)
    Vd_d = dscr("Vd_s", [NT, 128, 768], BF16)
    attnT_d = dscr("attnT_s", [8, 128, S], BF16)
    x1_d = dscr("x1_s", [S, D], F32)
    h2_d = dscr("h2_s", [S, D], BF16)
    moe_d = dscr("moe_s", [S, D], F32)
    aff_d = dscr("aff_s", [128, NT * NE], F32)
    idx_d = dscr("idx_s", [128, NE * 4 * 8], F32)

    kb = KB(nc, es)
    B_mod, B_q, B_k, B_vm, B_dq, B_dk, B_vd = (Buf(n) for n in ("mod", "q", "k", "vm", "dq", "dk", "vd"))
    B_attn, B_x1, B_h2, B_moe = Buf("attn"), Buf("x1"), Buf("h2"), Buf("moe")

    ps = es.enter_context(nc.psum_tensor("ps", [128, 4096], F32))
    PB = [Buf("bank%d" % i, excl=True) for i in range(8)]
    bank_ctr = [0]

    def bank():
        i = bank_ctr[0] % 8
        bank_ctr[0] += 1
        return ps[:, i * 512:(i + 1) * 512], PB[i]

    modfm = es.enter_context(nc.sbuf_tensor("modfm", [128, 16], F32))
    aff_all = es.enter_context(nc.sbuf_tensor("aff_all", [128, NT * NE], F32))
    B_modfm, B_aff = Buf("modfm"), Buf("aff")

    def bcast_row(dram_ap_row, n):
        return AP(dram_ap_row.tensor, dram_ap_row.offset, [[0, 128], [1, n]])

    with ExitStack() as s0:
        A = lambda n, sh, dt: s0.enter_context(nc.sbuf_tensor("p0_" + n, sh, dt))
        cfm = A("cfm", [128, 8], F32)
        sig = A("sig", [128, 8], F32)
        cond = A("cond", [128, 8], F32)
        bada = A("bada", [1, 6 * D], F32)
        modrow = A("modrow", [1, 6 * D], F32)
        wa = [A("wa%d" % i, [128, 8, 512], F32) for i in range(3)]
        Bc, Bcond, Bb, Bm = Buf(), Buf(), Buf(), Buf()
        Bwa = [Buf() for _ in range(3)]
        kb.dma("sp", [], [Bc], cfm[:], cfm_d[:, :])
        kb.dma("sp", [], [Bb], bada[:], bada_d[:, :])
        wada_v = wada_d.rearrange("(k p) n -> p k n", p=128)
        for j in range(2):
            kb.dma("sp", [], [Bwa[j]], wa[j][:], wada_v[:, :, j * 512:(j + 1) * 512])
        kb.op("act", [Bc], [Bcond], lambda e: e.activation(out=sig[:], in_=cfm[:], func=AF.Sigmoid))
        kb.op("dve", [Bc, Bcond], [Bcond], lambda e: e.tensor_tensor(out=cond[:], in0=cfm[:], in1=sig[:], op=ALU.mult))
        for j in range(12):
            if j + 2 < 12:
                kb.dma("sp", [], [Bwa[(j + 2) % 3]], wa[(j + 2) % 3][:], wada_v[:, :, (j + 2) * 512:(j + 3) * 512])
            bk, bb = bank()
            w = wa[j % 3]

            def mm(e, w=w, bk=bk):
                for k in range(8):
                    i = e.matmul(bk[0:1, :], cond[:, k:k + 1], w[:, k, :], start=(k == 0), stop=(k == 7))
                return i
            kb.op("pe", [Bcond, Bwa[j % 3]], [bb], mm)
            kb.op("dve", [bb, Bb], [], pwrites=[Bm], fn=lambda e, bk=bk, j=j: e.tensor_tensor(
                out=modrow[0:1, j * 512:(j + 1) * 512], in0=bk[0:1, :], in1=bada[0:1, j * 512:(j + 1) * 512], op=ALU.add))
        kb.dma("sp", [Bm], [B_mod], mod_d[:, :], modrow[:])
        onef = A("onef", [1, 1], F32)
        kb.op("pool", [], [Bc], lambda e: e.memset(onef[:], 1.0))
        bk, bb = bank()

        def mmT(e, bk=bk):
            for j in range(16):
                ins = e.matmul(bk[:, j:j + 1], modrow[0:1, j * 128:(j + 1) * 128], onef[0:1, 0:1], start=True, stop=True)
            return ins
        kb.op("pe", [Bm, Bc], [bb], mmT)
        kb.op("dve", [bb], [B_modfm], lambda e, bk=bk: e.tensor_copy(out=modfm[:], in_=bk[:, 0:16]))
        kb.op("dve", [B_modfm], [B_modfm], lambda e: e.tensor_scalar(
            out=modfm[:, 8:16], in0=modfm[:, 8:16], scalar1=1.0, scalar2=None, op0=ALU.add))
        kb.barrier()
    if stages <= 0:
        _finish(nc, kb, out_d, x_d)
        return

    with ExitStack() as s1:
        A = lambda n, sh, dt: s1.enter_context(nc.sbuf_tensor("p1_" + n, sh, dt))
        win = A("win", [128, 8, 2208], BF16)
        wkr = A("wkr", [128, 8, 192], BF16)
        wuq = A("wuq", [128, 3, 2, 768], BF16)
        wukvk = A("wukvk", [128, 2, 512], BF16)
        wukvv = A("wukvv", [128, 2, 512], BF16)
        ident = A("ident", [128, 128], F32)
        onesq = A("onesq", [128, 128], F32)
        oneskv = A("oneskv", [128, 128], F32)
        gq = A("gq", [128, 3], F32)
        gkv = A("gkv", [128, 2], F32)
        xg = [A("xg%d" % i, [128, 4, 1024], F32) for i in range(2)]
        hT = [A("hT%d" % i, [128, 8, 512], BF16) for i in range(2)]
        cT = A("cT", [128, 5, 512], F32)
        sq = A("sq", [128, 5, 512], F32)
        rstd = A("rstd", [128, 2, 512], F32)
        cn = [A("cn%d" % i, [128, 5, 512], BF16) for i in range(2)]
        cosr = [A("cosr%d" % i, [128, 512], F32) for i in range(2)]
        sinr = [A("sinr%d" % i, [128, 512], F32) for i in range(2)]
        tmpa = [A("tmpa%d" % i, [128, 512], F32) for i in range(2)]
        tmpb = [A("tmpb%d" % i, [128, 512], F32) for i in range(2)]
        NO = 6
        ob = [A("ob%d" % i, [128, 512], BF16) for i in range(NO)]
        vo = [A("vo%d" % i, [128, 768], BF16) for i in range(4)]
        Bw, Bid, Bones, Bg = Buf(), Buf(), Buf(), Buf()
        Bxg = [Buf(), Buf()]
        BhT = [Buf(), Buf()]
        BcT, Bsq, Brstd = [Buf() for _ in range(5)], [Buf() for _ in range(5)], [Buf(), Buf()]
        Bcn = [[Buf() for _ in range(5)] for _ in range(2)]
        Brope = [Buf(), Buf()]
        Bta, Btb = [Buf(), Buf()], [Buf(), Buf()]
        Bob = [Buf() for _ in range(NO)]
        Bvo = [Buf() for _ in range(4)]
        ob_ctr, vo_ctr, t_ctr = [0], [0], [0]

        SKIP = os.environ.get('P1SKIP', '')
        for k in range(0 if 'w' in SKIP else 8):
            kb.dma("pool", [], [], win[:, k, :], win_d[k * 128:(k + 1) * 128, :], pwrites=[Bw])
            kb.dma("pool", [], [], wkr[:, k, :], wkr_d[k * 128:(k + 1) * 128, :], pwrites=[Bw])
        for c in range(0 if 'u' in SKIP else 3):
            kb.dma("pool", [], [], wuq[:, c, :, :], wuq_d[c * 128:(c + 1) * 128, :, :], pwrites=[Bw])
        for c in range(0 if 'v' in SKIP else 2):
            kb.dma("pool", [], [], wukvk[:, c, :], wukvk_d[c * 128:(c + 1) * 128, :], pwrites=[Bw])
            kb.dma("pool", [], [], wukvv[:, c, :], wukvv_d[c * 128:(c + 1) * 128, :], pwrites=[Bw])
        kb.dma("sp", [], [Bid], ident[:], ident_d[:, :])
        kb.dma("sp", [], [], gq[:], gq_d[:, :], pwrites=[Bg])
        kb.dma("sp", [], [], gkv[:], gkv_d[:, :], pwrites=[Bg])
        kb.op("pool", [], [], lambda e: e.memset(onesq[:], 1.0 / 384.0), pwrites=[Bones])
        kb.op("pool", [], [], lambda e: e.memset(oneskv[:], 1.0 / 256.0), pwrites=[Bones])
        for i in range(4):
            kb.op("pool", [], [Bvo[i]], lambda e, i=i: e.memset(vo[i][:], 1.0))

        x_v = x_d.rearrange("(t p) d -> p t d", p=128)

        def load_x(g):
            kb.dma("sp", [], [Bxg[g % 2]], xg[g % 2][:], x_v[:, g * 4:(g + 1) * 4, :])
            kb.dma("sp", [], [Brope[g % 2]], cosr[g % 2][64:96, :], cos_d[:, g * 512:(g + 1) * 512])
            kb.dma("sp", [], [Brope[g % 2]], sinr[g % 2][64:96, :], sin_d[:, g * 512:(g + 1) * 512])

        def next_ob():
            i = ob_ctr[0] % NO
            ob_ctr[0] += 1
            return ob[i], Bob[i]

        def evac_copy(eng, src, bsrc, dst, bdst):
            if eng == "act":
                kb.op("act", [bsrc], [bdst], lambda e: e.copy(out=dst, in_=src))
            else:
                kb.op("dve", [bsrc], [bdst], lambda e: e.tensor_copy(out=dst, in_=src))

        def rope_rows(bm, bbm, br, bbr, g, dst, bdst, full):
            i = t_ctr[0] % 2
            t_ctr[0] += 1
            kb.op("dve", [bbm, Brope[g % 2]], [Bta[i]], lambda e: e.tensor_tensor(
                out=tmpa[i][64:96, :], in0=bm[64:96, :], in1=cosr[g % 2][64:96, :], op=ALU.mult))
            kb.op("dve", [bbr, Brope[g % 2]], [Btb[i]], lambda e: e.tensor_tensor(
                out=tmpb[i][64:96, :], in0=br[64:96, :], in1=sinr[g % 2][64:96, :], op=ALU.mult))
            kb.op("dve", [Bta[i], Btb[i]], [bdst] if full else [], lambda e: e.tensor_tensor(
                out=dst[64:96, :], in0=tmpa[i][64:96, :], in1=tmpb[i][64:96, :], op=ALU.add),
                pwrites=[] if full else [bdst])

        def vo_views(vt, bk):
            o = AP(vt, 0, [pdim(vt[:]), [192, 4], [128, 2], [1, 64]])
            i = AP(bk.tensor, bk.offset, [pdim(bk), [128, 4], [64, 2], [1, 64]])
            return o, i

        load_x(0)
        LIM = float(os.environ.get('P1LIM', '99'))
        SUB = float(os.environ.get('P1SUB', '99'))
        for g in range(NG if LIM >= 99 else 1):
            if g + 1 < NG:
                load_x(g + 1)
            X, BX = xg[g % 2], Bxg[g % 2]
            H, BH = hT[g % 2], BhT[g % 2]
            CN, BCN = cn[g % 2], Bcn[g % 2]
            cols = slice(g * 512, (g + 1) * 512)
            for k in range(8):
                bk, bb = bank()

                def tp(e, bk=bk, k=k):
                    for i in range(4):
                        ins = e.transpose(bk[:, i * 128:(i + 1) * 128], X[:, i, k * 128:(k + 1) * 128], ident[:])
                    return ins
                kb.op("pe", [BX, Bid], [bb], tp)
                kb.op("act", [bb, B_modfm], [BH] if k == 0 else [], lambda e, bk=bk, k=k: e.activation(
                    out=H[:, k, :], in_=bk, func=AF.Identity, scale=modfm[:, 8 + k:9 + k], bias=modfm[:, k:k + 1]),
                    pwrites=[] if k == 0 else [BH])
            if LIM < 1:
                break
            for c in range(5):
                bk, bb = bank()

                def mm(e, bk=bk, c=c):
                    for k in range(8):
                        ins = e.matmul(bk, win[:, k, c * 128:(c + 1) * 128], H[:, k, :], start=(k == 0), stop=(k == 7))
                    return ins
                kb.op("pe", [BH, Bw], [bb], mm)
                if SUB >= 0.1:
                    kb.op("act", [bb], [Bsq[c]], lambda e, bk=bk, c=c: e.activation(out=sq[:, c, :], in_=bk, func=AF.Square))
                if SUB >= 0.15:
                    kb.op("dve", [bb, Bsq[c]], [BcT[c]], lambda e, bk=bk, c=c: e.tensor_copy(out=cT[:, c, :], in_=bk))
            if SUB < 0.3:
                break
            for which, (cs, ones_t) in enumerate((((0, 1, 2), onesq), ((3, 4), oneskv))):
                bk, bb = bank()

                def mm(e, bk=bk, cs=cs, ones_t=ones_t):
                    for n, c in enumerate(cs):
                        ins = e.matmul(bk, ones_t[:], sq[:, c, :], start=(n == 0), stop=(n == len(cs) - 1))
                    return ins
                kb.op("pe", [Bsq[c] for c in cs] + [Bones], [bb], mm)
                kb.op("dve", [bb], [Brstd[which]], lambda e, bk=bk, which=which: e.tensor_scalar(
                    out=rstd[:, which, :], in0=bk, scalar1=EPS, scalar2=None, op0=ALU.add))
                if SUB < 0.5:
                    continue
                kb.op("act", [Brstd[which]], [Brstd[which]], lambda e, which=which: e.activation(
                    out=rstd[:, which, :], in_=rstd[:, which, :], func=AF.Sqrt))
                kb.op("dve", [Brstd[which]], [Brstd[which]], lambda e, which=which: e.reciprocal(
                    out=rstd[:, which, :], in_=rstd[:, which, :]))
                if SUB < 0.7:
                    continue
                for c in cs:
                    gsc = gq[:, c:c + 1] if which == 0 else gkv[:, c - 3:c - 2]
                    kb.op("dve", [BcT[c], Brstd[which], Bg], [BCN[c]], lambda e, c=c, gsc=gsc, which=which: e.scalar_tensor_tensor(
                        out=CN[:, c, :], in0=cT[:, c, :], scalar=gsc, in1=rstd[:, which, :], op0=ALU.mult, op1=ALU.mult))
            if LIM < 2:
                break
            for h in range(8):
                bm, bbm = bank()
                br, bbr = bank()

                def mm(e, h=h, bm=bm, br=br):
                    for which, bk in ((0, bm), (1, br)):
                        for c in range(3):
                            ins = e.matmul(bk[0:96, :], wuq[:, c, which, h * 96:(h + 1) * 96], CN[:, c, :],
                                           start=(c == 0), stop=(c == 2))
                    return ins
                kb.op("pe", [BCN[0], BCN[1], BCN[2], Bw], [bbm, bbr], mm)
                o, bo = next_ob()
                kb.op("act", [bbm], [bo], lambda e, o=o, bm=bm: e.copy(out=o[0:64, :], in_=bm[0:64, :]))
                rope_rows(bm, bbm, br, bbr, g, o, bo, False)
                kb.dma("sp", [bo], [], qT_d[h, :, cols], o[0:96, :], pwrites=[B_q])
            if LIM < 3:
                break
            for hp in range(4):
                bk, bb = bank()

                def mm(e, hp=hp, bk=bk):
                    for c in range(2):
                        ins = e.matmul(bk, wukvk[:, c, hp * 128:(hp + 1) * 128], CN[:, 3 + c, :], start=(c == 0), stop=(c == 1))
                    return ins
                kb.op("pe", [BCN[3], BCN[4], Bw], [bb], mm)
                o, bo = next_ob()
                evac_copy("act", bk, bb, o[:, :], bo)
                kb.dma("sp", [bo], [], kT_d[2 * hp, 0:64, cols], o[0:64, :], pwrites=[B_k])
                kb.dma("sp", [bo], [], kT_d[2 * hp + 1, 0:64, cols], o[64:128, :], pwrites=[B_k])
            if LIM < 4:
                break
            bm, bbm = bank()
            br, bbr = bank()

            def mm(e, bm=bm, br=br):
                for which, bk in ((0, bm), (1, br)):
                    for k in range(8):
                        ins = e.matmul(bk[0:96, :], wkr[:, k, which * 96:(which + 1) * 96], H[:, k, :],
                                       start=(k == 0), stop=(k == 7))
                return ins
            kb.op("pe", [BH, Bw], [bbm, bbr], mm)
            o, bo = next_ob()
            rope_rows(bm, bbm, br, bbr, g, o, bo, True)
            for h in range(8):
                kb.dma("sp", [bo], [], kT_d[h, 64:96, cols], o[64:96, :], pwrites=[B_k])
            if LIM < 5:
                break
            for i in range(4):
                bk, bb = bank()

                def mm(e, i=i, bk=bk):
                    for c in range(2):
                        ins = e.matmul(bk, CN[:, 3 + c, i * 128:(i + 1) * 128], wukvv[:, c, :], start=(c == 0), stop=(c == 1))
                    return ins
                kb.op("pe", [BCN[3], BCN[4], Bw], [bb], mm)
                vi = vo_ctr[0] % 4
                vo_ctr[0] += 1
                ov, iv = vo_views(vo[vi], bk)
                kb.op("act", [bb], [Bvo[vi]], lambda e, ov=ov, iv=iv: e.copy(out=ov, in_=iv))
                kb.dma("sp", [Bvo[vi]], [], Vm_d[g * 4 + i, :, :], vo[vi][:], pwrites=[B_vm])
            if LIM < 6:
                break
            for base, dst, bd in ((672, dqT_d, B_dq), (1184, dkT_d, B_dk)):
                for hp in range(4):
                    bk, bb = bank()

                    def mm(e, hp=hp, bk=bk, base=base):
                        for k in range(8):
                            ins = e.matmul(bk, win[:, k, base + hp * 128: base + (hp + 1) * 128], H[:, k, :],
                                           start=(k == 0), stop=(k == 7))
                        return ins
                    kb.op("pe", [BH, Bw], [bb], mm)
                    o, bo = next_ob()
                    evac_copy("act" if hp % 2 == 0 else "dve", bk, bb, o[:, :], bo)
                    kb.dma("sp", [bo], [], dst[hp, :, cols], o[:, :], pwrites=[bd])
            if LIM < 7:
                break
            for i in range(4):
                bk, bb = bank()

                def mm(e, i=i, bk=bk):
                    for k in range(8):
                        ins = e.matmul(bk, H[:, k, i * 128:(i + 1) * 128], win[:, k, 1696:2208], start=(k == 0), stop=(k == 7))
                    return ins
                kb.op("pe", [BH, Bw], [bb], mm)
                vi = vo_ctr[0] % 4
                vo_ctr[0] += 1
                ov, iv = vo_views(vo[vi], bk)
                kb.op("act", [bb], [Bvo[vi]], lambda e, ov=ov, iv=iv: e.copy(out=ov, in_=iv))
                kb.dma("sp", [Bvo[vi]], [], Vd_d[g * 4 + i, :, :], vo[vi][:], pwrites=[B_vd])
        kb.barrier()
    if stages <= 1:
        _finish(nc, kb, out_d, x_d)
        return

    def attention(tag, kT_src, qT_src, V_src, per_pair_kq, krows, scale, chunk0, dilated, Bk_src, Bq_src, Bv_src):
        with ExitStack() as sa:
            A = lambda n, sh, dt: sa.enter_context(nc.sbuf_tensor(tag + "_" + n, sh, dt))
            Vp = [A("Vp%d" % i, [128, 32, 192], BF16) for i in range(2)]
            Kt = [A("Kt%d" % i, [128, S], BF16) for i in range(2)]
            Qt = [A("Qt%d" % i, [128, S], BF16) for i in range(2)]
            pte = [A("pte%d" % i, [128, 1024], BF16) for i in range(3)]
            ptm = [A("ptm%d" % i, [128, 1024], BF16) for i in range(3)] if dilated else None
            ao = [A("ao%d" % i, [128, S], BF16) for i in range(2)]
            rec = [A("rec%d" % i, [128, 512], F32) for i in range(2)]
            BVp, BKt, BQt = [Buf(), Buf()], [Buf(), Buf()], [Buf(), Buf()]
            Bpte, Bptm = [Buf() for _ in range(3)], [Buf() for _ in range(3)]
            Bao, Brec = [Buf(), Buf()], [Buf(), Buf()]
            SB = [Buf("S%d" % i, excl=True) for i in range(3)]
            AB = [Buf("acc%d" % i, excl=True) for i in range(2)]
            Sap = [ps[:, i * 1024:(i + 1) * 1024] for i in range(3)]
            Aap = [ps[:, 3072 + i * 512: 3072 + (i + 1) * 512] for i in range(2)]
            if dilated:
                toep_st = A("toep", [128, MW], F32)
                multb = A("multb", [128, MW], BF16)
                master = [A("master%d" % i, [128, MW], BF16) for i in range(2)]
                Btoep, Bmult, Bmaster = Buf(), Buf(), [Buf(), Buf()]
                kb.dma("pool", [], [Bmult], multb[:], mult_d[:, :])

            def load_pair(hp):
                for j in range(4):
                    kb.dma("sp", [Bv_src], [], Vp[hp % 2][:, j * 8:(j + 1) * 8, :],
                           V_src[j * 8:(j + 1) * 8, :, hp * 192:(hp + 1) * 192].rearrange("t p c -> p t c"),
                           pwrites=[BVp[hp % 2]])

            def load_kq(u):
                if per_pair_kq:
                    kb.dma("sp", [Bk_src], [BKt[u % 2]], Kt[u % 2][:, :], kT_src[u, :, :])
                    kb.dma("sp", [Bq_src], [BQt[u % 2]], Qt[u % 2][:, :], qT_src[u, :, :])
                else:
                    kb.dma("sp", [Bk_src], [BKt[u % 2]], Kt[u % 2][0:krows, :], kT_src[u, :, :])
                    kb.dma("sp", [Bq_src], [BQt[u % 2]], Qt[u % 2][0:krows, :], qT_src[u, :, :])

            def prep_mask(h):
                kb.dma("sp", [], [Btoep], toep_st[:], toep_d[h, :, :])
                kb.op("act", [Btoep], [Btoep], lambda e: e.activation(out=toep_st[:], in_=toep_st[:], func=AF.Exp))
                kb.op("dve", [Btoep, Bmult], [Bmaster[h % 2]], lambda e: e.tensor_tensor(
                    out=master[h % 2][:], in0=toep_st[:], in1=multb[:], op=ALU.mult))

            items = []
            for h in range(8):
                hp, hh = h // 2, h % 2
                for Q in range(8):
                    if dilated:
                        Ts = list(range(max(0, 4 * Q - 8), min(31, 4 * Q + 11) + 1))
                    else:
                        Ts = list(range(32))
                    groups = []
                    n = 0
                    while n < len(Ts):
                        if n + 1 < len(Ts):
                            groups.append([Ts[n + 1], Ts[n]])
                            n += 2
                        else:
                            groups.append([Ts[n]])
                            n += 1
                    for gi, gT in enumerate(groups):
                        items.append(dict(h=h, hp=hp, hh=hh, Q=Q, Ts=gT, first=(gi == 0), last=(gi == len(groups) - 1),
                                          hq=h * 8 + Q))
            N = len(items)
            for i, it in enumerate(items):
                it["i"] = i

            def kq_unit(it):
                return it["hp"] if per_pair_kq else it["h"]

            def rows_of(it):
                if per_pair_kq:
                    return it["hh"] * 64, it["hh"] * 64 + 64
                return 0, krows

            def emit_qk(it):
                i = it["i"]
                u = kq_unit(it)
                r0, r1 = rows_of(it)
                K_, Q_ = Kt[u % 2], Qt[u % 2]

                def f(e):
                    for j, T in enumerate(it["Ts"]):
                        ins = e.matmul(Sap[i % 3][:, j * 512:(j + 1) * 512], K_[r0:r1, T * 128:(T + 1) * 128],
                                       Q_[r0:r1, it["Q"] * 512:(it["Q"] + 1) * 512], start=True, stop=True)
                    return ins
                kb.op("pe", [BKt[u % 2], BQt[u % 2]], [SB[i % 3]], f)

            def emit_exp(it):
                i = it["i"]
                w = 512 * len(it["Ts"])
                kb.op("act", [SB[i % 3]], [Bpte[i % 3]], lambda e: e.activation(
                    out=pte[i % 3][:, 0:w], in_=Sap[i % 3][:, 0:w], func=AF.Exp, scale=scale))

            def emit_mask(it):
                i = it["i"]
                n = len(it["Ts"])
                h = it["h"]
                c0 = C0 - 128 * it["Ts"][0] + 512 * it["Q"]
                m = master[h % 2]
                in1 = AP(m, c0, [pdim(m[:]), [128, n], [1, 512]])
                o = AP(ptm[i % 3], 0, [pdim(ptm[i % 3][:]), [512, n], [1, 512]])
                i0 = AP(pte[i % 3], 0, [pdim(pte[i % 3][:]), [512, n], [1, 512]])
                kb.op("dve", [Bpte[i % 3], Bmaster[h % 2]], [Bptm[i % 3]], lambda e: e.tensor_tensor(
                    out=o, in0=i0, in1=in1, op=ALU.mult))

            def emit_pv(it):
                i = it["i"]
                P_, BP_ = (ptm[i % 3], Bptm[i % 3]) if dilated else (pte[i % 3], Bpte[i % 3])
                acc = Aap[it["hq"] % 2]
                V_ = Vp[it["hp"] % 2]
                vc = 64 * it["hh"]
                nT = len(it["Ts"])

                def f(e):
                    for j, T in enumerate(it["Ts"]):
                        ins = e.matmul(acc, V_[:, T, vc:vc + 128], P_[:, j * 512:(j + 1) * 512],
                                       start=(it["first"] and j == 0), stop=(it["last"] and j == nT - 1))
                    return ins
                if it["first"]:
                    kb.op("pe", [BP_, BVp[it["hp"] % 2]], [AB[it["hq"] % 2]], f)
                else:
                    kb.op("pe", [BP_, BVp[it["hp"] % 2]], [], f, pwrites=[AB[it["hq"] % 2]])

            def emit_fin(it):
                a = it["hq"] % 2
                acc = Aap[a]
                hh, hp, Q = it["hh"], it["hp"], it["Q"]
                nr = slice(64 * hh, 64 * hh + 64)
                dr = slice(64 * (1 - hh), 64 * (1 - hh) + 64)
                kb.op("dve", [AB[a]], [Brec[a]], lambda e: e.reciprocal(out=rec[a][nr, :], in_=acc[dr, :]))
                first_of_pair = (hh == 0 and Q == 0)
                kb.op("dve", [AB[a], Brec[a]], [Bao[hp % 2]] if first_of_pair else [], lambda e: e.tensor_tensor(
                    out=ao[hp % 2][nr, Q * 512:(Q + 1) * 512], in0=acc[nr, :], in1=rec[a][nr, :], op=ALU.mult),
                    pwrites=[] if first_of_pair else [Bao[hp % 2]])
                if hh == 1 and Q == 7:
                    kb.dma("sp", [Bao[hp % 2]], [], attnT_d[chunk0 + hp, :, :], ao[hp % 2][:, :], pwrites=[B_attn])

            def unit_start(it):
                h = it["h"]
                if it["Q"] == 0 and it["first"]:
                    if h + 1 < 8:
                        if per_pair_kq:
                            if (h + 1) % 2 == 0:
                                load_kq((h + 1) // 2)
                        else:
                            load_kq(h + 1)
                        if (h + 1) % 2 == 0:
                            load_pair((h + 1) // 2)
                        if dilated:
                            prep_mask(h + 1)

            load_pair(0)
            load_kq(0)
            if dilated:
                prep_mask(0)
            emit_qk(items[0])
            emit_exp(items[0])
            if dilated:
                emit_mask(items[0])
            if N > 1:
                emit_qk(items[1])
            for i in range(N):
                it = items[i]
                unit_start(it)
                if i + 1 < N:
                    emit_exp(items[i + 1])
                    if dilated:
                        emit_mask(items[i + 1])
                if i + 2 < N:
                    emit_qk(items[i + 2])
                emit_pv(it)
                if it["last"]:
                    emit_fin(it)
            kb.barrier()

    run_dil = os.environ.get("SKIPDIL", "") == ""
    run_mla = os.environ.get("SKIPMLA", "") == ""
    if run_dil:
        attention("dil", dkT_d, dqT_d, Vd_d, True, 64, 64.0 ** -0.5, 4, True, B_dk, B_dq, B_vd)
    if run_mla:
        attention("mla", kT_d, qT_d, Vm_d, False, 96, 96.0 ** -0.5, 0, False, B_k, B_q, B_vm)
    if stages <= 3:
        _finish(nc, kb, out_d, x_d)
        return

    def bank2():
        if bank_ctr[0] % 2:
            bank_ctr[0] += 1
        i = bank_ctr[0] % 8
        bank_ctr[0] += 2
        return ps[:, i * 512:(i + 2) * 512], [PB[i], PB[i + 1]]

    class LNState:
        def __init__(self, A, tag):
            self.junk = A(tag + "junk", [128, D], F32)
            self.st = [A(tag + "st%d" % i, [128, 8], F32) for i in range(2)]
            self.Bjunk = Buf()
            self.Bst = [Buf(), Buf()]
            self.n = 0

    def ln_stats(L, y, By):
        i = L.n % 2
        L.n += 1
        st, Bst = L.st[i], L.Bst[i]
        kb.op("pool", [], [Bst], lambda e: e.memset(st[:], 0.0))
        kb.op("act", [By, Bst], [L.Bjunk], lambda e: e.activation(out=L.junk[:], in_=y[:], func=AF.Identity, accum_out=st[:, 0:1]),
              pwrites=[Bst])
        kb.op("act", [By, Bst], [L.Bjunk], lambda e: e.activation(out=L.junk[:], in_=y[:], func=AF.Square, accum_out=st[:, 1:2]),
              pwrites=[Bst])
        return st, Bst

    def ln_apply(st, Bst, y, By, xh, Bxh):
        kb.op("dve", [Bst], [], lambda e: e.tensor_scalar(out=st[:, 2:3], in0=st[:, 0:1], scalar1=1.0 / D, scalar2=None, op0=ALU.mult), pwrites=[Bst])
        kb.op("dve", [Bst], [], lambda e: e.tensor_tensor(out=st[:, 3:4], in0=st[:, 2:3], in1=st[:, 2:3], op=ALU.mult), pwrites=[Bst])
        kb.op("dve", [Bst], [], lambda e: e.scalar_tensor_tensor(out=st[:, 4:5], in0=st[:, 1:2], scalar=1.0 / D, in1=st[:, 3:4],
                                                                   op0=ALU.mult, op1=ALU.subtract), pwrites=[Bst])
        kb.op("dve", [Bst], [], lambda e: e.tensor_scalar(out=st[:, 4:5], in0=st[:, 4:5], scalar1=EPS, scalar2=None, op0=ALU.add), pwrites=[Bst])
        kb.op("act", [Bst], [], lambda e: e.activation(out=st[:, 5:6], in_=st[:, 4:5], func=AF.Ln), pwrites=[Bst])
        kb.op("act", [Bst], [], lambda e: e.activation(out=st[:, 5:6], in_=st[:, 5:6], func=AF.Exp, scale=-0.5), pwrites=[Bst])
        kb.op("dve", [Bst], [], lambda e: e.scalar_tensor_tensor(out=st[:, 6:7], in0=st[:, 2:3], scalar=-1.0, in1=st[:, 5:6],
                                                                   op0=ALU.mult, op1=ALU.mult), pwrites=[Bst])
        kb.op("act", [By, Bst], [Bxh], lambda e: e.activation(out=xh[:], in_=y[:], func=AF.Identity, scale=st[:, 5:6], bias=st[:, 6:7]))

    with ExitStack() as s4:
        A = lambda n, sh, dt: s4.enter_context(nc.sbuf_tensor("p4_" + n, sh, dt))
        wout = A("wout", [128, 8, D], BF16)
        g1bc, lngbc, lnbbc, sc2bc, sh2bc = (A(n, [128, D], F32) for n in ("g1bc", "lngbc", "lnbbc", "sc2bc", "sh2bc"))
        wr = A("wr", [128, 8, NE], F32)
        ident = A("ident", [128, 128], F32)
        at = [A("at%d" % i, [128, 8, 512], BF16) for i in range(2)]
        xt = [A("xt%d" % i, [128, D], F32) for i in range(2)]
        t1 = [A("t1%d" % i, [128, D], F32) for i in range(2)]
        yy = [A("y%d" % i, [128, D], F32) for i in range(2)]
        xh = [A("xh%d" % i, [128, D], F32) for i in range(2)]
        x1 = [A("x1%d" % i, [128, D], F32) for i in range(2)]
        h2f = [A("h2f%d" % i, [128, D], F32) for i in range(2)]
        h2b = [A("h2b%d" % i, [128, D], BF16) for i in range(2)]
        h2T = [A("h2T%d" % i, [128, 8, 128], F32) for i in range(2)]
        sm = [A("sm%d" % i, [128, 8], F32) for i in range(2)]
        ee = [A("ee%d" % i, [128, NE], F32) for i in range(2)]
        L = LNState(A, "ln")
        Bc4 = Buf()
        Bat, Bxt, Bt1, By, Bxh, Bx1, Bh2f, Bh2b, Bh2T, Bsm, Bee = ([Buf(), Buf()] for _ in range(11))
        for c in range(8):
            kb.dma("pool", [], [], wout[:, c, :], wout_d[c * 128:(c + 1) * 128, :], pwrites=[Bc4])
        kb.dma("sp", [B_mod], [], g1bc[:], bcast_row(mod_d[0:1, 2048:3072], D), pwrites=[Bc4])
        kb.dma("sp", [B_mod], [], sh2bc[:], bcast_row(mod_d[0:1, 3072:4096], D), pwrites=[Bc4])
        kb.dma("sp", [B_mod], [], sc2bc[:], bcast_row(mod_d[0:1, 4096:5120], D), pwrites=[Bc4])
        kb.dma("sp", [], [], lngbc[:], bcast_row(ln1g_d[0:1, :], D), pwrites=[Bc4])
        kb.dma("sp", [], [], lnbbc[:], bcast_row(ln1b_d[0:1, :], D), pwrites=[Bc4])
        kb.dma("sp", [], [], wr[:], wr_d.rearrange("(k p) e -> p k e", p=128), pwrites=[Bc4])
        kb.dma("sp", [], [], ident[:], ident_d[:, :], pwrites=[Bc4])
        kb.op("dve", [Bc4], [], lambda e: e.tensor_scalar(out=sc2bc[:], in0=sc2bc[:], scalar1=1.0, scalar2=None, op0=ALU.add), pwrites=[Bc4])
        attn_v = attnT_d.rearrange("c p n -> p c n")
        x_t = x_d.rearrange("(t p) d -> t p d", p=128)
        x1_t = x1_d.rearrange("(t p) d -> t p d", p=128)
        h2_t = h2_d.rearrange("(t p) d -> t p d", p=128)
        stA = {}

        def stageA(t):
            g, i = t // 4, t % 4
            if i == 0:
                kb.dma("sp", [B_attn], [Bat[g % 2]], at[g % 2][:], attn_v[:, :, g * 512:(g + 1) * 512])
            b = t % 2
            kb.dma("sp", [], [Bxt[b]], xt[b][:], x_t[t])
            bk2, bb2 = bank2()

            def mm(e):
                for half in range(2):
                    for c in range(8):
                        ins = e.matmul(bk2[:, half * 512:(half + 1) * 512], at[g % 2][:, c, i * 128:(i + 1) * 128],
                                       wout[:, c, half * 512:(half + 1) * 512], start=(c == 0), stop=(c == 7))
                return ins
            kb.op("pe", [Bat[g % 2], Bc4], bb2, mm)
            kb.op("dve", bb2 + [Bc4], [Bt1[b]], lambda e: e.tensor_tensor(out=t1[b][:], in0=bk2, in1=g1bc[:], op=ALU.mult))
            kb.op("dve", [Bxt[b], Bt1[b]], [By[b]], lambda e: e.scalar_tensor_tensor(
                out=yy[b][:], in0=xt[b][:], scalar=ALPHA, in1=t1[b][:], op0=ALU.mult, op1=ALU.add))
            stA[t] = ln_stats(L, yy[b], By[b])

        def stageB(t):
            b = t % 2
            st, Bst = stA.pop(t)
            ln_apply(st, Bst, yy[b], By[b], xh[b], Bxh[b])
            if P4LIM < 3:
                return
            kb.op("pool", [Bxh[b], Bc4], [Bx1[b]], lambda e: e.tensor_tensor(out=x1[b][:], in0=xh[b][:], in1=lngbc[:], op=ALU.mult))
            kb.op("pool", [Bx1[b], Bc4], [Bx1[b]], lambda e: e.tensor_tensor(out=x1[b][:], in0=x1[b][:], in1=lnbbc[:], op=ALU.add))
            kb.dma("sp", [Bx1[b]], [], x1_t[t], x1[b][:], pwrites=[B_x1])
            kb.op("pool", [Bx1[b], Bc4], [Bh2f[b]], lambda e: e.tensor_tensor(out=h2f[b][:], in0=x1[b][:], in1=sc2bc[:], op=ALU.mult))
            kb.op("pool", [Bh2f[b], Bc4], [Bh2f[b]], lambda e: e.tensor_tensor(out=h2f[b][:], in0=h2f[b][:], in1=sh2bc[:], op=ALU.add))
            kb.op("act", [Bh2f[b]], [Bh2b[b]], lambda e: e.copy(out=h2b[b][:], in_=h2f[b][:]))
            kb.dma("sp", [Bh2b[b]], [], h2_t[t], h2b[b][:], pwrites=[B_h2])
            if P4LIM < 4:
                return
            bk2, bb2 = bank2()

            def tp(e):
                for k in range(8):
                    ins = e.transpose(bk2[:, k * 128:(k + 1) * 128], h2f[b][:, k * 128:(k + 1) * 128], ident[:])
                return ins
            kb.op("pe", [Bh2f[b], Bc4], bb2, tp)
            kb.op("act", bb2, [Bh2T[b]], lambda e: e.copy(out=h2T[b][:].rearrange("p k n -> p (k n)"), in_=bk2))
            bk, bb = bank()

            def lg(e):
                for k in range(8):
                    ins = e.matmul(bk[:, 0:NE], h2T[b][:, k, :], wr[:, k, :], start=(k == 0), stop=(k == 7))
                return ins
            kb.op("pe", [Bh2T[b], Bc4], [bb], lg)
            if P4LIM < 5:
                return
            kb.op("pool", [], [Bsm[b]], lambda e: e.memset(sm[b][:], 0.0))
            kb.op("dve", [bb], [], lambda e: e.reduce_max(out=sm[b][:, 0:1], in_=bk[:, 0:NE], axis=AX.X), pwrites=[Bsm[b]])
            kb.op("dve", [Bsm[b]], [], lambda e: e.tensor_scalar(out=sm[b][:, 1:2], in0=sm[b][:, 0:1], scalar1=-1.0, scalar2=None, op0=ALU.mult),
                  pwrites=[Bsm[b]])
            kb.op("act", [bb, Bsm[b]], [Bee[b]], lambda e: e.activation(out=ee[b][:], in_=bk[:, 0:NE], func=AF.Exp, bias=sm[b][:, 1:2],
                                                                       accum_out=sm[b][:, 2:3]), pwrites=[Bsm[b]])
            kb.op("dve", [Bsm[b]], [], lambda e: e.reciprocal(out=sm[b][:, 3:4], in_=sm[b][:, 2:3]), pwrites=[Bsm[b]])
            kb.op("dve", [Bee[b], Bsm[b]], [], lambda e: e.tensor_scalar(out=aff_all[:, t * NE:(t + 1) * NE], in0=ee[b][:], scalar1=sm[b][:, 3:4],
                                                                        scalar2=None, op0=ALU.mult), pwrites=[B_aff])

        P4LIM = float(os.environ.get("P4LIM", "99"))
        if P4LIM >= 1:
            stageA(0)
        P4NT = int(os.environ.get('P4NT', str(NT)))
        for t in range(P4NT if P4LIM >= 99 else 1):
            if P4LIM < 2:
                break
            if t + 1 < P4NT:
                stageA(t + 1)
            stageB(t)
        if "aff_s" in dbg:
            kb.dma("sp", [B_aff], [Buf()], aff_d[:, :], aff_all[:])
        kb.barrier()
    if stages <= 4:
        _finish(nc, kb, out_d, x_d)
        return

    def moe_phase():
        with ExitStack() as s5:
            A = lambda n, sh, dt: s5.enter_context(nc.sbuf_tensor("p5_" + n, sh, dt))
            onesf = A("onesf", [128, 128], F32)
            onesb = A("onesb", [128, 128], BF16)
            triub = A("triub", [128, 128], BF16)
            identb = A("identb", [128, 128], BF16)
            iota = A("iota", [128, 512], F32)
            tokhl = A("tokhl", [128, NT, 2], F32)
            zero = A("zero", [128, D], F32)
            lo = A("lo", [128, NE], F32)
            tau = A("tau", [128, NE], F32)
            part = A("part", [128, NE], F32)
            gw = A("gw", [128, NE], F32)
            cmp = A("cmp", [128, NT * NE], F32)
            self_ = A("self", [128, NT * NE], F32)
            selb = A("selb", [128, NT * NE], BF16)
            posm = A("posm", [128, NT * NE], F32)
            r1 = A("r1", [128, NT * NE], F32)
            R = A("R", [128, NT * NE * 5], BF16)
            OH = [A("OH%d" % i, [128, 512], BF16) for i in range(4)]
            idxv = [A("idxv%d" % i, [128, 32], F32) for i in range(2)]
            idxf = [A("idxf%d" % i, [128, 4], F32) for i in range(2)]
            idxi = [A("idxi%d" % i, [128, 4], I32) for i in range(2)]
            val = [A("val%d" % i, [128, 4], F32) for i in range(2)]
            xin = [A("xin%d" % i, [128, 4, D], BF16) for i in range(2)]
            xinT = [A("xinT%d" % i, [128, 8, 512], BF16) for i in range(2)]
            wg = [A("wg%d" % i, [128, 8, 512], BF16) for i in range(3)]
            wu = [A("wu%d" % i, [128, 8, 512], BF16) for i in range(3)]
            wd = [A("wd%d" % i, [128, 6, D], BF16) for i in range(3)]
            sg = [A("sg%d" % i, [128, 512], F32) for i in range(2)]
            act = [A("act%d" % i, [128, 12, 512], BF16) for i in range(2)]
            ys = [A("ys%d" % i, [128, D], F32) for i in range(4)]
            mo = [A("mo%d" % i, [128, D], F32) for i in range(4)]
            Bc = Buf()
            Blo, Btau, Bpart, Bgw, Bcmp, Bsel, Bposm, BR = (Buf() for _ in range(8))
            BOH = [Buf() for _ in range(4)]
            Bidxv, Bidx, Bval = [Buf(), Buf()], [Buf(), Buf()], [Buf(), Buf()]
            Bxin, BxinT = [Buf(), Buf()], [Buf(), Buf()]
            Bwg, Bwu, Bwd = [Buf() for _ in range(3)], [Buf() for _ in range(3)], [Buf() for _ in range(3)]
            Bsg, Bact = [Buf(), Buf()], [Buf(), Buf()]
            Bys, Bmo = [Buf() for _ in range(4)], [Buf() for _ in range(4)]

            kb.op("pool", [], [], lambda e: e.memset(onesf[:], 1.0), pwrites=[Bc])
            kb.op("pool", [], [], lambda e: e.memset(onesb[:], 1.0), pwrites=[Bc])
            kb.op("pool", [], [], lambda e: e.memset(zero[:], 0.0), pwrites=[Bc])
            kb.op("pool", [], [Blo], lambda e: e.memset(lo[:], 0.0))
            kb.dma("pool", [], [], triub[:], triu_d[:, :], pwrites=[Bc])
            kb.dma("pool", [], [], identb[:], ident_d[:, :], pwrites=[Bc])
            kb.dma("sp", [], [], iota[:], iota_d[:, :], pwrites=[Bc])
            kb.dma("sp", [], [], tokhl[:], tokhl_d[:, :, :], pwrites=[Bc])
            moe_t = moe_d.rearrange("(t p) d -> t p d", p=128)
            for t in range(NT):
                kb.dma("sp", [Bc], [], moe_t[t], zero[:], pwrites=[B_moe])

            def issue_gu(n):
                if n >= NE * 3:
                    return
                e, fb = n // 3, n % 3
                b = n % 3
                kb.dma("pool", [], [Bwg[b]], wg[b][:], wg_d[e, :, fb * 512:(fb + 1) * 512].rearrange("(k p) f -> p k f", p=128))
                kb.dma("pool", [], [Bwu[b]], wu[b][:], wu_d[e, :, fb * 512:(fb + 1) * 512].rearrange("(k p) f -> p k f", p=128))

            def issue_d(m):
                if m >= NE * 2:
                    return
                e, half = m // 2, m % 2
                b = m % 3
                kb.dma("pool", [], [Bwd[b]], wd[b][:], wd_d[e, half * 768:(half + 1) * 768, :].rearrange("(k p) n -> p k n", p=128))

            issue_gu(0)
            issue_gu(1)
            issue_d(0)
            issue_d(1)
            issue_d(2)

            aff3 = AP(aff_all, 0, [pdim(aff_all[:]), [NE, NT], [1, NE]])
            cmp3 = AP(cmp, 0, [pdim(cmp[:]), [NE, NT], [1, NE]])
            cmpT = AP(cmp, 0, [pdim(cmp[:]), [1, NE], [NE, NT]])

            def bc16(tile_):
                return AP(tile_, 0, [pdim(tile_[:]), [0, NT], [1, NE]])

            for it in range(1, NBIS + 1):
                w = 2.0 ** (-it)
                kb.op("dve", [Blo], [Btau], lambda e: e.tensor_scalar(out=tau[:], in0=lo[:], scalar1=w, scalar2=None, op0=ALU.add))
                kb.op("dve", [B_aff, Btau], [Bcmp], lambda e: e.tensor_tensor(out=cmp3, in0=aff3, in1=bc16(tau), op=ALU.is_ge))
                kb.op("dve", [Bcmp], [Bpart], lambda e: e.tensor_reduce(out=part[:], in_=cmpT, axis=AX.X, op=ALU.add))
                bk, bb = bank()
                kb.op("pe", [Bpart, Bc], [bb], lambda e: e.matmul(bk[:, 0:NE], onesf[:], part[:], start=True, stop=True))
                kb.op("dve", [bb], [Bgw], lambda e: e.tensor_scalar(out=gw[:], in0=bk[:, 0:NE], scalar1=CAP - 0.5, scalar2=w,
                                                                  op0=ALU.is_ge, op1=ALU.mult))
                kb.op("dve", [Bgw, Blo], [Blo], lambda e: e.tensor_tensor(out=lo[:], in0=lo[:], in1=gw[:], op=ALU.add))
            self3 = AP(self_, 0, [pdim(self_[:]), [NE, NT], [1, NE]])
            kb.op("dve", [B_aff, Blo], [Bsel], lambda e: e.tensor_tensor(out=self3, in0=aff3, in1=bc16(lo), op=ALU.is_ge))
            kb.op("dve", [Bsel], [], lambda e: e.tensor_copy(out=selb[:], in_=self_[:]), pwrites=[Bsel])
            bk, bb = bank()

            def prefix(e):
                for t in range(NT):
                    o = bk[:, t * NE:(t + 1) * NE]
                    for tp in range(t):
                        e.matmul(o, onesb[:], selb[:, tp * NE:(tp + 1) * NE], start=(tp == 0), stop=False)
                    ins = e.matmul(o, triub[:], selb[:, t * NE:(t + 1) * NE], start=(t == 0), stop=True)
                return ins
            kb.op("pe", [Bsel, Bc], [bb], prefix)
            kb.op("dve", [bb, Bsel], [Bposm], lambda e: e.scalar_tensor_tensor(out=posm[:], in0=bk, scalar=1.0, in1=self_[:],
                                                                            op0=ALU.add, op1=ALU.mult))
            kb.op("dve", [Bposm], [Bposm], lambda e: e.tensor_scalar(out=posm[:], in0=posm[:], scalar1=-1.0, scalar2=None, op0=ALU.add))
            Rv = lambda j: AP(R, j, [pdim(R[:]), [5, NT * NE]])
            kb.op("dve", [Bc], [BR], lambda e: e.tensor_copy(
                out=AP(R, 0, [pdim(R[:]), [5 * NE, NT], [5, NE], [1, 2]]),
                in_=AP(tokhl, 0, [pdim(tokhl[:]), [2, NT], [0, NE], [1, 2]])))
            kb.op("dve", [B_aff], [], lambda e: e.tensor_copy(out=Rv(2), in_=aff_all[:]), pwrites=[BR])
            kb.op("dve", [B_aff, BR], [Bcmp], lambda e: e.tensor_tensor(out=r1[:], in0=aff_all[:], in1=Rv(2), op=ALU.subtract))
            kb.op("dve", [Bcmp], [], lambda e: e.tensor_copy(out=Rv(3), in_=r1[:]), pwrites=[BR])
            kb.op("dve", [Bcmp, BR], [Bcmp], lambda e: e.tensor_tensor(out=r1[:], in0=r1[:], in1=Rv(3), op=ALU.subtract))
            kb.op("dve", [Bcmp], [], lambda e: e.tensor_copy(out=Rv(4), in_=r1[:]), pwrites=[BR])

            h2rows = h2_d
            oh_ctr = [0]

            def route(e):
                b = e % 2
                bks = [bank() for _ in range(4)]
                for t in range(NT):
                    oi = oh_ctr[0] % 4
                    oh_ctr[0] += 1
                    eng = "dve" if t % 2 == 0 else "pool"
                    kb.op(eng, [Bposm, Bc], [BOH[oi]], lambda en: en.tensor_scalar(
                        out=OH[oi][:], in0=iota[:], scalar1=posm[:, t * NE + e:t * NE + e + 1], scalar2=None, op0=ALU.is_equal))

                    def mm(en):
                        for cj in range(4):
                            ins = en.matmul(bks[cj][0][:, 0:5], OH[oi][:, cj * 128:(cj + 1) * 128],
                                            R[:, (t * NE + e) * 5:(t * NE + e) * 5 + 5], start=(t == 0), stop=(t == NT - 1))
                        return ins
                    if t == 0:
                        kb.op("pe", [BOH[oi], BR], [x[1] for x in bks], mm)
                    else:
                        kb.op("pe", [BOH[oi], BR], [], mm, pwrites=[x[1] for x in bks])
                for cj in range(4):
                    kb.op("dve", [bks[cj][1]], [] if cj else [Bidxv[b]], lambda en: en.tensor_copy(
                        out=idxv[b][:, cj * 8:cj * 8 + 5], in_=bks[cj][0][:, 0:5]), pwrites=[Bidxv[b]] if cj else [])
                v3 = lambda j: AP(idxv[b], j, [pdim(idxv[b][:]), [8, 4]])
                kb.op("dve", [Bidxv[b]], [Bidx[b]], lambda en: en.scalar_tensor_tensor(
                    out=idxf[b][:], in0=v3(0), scalar=128.0, in1=v3(1), op0=ALU.mult, op1=ALU.add))
                kb.op("dve", [Bidx[b]], [], lambda en: en.tensor_copy(out=idxi[b][:], in_=idxf[b][:]), pwrites=[Bidx[b]])
                kb.op("dve", [Bidxv[b]], [Bval[b]], lambda en: en.tensor_tensor(out=val[b][:], in0=v3(2), in1=v3(3), op=ALU.add))
                kb.op("dve", [Bidxv[b], Bval[b]], [Bval[b]], lambda en: en.tensor_tensor(out=val[b][:], in0=val[b][:], in1=v3(4), op=ALU.add))
                for cj in range(4):
                    kb.dma("pool", [Bidx[b], B_h2], [] if cj else [Bxin[b]], xin[b][:, cj, :], h2rows[:, :],
                           pwrites=[Bxin[b]] if cj else [],
                           indirect=dict(out_offset=None, in_offset=bass.IndirectOffsetOnAxis(ap=idxi[b][:, cj:cj + 1], axis=0)))
                for k in range(8):
                    bk2, bb2 = bank()
                    bkb = bk2.bitcast(BF16)

                    def tp(en):
                        for cj in range(4):
                            ins = en.transpose(bkb[:, cj * 128:(cj + 1) * 128], xin[b][:, cj, k * 128:(k + 1) * 128], identb[:])
                        return ins
                    kb.op("pe", [Bxin[b], Bc], [bb2], tp)
                    kb.op("dve" if k % 2 else "act", [bb2], [] if k else [BxinT[b]],
                          (lambda en: en.tensor_copy(out=xinT[b][:, k, :], in_=bkb[:, 0:512])) if k % 2 else
                          (lambda en: en.copy(out=xinT[b][:, k, :], in_=bkb[:, 0:512])),
                          pwrites=[BxinT[b]] if k else [])

            def compute(e):
                b = e % 2
                for fb in range(3):
                    n = e * 3 + fb
                    wb = n % 3
                    for j in range(4):
                        bg, bbg = bank()
                        bu, bbu = bank()

                        def mm(en):
                            for bk_, w_ in ((bg, wg[wb]), (bu, wu[wb])):
                                for k in range(8):
                                    ins = en.matmul(bk_, w_[:, k, j * 128:(j + 1) * 128], xinT[b][:, k, :], start=(k == 0), stop=(k == 7))
                            return ins
                        kb.op("pe", [BxinT[b], Bwg[wb], Bwu[wb]], [bbg, bbu], mm)
                        si = (fb * 4 + j) % 2
                        kb.op("act", [bbg], [Bsg[si]], lambda en: en.activation(out=sg[si][:], in_=bg, func=AF.Silu))
                        fi = fb * 4 + j
                        kb.op("dve", [Bsg[si], bbu], [] if fi else [Bact[b]], lambda en: en.tensor_tensor(
                            out=act[b][:, fi, :], in0=sg[si][:], in1=bu, op=ALU.mult), pwrites=[Bact[b]] if fi else [])
                    issue_gu(n + 2)
                    if fb == 0 and e + 1 < NE:
                        route(e + 1)
                for cj in range(4):
                    yi = cj
                    for half in range(2):
                        bk, bb = bank()

                        def mm(en):
                            for fi in range(12):
                                m = e * 2 + fi // 6
                                ins = en.matmul(bk, act[b][:, fi, cj * 128:(cj + 1) * 128], wd[m % 3][:, fi % 6, half * 512:(half + 1) * 512],
                                                start=(fi == 0), stop=(fi == 11))
                            return ins
                        kb.op("pe", [Bact[b], Bwd[(e * 2) % 3], Bwd[(e * 2 + 1) % 3]], [bb], mm)
                        kb.op("act", [bb, Bval[b]], [] if half else [Bys[yi]], lambda en: en.activation(
                            out=ys[yi][:, half * 512:(half + 1) * 512], in_=bk, func=AF.Copy, scale=val[b][:, cj:cj + 1]),
                            pwrites=[Bys[yi]] if half else [])
                issue_d(e * 2 + 3)
                issue_d(e * 2 + 4)
                for cj in range(4):
                    kb.dma("pool", [Bidx[b], B_moe], [Bmo[cj]], mo[cj][:, :], moe_d[:, :],
                           indirect=dict(out_offset=None, in_offset=bass.IndirectOffsetOnAxis(ap=idxi[b][:, cj:cj + 1], axis=0)))
                for cj in range(4):
                    kb.op("pool", [Bmo[cj], Bys[cj]], [Bmo[cj]], lambda en: en.tensor_tensor(out=mo[cj][:], in0=mo[cj][:], in1=ys[cj][:], op=ALU.add))
                for cj in range(4):
                    kb.dma("pool", [Bidx[b], Bmo[cj]], [], moe_d[:, :], mo[cj][:, :], pwrites=[B_moe],
                           indirect=dict(out_offset=bass.IndirectOffsetOnAxis(ap=idxi[b][:, cj:cj + 1], axis=0), in_offset=None))

            route(0)
            for e in range(int(os.environ.get("NEXP", str(NE)))):
                compute(e)
            kb.barrier()

    if os.environ.get("SKIPMOE", "") == "":
        moe_phase()

    with ExitStack() as s6:
        A = lambda n, sh, dt: s6.enter_context(nc.sbuf_tensor("p6_" + n, sh, dt))
        g2bc, lngbc, lnbbc = (A(n, [128, D], F32) for n in ("g2bc", "lngbc", "lnbbc"))
        xt = [A("xt%d" % i, [128, D], F32) for i in range(2)]
        mt = [A("mt%d" % i, [128, D], F32) for i in range(2)]
        yy = [A("y%d" % i, [128, D], F32) for i in range(2)]
        xh = [A("xh%d" % i, [128, D], F32) for i in range(2)]
        oo = [A("o%d" % i, [128, D], F32) for i in range(2)]
        L = LNState(A, "ln")
        Bc6 = Buf()
        Bxt, Bmt, By, Bxh, Boo = ([Buf(), Buf()] for _ in range(5))
        kb.dma("sp", [B_mod], [], g2bc[:], bcast_row(mod_d[0:1, 5120:6144], D), pwrites=[Bc6])
        kb.dma("sp", [], [], lngbc[:], bcast_row(ln2g_d[0:1, :], D), pwrites=[Bc6])
        kb.dma("sp", [], [], lnbbc[:], bcast_row(ln2b_d[0:1, :], D), pwrites=[Bc6])
        x1_t = x1_d.rearrange("(t p) d -> t p d", p=128)
        moe_t = moe_d.rearrange("(t p) d -> t p d", p=128)
        out_t = out_d.rearrange("(t p) d -> t p d", p=128)
        stA = {}

        def stageA6(t):
            b = t % 2
            kb.dma("sp", [B_x1], [Bxt[b]], xt[b][:], x1_t[t])
            kb.dma("sp", [B_moe], [Bmt[b]], mt[b][:], moe_t[t])
            kb.op("pool", [Bmt[b], Bc6], [Bmt[b]], lambda e: e.tensor_tensor(out=mt[b][:], in0=mt[b][:], in1=g2bc[:], op=ALU.mult))
            kb.op("dve", [Bxt[b], Bmt[b]], [By[b]], lambda e: e.scalar_tensor_tensor(
                out=yy[b][:], in0=xt[b][:], scalar=ALPHA, in1=mt[b][:], op0=ALU.mult, op1=ALU.add))
            stA[t] = ln_stats(L, yy[b], By[b])

        def stageB6(t):
            b = t % 2
            st, Bst = stA.pop(t)
            ln_apply(st, Bst, yy[b], By[b], xh[b], Bxh[b])
            kb.op("dve", [Bxh[b], Bc6], [Boo[b]], lambda e: e.tensor_tensor(out=oo[b][:], in0=xh[b][:], in1=lngbc[:], op=ALU.mult))
            kb.op("pool", [Boo[b], Bc6], [Boo[b]], lambda e: e.tensor_tensor(out=oo[b][:], in0=oo[b][:], in1=lnbbc[:], op=ALU.add))
            kb.dma("sp", [Boo[b]], [Buf()], out_t[t], oo[b][:])

        stageA6(0)
        for t in range(NT):
            if t + 1 < NT:
                stageA6(t + 1)
            stageB6(t)
        kb.barrier()


def _finish(nc, kb, out_d, x_d):
    kb.barrier()
    tok = kb.dma("sp", [], [Buf()], out_d[0:128, :], x_d[0:128, :])
    kb._wait("sp", [tok])


def _t5_bucket(rel):
    half = 16
    ret = (rel > 0).astype(np.int32) * half
    n = np.abs(rel)
    large = 8 + (np.log(np.maximum(n, 1) / 8) / np.log(1024 / 8) * (half - 8)).astype(np.int32)
    large = np.minimum(large, half - 1)
    return ret + np.where(n < 8, n, large).astype(np.int32)


def _static_tables():
    a = np.arange(128)[:, None]
    c = np.arange(MW)[None, :]
    rel = a - c + C0
    mult = ((np.abs(rel) <= 64).astype(np.float32)
            + ((rel % 4 == 0) & (np.abs(rel) <= 256)).astype(np.float32)
            + ((rel % 16 == 0) & (np.abs(rel) <= 1024)).astype(np.float32))
    bucket = _t5_bucket(rel)
    inv = (10000.0 ** (-(np.arange(0, 32, 2, dtype=np.float32)) / np.float32(32))).astype(np.float32)
    ang = (np.arange(S, dtype=np.float32)[:, None] * inv[None, :]).astype(np.float32)
    cos = np.cos(ang.astype(np.float64)).astype(np.float32).T
    sin = np.sin(ang.astype(np.float64)).astype(np.float32).T
    rope_cos = np.ascontiguousarray(np.concatenate([cos, cos], axis=0))
    rope_sin = np.ascontiguousarray(np.concatenate([-sin, sin], axis=0))
    tokhl = np.zeros((128, NT, 2), np.float32)
    tokhl[:, :, 0] = np.arange(NT)[None, :]
    tokhl[:, :, 1] = np.arange(128)[:, None]
    return dict(
        toep_mult=mult.astype(np.float32), bucket=bucket,
        rope_cos=rope_cos, rope_sin=rope_sin,
        ident=np.eye(128, dtype=np.float32),
        triu=np.triu(np.ones((128, 128), np.float32), k=1),
        iota512=np.tile(np.arange(512, dtype=np.float32)[None, :], (128, 1)),
        tokhl=tokhl,
    )


def make_in_maps(inputs):
    f = lambda a: np.ascontiguousarray(np.asarray(a, dtype=np.float32))
    st = _static_tables()
    x = f(inputs["x"])
    c = f(inputs["c"])
    w_in = f(inputs["w_in"][0])
    kr = w_in[:, 640:672]
    w_kr = np.zeros((D, 192), np.float32)
    w_kr[:, 64:96] = kr
    w_kr[:, 96 + 64:96 + 80] = kr[:, 16:32]
    w_kr[:, 96 + 80:96 + 96] = kr[:, 0:16]
    w_uq = f(inputs["w_uq"][0]).reshape(384, 8, 96)
    w_uq_rot = w_uq.copy()
    w_uq_rot[:, :, 64:80] = w_uq[:, :, 80:96]
    w_uq_rot[:, :, 80:96] = w_uq[:, :, 64:80]
    w_uq2 = np.ascontiguousarray(np.stack([w_uq.reshape(384, 768), w_uq_rot.reshape(384, 768)], axis=1))
    w_ukv = f(inputs["w_ukv"][0]).reshape(256, 8, 128)
    rel_bias = f(inputs["rel_bias"])
    toep = np.ascontiguousarray(np.transpose(rel_bias[st["bucket"]], (2, 0, 1)))
    shared = dict(
        w_ada=f(inputs["w_ada"][0]), b_ada=f(inputs["b_ada"][0]).reshape(1, -1),
        w_in=w_in, w_kr=w_kr,
        g_q=np.ascontiguousarray(f(inputs["q_norm_g"][0]).reshape(3, 128).T),
        g_kv=np.ascontiguousarray(f(inputs["kv_norm_g"][0]).reshape(2, 128).T),
        w_uq2=w_uq2,
        w_ukv_k=np.ascontiguousarray(w_ukv[:, :, 0:64].reshape(256, 512)),
        w_ukv_v=np.ascontiguousarray(w_ukv[:, :, 64:128].reshape(256, 512)),
        rope_cos=st["rope_cos"], rope_sin=st["rope_sin"],
        relb_toep=toep, toep_mult=st["toep_mult"],
        w_out=f(inputs["w_out"][0]), ln1_g=f(inputs["ln1_g"][0]).reshape(1, -1), ln1_b=f(inputs["ln1_b"][0]).reshape(1, -1),
        w_router=f(inputs["w_router"][0]),
        w_gate=f(inputs["w_gate"][0]), w_up=f(inputs["w_up"][0]), w_down=f(inputs["w_down"][0]),
        ln2_g=f(inputs["ln2_g"][0]).reshape(1, -1), ln2_b=f(inputs["ln2_b"][0]).reshape(1, -1),
        ident=st["ident"], triu=st["triu"], iota512=st["iota512"], tokhl=st["tokhl"],
    )
    maps = []
    for b in range(x.shape[0]):
        m = dict(shared)
        m["x"] = x[b]
        m["c_fm"] = np.ascontiguousarray(c[b].reshape(8, 128).T)
        maps.append(m)
    return maps


def kernel(**inputs):
    maps = make_in_maps(inputs)
    nc = build_program()
    res = run_bass_kernel_spmd(nc, maps, core_ids=list(range(len(maps))))
    return np.stack([np.asarray(r["out"], dtype=np.float32) for r in res.results], axis=0)
```

```python
import math
import os
from contextlib import ExitStack

import numpy as np
import concourse.bass as bass
import concourse.mybir as mybir
from concourse.bass_utils import run_bass_kernel_spmd

F32 = mybir.dt.float32
BF16 = mybir.dt.bfloat16
I32 = mybir.dt.int32
AF = mybir.ActivationFunctionType
ALU = mybir.AluOpType
AX = mybir.AxisListType

D = 1024
S = 4096
NT = 32
NG = 8
ALPHA = 2.0 ** 0.25
EPS = 1e-6
NE = 16
CAP = 512
DFF = 1536
C0 = 1408
MW = 2944
NBIS = 26


class Buf:
    __slots__ = ("w", "r", "name", "excl")

    def __init__(self, name="", excl=False):
        self.excl = excl
        self.w = []
        self.r = []
        self.name = name


def _compact(toks):
    best = {}
    for t in toks:
        if t[0] not in best or best[t[0]][2] < t[2]:
            best[t[0]] = t
    return list(best.values())


class KB:
    COMPUTE = ("pe", "act", "dve", "pool")
    NDS = 12

    def __init__(self, nc, es):
        self.nc = nc
        self.E = {"pe": nc.tensor, "act": nc.scalar, "dve": nc.vector, "pool": nc.gpsimd, "sp": nc.sync}
        self.csem = {e: es.enter_context(nc.semaphore("c_" + e)) for e in self.COMPUTE}
        self.ccnt = {e: 0 for e in self.COMPUTE}
        self.dsem = {q: [es.enter_context(nc.semaphore("d_%s%d" % (q, i))) for i in range(self.NDS)]
                     for q in ("sp", "pool", "act")}
        self.dcnt = {q: 0 for q in self.dsem}
        self.dtok = {q: [None] * self.NDS for q in self.dsem}
        self.seen = {e: {} for e in self.E}
        self.nwait = 0

    def _wait(self, eng, toks):
        need = {}
        for t in toks:
            if t is None:
                continue
            key, sem, val = t
            if self.seen[eng].get(key, 0) >= val:
                continue
            if need.get(key, (None, 0))[1] < val:
                need[key] = (sem, val)
        for key, (sem, val) in need.items():
            self.E[eng].wait_ge(sem, val)
            self.seen[eng][key] = val
            self.nwait += 1

    def _deps(self, eng, reads, writes, pwrites, is_dma):
        own = None if is_dma else "c_" + eng
        toks = []
        for b in reads:
            for t in b.w:
                if not (eng == "pe" and t[0] == own):
                    toks.append(t)
            if b.excl:
                toks.extend(t for t in b.r if t[0] != own)
        for b in writes:
            toks.extend(t for t in b.w if t[0] != own)
            toks.extend(t for t in b.r if t[0] != own)
        for b in pwrites:
            toks.extend(t for t in b.r if t[0] != own)
        return toks

    def _commit(self, tok, reads, writes, pwrites):
        for b in reads:
            b.r.append(tok)
            if len(b.r) > 16:
                b.r = _compact(b.r)
        for b in writes:
            b.w = [tok]
            b.r = []
        for b in pwrites:
            b.w.append(tok)
            if len(b.w) > 16:
                b.w = _compact(b.w)

    def op(self, eng, reads, writes, fn, pwrites=()):
        self._wait(eng, self._deps(eng, reads, writes, pwrites, False))
        inst = fn(self.E[eng])
        self.ccnt[eng] += 1
        tok = ("c_" + eng, self.csem[eng], self.ccnt[eng])
        inst.then_inc(self.csem[eng], 1)
        self._commit(tok, reads, writes, pwrites)
        return tok

    def dma(self, q, reads, writes, out, in_, pwrites=(), indirect=None, **kw):
        i = self.dcnt[q]
        slot = i % self.NDS
        self._wait(q, self._deps(q, reads, writes, pwrites, True) + [self.dtok[q][slot]])
        if indirect is None:
            inst = self.E[q].dma_start(out=out, in_=in_, **kw)
        else:
            inst = self.E[q].indirect_dma_start(out=out, in_=in_, **indirect)
        val = 16 * (i // self.NDS + 1)
        sem = self.dsem[q][slot]
        inst.then_inc(sem, 16)
        tok = ("d_%s%d" % (q, slot), sem, val)
        self.dtok[q][slot] = tok
        self.dcnt[q] += 1
        self._commit(tok, reads, writes, pwrites)
        return tok

    def barrier(self, engines=None):
        toks = []
        for e in self.COMPUTE:
            if self.ccnt[e]:
                toks.append(("c_" + e, self.csem[e], self.ccnt[e]))
        for q in self.dsem:
            toks.extend(t for t in self.dtok[q] if t is not None)
        for e in (engines or self.E):
            self._wait(e, toks)


def AP(t, off, dims):
    return bass.AP(t, off, [list(d) for d in dims])


def pdim(ap):
    return list(ap.ap[0])


def build_program(stages=99, dbg=()):
    nc = bass.Bass("TRN2", target_bir_lowering=False)
    es = ExitStack()
    with es:
        _build(nc, es, stages, dbg)
    return nc


def _build(nc, es, stages, dbg):
    def din(name, shape, dt=F32):
        return nc.dram_tensor(name, list(shape), dt, kind="ExternalInput").ap()

    def dscr(name, shape, dt):
        kind = "ExternalOutput" if name in dbg else "Internal"
        return nc.dram_tensor(name, list(shape), dt, kind=kind).ap()

    x_d = din("x", [S, D])
    cfm_d = din("c_fm", [128, 8])
    wada_d = din("w_ada", [D, 6 * D])
    bada_d = din("b_ada", [1, 6 * D])
    win_d = din("w_in", [D, 2208])
    wkr_d = din("w_kr", [D, 192])
    gq_d = din("g_q", [128, 3])
    gkv_d = din("g_kv", [128, 2])
    wuq_d = din("w_uq2", [384, 2, 768])
    wukvk_d = din("w_ukv_k", [256, 512])
    wukvv_d = din("w_ukv_v", [256, 512])
    cos_d = din("rope_cos", [32, S])
    sin_d = din("rope_sin", [32, S])
    toep_d = din("relb_toep", [8, 128, MW])
    mult_d = din("toep_mult", [128, MW])
    wout_d = din("w_out", [D, D])
    ln1g_d = din("ln1_g", [1, D])
    ln1b_d = din("ln1_b", [1, D])
    wr_d = din("w_router", [D, NE])
    wg_d = din("w_gate", [NE, D, DFF])
    wu_d = din("w_up", [NE, D, DFF])
    wd_d = din("w_down", [NE, DFF, D])
    ln2g_d = din("ln2_g", [1, D])
    ln2b_d = din("ln2_b", [1, D])
    ident_d = din("ident", [128, 128])
    triu_d = din("triu", [128, 128])
    iota_d = din("iota512", [128, 512])
    tokhl_d = din("tokhl", [128, NT, 2])
    out_d = nc.dram_tensor("out", [S, D], F32, kind="ExternalOutput").ap()

    mod_d = dscr("mod_s", [1, 6 * D], F32)
    qT_d = dscr("qT_s", [8, 96, S], BF16)
    kT_d = dscr("kT_s", [8, 96, S], BF16)
    Vm_d = dscr("Vm_s", [NT, 128, 768], BF16)
    dqT_d = dscr("dqT_s", [4, 128, S], BF16)
    dkT_d = dscr("dkT_s", [4, 128, S], BF16)
    Vd_d = dscr("Vd_s", [NT, 128, 768], BF16)
    attnT_d = dscr("attnT_s", [8, 128, S], BF16)
    x1_d = dscr("x1_s", [S, D], F32)
    h2_d = dscr("h2_s", [S, D], BF16)
    moe_d = dscr("moe_s", [S, D], F32)
    aff_d = dscr("aff_s", [128, NT * NE], F32)
    idx_d = dscr("idx_s", [128, NE * 4 * 8], F32)

    kb = KB(nc, es)
    B_mod, B_q, B_k, B_vm, B_dq, B_dk, B_vd = (Buf(n) for n in ("mod", "q", "k", "vm", "dq", "dk", "vd"))
    B_attn, B_x1, B_h2, B_moe = Buf("attn"), Buf("x1"), Buf("h2"), Buf("moe")

    ps = es.enter_context(nc.psum_tensor("ps", [128, 4096], F32))
    PB = [Buf("bank%d" % i, excl=True) for i in range(8)]
    bank_ctr = [0]

    def bank():
        i = bank_ctr[0] % 8
        bank_ctr[0] += 1
        return ps[:, i * 512:(i + 1) * 512], PB[i]

    modfm = es.enter_context(nc.sbuf_tensor("modfm", [128, 16], F32))
    aff_all = es.enter_context(nc.sbuf_tensor("aff_all", [128, NT * NE], F32))
    B_modfm, B_aff = Buf("modfm"), Buf("aff")

    def bcast_row(dram_ap_row, n):
        return AP(dram_ap_row.tensor, dram_ap_row.offset, [[0, 128], [1, n]])

    with ExitStack() as s0:
        A = lambda n, sh, dt: s0.enter_context(nc.sbuf_tensor("p0_" + n, sh, dt))
        cfm = A("cfm", [128, 8], F32)
        sig = A("sig", [128, 8], F32)
        cond = A("cond", [128, 8], F32)
        bada = A("bada", [1, 6 * D], F32)
        modrow = A("modrow", [1, 6 * D], F32)
        wa = [A("wa%d" % i, [128, 8, 512], F32) for i in range(3)]
        Bc, Bcond, Bb, Bm = Buf(), Buf(), Buf(), Buf()
        Bwa = [Buf() for _ in range(3)]
        kb.dma("sp", [], [Bc], cfm[:], cfm_d[:, :])
        kb.dma("sp", [], [Bb], bada[:], bada_d[:, :])
        wada_v = wada_d.rearrange("(k p) n -> p k n", p=128)
        for j in range(2):
            kb.dma("sp", [], [Bwa[j]], wa[j][:], wada_v[:, :, j * 512:(j + 1) * 512])
        kb.op("act", [Bc], [Bcond], lambda e: e.activation(out=sig[:], in_=cfm[:], func=AF.Sigmoid))
        kb.op("dve", [Bc, Bcond], [Bcond], lambda e: e.tensor_tensor(out=cond[:], in0=cfm[:], in1=sig[:], op=ALU.mult))
        for j in range(12):
            if j + 2 < 12:
                kb.dma("sp", [], [Bwa[(j + 2) % 3]], wa[(j + 2) % 3][:], wada_v[:, :, (j + 2) * 512:(j + 3) * 512])
            bk, bb = bank()
            w = wa[j % 3]

            def mm(e, w=w, bk=bk):
                for k in range(8):
                    i = e.matmul(bk[0:1, :], cond[:, k:k + 1], w[:, k, :], start=(k == 0), stop=(k == 7))
                return i
            kb.op("pe", [Bcond, Bwa[j % 3]], [bb], mm)
            kb.op("dve", [bb, Bb], [], pwrites=[Bm], fn=lambda e, bk=bk, j=j: e.tensor_tensor(
                out=modrow[0:1, j * 512:(j + 1) * 512], in0=bk[0:1, :], in1=bada[0:1, j * 512:(j + 1) * 512], op=ALU.add))
        kb.dma("sp", [Bm], [B_mod], mod_d[:, :], modrow[:])
        onef = A("onef", [1, 1], F32)
        kb.op("pool", [], [Bc], lambda e: e.memset(onef[:], 1.0))
        bk, bb = bank()

        def mmT(e, bk=bk):
            for j in range(16):
                ins = e.matmul(bk[:, j:j + 1], modrow[0:1, j * 128:(j + 1) * 128], onef[0:1, 0:1], start=True, stop=True)
            return ins
        kb.op("pe", [Bm, Bc], [bb], mmT)
        kb.op("dve", [bb], [B_modfm], lambda e, bk=bk: e.tensor_copy(out=modfm[:], in_=bk[:, 0:16]))
        kb.op("dve", [B_modfm], [B_modfm], lambda e: e.tensor_scalar(
            out=modfm[:, 8:16], in0=modfm[:, 8:16], scalar1=1.0, scalar2=None, op0=ALU.add))
        kb.barrier()
    if stages <= 0:
        _finish(nc, kb, out_d, x_d)
        return

    with ExitStack() as s1:
        A = lambda n, sh, dt: s1.enter_context(nc.sbuf_tensor("p1_" + n, sh, dt))
        win = A("win", [128, 8, 2208], BF16)
        wkr = A("wkr", [128, 8, 192], BF16)
        wuq = A("wuq", [128, 3, 2, 768], BF16)
        wukvk = A("wukvk", [128, 2, 512], BF16)
        wukvv = A("wukvv", [128, 2, 512], BF16)
        ident = A("ident", [128, 128], F32)
        onesq = A("onesq", [128, 128], F32)
        oneskv = A("oneskv", [128, 128], F32)
        gq = A("gq", [128, 3], F32)
        gkv = A("gkv", [128, 2], F32)
        xg = [A("xg%d" % i, [128, 4, 1024], F32) for i in range(2)]
        hT = [A("hT%d" % i, [128, 8, 512], BF16) for i in range(2)]
        cT = A("cT", [128, 5, 512], F32)
        sq = A("sq", [128, 5, 512], F32)
        rstd = A("rstd", [128, 2, 512], F32)
        cn = [A("cn%d" % i, [128, 5, 512], BF16) for i in range(2)]
        cosr = [A("cosr%d" % i, [128, 512], F32) for i in range(2)]
        sinr = [A("sinr%d" % i, [128, 512], F32) for i in range(2)]
        tmpa = [A("tmpa%d" % i, [128, 512], F32) for i in range(2)]
        tmpb = [A("tmpb%d" % i, [128, 512], F32) for i in range(2)]
        NO = 6
        ob = [A("ob%d" % i, [128, 512], BF16) for i in range(NO)]
        vo = [A("vo%d" % i, [128, 768], BF16) for i in range(4)]
        Bw, Bid, Bones, Bg = Buf(), Buf(), Buf(), Buf()
        Bxg = [Buf(), Buf()]
        BhT = [Buf(), Buf()]
        BcT, Bsq, Brstd = [Buf() for _ in range(5)], [Buf() for _ in range(5)], [Buf(), Buf()]
        Bcn = [[Buf() for _ in range(5)] for _ in range(2)]
        Brope = [Buf(), Buf()]
        Bta, Btb = [Buf(), Buf()], [Buf(), Buf()]
        Bob = [Buf() for _ in range(NO)]
        Bvo = [Buf() for _ in range(4)]
        ob_ctr, vo_ctr, t_ctr = [0], [0], [0]

        SKIP = os.environ.get('P1SKIP', '')
        for k in range(0 if 'w' in SKIP else 8):
            kb.dma("pool", [], [], win[:, k, :], win_d[k * 128:(k + 1) * 128, :], pwrites=[Bw])
            kb.dma("pool", [], [], wkr[:, k, :], wkr_d[k * 128:(k + 1) * 128, :], pwrites=[Bw])
        for c in range(0 if 'u' in SKIP else 3):
            kb.dma("pool", [], [], wuq[:, c, :, :], wuq_d[c * 128:(c + 1) * 128, :, :], pwrites=[Bw])
        for c in range(0 if 'v' in SKIP else 2):
            kb.dma("pool", [], [], wukvk[:, c, :], wukvk_d[c * 128:(c + 1) * 128, :], pwrites=[Bw])
            kb.dma("pool", [], [], wukvv[:, c, :], wukvv_d[c * 128:(c + 1) * 128, :], pwrites=[Bw])
        kb.dma("sp", [], [Bid], ident[:], ident_d[:, :])
        kb.dma("sp", [], [], gq[:], gq_d[:, :], pwrites=[Bg])
        kb.dma("sp", [], [], gkv[:], gkv_d[:, :], pwrites=[Bg])
        kb.op("pool", [], [], lambda e: e.memset(onesq[:], 1.0 / 384.0), pwrites=[Bones])
        kb.op("pool", [], [], lambda e: e.memset(oneskv[:], 1.0 / 256.0), pwrites=[Bones])
        for i in range(4):
            kb.op("pool", [], [Bvo[i]], lambda e, i=i: e.memset(vo[i][:], 1.0))

        x_v = x_d.rearrange("(t p) d -> p t d", p=128)

        def load_x(g):
            kb.dma("sp", [], [Bxg[g % 2]], xg[g % 2][:], x_v[:, g * 4:(g + 1) * 4, :])
            kb.dma("sp", [], [Brope[g % 2]], cosr[g % 2][64:96, :], cos_d[:, g * 512:(g + 1) * 512])
            kb.dma("sp", [], [Brope[g % 2]], sinr[g % 2][64:96, :], sin_d[:, g * 512:(g + 1) * 512])

        def next_ob():
            i = ob_ctr[0] % NO
            ob_ctr[0] += 1
            return ob[i], Bob[i]

        def evac_copy(eng, src, bsrc, dst, bdst):
            if eng == "act":
                kb.op("act", [bsrc], [bdst], lambda e: e.copy(out=dst, in_=src))
            else:
                kb.op("dve", [bsrc], [bdst], lambda e: e.tensor_copy(out=dst, in_=src))

        def rope_rows(bm, bbm, br, bbr, g, dst, bdst, full):
            i = t_ctr[0] % 2
            t_ctr[0] += 1
            kb.op("dve", [bbm, Brope[g % 2]], [Bta[i]], lambda e: e.tensor_tensor(
                out=tmpa[i][64:96, :], in0=bm[64:96, :], in1=cosr[g % 2][64:96, :], op=ALU.mult))
            kb.op("dve", [bbr, Brope[g % 2]], [Btb[i]], lambda e: e.tensor_tensor(
                out=tmpb[i][64:96, :], in0=br[64:96, :], in1=sinr[g % 2][64:96, :], op=ALU.mult))
            kb.op("dve", [Bta[i], Btb[i]], [bdst] if full else [], lambda e: e.tensor_tensor(
                out=dst[64:96, :], in0=tmpa[i][64:96, :], in1=tmpb[i][64:96, :], op=ALU.add),
                pwrites=[] if full else [bdst])

        def vo_views(vt, bk):
            o = AP(vt, 0, [pdim(vt[:]), [192, 4], [128, 2], [1, 64]])
            i = AP(bk.tensor, bk.offset, [pdim(bk), [128, 4], [64, 2], [1, 64]])
            return o, i

        load_x(0)
        LIM = float(os.environ.get('P1LIM', '99'))
        SUB = float(os.environ.get('P1SUB', '99'))
        for g in range(NG if LIM >= 99 else 1):
            if g + 1 < NG:
                load_x(g + 1)
            X, BX = xg[g % 2], Bxg[g % 2]
            H, BH = hT[g % 2], BhT[g % 2]
            CN, BCN = cn[g % 2], Bcn[g % 2]
            cols = slice(g * 512, (g + 1) * 512)
            for k in range(8):
                bk, bb = bank()

                def tp(e, bk=bk, k=k):
                    for i in range(4):
                        ins = e.transpose(bk[:, i * 128:(i + 1) * 128], X[:, i, k * 128:(k + 1) * 128], ident[:])
                    return ins
                kb.op("pe", [BX, Bid], [bb], tp)
                kb.op("act", [bb, B_modfm], [BH] if k == 0 else [], lambda e, bk=bk, k=k: e.activation(
                    out=H[:, k, :], in_=bk, func=AF.Identity, scale=modfm[:, 8 + k:9 + k], bias=modfm[:, k:k + 1]),
                    pwrites=[] if k == 0 else [BH])
            if LIM < 1:
                break
            for c in range(5):
                bk, bb = bank()

                def mm(e, bk=bk, c=c):
                    for k in range(8):
                        ins = e.matmul(bk, win[:, k, c * 128:(c + 1) * 128], H[:, k, :], start=(k == 0), stop=(k == 7))
                    return ins
                kb.op("pe", [BH, Bw], [bb], mm)
                if SUB >= 0.1:
                    kb.op("act", [bb], [Bsq[c]], lambda e, bk=bk, c=c: e.activation(out=sq[:, c, :], in_=bk, func=AF.Square))
                if SUB >= 0.15:
                    kb.op("dve", [bb, Bsq[c]], [BcT[c]], lambda e, bk=bk, c=c: e.tensor_copy(out=cT[:, c, :], in_=bk))
            if SUB < 0.3:
                break
            for which, (cs, ones_t) in enumerate((((0, 1, 2), onesq), ((3, 4), oneskv))):
                bk, bb = bank()

                def mm(e, bk=bk, cs=cs, ones_t=ones_t):
                    for n, c in enumerate(cs):
                        ins = e.matmul(bk, ones_t[:], sq[:, c, :], start=(n == 0), stop=(n == len(cs) - 1))
                    return ins
                kb.op("pe", [Bsq[c] for c in cs] + [Bones], [bb], mm)
                kb.op("dve", [bb], [Brstd[which]], lambda e, bk=bk, which=which: e.tensor_scalar(
                    out=rstd[:, which, :], in0=bk, scalar1=EPS, scalar2=None, op0=ALU.add))
                if SUB < 0.5:
                    continue
                kb.op("act", [Brstd[which]], [Brstd[which]], lambda e, which=which: e.activation(
                    out=rstd[:, which, :], in_=rstd[:, which, :], func=AF.Sqrt))
                kb.op("dve", [Brstd[which]], [Brstd[which]], lambda e, which=which: e.reciprocal(
                    out=rstd[:, which, :], in_=rstd[:, which, :]))
                if SUB < 0.7:
                    continue
                for c in cs:
                    gsc = gq[:, c:c + 1] if which == 0 else gkv[:, c - 3:c - 2]
                    kb.op("dve", [BcT[c], Brstd[which], Bg], [BCN[c]], lambda e, c=c, gsc=gsc, which=which: e.scalar_tensor_tensor(
                        out=CN[:, c, :], in0=cT[:, c, :], scalar=gsc, in1=rstd[:, which, :], op0=ALU.mult, op1=ALU.mult))
            if LIM < 2:
                break
            for h in range(8):
                bm, bbm = bank()
                br, bbr = bank()

                def mm(e, h=h, bm=bm, br=br):
                    for which, bk in ((0, bm), (1, br)):
                        for c in range(3):
                            ins = e.matmul(bk[0:96, :], wuq[:, c, which, h * 96:(h + 1) * 96], CN[:, c, :],
                                           start=(c == 0), stop=(c == 2))
                    return ins
                kb.op("pe", [BCN[0], BCN[1], BCN[2], Bw], [bbm, bbr], mm)
                o, bo = next_ob()
                kb.op("act", [bbm], [bo], lambda e, o=o, bm=bm: e.copy(out=o[0:64, :], in_=bm[0:64, :]))
                rope_rows(bm, bbm, br, bbr, g, o, bo, False)
                kb.dma("sp", [bo], [], qT_d[h, :, cols], o[0:96, :], pwrites=[B_q])
            if LIM < 3:
                break
            for hp in range(4):
                bk, bb = bank()

                def mm(e, hp=hp, bk=bk):
                    for c in range(2):
                        ins = e.matmul(bk, wukvk[:, c, hp * 128:(hp + 1) * 128], CN[:, 3 + c, :], start=(c == 0), stop=(c == 1))
                    return ins
                kb.op("pe", [BCN[3], BCN[4], Bw], [bb], mm)
                o, bo = next_ob()
                evac_copy("act", bk, bb, o[:, :], bo)
                kb.dma("sp", [bo], [], kT_d[2 * hp, 0:64, cols], o[0:64, :], pwrites=[B_k])
                kb.dma("sp", [bo], [], kT_d[2 * hp + 1, 0:64, cols], o[64:128, :], pwrites=[B_k])
            if LIM < 4:
                break
            bm, bbm = bank()
            br, bbr = bank()

            def mm(e, bm=bm, br=br):
                for which, bk in ((0, bm), (1, br)):
                    for k in range(8):
                        ins = e.matmul(bk[0:96, :], wkr[:, k, which * 96:(which + 1) * 96], H[:, k, :],
                                       start=(k == 0), stop=(k == 7))
                return ins
            kb.op("pe", [BH, Bw], [bbm, bbr], mm)
            o, bo = next_ob()
            rope_rows(bm, bbm, br, bbr, g, o, bo, True)
            for h in range(8):
                kb.dma("sp", [bo], [], kT_d[h, 64:96, cols], o[64:96, :], pwrites=[B_k])
            if LIM < 5:
                break
            for i in range(4):
                bk, bb = bank()

                def mm(e, i=i, bk=bk):
                    for c in range(2):
                        ins = e.matmul(bk, CN[:, 3 + c, i * 128:(i + 1) * 128], wukvv[:, c, :], start=(c == 0), stop=(c == 1))
                    return ins
                kb.op("pe", [BCN[3], BCN[4], Bw], [bb], mm)
                vi = vo_ctr[0] % 4
                vo_ctr[0] += 1
                ov, iv = vo_views(vo[vi], bk)
                kb.op("act", [bb], [Bvo[vi]], lambda e, ov=ov, iv=iv: e.copy(out=ov, in_=iv))
                kb.dma("sp", [Bvo[vi]], [], Vm_d[g * 4 + i, :, :], vo[vi][:], pwrites=[B_vm])
            if LIM < 6:
                break
            for base, dst, bd in ((672, dqT_d, B_dq), (1184, dkT_d, B_dk)):
                for hp in range(4):
                    bk, bb = bank()

                    def mm(e, hp=hp, bk=bk, base=base):
                        for k in range(8):
                            ins = e.matmul(bk, win[:, k, base + hp * 128: base + (hp + 1) * 128], H[:, k, :],
                                           start=(k == 0), stop=(k == 7))
                        return ins
                    kb.op("pe", [BH, Bw], [bb], mm)
                    o, bo = next_ob()
                    evac_copy("act" if hp % 2 == 0 else "dve", bk, bb, o[:, :], bo)
                    kb.dma("sp", [bo], [], dst[hp, :, cols], o[:, :], pwrites=[bd])
            if LIM < 7:
                break
            for i in range(4):
                bk, bb = bank()

                def mm(e, i=i, bk=bk):
                    for k in range(8):
                        ins = e.matmul(bk, H[:, k, i * 128:(i + 1) * 128], win[:, k, 1696:2208], start=(k == 0), stop=(k == 7))
                    return ins
                kb.op("pe", [BH, Bw], [bb], mm)
                vi = vo_ctr[0] % 4
                vo_ctr[0] += 1
                ov, iv = vo_views(vo[vi], bk)
                kb.op("act", [bb], [Bvo[vi]], lambda e, ov=ov, iv=iv: e.copy(out=ov, in_=iv))
                kb.dma("sp", [Bvo[vi]], [], Vd_d[g * 4 + i, :, :], vo[vi][:], pwrites=[B_vd])
        kb.barrier()
    if stages <= 1:
        _finish(nc, kb, out_d, x_d)
        return

    def attention(tag, kT_src, qT_src, V_src, per_pair_kq, krows, scale, chunk0, dilated, Bk_src, Bq_src, Bv_src):
        with ExitStack() as sa:
            A = lambda n, sh, dt: sa.enter_context(nc.sbuf_tensor(tag + "_" + n, sh, dt))
            Vp = [A("Vp%d" % i, [128, 32, 192], BF16) for i in range(2)]
            Kt = [A("Kt%d" % i, [128, S], BF16) for i in range(2)]
            Qt = [A("Qt%d" % i, [128, S], BF16) for i in range(2)]
            pte = [A("pte%d" % i, [128, 1024], BF16) for i in range(3)]
            ptm = [A("ptm%d" % i, [128, 1024], BF16) for i in range(3)] if dilated else None
            ao = [A("ao%d" % i, [128, S], BF16) for i in range(2)]
            rec = [A("rec%d" % i, [128, 512], F32) for i in range(2)]
            BVp, BKt, BQt = [Buf(), Buf()], [Buf(), Buf()], [Buf(), Buf()]
            Bpte, Bptm = [Buf() for _ in range(3)], [Buf() for _ in range(3)]
            Bao, Brec = [Buf(), Buf()], [Buf(), Buf()]
            SB = [Buf("S%d" % i, excl=True) for i in range(3)]
            AB = [Buf("acc%d" % i, excl=True) for i in range(2)]
            Sap = [ps[:, i * 1024:(i + 1) * 1024] for i in range(3)]
            Aap = [ps[:, 3072 + i * 512: 3072 + (i + 1) * 512] for i in range(2)]
            Bzk = Buf()
            if per_pair_kq:
                KtB = [A("KtB%d" % i, [128, S], BF16) for i in range(2)]
                for i in range(2):
                    kb.op("dve", [], [], lambda e, i=i: e.memset(Kt[i][64:128, :], 0.0), pwrites=[Bzk])
                    kb.op("dve", [], [], lambda e, i=i: e.memset(KtB[i][0:64, :], 0.0), pwrites=[Bzk])
            if dilated:
                toep_st = A("toep", [128, MW], F32)
                multb = A("multb", [128, MW], BF16)
                master = [A("master%d" % i, [128, MW], BF16) for i in range(2)]
                Btoep, Bmult, Bmaster = Buf(), Buf(), [Buf(), Buf()]
                kb.dma("pool", [], [Bmult], multb[:], mult_d[:, :])

            def load_pair(hp):
                for j in range(4):
                    kb.dma("sp", [Bv_src], [], Vp[hp % 2][:, j * 8:(j + 1) * 8, :],
                           V_src[j * 8:(j + 1) * 8, :, hp * 192:(hp + 1) * 192].rearrange("t p c -> p t c"),
                           pwrites=[BVp[hp % 2]])

            def load_kq(u):
                if per_pair_kq:
                    kb.dma("sp", [Bk_src, Bzk], [BKt[u % 2]], Kt[u % 2][0:64, :], kT_src[u, 0:64, :])
                    kb.dma("sp", [Bk_src, Bzk], [], KtB[u % 2][64:128, :], kT_src[u, 64:128, :], pwrites=[BKt[u % 2]])
                    kb.dma("sp", [Bq_src], [BQt[u % 2]], Qt[u % 2][:, :], qT_src[u, :, :])
                else:
                    kb.dma("sp", [Bk_src], [BKt[u % 2]], Kt[u % 2][0:krows, :], kT_src[u, :, :])
                    kb.dma("sp", [Bq_src], [BQt[u % 2]], Qt[u % 2][0:krows, :], qT_src[u, :, :])

            def prep_mask(h):
                kb.dma("sp", [], [Btoep], toep_st[:], toep_d[h, :, :])
                kb.op("act", [Btoep], [Btoep], lambda e: e.activation(out=toep_st[:], in_=toep_st[:], func=AF.Exp))
                kb.op("dve", [Btoep, Bmult], [Bmaster[h % 2]], lambda e: e.tensor_tensor(
                    out=master[h % 2][:], in0=toep_st[:], in1=multb[:], op=ALU.mult))

            items = []
            for h in range(8):
                hp, hh = h // 2, h % 2
                for Q in range(8):
                    if dilated:
                        Ts = list(range(max(0, 4 * Q - 8), min(31, 4 * Q + 11) + 1))
                    else:
                        Ts = list(range(32))
                    groups = []
                    n = 0
                    while n < len(Ts):
                        if n + 1 < len(Ts):
                            groups.append([Ts[n + 1], Ts[n]])
                            n += 2
                        else:
                            groups.append([Ts[n]])
                            n += 1
                    for gi, gT in enumerate(groups):
                        items.append(dict(h=h, hp=hp, hh=hh, Q=Q, Ts=gT, first=(gi == 0), last=(gi == len(groups) - 1),
                                          hq=h * 8 + Q))
            N = len(items)
            for i, it in enumerate(items):
                it["i"] = i

            def kq_unit(it):
                return it["hp"] if per_pair_kq else it["h"]

            def rows_of(it):
                if per_pair_kq:
                    return it["hh"] * 64, it["hh"] * 64 + 64
                return 0, krows

            def emit_qk(it):
                i = it["i"]
                u = kq_unit(it)
                r0, r1 = rows_of(it)
                K_, Q_ = Kt[u % 2], Qt[u % 2]
                if per_pair_kq:
                    r0, r1 = 0, 128
                    if it["hh"] == 1:
                        K_ = KtB[u % 2]

                def f(e):
                    for j, T in enumerate(it["Ts"]):
                        ins = e.matmul(Sap[i % 3][:, j * 512:(j + 1) * 512], K_[r0:r1, T * 128:(T + 1) * 128],
                                       Q_[r0:r1, it["Q"] * 512:(it["Q"] + 1) * 512], start=True, stop=True)
                    return ins
                kb.op("pe", [BKt[u % 2], BQt[u % 2]], [SB[i % 3]], f)

            def emit_exp(it):
                i = it["i"]
                w = 512 * len(it["Ts"])
                kb.op("act", [SB[i % 3]], [Bpte[i % 3]], lambda e: e.activation(
                    out=pte[i % 3][:, 0:w], in_=Sap[i % 3][:, 0:w], func=AF.Exp, scale=scale))

            def emit_mask(it):
                i = it["i"]
                n = len(it["Ts"])
                h = it["h"]
                c0 = C0 - 128 * it["Ts"][0] + 512 * it["Q"]
                m = master[h % 2]
                in1 = AP(m, c0, [pdim(m[:]), [128, n], [1, 512]])
                o = AP(ptm[i % 3], 0, [pdim(ptm[i % 3][:]), [512, n], [1, 512]])
                i0 = AP(pte[i % 3], 0, [pdim(pte[i % 3][:]), [512, n], [1, 512]])
                kb.op("dve", [Bpte[i % 3], Bmaster[h % 2]], [Bptm[i % 3]], lambda e: e.tensor_tensor(
                    out=o, in0=i0, in1=in1, op=ALU.mult))

            def emit_pv(it):
                i = it["i"]
                P_, BP_ = (ptm[i % 3], Bptm[i % 3]) if dilated else (pte[i % 3], Bpte[i % 3])
                acc = Aap[it["hq"] % 2]
                V_ = Vp[it["hp"] % 2]
                vc = 64 * it["hh"]
                nT = len(it["Ts"])

                def f(e):
                    for j, T in enumerate(it["Ts"]):
                        ins = e.matmul(acc, V_[:, T, vc:vc + 128], P_[:, j * 512:(j + 1) * 512],
                                       start=(it["first"] and j == 0), stop=(it["last"] and j == nT - 1))
                    return ins
                if it["first"]:
                    kb.op("pe", [BP_, BVp[it["hp"] % 2]], [AB[it["hq"] % 2]], f)
                else:
                    kb.op("pe", [BP_, BVp[it["hp"] % 2]], [], f, pwrites=[AB[it["hq"] % 2]])

            def emit_fin(it):
                a = it["hq"] % 2
                acc = Aap[a]
                hh, hp, Q = it["hh"], it["hp"], it["Q"]
                nr = slice(64 * hh, 64 * hh + 64)
                dr = slice(64 * (1 - hh), 64 * (1 - hh) + 64)
                if dilated:
                    kb.op("act", [AB[a]], [Brec[a]], lambda e: e.activation(out=rec[a][nr, :], in_=acc[dr, :], func=AF.Ln))
                    kb.op("act", [Brec[a]], [Brec[a]], lambda e: e.activation(out=rec[a][nr, :], in_=rec[a][nr, :], func=AF.Exp, scale=-1.0))
                else:
                    kb.op("dve", [AB[a]], [Brec[a]], lambda e: e.reciprocal(out=rec[a][nr, :], in_=acc[dr, :]))
                first_of_pair = (hh == 0 and Q == 0)
                kb.op("dve", [AB[a], Brec[a]], [Bao[hp % 2]] if first_of_pair else [], lambda e: e.tensor_tensor(
                    out=ao[hp % 2][nr, Q * 512:(Q + 1) * 512], in0=acc[nr, :], in1=rec[a][nr, :], op=ALU.mult),
                    pwrites=[] if first_of_pair else [Bao[hp % 2]])
                if hh == 1 and Q == 7:
                    kb.dma("sp", [Bao[hp % 2]], [], attnT_d[chunk0 + hp, :, :], ao[hp % 2][:, :], pwrites=[B_attn])

            def unit_start(it):
                h = it["h"]
                if it["Q"] == 0 and it["first"]:
                    if h + 1 < 8:
                        if per_pair_kq:
                            if (h + 1) % 2 == 0:
                                load_kq((h + 1) // 2)
                        else:
                            load_kq(h + 1)
                        if (h + 1) % 2 == 0:
                            load_pair((h + 1) // 2)
                        if dilated:
                            prep_mask(h + 1)

            load_pair(0)
            load_kq(0)
            if dilated:
                prep_mask(0)
            emit_qk(items[0])
            emit_exp(items[0])
            if dilated:
                emit_mask(items[0])
            if N > 1:
                emit_qk(items[1])
            for i in range(N):
                it = items[i]
                unit_start(it)
                if i + 1 < N:
                    emit_exp(items[i + 1])
                    if dilated:
                        emit_mask(items[i + 1])
                if i + 2 < N:
                    emit_qk(items[i + 2])
                emit_pv(it)
                if it["last"]:
                    emit_fin(it)
            kb.barrier()

    run_dil = os.environ.get("SKIPDIL", "") == ""
    run_mla = os.environ.get("SKIPMLA", "") == ""
    if run_dil:
        attention("dil", dkT_d, dqT_d, Vd_d, True, 64, 64.0 ** -0.5, 4, True, B_dk, B_dq, B_vd)
    if run_mla:
        attention("mla", kT_d, qT_d, Vm_d, False, 96, 96.0 ** -0.5, 0, False, B_k, B_q, B_vm)
    if stages <= 3:
        _finish(nc, kb, out_d, x_d)
        return

    def bank2():
        if bank_ctr[0] % 2:
            bank_ctr[0] += 1
        i = bank_ctr[0] % 8
        bank_ctr[0] += 2
        return ps[:, i * 512:(i + 2) * 512], [PB[i], PB[i + 1]]

    class LNState:
        def __init__(self, A, tag, nb=2):
            self.junk = A(tag + "junk", [128, D], F32)
            self.st = [A(tag + "st%d" % i, [128, 8], F32) for i in range(nb)]
            self.Bjunk = Buf()
            self.Bst = [Buf() for _ in range(nb)]
            self.nb = nb
            self.n = 0

    def ln_stats(L, y, By):
        i = L.n % L.nb
        L.n += 1
        st, Bst = L.st[i], L.Bst[i]
        kb.op("dve", [], [Bst], lambda e: e.memset(st[:], 0.0))
        kb.op("act", [By, Bst], [L.Bjunk], lambda e: e.activation(out=L.junk[:], in_=y[:], func=AF.Identity, accum_out=st[:, 0:1]),
              pwrites=[Bst])
        kb.op("act", [By, Bst], [L.Bjunk], lambda e: e.activation(out=L.junk[:], in_=y[:], func=AF.Square, accum_out=st[:, 1:2]),
              pwrites=[Bst])
        return st, Bst

    def ln_apply(st, Bst, y, By, xh, Bxh):
        kb.op("dve", [Bst], [], lambda e: e.tensor_scalar(out=st[:, 2:3], in0=st[:, 0:1], scalar1=1.0 / D, scalar2=None, op0=ALU.mult), pwrites=[Bst])
        kb.op("dve", [Bst], [], lambda e: e.tensor_tensor(out=st[:, 3:4], in0=st[:, 2:3], in1=st[:, 2:3], op=ALU.mult), pwrites=[Bst])
        kb.op("dve", [Bst], [], lambda e: e.scalar_tensor_tensor(out=st[:, 4:5], in0=st[:, 1:2], scalar=1.0 / D, in1=st[:, 3:4],
                                                                   op0=ALU.mult, op1=ALU.subtract), pwrites=[Bst])
        kb.op("dve", [Bst], [], lambda e: e.tensor_scalar(out=st[:, 4:5], in0=st[:, 4:5], scalar1=EPS, scalar2=None, op0=ALU.add), pwrites=[Bst])
        kb.op("act", [Bst], [], lambda e: e.activation(out=st[:, 5:6], in_=st[:, 4:5], func=AF.Ln), pwrites=[Bst])
        kb.op("act", [Bst], [], lambda e: e.activation(out=st[:, 5:6], in_=st[:, 5:6], func=AF.Exp, scale=-0.5), pwrites=[Bst])
        kb.op("dve", [Bst], [], lambda e: e.scalar_tensor_tensor(out=st[:, 6:7], in0=st[:, 2:3], scalar=-1.0, in1=st[:, 5:6],
                                                                   op0=ALU.mult, op1=ALU.mult), pwrites=[Bst])
        kb.op("act", [By, Bst], [Bxh], lambda e: e.activation(out=xh[:], in_=y[:], func=AF.Identity, scale=st[:, 5:6], bias=st[:, 6:7]))

    with ExitStack() as s4:
        A = lambda n, sh, dt: s4.enter_context(nc.sbuf_tensor("p4_" + n, sh, dt))
        wout = A("wout", [128, 8, D], BF16)
        g1bc, lngbc, lnbbc, sc2bc, sh2bc = (A(n, [128, D], F32) for n in ("g1bc", "lngbc", "lnbbc", "sc2bc", "sh2bc"))
        wr = A("wr", [128, 8, NE], F32)
        ident = A("ident", [128, 128], F32)
        at = [A("at%d" % i, [128, 8, 512], BF16) for i in range(2)]
        xt = [A("xt%d" % i, [128, D], F32) for i in range(2)]
        t1 = [A("t1%d" % i, [128, D], F32) for i in range(2)]
        yy = [A("y%d" % i, [128, D], F32) for i in range(2)]
        xh = [A("xh%d" % i, [128, D], F32) for i in range(2)]
        x1 = [A("x1%d" % i, [128, D], F32) for i in range(2)]
        h2f = [A("h2f%d" % i, [128, D], F32) for i in range(2)]
        h2b = [A("h2b%d" % i, [128, D], BF16) for i in range(2)]
        h2T = [A("h2T%d" % i, [128, 8, 128], F32) for i in range(2)]
        sm = [A("sm%d" % i, [128, 8], F32) for i in range(2)]
        ee = [A("ee%d" % i, [128, NE], F32) for i in range(2)]
        L = LNState(A, "ln", 3)
        Bc4 = Buf()
        Bat, Bxt, Bt1, By, Bxh, Bx1, Bh2f, Bh2b, Bh2T, Bsm, Bee = ([Buf(), Buf()] for _ in range(11))
        kb.dma("sp", [B_mod], [], g1bc[:], bcast_row(mod_d[0:1, 2048:3072], D), pwrites=[Bc4])
        Bws = [Buf(), Buf()]
        for c in range(8):
            kb.dma("sp", [], [Bws[c % 2]], t1[c % 2][:], wout_d[c * 128:(c + 1) * 128, :])
            kb.op("dve", [Bws[c % 2], Bc4], [], lambda e, c=c: e.tensor_tensor(out=wout[:, c, :], in0=t1[c % 2][:], in1=g1bc[:], op=ALU.mult),
                  pwrites=[Bc4])
        kb.dma("sp", [B_mod], [], sh2bc[:], bcast_row(mod_d[0:1, 3072:4096], D), pwrites=[Bc4])
        kb.dma("sp", [B_mod], [], sc2bc[:], bcast_row(mod_d[0:1, 4096:5120], D), pwrites=[Bc4])
        kb.dma("sp", [], [], lngbc[:], bcast_row(ln1g_d[0:1, :], D), pwrites=[Bc4])
        kb.dma("sp", [], [], lnbbc[:], bcast_row(ln1b_d[0:1, :], D), pwrites=[Bc4])
        kb.dma("sp", [], [], wr[:], wr_d.rearrange("(k p) e -> p k e", p=128), pwrites=[Bc4])
        kb.dma("sp", [], [], ident[:], ident_d[:, :], pwrites=[Bc4])
        kb.op("dve", [Bc4], [], lambda e: e.tensor_scalar(out=sc2bc[:], in0=sc2bc[:], scalar1=1.0, scalar2=None, op0=ALU.add), pwrites=[Bc4])
        attn_v = attnT_d.rearrange("c p n -> p c n")
        x_t = x_d.rearrange("(t p) d -> t p d", p=128)
        x1_t = x1_d.rearrange("(t p) d -> t p d", p=128)
        h2_t = h2_d.rearrange("(t p) d -> t p d", p=128)
        stA = {}
        stC = {}

        def stageA(t):
            g, i = t // 4, t % 4
            if i == 0:
                kb.dma("sp", [B_attn], [Bat[g % 2]], at[g % 2][:], attn_v[:, :, g * 512:(g + 1) * 512])
            b = t % 2
            kb.dma("sp", [], [Bxt[b]], xt[b][:], x_t[t])
            bk2, bb2 = bank2()

            def mm(e):
                for half in range(2):
                    for c in range(8):
                        ins = e.matmul(bk2[:, half * 512:(half + 1) * 512], at[g % 2][:, c, i * 128:(i + 1) * 128],
                                       wout[:, c, half * 512:(half + 1) * 512], start=(c == 0), stop=(c == 7))
                return ins
            kb.op("pe", [Bat[g % 2], Bc4], bb2, mm)
            kb.op("dve", bb2 + [Bxt[b]] + Bws, [By[b]], lambda e: e.scalar_tensor_tensor(
                out=yy[b][:], in0=xt[b][:], scalar=ALPHA, in1=bk2, op0=ALU.mult, op1=ALU.add))
            stA[t] = ln_stats(L, yy[b], By[b])

        def stageB1(t):
            b = t % 2
            st, Bst = stA.pop(t)
            ln_apply(st, Bst, yy[b], By[b], xh[b], Bxh[b])

        def stageB(t):
            b = t % 2
            kb.op("dve", [Bxh[b], Bc4], [Bx1[b]], lambda e: e.tensor_tensor(out=x1[b][:], in0=xh[b][:], in1=lngbc[:], op=ALU.mult))
            kb.op("dve", [Bx1[b], Bc4], [Bx1[b]], lambda e: e.tensor_tensor(out=x1[b][:], in0=x1[b][:], in1=lnbbc[:], op=ALU.add))
            kb.dma("sp", [Bx1[b]], [], x1_t[t], x1[b][:], pwrites=[B_x1])
            kb.op("dve", [Bx1[b], Bc4], [Bh2f[b]], lambda e: e.tensor_tensor(out=h2f[b][:], in0=x1[b][:], in1=sc2bc[:], op=ALU.mult))
            kb.op("dve", [Bh2f[b], Bc4], [Bh2f[b]], lambda e: e.tensor_tensor(out=h2f[b][:], in0=h2f[b][:], in1=sh2bc[:], op=ALU.add))
            kb.op("act", [Bh2f[b]], [Bh2b[b]], lambda e: e.copy(out=h2b[b][:], in_=h2f[b][:]))
            kb.dma("sp", [Bh2b[b]], [], h2_t[t], h2b[b][:], pwrites=[B_h2])
            bk2, bb2 = bank2()

            def tp(e):
                for k in range(8):
                    ins = e.transpose(bk2[:, k * 128:(k + 1) * 128], h2f[b][:, k * 128:(k + 1) * 128], ident[:])
                return ins
            kb.op("pe", [Bh2f[b], Bc4], bb2, tp)
            stC[t] = (bk2, bb2)

        def stageC(t):
            b = t % 2
            bk2, bb2 = stC.pop(t)
            kb.op("act", bb2, [Bh2T[b]], lambda e: e.copy(out=h2T[b][:].rearrange("p k n -> p (k n)"), in_=bk2))
            bk, bb = bank()

            def lg(e):
                for k in range(8):
                    ins = e.matmul(bk[:, 0:NE], h2T[b][:, k, :], wr[:, k, :], start=(k == 0), stop=(k == 7))
                return ins
            kb.op("pe", [Bh2T[b], Bc4], [bb], lg)
            kb.op("dve", [], [Bsm[b]], lambda e: e.memset(sm[b][:], 0.0))
            kb.op("dve", [bb], [], lambda e: e.reduce_max(out=sm[b][:, 0:1], in_=bk[:, 0:NE], axis=AX.X), pwrites=[Bsm[b]])
            kb.op("dve", [Bsm[b]], [], lambda e: e.tensor_scalar(out=sm[b][:, 1:2], in0=sm[b][:, 0:1], scalar1=-1.0, scalar2=None, op0=ALU.mult),
                  pwrites=[Bsm[b]])
            kb.op("act", [bb, Bsm[b]], [Bee[b]], lambda e: e.activation(out=ee[b][:], in_=bk[:, 0:NE], func=AF.Exp, bias=sm[b][:, 1:2],
                                                                       accum_out=sm[b][:, 2:3]), pwrites=[Bsm[b]])
            kb.op("dve", [Bsm[b]], [], lambda e: e.reciprocal(out=sm[b][:, 3:4], in_=sm[b][:, 2:3]), pwrites=[Bsm[b]])
            kb.op("dve", [Bee[b], Bsm[b]], [], lambda e: e.tensor_scalar(out=aff_all[:, t * NE:(t + 1) * NE], in0=ee[b][:], scalar1=sm[b][:, 3:4],
                                                                        scalar2=None, op0=ALU.mult), pwrites=[B_aff])

        for step in range(NT + 3):
            if 0 <= step - 3 < NT:
                stageC(step - 3)
            if 0 <= step - 2 < NT:
                stageB(step - 2)
            if 0 <= step - 1 < NT:
                stageB1(step - 1)
            if step < NT:
                stageA(step)
        if "aff_s" in dbg:
            kb.dma("sp", [B_aff], [Buf()], aff_d[:, :], aff_all[:])
        kb.barrier()
    if stages <= 4:
        _finish(nc, kb, out_d, x_d)
        return

    def moe_phase():
        with ExitStack() as s5:
            A = lambda n, sh, dt: s5.enter_context(nc.sbuf_tensor("p5_" + n, sh, dt))
            onesf = A("onesf", [128, 128], F32)
            onesb = A("onesb", [128, 128], BF16)
            triub = A("triub", [128, 128], BF16)
            identb = A("identb", [128, 128], BF16)
            iota = A("iota", [128, 512], F32)
            tokhl = A("tokhl", [128, NT, 2], F32)
            zero = A("zero", [128, D], F32)
            lo = A("lo", [128, NE], F32)
            tau = A("tau", [128, NE], F32)
            part = A("part", [128, NE], F32)
            gw = A("gw", [128, NE], F32)
            cmp = A("cmp", [128, NT * NE], F32)
            self_ = A("self", [128, NT * NE], F32)
            selb = A("selb", [128, NT * NE], BF16)
            posm = A("posm", [128, NT * NE], F32)
            r1 = A("r1", [128, NT * NE], F32)
            R = A("R", [128, NT * NE * 5], BF16)
            OH = [A("OH%d" % i, [128, 512], BF16) for i in range(4)]
            idxv = [A("idxv%d" % i, [128, 32], F32) for i in range(2)]
            idxf = [A("idxf%d" % i, [128, 4], F32) for i in range(2)]
            idxi = [A("idxi%d" % i, [128, 4], I32) for i in range(2)]
            val = [A("val%d" % i, [128, 4], F32) for i in range(2)]
            xin = [A("xin%d" % i, [128, 4, D], BF16) for i in range(2)]
            xinT = [A("xinT%d" % i, [128, 8, 512], BF16) for i in range(2)]
            wg = [A("wg%d" % i, [128, 8, 512], BF16) for i in range(3)]
            wu = [A("wu%d" % i, [128, 8, 512], BF16) for i in range(3)]
            wd = [A("wd%d" % i, [128, 6, D], BF16) for i in range(3)]
            sg = [A("sg%d" % i, [128, 512], F32) for i in range(2)]
            act = [A("act%d" % i, [128, 12, 512], BF16) for i in range(2)]
            ys = [A("ys%d" % i, [128, D], F32) for i in range(4)]
            mo = [A("mo%d" % i, [128, D], F32) for i in range(4)]
            Bc = Buf()
            Blo, Btau, Bpart, Bgw, Bcmp, Bsel, Bposm, BR = (Buf() for _ in range(8))
            BOH = [Buf() for _ in range(4)]
            Bidxv, Bidx, Bval = [Buf(), Buf()], [Buf(), Buf()], [Buf(), Buf()]
            Bxin, BxinT = [Buf(), Buf()], [Buf(), Buf()]
            Bwg, Bwu, Bwd = [Buf() for _ in range(3)], [Buf() for _ in range(3)], [Buf() for _ in range(3)]
            Bsg, Bact = [Buf(), Buf()], [Buf(), Buf()]
            Bys, Bmo = [Buf() for _ in range(4)], [Buf() for _ in range(4)]

            kb.op("pool", [], [], lambda e: e.memset(onesf[:], 1.0), pwrites=[Bc])
            kb.op("pool", [], [], lambda e: e.memset(onesb[:], 1.0), pwrites=[Bc])
            kb.op("pool", [], [], lambda e: e.memset(zero[:], 0.0), pwrites=[Bc])
            kb.op("pool", [], [Blo], lambda e: e.memset(lo[:], 0.0))
            kb.dma("pool", [], [], triub[:], triu_d[:, :], pwrites=[Bc])
            kb.dma("pool", [], [], identb[:], ident_d[:, :], pwrites=[Bc])
            kb.dma("sp", [], [], iota[:], iota_d[:, :], pwrites=[Bc])
            kb.dma("sp", [], [], tokhl[:], tokhl_d[:, :, :], pwrites=[Bc])
            moe_t = moe_d.rearrange("(t p) d -> t p d", p=128)
            for t in range(NT):
                kb.dma("sp", [Bc], [], moe_t[t], zero[:], pwrites=[B_moe])

            def issue_gu(n):
                if n >= NE * 3:
                    return
                e, fb = n // 3, n % 3
                b = n % 3
                kb.dma("pool", [], [Bwg[b]], wg[b][:], wg_d[e, :, fb * 512:(fb + 1) * 512].rearrange("(k p) f -> p k f", p=128))
                kb.dma("pool", [], [Bwu[b]], wu[b][:], wu_d[e, :, fb * 512:(fb + 1) * 512].rearrange("(k p) f -> p k f", p=128))

            def issue_d(m):
                if m >= NE * 2:
                    return
                e, half = m // 2, m % 2
                b = m % 3
                kb.dma("pool", [], [Bwd[b]], wd[b][:], wd_d[e, half * 768:(half + 1) * 768, :].rearrange("(k p) n -> p k n", p=128))

            issue_gu(0)
            issue_gu(1)
            issue_d(0)
            issue_d(1)
            issue_d(2)

            aff3 = AP(aff_all, 0, [pdim(aff_all[:]), [NE, NT], [1, NE]])
            cmp3 = AP(cmp, 0, [pdim(cmp[:]), [NE, NT], [1, NE]])
            cmpT = AP(cmp, 0, [pdim(cmp[:]), [1, NE], [NE, NT]])

            def bc16(tile_):
                return AP(tile_, 0, [pdim(tile_[:]), [0, NT], [1, NE]])

            for it in range(1, NBIS + 1):
                w = 2.0 ** (-it)
                kb.op("dve", [Blo], [Btau], lambda e: e.tensor_scalar(out=tau[:], in0=lo[:], scalar1=w, scalar2=None, op0=ALU.add))
                kb.op("dve", [B_aff, Btau], [Bcmp], lambda e: e.tensor_tensor(out=cmp3, in0=aff3, in1=bc16(tau), op=ALU.is_ge))
                kb.op("dve", [Bcmp], [Bpart], lambda e: e.tensor_reduce(out=part[:], in_=cmpT, axis=AX.X, op=ALU.add))
                bk, bb = bank()
                kb.op("pe", [Bpart, Bc], [bb], lambda e: e.matmul(bk[:, 0:NE], onesf[:], part[:], start=True, stop=True))
                kb.op("dve", [bb], [Bgw], lambda e: e.tensor_scalar(out=gw[:], in0=bk[:, 0:NE], scalar1=CAP - 0.5, scalar2=w,
                                                                  op0=ALU.is_ge, op1=ALU.mult))
                kb.op("dve", [Bgw, Blo], [Blo], lambda e: e.tensor_tensor(out=lo[:], in0=lo[:], in1=gw[:], op=ALU.add))
            self3 = AP(self_, 0, [pdim(self_[:]), [NE, NT], [1, NE]])
            kb.op("dve", [B_aff, Blo], [Bsel], lambda e: e.tensor_tensor(out=self3, in0=aff3, in1=bc16(lo), op=ALU.is_ge))
            kb.op("dve", [Bsel], [], lambda e: e.tensor_copy(out=selb[:], in_=self_[:]), pwrites=[Bsel])
            bk, bb = bank()

            def prefix(e):
                for t in range(NT):
                    o = bk[:, t * NE:(t + 1) * NE]
                    for tp in range(t):
                        e.matmul(o, onesb[:], selb[:, tp * NE:(tp + 1) * NE], start=(tp == 0), stop=False)
                    ins = e.matmul(o, triub[:], selb[:, t * NE:(t + 1) * NE], start=(t == 0), stop=True)
                return ins
            kb.op("pe", [Bsel, Bc], [bb], prefix)
            kb.op("dve", [bb, Bsel], [Bposm], lambda e: e.scalar_tensor_tensor(out=posm[:], in0=bk, scalar=1.0, in1=self_[:],
                                                                            op0=ALU.add, op1=ALU.mult))
            kb.op("dve", [Bposm], [Bposm], lambda e: e.tensor_scalar(out=posm[:], in0=posm[:], scalar1=-1.0, scalar2=None, op0=ALU.add))
            Rv = lambda j: AP(R, j, [pdim(R[:]), [5, NT * NE]])
            kb.op("dve", [Bc], [BR], lambda e: e.tensor_copy(
                out=AP(R, 0, [pdim(R[:]), [5 * NE, NT], [5, NE], [1, 2]]),
                in_=AP(tokhl, 0, [pdim(tokhl[:]), [2, NT], [0, NE], [1, 2]])))
            kb.op("dve", [B_aff], [], lambda e: e.tensor_copy(out=Rv(2), in_=aff_all[:]), pwrites=[BR])
            kb.op("dve", [B_aff, BR], [Bcmp], lambda e: e.tensor_tensor(out=r1[:], in0=aff_all[:], in1=Rv(2), op=ALU.subtract))
            kb.op("dve", [Bcmp], [], lambda e: e.tensor_copy(out=Rv(3), in_=r1[:]), pwrites=[BR])
            kb.op("dve", [Bcmp, BR], [Bcmp], lambda e: e.tensor_tensor(out=r1[:], in0=r1[:], in1=Rv(3), op=ALU.subtract))
            kb.op("dve", [Bcmp], [], lambda e: e.tensor_copy(out=Rv(4), in_=r1[:]), pwrites=[BR])

            h2rows = h2_d
            oh_ctr = [0]

            def route(e):
                b = e % 2
                bks = [bank() for _ in range(4)]
                for t in range(NT):
                    oi = oh_ctr[0] % 4
                    oh_ctr[0] += 1
                    eng = "dve"
                    kb.op(eng, [Bposm, Bc], [BOH[oi]], lambda en: en.tensor_scalar(
                        out=OH[oi][:], in0=iota[:], scalar1=posm[:, t * NE + e:t * NE + e + 1], scalar2=None, op0=ALU.is_equal))

                    def mm(en):
                        for cj in range(4):
                            ins = en.matmul(bks[cj][0][:, 0:5], OH[oi][:, cj * 128:(cj + 1) * 128],
                                            R[:, (t * NE + e) * 5:(t * NE + e) * 5 + 5], start=(t == 0), stop=(t == NT - 1))
                        return ins
                    if t == 0:
                        kb.op("pe", [BOH[oi], BR], [x[1] for x in bks], mm)
                    else:
                        kb.op("pe", [BOH[oi], BR], [], mm, pwrites=[x[1] for x in bks])
                for cj in range(4):
                    kb.op("dve", [bks[cj][1]], [] if cj else [Bidxv[b]], lambda en: en.tensor_copy(
                        out=idxv[b][:, cj * 8:cj * 8 + 5], in_=bks[cj][0][:, 0:5]), pwrites=[Bidxv[b]] if cj else [])
                v3 = lambda j: AP(idxv[b], j, [pdim(idxv[b][:]), [8, 4]])
                kb.op("dve", [Bidxv[b]], [Bidx[b]], lambda en: en.scalar_tensor_tensor(
                    out=idxf[b][:], in0=v3(0), scalar=128.0, in1=v3(1), op0=ALU.mult, op1=ALU.add))
                kb.op("dve", [Bidx[b]], [], lambda en: en.tensor_copy(out=idxi[b][:], in_=idxf[b][:]), pwrites=[Bidx[b]])
                kb.op("dve", [Bidxv[b]], [Bval[b]], lambda en: en.tensor_tensor(out=val[b][:], in0=v3(2), in1=v3(3), op=ALU.add))
                kb.op("dve", [Bidxv[b], Bval[b]], [Bval[b]], lambda en: en.tensor_tensor(out=val[b][:], in0=val[b][:], in1=v3(4), op=ALU.add))
                for cj in range(4):
                    kb.dma("pool", [Bidx[b], B_h2], [] if cj else [Bxin[b]], xin[b][:, cj, :], h2rows[:, :],
                           pwrites=[Bxin[b]] if cj else [],
                           indirect=dict(out_offset=None, in_offset=bass.IndirectOffsetOnAxis(ap=idxi[b][:, cj:cj + 1], axis=0)))
                for k in range(8):
                    bk2, bb2 = bank()
                    bkb = bk2.bitcast(BF16)

                    def tp(en):
                        for cj in range(4):
                            ins = en.transpose(bkb[:, cj * 128:(cj + 1) * 128], xin[b][:, cj, k * 128:(k + 1) * 128], identb[:])
                        return ins
                    kb.op("pe", [Bxin[b], Bc], [bb2], tp)
                    kb.op("dve" if k % 2 else "act", [bb2], [] if k else [BxinT[b]],
                          (lambda en: en.tensor_copy(out=xinT[b][:, k, :], in_=bkb[:, 0:512])) if k % 2 else
                          (lambda en: en.copy(out=xinT[b][:, k, :], in_=bkb[:, 0:512])),
                          pwrites=[BxinT[b]] if k else [])

            def compute(e):
                b = e % 2
                for fb in range(3):
                    n = e * 3 + fb
                    wb = n % 3
                    for j in range(4):
                        bg, bbg = bank()
                        bu, bbu = bank()

                        def mm(en):
                            for bk_, w_ in ((bg, wg[wb]), (bu, wu[wb])):
                                for k in range(8):
                                    ins = en.matmul(bk_, w_[:, k, j * 128:(j + 1) * 128], xinT[b][:, k, :], start=(k == 0), stop=(k == 7))
                            return ins
                        kb.op("pe", [BxinT[b], Bwg[wb], Bwu[wb]], [bbg, bbu], mm)
                        si = (fb * 4 + j) % 2
                        kb.op("act", [bbg], [Bsg[si]], lambda en: en.activation(out=sg[si][:], in_=bg, func=AF.Silu))
                        fi = fb * 4 + j
                        kb.op("dve", [Bsg[si], bbu], [] if fi else [Bact[b]], lambda en: en.tensor_tensor(
                            out=act[b][:, fi, :], in0=sg[si][:], in1=bu, op=ALU.mult), pwrites=[Bact[b]] if fi else [])
                    issue_gu(n + 2)
                    if fb == 0 and e + 1 < NE:
                        route(e + 1)
                for cj in range(4):
                    yi = cj
                    for half in range(2):
                        bk, bb = bank()

                        def mm(en):
                            for fi in range(12):
                                m = e * 2 + fi // 6
                                ins = en.matmul(bk, act[b][:, fi, cj * 128:(cj + 1) * 128], wd[m % 3][:, fi % 6, half * 512:(half + 1) * 512],
                                                start=(fi == 0), stop=(fi == 11))
                            return ins
                        kb.op("pe", [Bact[b], Bwd[(e * 2) % 3], Bwd[(e * 2 + 1) % 3]], [bb], mm)
                        kb.op("act", [bb, Bval[b]], [] if half else [Bys[yi]], lambda en: en.activation(
                            out=ys[yi][:, half * 512:(half + 1) * 512], in_=bk, func=AF.Copy, scale=val[b][:, cj:cj + 1]),
                            pwrites=[Bys[yi]] if half else [])
                issue_d(e * 2 + 3)
                issue_d(e * 2 + 4)
                for cj in range(4):
                    kb.dma("pool", [Bidx[b], B_moe], [Bmo[cj]], mo[cj][:, :], moe_d[:, :],
                           indirect=dict(out_offset=None, in_offset=bass.IndirectOffsetOnAxis(ap=idxi[b][:, cj:cj + 1], axis=0)))
                for cj in range(4):
                    kb.op("dve", [Bmo[cj], Bys[cj]], [Bmo[cj]], lambda en: en.tensor_tensor(out=mo[cj][:], in0=mo[cj][:], in1=ys[cj][:], op=ALU.add))
                for cj in range(4):
                    kb.dma("pool", [Bidx[b], Bmo[cj]], [], moe_d[:, :], mo[cj][:, :], pwrites=[B_moe],
                           indirect=dict(out_offset=bass.IndirectOffsetOnAxis(ap=idxi[b][:, cj:cj + 1], axis=0), in_offset=None))

            route(0)
            for e in range(int(os.environ.get("NEXP", str(NE)))):
                compute(e)
            kb.barrier()

    if os.environ.get("SKIPMOE", "") == "":
        moe_phase()

    with ExitStack() as s6:
        A = lambda n, sh, dt: s6.enter_context(nc.sbuf_tensor("p6_" + n, sh, dt))
        g2bc, lngbc, lnbbc = (A(n, [128, D], F32) for n in ("g2bc", "lngbc", "lnbbc"))
        NB6 = 3
        xt = [A("xt%d" % i, [128, D], F32) for i in range(NB6)]
        mt = [A("mt%d" % i, [128, D], F32) for i in range(NB6)]
        yy = [A("y%d" % i, [128, D], F32) for i in range(NB6)]
        xh = [A("xh%d" % i, [128, D], F32) for i in range(NB6)]
        oo = [A("o%d" % i, [128, D], F32) for i in range(NB6)]
        L = LNState(A, "ln", 3)
        Bc6 = Buf()
        Bxt, Bmt, By, Bxh, Boo = ([Buf() for _ in range(NB6)] for _ in range(5))
        kb.dma("sp", [B_mod], [], g2bc[:], bcast_row(mod_d[0:1, 5120:6144], D), pwrites=[Bc6])
        kb.dma("sp", [], [], lngbc[:], bcast_row(ln2g_d[0:1, :], D), pwrites=[Bc6])
        kb.dma("sp", [], [], lnbbc[:], bcast_row(ln2b_d[0:1, :], D), pwrites=[Bc6])
        x1_t = x1_d.rearrange("(t p) d -> t p d", p=128)
        moe_t = moe_d.rearrange("(t p) d -> t p d", p=128)
        out_t = out_d.rearrange("(t p) d -> t p d", p=128)
        stA = {}

        def stageA6(t):
            b = t % NB6
            kb.dma("sp", [B_x1], [Bxt[b]], xt[b][:], x1_t[t])
            kb.dma("sp", [B_moe], [Bmt[b]], mt[b][:], moe_t[t])
            kb.op("dve", [Bmt[b], Bc6], [Bmt[b]], lambda e: e.tensor_tensor(out=mt[b][:], in0=mt[b][:], in1=g2bc[:], op=ALU.mult))
            kb.op("dve", [Bxt[b], Bmt[b]], [By[b]], lambda e: e.scalar_tensor_tensor(
                out=yy[b][:], in0=xt[b][:], scalar=ALPHA, in1=mt[b][:], op0=ALU.mult, op1=ALU.add))
            stA[t] = ln_stats(L, yy[b], By[b])

        def stageB6(t):
            b = t % NB6
            st, Bst = stA.pop(t)
            ln_apply(st, Bst, yy[b], By[b], xh[b], Bxh[b])

        def stageC6(t):
            b = t % NB6
            kb.op("dve", [Bxh[b], Bc6], [Boo[b]], lambda e: e.tensor_tensor(out=oo[b][:], in0=xh[b][:], in1=lngbc[:], op=ALU.mult))
            kb.op("dve", [Boo[b], Bc6], [Boo[b]], lambda e: e.tensor_tensor(out=oo[b][:], in0=oo[b][:], in1=lnbbc[:], op=ALU.add))
            kb.dma("sp", [Boo[b]], [Buf()], out_t[t], oo[b][:])

        for step in range(NT + 2):
            if 0 <= step - 2 < NT:
                stageC6(step - 2)
            if 0 <= step - 1 < NT:
                stageB6(step - 1)
            if step < NT:
                stageA6(step)
        kb.barrier()


def _finish(nc, kb, out_d, x_d):
    kb.barrier()
    tok = kb.dma("sp", [], [Buf()], out_d[0:128, :], x_d[0:128, :])
    kb._wait("sp", [tok])


def _t5_bucket(rel):
    half = 16
    ret = (rel > 0).astype(np.int32) * half
    n = np.abs(rel)
    large = 8 + (np.log(np.maximum(n, 1) / 8) / np.log(1024 / 8) * (half - 8)).astype(np.int32)
    large = np.minimum(large, half - 1)
    return ret + np.where(n < 8, n, large).astype(np.int32)


def _static_tables():
    a = np.arange(128)[:, None]
    c = np.arange(MW)[None, :]
    rel = a - c + C0
    mult = ((np.abs(rel) <= 64).astype(np.float32)
            + ((rel % 4 == 0) & (np.abs(rel) <= 256)).astype(np.float32)
            + ((rel % 16 == 0) & (np.abs(rel) <= 1024)).astype(np.float32))
    bucket = _t5_bucket(rel)
    inv = (10000.0 ** (-(np.arange(0, 32, 2, dtype=np.float32)) / np.float32(32))).astype(np.float32)
    ang = (np.arange(S, dtype=np.float32)[:, None] * inv[None, :]).astype(np.float32)
    cos = np.cos(ang.astype(np.float64)).astype(np.float32).T
    sin = np.sin(ang.astype(np.float64)).astype(np.float32).T
    rope_cos = np.ascontiguousarray(np.concatenate([cos, cos], axis=0))
    rope_sin = np.ascontiguousarray(np.concatenate([-sin, sin], axis=0))
    tokhl = np.zeros((128, NT, 2), np.float32)
    tokhl[:, :, 0] = np.arange(NT)[None, :]
    tokhl[:, :, 1] = np.arange(128)[:, None]
    return dict(
        toep_mult=mult.astype(np.float32), bucket=bucket,
        rope_cos=rope_cos, rope_sin=rope_sin,
        ident=np.eye(128, dtype=np.float32),
        triu=np.triu(np.ones((128, 128), np.float32), k=1),
        iota512=np.tile(np.arange(512, dtype=np.float32)[None, :], (128, 1)),
        tokhl=tokhl,
    )


def make_in_maps(inputs):
    f = lambda a: np.ascontiguousarray(np.asarray(a, dtype=np.float32))
    st = _static_tables()
    x = f(inputs["x"])
    c = f(inputs["c"])
    w_in = f(inputs["w_in"][0])
    kr = w_in[:, 640:672]
    w_kr = np.zeros((D, 192), np.float32)
    w_kr[:, 64:96] = kr
    w_kr[:, 96 + 64:96 + 80] = kr[:, 16:32]
    w_kr[:, 96 + 80:96 + 96] = kr[:, 0:16]
    w_uq = f(inputs["w_uq"][0]).reshape(384, 8, 96)
    w_uq_rot = w_uq.copy()
    w_uq_rot[:, :, 64:80] = w_uq[:, :, 80:96]
    w_uq_rot[:, :, 80:96] = w_uq[:, :, 64:80]
    w_uq2 = np.ascontiguousarray(np.stack([w_uq.reshape(384, 768), w_uq_rot.reshape(384, 768)], axis=1))
    w_ukv = f(inputs["w_ukv"][0]).reshape(256, 8, 128)
    rel_bias = f(inputs["rel_bias"])
    toep = np.ascontiguousarray(np.transpose(rel_bias[st["bucket"]], (2, 0, 1)))
    shared = dict(
        w_ada=f(inputs["w_ada"][0]), b_ada=f(inputs["b_ada"][0]).reshape(1, -1),
        w_in=w_in, w_kr=w_kr,
        g_q=np.ascontiguousarray(f(inputs["q_norm_g"][0]).reshape(3, 128).T),
        g_kv=np.ascontiguousarray(f(inputs["kv_norm_g"][0]).reshape(2, 128).T),
        w_uq2=w_uq2,
        w_ukv_k=np.ascontiguousarray(w_ukv[:, :, 0:64].reshape(256, 512)),
        w_ukv_v=np.ascontiguousarray(w_ukv[:, :, 64:128].reshape(256, 512)),
        rope_cos=st["rope_cos"], rope_sin=st["rope_sin"],
        relb_toep=toep, toep_mult=st["toep_mult"],
        w_out=f(inputs["w_out"][0]), ln1_g=f(inputs["ln1_g"][0]).reshape(1, -1), ln1_b=f(inputs["ln1_b"][0]).reshape(1, -1),
        w_router=f(inputs["w_router"][0]),
        w_gate=f(inputs["w_gate"][0]), w_up=f(inputs["w_up"][0]), w_down=f(inputs["w_down"][0]),
        ln2_g=f(inputs["ln2_g"][0]).reshape(1, -1), ln2_b=f(inputs["ln2_b"][0]).reshape(1, -1),
        ident=st["ident"], triu=st["triu"], iota512=st["iota512"], tokhl=st["tokhl"],
    )
    maps = []
    for b in range(x.shape[0]):
        m = dict(shared)
        m["x"] = x[b]
        m["c_fm"] = np.ascontiguousarray(c[b].reshape(8, 128).T)
        maps.append(m)
    return maps


def kernel(**inputs):
    maps = make_in_maps(inputs)
    nc = build_program()
    res = run_bass_kernel_spmd(nc, maps, core_ids=list(range(len(maps))))
    return np.stack([np.asarray(r["out"], dtype=np.float32) for r in res.results], axis=0)
```

```python
import math
import os
from contextlib import ExitStack

import numpy as np
import concourse.bass as bass
import concourse.mybir as mybir
from concourse.bass_utils import run_bass_kernel_spmd

F32 = mybir.dt.float32
BF16 = mybir.dt.bfloat16
I32 = mybir.dt.int32
AF = mybir.ActivationFunctionType
ALU = mybir.AluOpType
AX = mybir.AxisListType

D = 1024
S = 4096
NT = 32
NG = 8
ALPHA = 2.0 ** 0.25
EPS = 1e-6
NE = 16
CAP = 512
DFF = 1536
C0 = 1408
MW = 2944
NBIS = 26


class Buf:
    __slots__ = ("w", "r", "name", "excl")

    def __init__(self, name="", excl=False):
        self.excl = excl
        self.w = []
        self.r = []
        self.name = name


def _compact(toks):
    best = {}
    for t in toks:
        if t[0] not in best or best[t[0]][2] < t[2]:
            best[t[0]] = t
    return list(best.values())


class KB:
    COMPUTE = ("pe", "act", "dve", "pool")
    NDS = 12

    def __init__(self, nc, es):
        self.nc = nc
        self.E = {"pe": nc.tensor, "act": nc.scalar, "dve": nc.vector, "pool": nc.gpsimd, "sp": nc.sync}
        self.csem = {e: es.enter_context(nc.semaphore("c_" + e)) for e in self.COMPUTE}
        self.ccnt = {e: 0 for e in self.COMPUTE}
        self.dsem = {q: [es.enter_context(nc.semaphore("d_%s%d" % (q, i))) for i in range(self.NDS)]
                     for q in ("sp", "pool", "act")}
        self.dcnt = {q: 0 for q in self.dsem}
        self.dtok = {q: [None] * self.NDS for q in self.dsem}
        self.seen = {e: {} for e in self.E}
        self.nwait = 0

    def _wait(self, eng, toks):
        need = {}
        for t in toks:
            if t is None:
                continue
            key, sem, val = t
            if self.seen[eng].get(key, 0) >= val:
                continue
            if need.get(key, (None, 0))[1] < val:
                need[key] = (sem, val)
        for key, (sem, val) in need.items():
            self.E[eng].wait_ge(sem, val)
            self.seen[eng][key] = val
            self.nwait += 1

    def _deps(self, eng, reads, writes, pwrites, is_dma):
        own = None if is_dma else "c_" + eng
        toks = []
        for b in reads:
            for t in b.w:
                if not (eng == "pe" and t[0] == own):
                    toks.append(t)
            if b.excl:
                toks.extend(t for t in b.r if t[0] != own)
        for b in writes:
            toks.extend(t for t in b.w if t[0] != own)
            toks.extend(t for t in b.r if t[0] != own)
        for b in pwrites:
            toks.extend(t for t in b.r if t[0] != own)
        return toks

    def _commit(self, tok, reads, writes, pwrites):
        for b in reads:
            b.r.append(tok)
            if len(b.r) > 16:
                b.r = _compact(b.r)
        for b in writes:
            b.w = [tok]
            b.r = []
        for b in pwrites:
            b.w.append(tok)
            if len(b.w) > 16:
                b.w = _compact(b.w)

    def op(self, eng, reads, writes, fn, pwrites=()):
        self._wait(eng, self._deps(eng, reads, writes, pwrites, False))
        inst = fn(self.E[eng])
        self.ccnt[eng] += 1
        tok = ("c_" + eng, self.csem[eng], self.ccnt[eng])
        inst.then_inc(self.csem[eng], 1)
        self._commit(tok, reads, writes, pwrites)
        return tok

    def dma(self, q, reads, writes, out, in_, pwrites=(), indirect=None, **kw):
        i = self.dcnt[q]
        slot = i % self.NDS
        self._wait(q, self._deps(q, reads, writes, pwrites, True) + [self.dtok[q][slot]])
        if indirect is None:
            inst = self.E[q].dma_start(out=out, in_=in_, **kw)
        else:
            inst = self.E[q].indirect_dma_start(out=out, in_=in_, **indirect)
        val = 16 * (i // self.NDS + 1)
        sem = self.dsem[q][slot]
        inst.then_inc(sem, 16)
        tok = ("d_%s%d" % (q, slot), sem, val)
        self.dtok[q][slot] = tok
        self.dcnt[q] += 1
        self._commit(tok, reads, writes, pwrites)
        return tok

    def barrier(self, engines=None):
        toks = []
        for e in self.COMPUTE:
            if self.ccnt[e]:
                toks.append(("c_" + e, self.csem[e], self.ccnt[e]))
        for q in self.dsem:
            toks.extend(t for t in self.dtok[q] if t is not None)
        for e in (engines or self.E):
            self._wait(e, toks)


def AP(t, off, dims):
    return bass.AP(t, off, [list(d) for d in dims])


def pdim(ap):
    return list(ap.ap[0])


def build_program(stages=99, dbg=()):
    nc = bass.Bass("TRN2", target_bir_lowering=False)
    es = ExitStack()
    with es:
        _build(nc, es, stages, dbg)
    return nc


def _build(nc, es, stages, dbg):
    def din(name, shape, dt=F32):
        return nc.dram_tensor(name, list(shape), dt, kind="ExternalInput").ap()

    def dscr(name, shape, dt):
        kind = "ExternalOutput" if name in dbg else "Internal"
        return nc.dram_tensor(name, list(shape), dt, kind=kind).ap()

    x_d = din("x", [S, D])
    cfm_d = din("c_fm", [128, 8])
    wada_d = din("w_ada", [D, 6 * D])
    bada_d = din("b_ada", [1, 6 * D])
    win_d = din("w_in", [D, 2208])
    wkr_d = din("w_kr", [D, 192])
    gq_d = din("g_q", [128, 3])
    gkv_d = din("g_kv", [128, 2])
    wuq_d = din("w_uq2", [384, 2, 768])
    wukvk_d = din("w_ukv_k", [256, 512])
    wukvv_d = din("w_ukv_v", [256, 512])
    cos_d = din("rope_cos", [32, S])
    sin_d = din("rope_sin", [32, S])
    toep_d = din("relb_toep", [8, 128, MW])
    mult_d = din("toep_mult", [128, MW])
    wout_d = din("w_out", [D, D])
    ln1g_d = din("ln1_g", [1, D])
    ln1b_d = din("ln1_b", [1, D])
    wr_d = din("w_router", [D, NE])
    wg_d = din("w_gate", [NE, D, DFF])
    wu_d = din("w_up", [NE, D, DFF])
    wd_d = din("w_down", [NE, DFF, D])
    ln2g_d = din("ln2_g", [1, D])
    ln2b_d = din("ln2_b", [1, D])
    ident_d = din("ident", [128, 128])
    triu_d = din("triu", [128, 128])
    iota_d = din("iota512", [128, 512])
    tokhl_d = din("tokhl", [128, NT, 2])
    out_d = nc.dram_tensor("out", [S, D], F32, kind="ExternalOutput").ap()

    mod_d = dscr("mod_s", [1, 6 * D], F32)
    qT_d = dscr("qT_s", [8, 96, S], BF16)
    kT_d = dscr("kT_s", [8, 96, S], BF16)
    Vm_d = dscr("Vm_s", [NT, 128, 768], BF16)
    dqT_d = dscr("dqT_s", [4, 128, S], BF16)
    dkT_d = dscr("dkT_s", [4, 128, S], BF16)
    Vd_d = dscr("Vd_s", [NT, 128, 768], BF16)
    attnT_d = dscr("attnT_s", [8, 128, S], BF16)
    x1_d = dscr("x1_s", [S, D], F32)
    h2_d = dscr("h2_s", [S, D], BF16)
    moe_d = dscr("moe_s", [S, D], F32)
    aff_d = dscr("aff_s", [128, NT * NE], F32)
    idx_d = dscr("idx_s", [128, NE * 4 * 8], F32)

    kb = KB(nc, es)
    B_mod, B_q, B_k, B_vm, B_dq, B_dk, B_vd = (Buf(n) for n in ("mod", "q", "k", "vm", "dq", "dk", "vd"))
    B_attn, B_x1, B_h2, B_moe = Buf("attn"), Buf("x1"), Buf("h2"), Buf("moe")

    ps = es.enter_context(nc.psum_tensor("ps", [128, 4096], F32))
    PB = [Buf("bank%d" % i, excl=True) for i in range(8)]
    bank_ctr = [0]

    nrot = [8]

    def bank():
        i = bank_ctr[0] % nrot[0]
        bank_ctr[0] += 1
        return ps[:, i * 512:(i + 1) * 512], PB[i]

    modfm = es.enter_context(nc.sbuf_tensor("modfm", [128, 16], F32))
    aff_all = es.enter_context(nc.sbuf_tensor("aff_all", [128, NT * NE], F32))
    B_modfm, B_aff = Buf("modfm"), Buf("aff")

    def bcast_row(dram_ap_row, n):
        return AP(dram_ap_row.tensor, dram_ap_row.offset, [[0, 128], [1, n]])

    with ExitStack() as s0:
        A = lambda n, sh, dt: s0.enter_context(nc.sbuf_tensor("p0_" + n, sh, dt))
        cfm = A("cfm", [128, 8], F32)
        sig = A("sig", [128, 8], F32)
        cond = A("cond", [128, 8], F32)
        bada = A("bada", [1, 6 * D], F32)
        modrow = A("modrow", [1, 6 * D], F32)
        wa = [A("wa%d" % i, [128, 8, 512], F32) for i in range(3)]
        Bc, Bcond, Bb, Bm = Buf(), Buf(), Buf(), Buf()
        Bwa = [Buf() for _ in range(3)]
        kb.dma("sp", [], [Bc], cfm[:], cfm_d[:, :])
        kb.dma("sp", [], [Bb], bada[:], bada_d[:, :])
        wada_v = wada_d.rearrange("(k p) n -> p k n", p=128)
        for j in range(2):
            kb.dma("sp", [], [Bwa[j]], wa[j][:], wada_v[:, :, j * 512:(j + 1) * 512])
        kb.op("act", [Bc], [Bcond], lambda e: e.activation(out=sig[:], in_=cfm[:], func=AF.Sigmoid))
        kb.op("dve", [Bc, Bcond], [Bcond], lambda e: e.tensor_tensor(out=cond[:], in0=cfm[:], in1=sig[:], op=ALU.mult))
        for j in range(12):
            if j + 2 < 12:
                kb.dma("sp", [], [Bwa[(j + 2) % 3]], wa[(j + 2) % 3][:], wada_v[:, :, (j + 2) * 512:(j + 3) * 512])
            bk, bb = bank()
            w = wa[j % 3]

            def mm(e, w=w, bk=bk):
                for k in range(8):
                    i = e.matmul(bk[0:1, :], cond[:, k:k + 1], w[:, k, :], start=(k == 0), stop=(k == 7))
                return i
            kb.op("pe", [Bcond, Bwa[j % 3]], [bb], mm)
            kb.op("dve", [bb, Bb], [], pwrites=[Bm], fn=lambda e, bk=bk, j=j: e.tensor_tensor(
                out=modrow[0:1, j * 512:(j + 1) * 512], in0=bk[0:1, :], in1=bada[0:1, j * 512:(j + 1) * 512], op=ALU.add))
        kb.dma("sp", [Bm], [B_mod], mod_d[:, :], modrow[:])
        onef = A("onef", [1, 1], F32)
        kb.op("pool", [], [Bc], lambda e: e.memset(onef[:], 1.0))
        bk, bb = bank()

        def mmT(e, bk=bk):
            for j in range(16):
                ins = e.matmul(bk[:, j:j + 1], modrow[0:1, j * 128:(j + 1) * 128], onef[0:1, 0:1], start=True, stop=True)
            return ins
        kb.op("pe", [Bm, Bc], [bb], mmT)
        kb.op("dve", [bb], [B_modfm], lambda e, bk=bk: e.tensor_copy(out=modfm[:], in_=bk[:, 0:16]))
        kb.op("dve", [B_modfm], [B_modfm], lambda e: e.tensor_scalar(
            out=modfm[:, 8:16], in0=modfm[:, 8:16], scalar1=1.0, scalar2=None, op0=ALU.add))
        kb.barrier()
    if stages <= 0:
        _finish(nc, kb, out_d, x_d)
        return

    with ExitStack() as s1:
        A = lambda n, sh, dt: s1.enter_context(nc.sbuf_tensor("p1_" + n, sh, dt))
        win = A("win", [128, 8, 2208], BF16)
        wkr = A("wkr", [128, 8, 192], BF16)
        wuq = A("wuq", [128, 3, 2, 768], BF16)
        wukvk = A("wukvk", [128, 2, 512], BF16)
        wukvv = A("wukvv", [128, 2, 512], BF16)
        ident = A("ident", [128, 128], F32)
        onesq = A("onesq", [128, 128], F32)
        oneskv = A("oneskv", [128, 128], F32)
        gq = A("gq", [128, 3], F32)
        gkv = A("gkv", [128, 2], F32)
        xg = [A("xg%d" % i, [128, 4, 1024], F32) for i in range(2)]
        hT = [A("hT%d" % i, [128, 8, 512], BF16) for i in range(2)]
        cT = A("cT", [128, 5, 512], F32)
        sq = A("sq", [128, 5, 512], F32)
        rstd = A("rstd", [128, 2, 512], F32)
        cn = [A("cn%d" % i, [128, 5, 512], BF16) for i in range(2)]
        cosr = [A("cosr%d" % i, [128, 512], F32) for i in range(2)]
        sinr = [A("sinr%d" % i, [128, 512], F32) for i in range(2)]
        tmpa = [A("tmpa%d" % i, [128, 512], F32) for i in range(2)]
        tmpb = [A("tmpb%d" % i, [128, 512], F32) for i in range(2)]
        NO = 6
        ob = [A("ob%d" % i, [128, 512], BF16) for i in range(NO)]
        vo = [A("vo%d" % i, [128, 768], BF16) for i in range(4)]
        Bw, Bid, Bones, Bg = Buf(), Buf(), Buf(), Buf()
        Bxg = [Buf(), Buf()]
        BhT = [Buf(), Buf()]
        BcT, Bsq, Brstd = [Buf() for _ in range(5)], [Buf() for _ in range(5)], [Buf(), Buf()]
        Bcn = [[Buf() for _ in range(5)] for _ in range(2)]
        Brope = [Buf(), Buf()]
        Bta, Btb = [Buf(), Buf()], [Buf(), Buf()]
        Bob = [Buf() for _ in range(NO)]
        Bvo = [Buf() for _ in range(4)]
        ob_ctr, vo_ctr, t_ctr = [0], [0], [0]

        SKIP = os.environ.get('P1SKIP', '')
        for k in range(0 if 'w' in SKIP else 8):
            kb.dma("pool", [], [], win[:, k, :], win_d[k * 128:(k + 1) * 128, :], pwrites=[Bw])
            kb.dma("pool", [], [], wkr[:, k, :], wkr_d[k * 128:(k + 1) * 128, :], pwrites=[Bw])
        for c in range(0 if 'u' in SKIP else 3):
            kb.dma("pool", [], [], wuq[:, c, :, :], wuq_d[c * 128:(c + 1) * 128, :, :], pwrites=[Bw])
        for c in range(0 if 'v' in SKIP else 2):
            kb.dma("pool", [], [], wukvk[:, c, :], wukvk_d[c * 128:(c + 1) * 128, :], pwrites=[Bw])
            kb.dma("pool", [], [], wukvv[:, c, :], wukvv_d[c * 128:(c + 1) * 128, :], pwrites=[Bw])
        kb.dma("sp", [], [Bid], ident[:], ident_d[:, :])
        kb.dma("sp", [], [], gq[:], gq_d[:, :], pwrites=[Bg])
        kb.dma("sp", [], [], gkv[:], gkv_d[:, :], pwrites=[Bg])
        kb.op("pool", [], [], lambda e: e.memset(onesq[:], 1.0 / 384.0), pwrites=[Bones])
        kb.op("pool", [], [], lambda e: e.memset(oneskv[:], 1.0 / 256.0), pwrites=[Bones])
        for i in range(4):
            kb.op("pool", [], [Bvo[i]], lambda e, i=i: e.memset(vo[i][:], 1.0))

        x_v = x_d.rearrange("(t p) d -> p t d", p=128)

        def load_x(g):
            kb.dma("sp", [], [Bxg[g % 2]], xg[g % 2][:], x_v[:, g * 4:(g + 1) * 4, :])
            kb.dma("sp", [], [Brope[g % 2]], cosr[g % 2][64:96, :], cos_d[:, g * 512:(g + 1) * 512])
            kb.dma("sp", [], [Brope[g % 2]], sinr[g % 2][64:96, :], sin_d[:, g * 512:(g + 1) * 512])

        def next_ob():
            i = ob_ctr[0] % NO
            ob_ctr[0] += 1
            return ob[i], Bob[i]

        def evac_copy(eng, src, bsrc, dst, bdst):
            if eng == "act":
                kb.op("act", [bsrc], [bdst], lambda e: e.copy(out=dst, in_=src))
            else:
                kb.op("dve", [bsrc], [bdst], lambda e: e.tensor_copy(out=dst, in_=src))

        def rope_rows(bm, bbm, br, bbr, g, dst, bdst, full):
            i = t_ctr[0] % 2
            t_ctr[0] += 1
            kb.op("dve", [bbm, Brope[g % 2]], [Bta[i]], lambda e: e.tensor_tensor(
                out=tmpa[i][64:96, :], in0=bm[64:96, :], in1=cosr[g % 2][64:96, :], op=ALU.mult))
            kb.op("dve", [bbr, Brope[g % 2]], [Btb[i]], lambda e: e.tensor_tensor(
                out=tmpb[i][64:96, :], in0=br[64:96, :], in1=sinr[g % 2][64:96, :], op=ALU.mult))
            kb.op("dve", [Bta[i], Btb[i]], [bdst] if full else [], lambda e: e.tensor_tensor(
                out=dst[64:96, :], in0=tmpa[i][64:96, :], in1=tmpb[i][64:96, :], op=ALU.add),
                pwrites=[] if full else [bdst])

        def vo_views(vt, bk):
            o = AP(vt, 0, [pdim(vt[:]), [192, 4], [128, 2], [1, 64]])
            i = AP(bk.tensor, bk.offset, [pdim(bk), [128, 4], [64, 2], [1, 64]])
            return o, i

        load_x(0)
        LIM = float(os.environ.get('P1LIM', '99'))
        SUB = float(os.environ.get('P1SUB', '99'))
        for g in range(NG if LIM >= 99 else 1):
            if g + 1 < NG:
                load_x(g + 1)
            X, BX = xg[g % 2], Bxg[g % 2]
            H, BH = hT[g % 2], BhT[g % 2]
            CN, BCN = cn[g % 2], Bcn[g % 2]
            cols = slice(g * 512, (g + 1) * 512)
            for k in range(8):
                bk, bb = bank()

                def tp(e, bk=bk, k=k):
                    for i in range(4):
                        ins = e.transpose(bk[:, i * 128:(i + 1) * 128], X[:, i, k * 128:(k + 1) * 128], ident[:])
                    return ins
                kb.op("pe", [BX, Bid], [bb], tp)
                kb.op("act", [bb, B_modfm], [BH] if k == 0 else [], lambda e, bk=bk, k=k: e.activation(
                    out=H[:, k, :], in_=bk, func=AF.Identity, scale=modfm[:, 8 + k:9 + k], bias=modfm[:, k:k + 1]),
                    pwrites=[] if k == 0 else [BH])
            if LIM < 1:
                break
            for c in range(5):
                bk, bb = bank()

                def mm(e, bk=bk, c=c):
                    for k in range(8):
                        ins = e.matmul(bk, win[:, k, c * 128:(c + 1) * 128], H[:, k, :], start=(k == 0), stop=(k == 7))
                    return ins
                kb.op("pe", [BH, Bw], [bb], mm)
                if SUB >= 0.1:
                    kb.op("act", [bb], [Bsq[c]], lambda e, bk=bk, c=c: e.activation(out=sq[:, c, :], in_=bk, func=AF.Square))
                if SUB >= 0.15:
                    kb.op("dve", [bb, Bsq[c]], [BcT[c]], lambda e, bk=bk, c=c: e.tensor_copy(out=cT[:, c, :], in_=bk))
            if SUB < 0.3:
                break
            for which, (cs, ones_t) in enumerate((((0, 1, 2), onesq), ((3, 4), oneskv))):
                bk, bb = bank()

                def mm(e, bk=bk, cs=cs, ones_t=ones_t):
                    for n, c in enumerate(cs):
                        ins = e.matmul(bk, ones_t[:], sq[:, c, :], start=(n == 0), stop=(n == len(cs) - 1))
                    return ins
                kb.op("pe", [Bsq[c] for c in cs] + [Bones], [bb], mm)
                kb.op("dve", [bb], [Brstd[which]], lambda e, bk=bk, which=which: e.tensor_scalar(
                    out=rstd[:, which, :], in0=bk, scalar1=EPS, scalar2=None, op0=ALU.add))
                if SUB < 0.5:
                    continue
                kb.op("act", [Brstd[which]], [Brstd[which]], lambda e, which=which: e.activation(
                    out=rstd[:, which, :], in_=rstd[:, which, :], func=AF.Sqrt))
                kb.op("dve", [Brstd[which]], [Brstd[which]], lambda e, which=which: e.reciprocal(
                    out=rstd[:, which, :], in_=rstd[:, which, :]))
                if SUB < 0.7:
                    continue
                for c in cs:
                    gsc = gq[:, c:c + 1] if which == 0 else gkv[:, c - 3:c - 2]
                    kb.op("dve", [BcT[c], Brstd[which], Bg], [BCN[c]], lambda e, c=c, gsc=gsc, which=which: e.scalar_tensor_tensor(
                        out=CN[:, c, :], in0=cT[:, c, :], scalar=gsc, in1=rstd[:, which, :], op0=ALU.mult, op1=ALU.mult))
            if LIM < 2:
                break
            for h in range(8):
                bm, bbm = bank()
                br, bbr = bank()

                def mm(e, h=h, bm=bm, br=br):
                    for which, bk in ((0, bm), (1, br)):
                        for c in range(3):
                            ins = e.matmul(bk[0:96, :], wuq[:, c, which, h * 96:(h + 1) * 96], CN[:, c, :],
                                           start=(c == 0), stop=(c == 2))
                    return ins
                kb.op("pe", [BCN[0], BCN[1], BCN[2], Bw], [bbm, bbr], mm)
                o, bo = next_ob()
                kb.op("act", [bbm], [bo], lambda e, o=o, bm=bm: e.copy(out=o[0:64, :], in_=bm[0:64, :]))
                rope_rows(bm, bbm, br, bbr, g, o, bo, False)
                kb.dma("sp", [bo], [], qT_d[h, :, cols], o[0:96, :], pwrites=[B_q])
            if LIM < 3:
                break
            for hp in range(4):
                bk, bb = bank()

                def mm(e, hp=hp, bk=bk):
                    for c in range(2):
                        ins = e.matmul(bk, wukvk[:, c, hp * 128:(hp + 1) * 128], CN[:, 3 + c, :], start=(c == 0), stop=(c == 1))
                    return ins
                kb.op("pe", [BCN[3], BCN[4], Bw], [bb], mm)
                o, bo = next_ob()
                evac_copy("act", bk, bb, o[:, :], bo)
                kb.dma("sp", [bo], [], kT_d[2 * hp, 0:64, cols], o[0:64, :], pwrites=[B_k])
                kb.dma("sp", [bo], [], kT_d[2 * hp + 1, 0:64, cols], o[64:128, :], pwrites=[B_k])
            if LIM < 4:
                break
            bm, bbm = bank()
            br, bbr = bank()

            def mm(e, bm=bm, br=br):
                for which, bk in ((0, bm), (1, br)):
                    for k in range(8):
                        ins = e.matmul(bk[0:96, :], wkr[:, k, which * 96:(which + 1) * 96], H[:, k, :],
                                       start=(k == 0), stop=(k == 7))
                return ins
            kb.op("pe", [BH, Bw], [bbm, bbr], mm)
            o, bo = next_ob()
            rope_rows(bm, bbm, br, bbr, g, o, bo, True)
            for h in range(8):
                kb.dma("sp", [bo], [], kT_d[h, 64:96, cols], o[64:96, :], pwrites=[B_k])
            if LIM < 5:
                break
            for i in range(4):
                bk, bb = bank()

                def mm(e, i=i, bk=bk):
                    for c in range(2):
                        ins = e.matmul(bk, CN[:, 3 + c, i * 128:(i + 1) * 128], wukvv[:, c, :], start=(c == 0), stop=(c == 1))
                    return ins
                kb.op("pe", [BCN[3], BCN[4], Bw], [bb], mm)
                vi = vo_ctr[0] % 4
                vo_ctr[0] += 1
                ov, iv = vo_views(vo[vi], bk)
                kb.op("act", [bb], [Bvo[vi]], lambda e, ov=ov, iv=iv: e.copy(out=ov, in_=iv))
                kb.dma("sp", [Bvo[vi]], [], Vm_d[g * 4 + i, :, :], vo[vi][:], pwrites=[B_vm])
            if LIM < 6:
                break
            for base, dst, bd in ((672, dqT_d, B_dq), (1184, dkT_d, B_dk)):
                for hp in range(4):
                    bk, bb = bank()

                    def mm(e, hp=hp, bk=bk, base=base):
                        for k in range(8):
                            ins = e.matmul(bk, win[:, k, base + hp * 128: base + (hp + 1) * 128], H[:, k, :],
                                           start=(k == 0), stop=(k == 7))
                        return ins
                    kb.op("pe", [BH, Bw], [bb], mm)
                    o, bo = next_ob()
                    evac_copy("act" if hp % 2 == 0 else "dve", bk, bb, o[:, :], bo)
                    kb.dma("sp", [bo], [], dst[hp, :, cols], o[:, :], pwrites=[bd])
            if LIM < 7:
                break
            for i in range(4):
                bk, bb = bank()

                def mm(e, i=i, bk=bk):
                    for k in range(8):
                        ins = e.matmul(bk, H[:, k, i * 128:(i + 1) * 128], win[:, k, 1696:2208], start=(k == 0), stop=(k == 7))
                    return ins
                kb.op("pe", [BH, Bw], [bb], mm)
                vi = vo_ctr[0] % 4
                vo_ctr[0] += 1
                ov, iv = vo_views(vo[vi], bk)
                kb.op("act", [bb], [Bvo[vi]], lambda e, ov=ov, iv=iv: e.copy(out=ov, in_=iv))
                kb.dma("sp", [Bvo[vi]], [], Vd_d[g * 4 + i, :, :], vo[vi][:], pwrites=[B_vd])
        kb.barrier()
    if stages <= 1:
        _finish(nc, kb, out_d, x_d)
        return

    def attention(tag, kT_src, qT_src, V_src, per_pair_kq, krows, scale, chunk0, dilated, Bk_src, Bq_src, Bv_src):
        with ExitStack() as sa:
            A = lambda n, sh, dt: sa.enter_context(nc.sbuf_tensor(tag + "_" + n, sh, dt))
            Vp = [A("Vp%d" % i, [128, 32, 192], BF16) for i in range(2)]
            Kt = [A("Kt%d" % i, [128, S], BF16) for i in range(2)]
            Qt = [A("Qt%d" % i, [128, S], BF16) for i in range(2)]
            pte = [A("pte%d" % i, [128, 1024], BF16) for i in range(3)]
            ptm = [A("ptm%d" % i, [128, 1024], BF16) for i in range(3)] if dilated else None
            ao = [A("ao%d" % i, [128, S], BF16) for i in range(2)]
            rec = [A("rec%d" % i, [128, 512], F32) for i in range(2)]
            BVp, BKt, BQt = [Buf(), Buf()], [Buf(), Buf()], [Buf(), Buf()]
            Bpte, Bptm = [Buf() for _ in range(3)], [Buf() for _ in range(3)]
            Bao, Brec = [Buf(), Buf()], [Buf(), Buf()]
            SB = [Buf("S%d" % i, excl=True) for i in range(3)]
            AB = [Buf("acc%d" % i, excl=True) for i in range(2)]
            Sap = [ps[:, i * 1024:(i + 1) * 1024] for i in range(3)]
            Aap = [ps[:, 3072 + i * 512: 3072 + (i + 1) * 512] for i in range(2)]
            Bzk = Buf()
            if per_pair_kq:
                KtB = [A("KtB%d" % i, [128, S], BF16) for i in range(2)]
                for i in range(2):
                    kb.op("dve", [], [], lambda e, i=i: e.memset(Kt[i][64:128, :], 0.0), pwrites=[Bzk])
                    kb.op("dve", [], [], lambda e, i=i: e.memset(KtB[i][0:64, :], 0.0), pwrites=[Bzk])
            if dilated:
                toep_st = A("toep", [128, MW], F32)
                multb = A("multb", [128, MW], BF16)
                master = [A("master%d" % i, [128, MW], BF16) for i in range(2)]
                Btoep, Bmult, Bmaster = Buf(), Buf(), [Buf(), Buf()]
                kb.dma("pool", [], [Bmult], multb[:], mult_d[:, :])

            def load_pair(hp):
                for j in range(4):
                    kb.dma("sp", [Bv_src], [], Vp[hp % 2][:, j * 8:(j + 1) * 8, :],
                           V_src[j * 8:(j + 1) * 8, :, hp * 192:(hp + 1) * 192].rearrange("t p c -> p t c"),
                           pwrites=[BVp[hp % 2]])

            def load_kq(u):
                if per_pair_kq:
                    kb.dma("sp", [Bk_src, Bzk], [BKt[u % 2]], Kt[u % 2][0:64, :], kT_src[u, 0:64, :])
                    kb.dma("sp", [Bk_src, Bzk], [], KtB[u % 2][64:128, :], kT_src[u, 64:128, :], pwrites=[BKt[u % 2]])
                    kb.dma("sp", [Bq_src], [BQt[u % 2]], Qt[u % 2][:, :], qT_src[u, :, :])
                else:
                    kb.dma("sp", [Bk_src], [BKt[u % 2]], Kt[u % 2][0:krows, :], kT_src[u, :, :])
                    kb.dma("sp", [Bq_src], [BQt[u % 2]], Qt[u % 2][0:krows, :], qT_src[u, :, :])

            def prep_mask(h):
                kb.dma("sp", [], [Btoep], toep_st[:], toep_d[h, :, :])
                kb.op("act", [Btoep], [Btoep], lambda e: e.activation(out=toep_st[:], in_=toep_st[:], func=AF.Exp))
                kb.op("dve", [Btoep, Bmult], [Bmaster[h % 2]], lambda e: e.tensor_tensor(
                    out=master[h % 2][:], in0=toep_st[:], in1=multb[:], op=ALU.mult))

            items = []
            for h in range(8):
                hp, hh = h // 2, h % 2
                for Q in range(8):
                    if dilated:
                        Ts = list(range(max(0, 4 * Q - 8), min(31, 4 * Q + 11) + 1))
                    else:
                        Ts = list(range(32))
                    groups = []
                    n = 0
                    while n < len(Ts):
                        if n + 1 < len(Ts):
                            groups.append([Ts[n + 1], Ts[n]])
                            n += 2
                        else:
                            groups.append([Ts[n]])
                            n += 1
                    for gi, gT in enumerate(groups):
                        items.append(dict(h=h, hp=hp, hh=hh, Q=Q, Ts=gT, first=(gi == 0), last=(gi == len(groups) - 1),
                                          hq=h * 8 + Q))
            N = len(items)
            for i, it in enumerate(items):
                it["i"] = i

            def kq_unit(it):
                return it["hp"] if per_pair_kq else it["h"]

            def rows_of(it):
                if per_pair_kq:
                    return it["hh"] * 64, it["hh"] * 64 + 64
                return 0, krows

            def emit_qk(it):
                i = it["i"]
                u = kq_unit(it)
                r0, r1 = rows_of(it)
                K_, Q_ = Kt[u % 2], Qt[u % 2]
                if per_pair_kq:
                    r0, r1 = 0, 128
                    if it["hh"] == 1:
                        K_ = KtB[u % 2]

                def f(e):
                    for j, T in enumerate(it["Ts"]):
                        ins = e.matmul(Sap[i % 3][:, j * 512:(j + 1) * 512], K_[r0:r1, T * 128:(T + 1) * 128],
                                       Q_[r0:r1, it["Q"] * 512:(it["Q"] + 1) * 512], start=True, stop=True)
                    return ins
                kb.op("pe", [BKt[u % 2], BQt[u % 2]], [SB[i % 3]], f)

            def emit_exp(it):
                i = it["i"]
                w = 512 * len(it["Ts"])
                kb.op("act", [SB[i % 3]], [Bpte[i % 3]], lambda e: e.activation(
                    out=pte[i % 3][:, 0:w], in_=Sap[i % 3][:, 0:w], func=AF.Exp, scale=scale))

            def emit_mask(it):
                i = it["i"]
                n = len(it["Ts"])
                h = it["h"]
                c0 = C0 - 128 * it["Ts"][0] + 512 * it["Q"]
                m = master[h % 2]
                in1 = AP(m, c0, [pdim(m[:]), [128, n], [1, 512]])
                o = AP(ptm[i % 3], 0, [pdim(ptm[i % 3][:]), [512, n], [1, 512]])
                i0 = AP(pte[i % 3], 0, [pdim(pte[i % 3][:]), [512, n], [1, 512]])
                kb.op("dve", [Bpte[i % 3], Bmaster[h % 2]], [Bptm[i % 3]], lambda e: e.tensor_tensor(
                    out=o, in0=i0, in1=in1, op=ALU.mult))

            def emit_pv(it):
                i = it["i"]
                P_, BP_ = (ptm[i % 3], Bptm[i % 3]) if dilated else (pte[i % 3], Bpte[i % 3])
                acc = Aap[it["hq"] % 2]
                V_ = Vp[it["hp"] % 2]
                vc = 64 * it["hh"]
                nT = len(it["Ts"])

                def f(e):
                    for j, T in enumerate(it["Ts"]):
                        ins = e.matmul(acc, V_[:, T, vc:vc + 128], P_[:, j * 512:(j + 1) * 512],
                                       start=(it["first"] and j == 0), stop=(it["last"] and j == nT - 1))
                    return ins
                if it["first"]:
                    kb.op("pe", [BP_, BVp[it["hp"] % 2]], [AB[it["hq"] % 2]], f)
                else:
                    kb.op("pe", [BP_, BVp[it["hp"] % 2]], [], f, pwrites=[AB[it["hq"] % 2]])

            def emit_fin(it):
                a = it["hq"] % 2
                acc = Aap[a]
                hh, hp, Q = it["hh"], it["hp"], it["Q"]
                nr = slice(64 * hh, 64 * hh + 64)
                dr = slice(64 * (1 - hh), 64 * (1 - hh) + 64)
                if dilated:
                    kb.op("act", [AB[a]], [Brec[a]], lambda e: e.activation(out=rec[a][nr, :], in_=acc[dr, :], func=AF.Ln))
                    kb.op("act", [Brec[a]], [Brec[a]], lambda e: e.activation(out=rec[a][nr, :], in_=rec[a][nr, :], func=AF.Exp, scale=-1.0))
                else:
                    kb.op("dve", [AB[a]], [Brec[a]], lambda e: e.reciprocal(out=rec[a][nr, :], in_=acc[dr, :]))
                first_of_pair = (hh == 0 and Q == 0)
                kb.op("dve", [AB[a], Brec[a]], [Bao[hp % 2]] if first_of_pair else [], lambda e: e.tensor_tensor(
                    out=ao[hp % 2][nr, Q * 512:(Q + 1) * 512], in0=acc[nr, :], in1=rec[a][nr, :], op=ALU.mult),
                    pwrites=[] if first_of_pair else [Bao[hp % 2]])
                if hh == 1 and Q == 7:
                    kb.dma("sp", [Bao[hp % 2]], [], attnT_d[chunk0 + hp, :, :], ao[hp % 2][:, :], pwrites=[B_attn])

            def unit_start(it):
                h = it["h"]
                if it["Q"] == 0 and it["first"]:
                    if h + 1 < 8:
                        if per_pair_kq:
                            if (h + 1) % 2 == 0:
                                load_kq((h + 1) // 2)
                        else:
                            load_kq(h + 1)
                        if (h + 1) % 2 == 0:
                            load_pair((h + 1) // 2)
                        if dilated:
                            prep_mask(h + 1)

            load_pair(0)
            load_kq(0)
            if dilated:
                prep_mask(0)
            emit_qk(items[0])
            emit_exp(items[0])
            if dilated:
                emit_mask(items[0])
            if N > 1:
                emit_qk(items[1])
            for i in range(N):
                it = items[i]
                unit_start(it)
                if i + 1 < N:
                    emit_exp(items[i + 1])
                    if dilated:
                        emit_mask(items[i + 1])
                if i + 2 < N:
                    emit_qk(items[i + 2])
                emit_pv(it)
                if it["last"]:
                    emit_fin(it)
            kb.barrier()

    run_dil = os.environ.get("SKIPDIL", "") == ""
    run_mla = os.environ.get("SKIPMLA", "") == ""
    if run_dil:
        attention("dil", dkT_d, dqT_d, Vd_d, True, 64, 64.0 ** -0.5, 4, True, B_dk, B_dq, B_vd)
    if run_mla:
        attention("mla", kT_d, qT_d, Vm_d, False, 96, 96.0 ** -0.5, 0, False, B_k, B_q, B_vm)
    if stages <= 3:
        _finish(nc, kb, out_d, x_d)
        return

    def bank2():
        if bank_ctr[0] % 2:
            bank_ctr[0] += 1
        i = bank_ctr[0] % 8
        bank_ctr[0] += 2
        return ps[:, i * 512:(i + 2) * 512], [PB[i], PB[i + 1]]

    class LNState:
        def __init__(self, A, tag, nb=2):
            self.junk = A(tag + "junk", [128, D], F32)
            self.st = [A(tag + "st%d" % i, [128, 8], F32) for i in range(nb)]
            self.Bjunk = Buf()
            self.Bst = [Buf() for _ in range(nb)]
            self.nb = nb
            self.n = 0

    def ln_stats(L, y, By):
        i = L.n % L.nb
        L.n += 1
        st, Bst = L.st[i], L.Bst[i]
        kb.op("dve", [], [Bst], lambda e: e.memset(st[:], 0.0))
        kb.op("act", [By, Bst], [L.Bjunk], lambda e: e.activation(out=L.junk[:], in_=y[:], func=AF.Identity, accum_out=st[:, 0:1]),
              pwrites=[Bst])
        kb.op("act", [By, Bst], [L.Bjunk], lambda e: e.activation(out=L.junk[:], in_=y[:], func=AF.Square, accum_out=st[:, 1:2]),
              pwrites=[Bst])
        return st, Bst

    def ln_apply(st, Bst, y, By, xh, Bxh):
        kb.op("dve", [Bst], [], lambda e: e.tensor_scalar(out=st[:, 2:3], in0=st[:, 0:1], scalar1=1.0 / D, scalar2=None, op0=ALU.mult), pwrites=[Bst])
        kb.op("dve", [Bst], [], lambda e: e.tensor_tensor(out=st[:, 3:4], in0=st[:, 2:3], in1=st[:, 2:3], op=ALU.mult), pwrites=[Bst])
        kb.op("dve", [Bst], [], lambda e: e.scalar_tensor_tensor(out=st[:, 4:5], in0=st[:, 1:2], scalar=1.0 / D, in1=st[:, 3:4],
                                                                   op0=ALU.mult, op1=ALU.subtract), pwrites=[Bst])
        kb.op("dve", [Bst], [], lambda e: e.tensor_scalar(out=st[:, 4:5], in0=st[:, 4:5], scalar1=EPS, scalar2=None, op0=ALU.add), pwrites=[Bst])
        kb.op("act", [Bst], [], lambda e: e.activation(out=st[:, 5:6], in_=st[:, 4:5], func=AF.Ln), pwrites=[Bst])
        kb.op("act", [Bst], [], lambda e: e.activation(out=st[:, 5:6], in_=st[:, 5:6], func=AF.Exp, scale=-0.5), pwrites=[Bst])
        kb.op("dve", [Bst], [], lambda e: e.scalar_tensor_tensor(out=st[:, 6:7], in0=st[:, 2:3], scalar=-1.0, in1=st[:, 5:6],
                                                                   op0=ALU.mult, op1=ALU.mult), pwrites=[Bst])
        kb.op("act", [By, Bst], [Bxh], lambda e: e.activation(out=xh[:], in_=y[:], func=AF.Identity, scale=st[:, 5:6], bias=st[:, 6:7]))

    with ExitStack() as s4:
        A = lambda n, sh, dt: s4.enter_context(nc.sbuf_tensor("p4_" + n, sh, dt))
        wout = A("wout", [128, 8, D], BF16)
        g1bc, lngbc, lnbbc, sc2bc, sh2bc = (A(n, [128, D], F32) for n in ("g1bc", "lngbc", "lnbbc", "sc2bc", "sh2bc"))
        wr = A("wr", [128, 8, NE], F32)
        ident = A("ident", [128, 128], F32)
        at = [A("at%d" % i, [128, 8, 512], BF16) for i in range(2)]
        xt = [A("xt%d" % i, [128, D], F32) for i in range(2)]
        t1 = [A("t1%d" % i, [128, D], F32) for i in range(2)]
        yy = [A("y%d" % i, [128, D], F32) for i in range(2)]
        xh = [A("xh%d" % i, [128, D], F32) for i in range(2)]
        x1 = [A("x1%d" % i, [128, D], F32) for i in range(2)]
        h2f = [A("h2f%d" % i, [128, D], F32) for i in range(2)]
        h2b = [A("h2b%d" % i, [128, D], BF16) for i in range(2)]
        h2T = [A("h2T%d" % i, [128, 8, 128], F32) for i in range(2)]
        sm = [A("sm%d" % i, [128, 8], F32) for i in range(2)]
        ee = [A("ee%d" % i, [128, NE], F32) for i in range(2)]
        L = LNState(A, "ln", 3)
        Bc4 = Buf()
        Bat, Bxt, Bt1, By, Bxh, Bx1, Bh2f, Bh2b, Bh2T, Bsm, Bee = ([Buf(), Buf()] for _ in range(11))
        kb.dma("sp", [B_mod], [], g1bc[:], bcast_row(mod_d[0:1, 2048:3072], D), pwrites=[Bc4])
        Bws = [Buf(), Buf()]
        for c in range(8):
            kb.dma("sp", [], [Bws[c % 2]], t1[c % 2][:], wout_d[c * 128:(c + 1) * 128, :])
            kb.op("dve", [Bws[c % 2], Bc4], [], lambda e, c=c: e.tensor_tensor(out=wout[:, c, :], in0=t1[c % 2][:], in1=g1bc[:], op=ALU.mult),
                  pwrites=[Bc4])
        kb.dma("sp", [B_mod], [], sh2bc[:], bcast_row(mod_d[0:1, 3072:4096], D), pwrites=[Bc4])
        kb.dma("sp", [B_mod], [], sc2bc[:], bcast_row(mod_d[0:1, 4096:5120], D), pwrites=[Bc4])
        kb.dma("sp", [], [], lngbc[:], bcast_row(ln1g_d[0:1, :], D), pwrites=[Bc4])
        kb.dma("sp", [], [], lnbbc[:], bcast_row(ln1b_d[0:1, :], D), pwrites=[Bc4])
        kb.dma("sp", [], [], wr[:], wr_d.rearrange("(k p) e -> p k e", p=128), pwrites=[Bc4])
        kb.dma("sp", [], [], ident[:], ident_d[:, :], pwrites=[Bc4])
        kb.op("dve", [Bc4], [], lambda e: e.tensor_scalar(out=sc2bc[:], in0=sc2bc[:], scalar1=1.0, scalar2=None, op0=ALU.add), pwrites=[Bc4])
        attn_v = attnT_d.rearrange("c p n -> p c n")
        x_t = x_d.rearrange("(t p) d -> t p d", p=128)
        x1_t = x1_d.rearrange("(t p) d -> t p d", p=128)
        h2_t = h2_d.rearrange("(t p) d -> t p d", p=128)
        stA = {}
        stC = {}

        def stageA(t):
            g, i = t // 4, t % 4
            if i == 0:
                kb.dma("sp", [B_attn], [Bat[g % 2]], at[g % 2][:], attn_v[:, :, g * 512:(g + 1) * 512])
            b = t % 2
            kb.dma("sp", [], [Bxt[b]], xt[b][:], x_t[t])
            bk2, bb2 = bank2()

            def mm(e):
                for half in range(2):
                    for c in range(8):
                        ins = e.matmul(bk2[:, half * 512:(half + 1) * 512], at[g % 2][:, c, i * 128:(i + 1) * 128],
                                       wout[:, c, half * 512:(half + 1) * 512], start=(c == 0), stop=(c == 7))
                return ins
            kb.op("pe", [Bat[g % 2], Bc4], bb2, mm)
            kb.op("dve", bb2 + [Bxt[b]] + Bws, [By[b]], lambda e: e.scalar_tensor_tensor(
                out=yy[b][:], in0=xt[b][:], scalar=ALPHA, in1=bk2, op0=ALU.mult, op1=ALU.add))
            stA[t] = ln_stats(L, yy[b], By[b])

        def stageB1(t):
            b = t % 2
            st, Bst = stA.pop(t)
            ln_apply(st, Bst, yy[b], By[b], xh[b], Bxh[b])

        def stageB(t):
            b = t % 2
            kb.op("dve", [Bxh[b], Bc4], [Bx1[b]], lambda e: e.tensor_tensor(out=x1[b][:], in0=xh[b][:], in1=lngbc[:], op=ALU.mult))
            kb.op("dve", [Bx1[b], Bc4], [Bx1[b]], lambda e: e.tensor_tensor(out=x1[b][:], in0=x1[b][:], in1=lnbbc[:], op=ALU.add))
            kb.dma("sp", [Bx1[b]], [], x1_t[t], x1[b][:], pwrites=[B_x1])
            kb.op("dve", [Bx1[b], Bc4], [Bh2f[b]], lambda e: e.tensor_tensor(out=h2f[b][:], in0=x1[b][:], in1=sc2bc[:], op=ALU.mult))
            kb.op("dve", [Bh2f[b], Bc4], [Bh2f[b]], lambda e: e.tensor_tensor(out=h2f[b][:], in0=h2f[b][:], in1=sh2bc[:], op=ALU.add))
            kb.op("act", [Bh2f[b]], [Bh2b[b]], lambda e: e.copy(out=h2b[b][:], in_=h2f[b][:]))
            kb.dma("sp", [Bh2b[b]], [], h2_t[t], h2b[b][:], pwrites=[B_h2])
            bk2, bb2 = bank2()

            def tp(e):
                for k in range(8):
                    ins = e.transpose(bk2[:, k * 128:(k + 1) * 128], h2f[b][:, k * 128:(k + 1) * 128], ident[:])
                return ins
            kb.op("pe", [Bh2f[b], Bc4], bb2, tp)
            stC[t] = (bk2, bb2)

        def stageC(t):
            b = t % 2
            bk2, bb2 = stC.pop(t)
            kb.op("act", bb2, [Bh2T[b]], lambda e: e.copy(out=h2T[b][:].rearrange("p k n -> p (k n)"), in_=bk2))
            bk, bb = bank()

            def lg(e):
                for k in range(8):
                    ins = e.matmul(bk[:, 0:NE], h2T[b][:, k, :], wr[:, k, :], start=(k == 0), stop=(k == 7))
                return ins
            kb.op("pe", [Bh2T[b], Bc4], [bb], lg)
            kb.op("dve", [], [Bsm[b]], lambda e: e.memset(sm[b][:], 0.0))
            kb.op("dve", [bb], [], lambda e: e.reduce_max(out=sm[b][:, 0:1], in_=bk[:, 0:NE], axis=AX.X), pwrites=[Bsm[b]])
            kb.op("dve", [Bsm[b]], [], lambda e: e.tensor_scalar(out=sm[b][:, 1:2], in0=sm[b][:, 0:1], scalar1=-1.0, scalar2=None, op0=ALU.mult),
                  pwrites=[Bsm[b]])
            kb.op("act", [bb, Bsm[b]], [Bee[b]], lambda e: e.activation(out=ee[b][:], in_=bk[:, 0:NE], func=AF.Exp, bias=sm[b][:, 1:2],
                                                                       accum_out=sm[b][:, 2:3]), pwrites=[Bsm[b]])
            kb.op("dve", [Bsm[b]], [], lambda e: e.reciprocal(out=sm[b][:, 3:4], in_=sm[b][:, 2:3]), pwrites=[Bsm[b]])
            kb.op("dve", [Bee[b], Bsm[b]], [], lambda e: e.tensor_scalar(out=aff_all[:, t * NE:(t + 1) * NE], in0=ee[b][:], scalar1=sm[b][:, 3:4],
                                                                        scalar2=None, op0=ALU.mult), pwrites=[B_aff])

        for step in range(NT + 3):
            if 0 <= step - 3 < NT:
                stageC(step - 3)
            if 0 <= step - 2 < NT:
                stageB(step - 2)
            if 0 <= step - 1 < NT:
                stageB1(step - 1)
            if step < NT:
                stageA(step)
        if "aff_s" in dbg:
            kb.dma("sp", [B_aff], [Buf()], aff_d[:, :], aff_all[:])
        kb.barrier()
    if stages <= 4:
        _finish(nc, kb, out_d, x_d)
        return

    def moe_phase():
        with ExitStack() as s5:
            A = lambda n, sh, dt: s5.enter_context(nc.sbuf_tensor("p5_" + n, sh, dt))
            onesf = A("onesf", [128, 128], F32)
            onesb = A("onesb", [128, 128], BF16)
            triub = A("triub", [128, 128], BF16)
            identb = A("identb", [128, 128], BF16)
            iota = A("iota", [128, 512], F32)
            tokhl = A("tokhl", [128, NT, 2], F32)
            zero = A("zero", [128, D], F32)
            lo = A("lo", [128, NE], F32)
            tau = A("tau", [128, NE], F32)
            part = A("part", [128, NE], F32)
            gw = A("gw", [128, NE], F32)
            cmp = A("cmp", [128, NT * NE], F32)
            self_ = A("self", [128, NT * NE], F32)
            selb = A("selb", [128, NT * NE], BF16)
            posm = A("posm", [128, NT * NE], F32)
            r1 = A("r1", [128, NT * NE], F32)
            R = A("R", [128, NT * NE * 5], BF16)
            OH = [A("OH%d" % i, [128, 512], BF16) for i in range(4)]
            idxv = [A("idxv%d" % i, [128, 32], F32) for i in range(2)]
            idxf = [A("idxf%d" % i, [128, 4], F32) for i in range(2)]
            idxi = [A("idxi%d" % i, [128, 4], I32) for i in range(2)]
            val = [A("val%d" % i, [128, 4], F32) for i in range(2)]
            xin = [A("xin%d" % i, [128, 4, D], BF16) for i in range(2)]
            xinT = [A("xinT%d" % i, [128, 8, 512], BF16) for i in range(2)]
            wg = [A("wg%d" % i, [128, 8, 512], BF16) for i in range(3)]
            wu = [A("wu%d" % i, [128, 8, 512], BF16) for i in range(3)]
            wd = [A("wd%d" % i, [128, 6, D], BF16) for i in range(3)]
            sg = [A("sg%d" % i, [128, 512], F32) for i in range(2)]
            act = [A("act%d" % i, [128, 12, 512], BF16) for i in range(2)]
            ys = [A("ys%d" % i, [128, D], F32) for i in range(4)]
            mo = [A("mo%d" % i, [128, D], F32) for i in range(4)]
            Bc = Buf()
            Blo, Btau, Bpart, Bgw, Bcmp, Bsel, Bposm, BR = (Buf() for _ in range(8))
            BOH = [Buf() for _ in range(4)]
            Bidxv, Bidx, Bval = [Buf(), Buf()], [Buf(), Buf()], [Buf(), Buf()]
            Bxin, BxinT = [Buf(), Buf()], [Buf(), Buf()]
            Bwg, Bwu, Bwd = [Buf() for _ in range(3)], [Buf() for _ in range(3)], [Buf() for _ in range(3)]
            Bsg, Bact = [Buf(), Buf()], [Buf(), Buf()]
            Bys, Bmo = [Buf() for _ in range(4)], [Buf() for _ in range(4)]

            kb.op("pool", [], [], lambda e: e.memset(onesf[:], 1.0), pwrites=[Bc])
            kb.op("pool", [], [], lambda e: e.memset(onesb[:], 1.0), pwrites=[Bc])
            kb.op("pool", [], [], lambda e: e.memset(zero[:], 0.0), pwrites=[Bc])
            kb.op("pool", [], [Blo], lambda e: e.memset(lo[:], 0.0))
            kb.dma("pool", [], [], triub[:], triu_d[:, :], pwrites=[Bc])
            kb.dma("pool", [], [], identb[:], ident_d[:, :], pwrites=[Bc])
            kb.dma("sp", [], [], iota[:], iota_d[:, :], pwrites=[Bc])
            kb.dma("sp", [], [], tokhl[:], tokhl_d[:, :, :], pwrites=[Bc])
            moe_t = moe_d.rearrange("(t p) d -> t p d", p=128)
            for t in range(NT):
                kb.dma("sp", [Bc], [], moe_t[t], zero[:], pwrites=[B_moe])

            def issue_gu(n):
                if n >= NE * 3:
                    return
                e, fb = n // 3, n % 3
                b = n % 3
                kb.dma("pool", [], [Bwg[b]], wg[b][:], wg_d[e, :, fb * 512:(fb + 1) * 512].rearrange("(k p) f -> p k f", p=128))
                kb.dma("pool", [], [Bwu[b]], wu[b][:], wu_d[e, :, fb * 512:(fb + 1) * 512].rearrange("(k p) f -> p k f", p=128))

            def issue_d(m):
                if m >= NE * 2:
                    return
                e, half = m // 2, m % 2
                b = m % 3
                kb.dma("pool", [], [Bwd[b]], wd[b][:], wd_d[e, half * 768:(half + 1) * 768, :].rearrange("(k p) n -> p k n", p=128))

            issue_gu(0)
            issue_gu(1)
            issue_d(0)
            issue_d(1)
            issue_d(2)

            aff3 = AP(aff_all, 0, [pdim(aff_all[:]), [NE, NT], [1, NE]])
            cmp3 = AP(cmp, 0, [pdim(cmp[:]), [NE, NT], [1, NE]])
            cmpT = AP(cmp, 0, [pdim(cmp[:]), [1, NE], [NE, NT]])

            def bc16(tile_):
                return AP(tile_, 0, [pdim(tile_[:]), [0, NT], [1, NE]])

            for it in range(1, NBIS + 1):
                w = 2.0 ** (-it)
                kb.op("dve", [Blo], [Btau], lambda e: e.tensor_scalar(out=tau[:], in0=lo[:], scalar1=w, scalar2=None, op0=ALU.add))
                kb.op("dve", [B_aff, Btau], [Bcmp], lambda e: e.tensor_tensor(out=cmp3, in0=aff3, in1=bc16(tau), op=ALU.is_ge))
                kb.op("dve", [Bcmp], [Bpart], lambda e: e.tensor_reduce(out=part[:], in_=cmpT, axis=AX.X, op=ALU.add))
                bk, bb = bank()
                kb.op("pe", [Bpart, Bc], [bb], lambda e: e.matmul(bk[:, 0:NE], onesf[:], part[:], start=True, stop=True))
                kb.op("dve", [bb], [Bgw], lambda e: e.tensor_scalar(out=gw[:], in0=bk[:, 0:NE], scalar1=CAP - 0.5, scalar2=w,
                                                                  op0=ALU.is_ge, op1=ALU.mult))
                kb.op("dve", [Bgw, Blo], [Blo], lambda e: e.tensor_tensor(out=lo[:], in0=lo[:], in1=gw[:], op=ALU.add))
            self3 = AP(self_, 0, [pdim(self_[:]), [NE, NT], [1, NE]])
            kb.op("dve", [B_aff, Blo], [Bsel], lambda e: e.tensor_tensor(out=self3, in0=aff3, in1=bc16(lo), op=ALU.is_ge))
            kb.op("dve", [Bsel], [], lambda e: e.tensor_copy(out=selb[:], in_=self_[:]), pwrites=[Bsel])
            bk, bb = bank()

            def prefix(e):
                for t in range(NT):
                    o = bk[:, t * NE:(t + 1) * NE]
                    for tp in range(t):
                        e.matmul(o, onesb[:], selb[:, tp * NE:(tp + 1) * NE], start=(tp == 0), stop=False)
                    ins = e.matmul(o, triub[:], selb[:, t * NE:(t + 1) * NE], start=(t == 0), stop=True)
                return ins
            kb.op("pe", [Bsel, Bc], [bb], prefix)
            kb.op("dve", [bb, Bsel], [Bposm], lambda e: e.scalar_tensor_tensor(out=posm[:], in0=bk, scalar=1.0, in1=self_[:],
                                                                            op0=ALU.add, op1=ALU.mult))
            kb.op("dve", [Bposm], [Bposm], lambda e: e.tensor_scalar(out=posm[:], in0=posm[:], scalar1=-1.0, scalar2=None, op0=ALU.add))
            Rv = lambda j: AP(R, j, [pdim(R[:]), [5, NT * NE]])
            kb.op("dve", [Bc], [BR], lambda e: e.tensor_copy(
                out=AP(R, 0, [pdim(R[:]), [5 * NE, NT], [5, NE], [1, 2]]),
                in_=AP(tokhl, 0, [pdim(tokhl[:]), [2, NT], [0, NE], [1, 2]])))
            kb.op("dve", [B_aff], [], lambda e: e.tensor_copy(out=Rv(2), in_=aff_all[:]), pwrites=[BR])
            kb.op("dve", [B_aff, BR], [Bcmp], lambda e: e.tensor_tensor(out=r1[:], in0=aff_all[:], in1=Rv(2), op=ALU.subtract))
            kb.op("dve", [Bcmp], [], lambda e: e.tensor_copy(out=Rv(3), in_=r1[:]), pwrites=[BR])
            kb.op("dve", [Bcmp, BR], [Bcmp], lambda e: e.tensor_tensor(out=r1[:], in0=r1[:], in1=Rv(3), op=ALU.subtract))
            kb.op("dve", [Bcmp], [], lambda e: e.tensor_copy(out=Rv(4), in_=r1[:]), pwrites=[BR])

            h2rows = h2_d
            oh_ctr = [0]

            rstate = {}

            def route_tiles(e, t0, t1):
                for t in range(t0, t1):
                    oi = oh_ctr[0] % 4
                    oh_ctr[0] += 1
                    kb.op("dve", [Bposm, Bc], [BOH[oi]], lambda en: en.tensor_scalar(
                        out=OH[oi][:], in0=iota[:], scalar1=posm[:, t * NE + e:t * NE + e + 1], scalar2=None, op0=ALU.is_equal))

                    def mm(en):
                        for cj in range(4):
                            c0 = 3072 + (cj * NT + t) * 8
                            ins = en.matmul(ps[:, c0:c0 + 5], OH[oi][:, cj * 128:(cj + 1) * 128],
                                            R[:, (t * NE + e) * 5:(t * NE + e) * 5 + 5], start=True, stop=True)
                        return ins
                    if t == 0:
                        kb.op("pe", [BOH[oi], BR], [PB[6], PB[7]], mm)
                    else:
                        kb.op("pe", [BOH[oi], BR], [], mm, pwrites=[PB[6], PB[7]])

            def route_idx(e):
                b = e % 2
                kb.op("dve", [PB[6], PB[7]], [Bidxv[b]], lambda en: en.tensor_reduce(
                    out=AP(idxv[b], 0, [pdim(idxv[b][:]), [8, 4], [1, 5]]),
                    in_=AP(ps, 3072, [pdim(ps[:]), [NT * 8, 4], [1, 5], [8, NT]]),
                    axis=AX.X, op=ALU.add))
                v3 = lambda j: AP(idxv[b], j, [pdim(idxv[b][:]), [8, 4]])
                kb.op("dve", [Bidxv[b]], [Bidx[b]], lambda en: en.scalar_tensor_tensor(
                    out=idxf[b][:], in0=v3(0), scalar=128.0, in1=v3(1), op0=ALU.mult, op1=ALU.add))
                kb.op("dve", [Bidx[b]], [], lambda en: en.tensor_copy(out=idxi[b][:], in_=idxf[b][:]), pwrites=[Bidx[b]])
                kb.op("dve", [Bidxv[b]], [Bval[b]], lambda en: en.tensor_tensor(out=val[b][:], in0=v3(2), in1=v3(3), op=ALU.add))
                kb.op("dve", [Bidxv[b], Bval[b]], [Bval[b]], lambda en: en.tensor_tensor(out=val[b][:], in0=val[b][:], in1=v3(4), op=ALU.add))
                for cj in range(4):
                    kb.dma("pool", [Bidx[b], B_h2], [] if cj else [Bxin[b]], xin[b][:, cj, :], h2rows[:, :],
                           pwrites=[Bxin[b]] if cj else [],
                           indirect=dict(out_offset=None, in_offset=bass.IndirectOffsetOnAxis(ap=idxi[b][:, cj:cj + 1], axis=0)))

            def route_T(e):
                b = e % 2
                for k in range(8):
                    bk2, bb2 = bank()
                    bkb = bk2.bitcast(BF16)

                    def tp(en):
                        for cj in range(4):
                            ins = en.transpose(bkb[:, cj * 128:(cj + 1) * 128], xin[b][:, cj, k * 128:(k + 1) * 128], identb[:])
                        return ins
                    kb.op("pe", [Bxin[b], Bc], [bb2], tp)
                    kb.op("act", [bb2], [] if k else [BxinT[b]],
                          (lambda en: en.copy(out=xinT[b][:, k, :], in_=bkb[:, 0:512])),
                          pwrites=[BxinT[b]] if k else [])

            def route(e):
                route_tiles(e, 0, NT)
                route_idx(e)
                route_T(e)

            def tail_gather(e):
                b = e % 2
                for cj in range(4):
                    kb.dma("pool", [Bidx[b], B_moe], [Bmo[cj]], mo[cj][:, :], moe_d[:, :],
                           indirect=dict(out_offset=None, in_offset=bass.IndirectOffsetOnAxis(ap=idxi[b][:, cj:cj + 1], axis=0)))

            def tail_add_scatter(e):
                b = e % 2
                for cj in range(4):
                    kb.op("dve", [Bmo[cj], Bys[cj]], [Bmo[cj]], lambda en: en.tensor_tensor(out=mo[cj][:], in0=mo[cj][:], in1=ys[cj][:], op=ALU.add))
                for cj in range(4):
                    kb.dma("pool", [Bidx[b], Bmo[cj]], [], moe_d[:, :], mo[cj][:, :], pwrites=[B_moe],
                           indirect=dict(out_offset=bass.IndirectOffsetOnAxis(ap=idxi[b][:, cj:cj + 1], axis=0), in_offset=None))

            def compute(e):
                b = e % 2
                for fb in range(3):
                    n = e * 3 + fb
                    wb = n % 3
                    for j in range(4):
                        bg, bbg = bank()
                        bu, bbu = bank()

                        def mm(en):
                            for bk_, w_ in ((bg, wg[wb]), (bu, wu[wb])):
                                for k in range(8):
                                    ins = en.matmul(bk_, w_[:, k, j * 128:(j + 1) * 128], xinT[b][:, k, :], start=(k == 0), stop=(k == 7))
                            return ins
                        kb.op("pe", [BxinT[b], Bwg[wb], Bwu[wb]], [bbg, bbu], mm)
                        if e + 1 < NE:
                            if fb * 4 + j < 11:
                                route_tiles(e + 1, 3 * (fb * 4 + j), min(NT, 3 * (fb * 4 + j) + 3))
                            else:
                                route_idx(e + 1)
                        if fb == 0 and j == 2 and e > 0:
                            tail_add_scatter(e - 1)
                        si = (fb * 4 + j) % 2
                        kb.op("act", [bbg], [Bsg[si]], lambda en: en.activation(out=sg[si][:], in_=bg, func=AF.Silu))
                        fi = fb * 4 + j
                        kb.op("dve", [Bsg[si], bbu], [] if fi else [Bact[b]], lambda en: en.tensor_tensor(
                            out=act[b][:, fi, :], in0=sg[si][:], in1=bu, op=ALU.mult), pwrites=[Bact[b]] if fi else [])
                    issue_gu(n + 2)
                for cj in range(4):
                    yi = cj
                    for half in range(2):
                        bk, bb = bank()

                        def mm(en):
                            for fi in range(12):
                                m = e * 2 + fi // 6
                                ins = en.matmul(bk, act[b][:, fi, cj * 128:(cj + 1) * 128], wd[m % 3][:, fi % 6, half * 512:(half + 1) * 512],
                                                start=(fi == 0), stop=(fi == 11))
                            return ins
                        kb.op("pe", [Bact[b], Bwd[(e * 2) % 3], Bwd[(e * 2 + 1) % 3]], [bb], mm)
                        kb.op("act", [bb, Bval[b]], [] if half else [Bys[yi]], lambda en: en.activation(
                            out=ys[yi][:, half * 512:(half + 1) * 512], in_=bk, func=AF.Copy, scale=val[b][:, cj:cj + 1]),
                            pwrites=[Bys[yi]] if half else [])
                issue_d(e * 2 + 3)
                issue_d(e * 2 + 4)
                if e + 1 < NE:
                    route_T(e + 1)
                tail_gather(e)
                if e == NE - 1:
                    tail_add_scatter(e)

            nrot[0] = 6
            bank_ctr[0] = 0
            route(0)
            for e in range(int(os.environ.get("NEXP", str(NE)))):
                compute(e)
            kb.barrier()
            nrot[0] = 8

    if os.environ.get("SKIPMOE", "") == "":
        moe_phase()

    with ExitStack() as s6:
        A = lambda n, sh, dt: s6.enter_context(nc.sbuf_tensor("p6_" + n, sh, dt))
        g2bc, lngbc, lnbbc = (A(n, [128, D], F32) for n in ("g2bc", "lngbc", "lnbbc"))
        NB6 = 3
        xt = [A("xt%d" % i, [128, D], F32) for i in range(NB6)]
        mt = [A("mt%d" % i, [128, D], F32) for i in range(NB6)]
        yy = [A("y%d" % i, [128, D], F32) for i in range(NB6)]
        xh = [A("xh%d" % i, [128, D], F32) for i in range(NB6)]
        oo = [A("o%d" % i, [128, D], F32) for i in range(NB6)]
        L = LNState(A, "ln", 3)
        Bc6 = Buf()
        Bxt, Bmt, By, Bxh, Boo = ([Buf() for _ in range(NB6)] for _ in range(5))
        kb.dma("sp", [B_mod], [], g2bc[:], bcast_row(mod_d[0:1, 5120:6144], D), pwrites=[Bc6])
        kb.dma("sp", [], [], lngbc[:], bcast_row(ln2g_d[0:1, :], D), pwrites=[Bc6])
        kb.dma("sp", [], [], lnbbc[:], bcast_row(ln2b_d[0:1, :], D), pwrites=[Bc6])
        x1_t = x1_d.rearrange("(t p) d -> t p d", p=128)
        moe_t = moe_d.rearrange("(t p) d -> t p d", p=128)
        out_t = out_d.rearrange("(t p) d -> t p d", p=128)
        stA = {}

        def stageA6(t):
            b = t % NB6
            kb.dma("sp", [B_x1], [Bxt[b]], xt[b][:], x1_t[t])
            kb.dma("sp", [B_moe], [Bmt[b]], mt[b][:], moe_t[t])
            kb.op("dve", [Bmt[b], Bc6], [Bmt[b]], lambda e: e.tensor_tensor(out=mt[b][:], in0=mt[b][:], in1=g2bc[:], op=ALU.mult))
            kb.op("dve", [Bxt[b], Bmt[b]], [By[b]], lambda e: e.scalar_tensor_tensor(
                out=yy[b][:], in0=xt[b][:], scalar=ALPHA, in1=mt[b][:], op0=ALU.mult, op1=ALU.add))
            stA[t] = ln_stats(L, yy[b], By[b])

        def stageB6(t):
            b = t % NB6
            st, Bst = stA.pop(t)
            ln_apply(st, Bst, yy[b], By[b], xh[b], Bxh[b])

        def stageC6(t):
            b = t % NB6
            kb.op("dve", [Bxh[b], Bc6], [Boo[b]], lambda e: e.tensor_tensor(out=oo[b][:], in0=xh[b][:], in1=lngbc[:], op=ALU.mult))
            kb.op("dve", [Boo[b], Bc6], [Boo[b]], lambda e: e.tensor_tensor(out=oo[b][:], in0=oo[b][:], in1=lnbbc[:], op=ALU.add))
            kb.dma("sp", [Boo[b]], [Buf()], out_t[t], oo[b][:])

        for step in range(NT + 2):
            if 0 <= step - 2 < NT:
                stageC6(step - 2)
            if 0 <= step - 1 < NT:
                stageB6(step - 1)
            if step < NT:
                stageA6(step)
        kb.barrier()


def _finish(nc, kb, out_d, x_d):
    kb.barrier()
    tok = kb.dma("sp", [], [Buf()], out_d[0:128, :], x_d[0:128, :])
    kb._wait("sp", [tok])


def _t5_bucket(rel):
    half = 16
    ret = (rel > 0).astype(np.int32) * half
    n = np.abs(rel)
    large = 8 + (np.log(np.maximum(n, 1) / 8) / np.log(1024 / 8) * (half - 8)).astype(np.int32)
    large = np.minimum(large, half - 1)
    return ret + np.where(n < 8, n, large).astype(np.int32)


def _static_tables():
    a = np.arange(128)[:, None]
    c = np.arange(MW)[None, :]
    rel = a - c + C0
    mult = ((np.abs(rel) <= 64).astype(np.float32)
            + ((rel % 4 == 0) & (np.abs(rel) <= 256)).astype(np.float32)
            + ((rel % 16 == 0) & (np.abs(rel) <= 1024)).astype(np.float32))
    bucket = _t5_bucket(rel)
    inv = (10000.0 ** (-(np.arange(0, 32, 2, dtype=np.float32)) / np.float32(32))).astype(np.float32)
    ang = (np.arange(S, dtype=np.float32)[:, None] * inv[None, :]).astype(np.float32)
    cos = np.cos(ang.astype(np.float64)).astype(np.float32).T
    sin = np.sin(ang.astype(np.float64)).astype(np.float32).T
    rope_cos = np.ascontiguousarray(np.concatenate([cos, cos], axis=0))
    rope_sin = np.ascontiguousarray(np.concatenate([-sin, sin], axis=0))
    tokhl = np.zeros((128, NT, 2), np.float32)
    tokhl[:, :, 0] = np.arange(NT)[None, :]
    tokhl[:, :, 1] = np.arange(128)[:, None]
    return dict(
        toep_mult=mult.astype(np.float32), bucket=bucket,
        rope_cos=rope_cos, rope_sin=rope_sin,
        ident=np.eye(128, dtype=np.float32),
        triu=np.triu(np.ones((128, 128), np.float32), k=1),
        iota512=np.tile(np.arange(512, dtype=np.float32)[None, :], (128, 1)),
        tokhl=tokhl,
    )


def make_in_maps(inputs):
    f = lambda a: np.ascontiguousarray(np.asarray(a, dtype=np.float32))
    st = _static_tables()
    x = f(inputs["x"])
    c = f(inputs["c"])
    w_in = f(inputs["w_in"][0])
    kr = w_in[:, 640:672]
    w_kr = np.zeros((D, 192), np.float32)
    w_kr[:, 64:96] = kr
    w_kr[:, 96 + 64:96 + 80] = kr[:, 16:32]
    w_kr[:, 96 + 80:96 + 96] = kr[:, 0:16]
    w_uq = f(inputs["w_uq"][0]).reshape(384, 8, 96)
    w_uq_rot = w_uq.copy()
    w_uq_rot[:, :, 64:80] = w_uq[:, :, 80:96]
    w_uq_rot[:, :, 80:96] = w_uq[:, :, 64:80]
    w_uq2 = np.ascontiguousarray(np.stack([w_uq.reshape(384, 768), w_uq_rot.reshape(384, 768)], axis=1))
    w_ukv = f(inputs["w_ukv"][0]).reshape(256, 8, 128)
    rel_bias = f(inputs["rel_bias"])
    toep = np.ascontiguousarray(np.transpose(rel_bias[st["bucket"]], (2, 0, 1)))
    shared = dict(
        w_ada=f(inputs["w_ada"][0]), b_ada=f(inputs["b_ada"][0]).reshape(1, -1),
        w_in=w_in, w_kr=w_kr,
        g_q=np.ascontiguousarray(f(inputs["q_norm_g"][0]).reshape(3, 128).T),
        g_kv=np.ascontiguousarray(f(inputs["kv_norm_g"][0]).reshape(2, 128).T),
        w_uq2=w_uq2,
        w_ukv_k=np.ascontiguousarray(w_ukv[:, :, 0:64].reshape(256, 512)),
        w_ukv_v=np.ascontiguousarray(w_ukv[:, :, 64:128].reshape(256, 512)),
        rope_cos=st["rope_cos"], rope_sin=st["rope_sin"],
        relb_toep=toep, toep_mult=st["toep_mult"],
        w_out=f(inputs["w_out"][0]), ln1_g=f(inputs["ln1_g"][0]).reshape(1, -1), ln1_b=f(inputs["ln1_b"][0]).reshape(1, -1),
        w_router=f(inputs["w_router"][0]),
        w_gate=f(inputs["w_gate"][0]), w_up=f(inputs["w_up"][0]), w_down=f(inputs["w_down"][0]),
        ln2_g=f(inputs["ln2_g"][0]).reshape(1, -1), ln2_b=f(inputs["ln2_b"][0]).reshape(1, -1),
        ident=st["ident"], triu=st["triu"], iota512=st["iota512"], tokhl=st["tokhl"],
    )
    maps = []
    for b in range(x.shape[0]):
        m = dict(shared)
        m["x"] = x[b]
        m["c_fm"] = np.ascontiguousarray(c[b].reshape(8, 128).T)
        maps.append(m)
    return maps


def kernel(**inputs):
    maps = make_in_maps(inputs)
    nc = build_program()
    res = run_bass_kernel_spmd(nc, maps, core_ids=list(range(len(maps))))
    return np.stack([np.asarray(r["out"], dtype=np.float32) for r in res.results], axis=0)
```

```python
import math
import os
from contextlib import ExitStack

import numpy as np
import concourse.bass as bass
import concourse.mybir as mybir
from concourse.bass_utils import run_bass_kernel_spmd

F32 = mybir.dt.float32
BF16 = mybir.dt.bfloat16
I32 = mybir.dt.int32
AF = mybir.ActivationFunctionType
ALU = mybir.AluOpType
AX = mybir.AxisListType

D = 1024
S = 4096
NT = 32
NG = 8
ALPHA = 2.0 ** 0.25
EPS = 1e-6
NE = 16
CAP = 512
DFF = 1536
C0 = 1408
MW = 2944
NBIS = 26


class Buf:
    __slots__ = ("w", "r", "name", "excl")

    def __init__(self, name="", excl=False):
        self.excl = excl
        self.w = []
        self.r = []
        self.name = name


def _compact(toks):
    best = {}
    for t in toks:
        if t[0] not in best or best[t[0]][2] < t[2]:
            best[t[0]] = t
    return list(best.values())


class KB:
    COMPUTE = ("pe", "act", "dve", "pool")
    NDS = 12

    def __init__(self, nc, es):
        self.nc = nc
        self.E = {"pe": nc.tensor, "act": nc.scalar, "dve": nc.vector, "pool": nc.gpsimd, "sp": nc.sync}
        self.csem = {e: es.enter_context(nc.semaphore("c_" + e)) for e in self.COMPUTE}
        self.ccnt = {e: 0 for e in self.COMPUTE}
        self.dsem = {q: [es.enter_context(nc.semaphore("d_%s%d" % (q, i))) for i in range(self.NDS)]
                     for q in ("sp", "pool", "act")}
        self.dcnt = {q: 0 for q in self.dsem}
        self.dtok = {q: [None] * self.NDS for q in self.dsem}
        self.seen = {e: {} for e in self.E}
        self.nwait = 0

    def _wait(self, eng, toks):
        need = {}
        for t in toks:
            if t is None:
                continue
            key, sem, val = t
            if self.seen[eng].get(key, 0) >= val:
                continue
            if need.get(key, (None, 0))[1] < val:
                need[key] = (sem, val)
        for key, (sem, val) in need.items():
            self.E[eng].wait_ge(sem, val)
            self.seen[eng][key] = val
            self.nwait += 1

    def _deps(self, eng, reads, writes, pwrites, is_dma):
        own = None if is_dma else "c_" + eng
        toks = []
        for b in reads:
            for t in b.w:
                if not (eng == "pe" and t[0] == own):
                    toks.append(t)
            if b.excl:
                toks.extend(t for t in b.r if t[0] != own)
        for b in writes:
            toks.extend(t for t in b.w if t[0] != own)
            toks.extend(t for t in b.r if t[0] != own)
        for b in pwrites:
            toks.extend(t for t in b.r if t[0] != own)
        return toks

    def _commit(self, tok, reads, writes, pwrites):
        for b in reads:
            b.r.append(tok)
            if len(b.r) > 16:
                b.r = _compact(b.r)
        for b in writes:
            b.w = [tok]
            b.r = []
        for b in pwrites:
            b.w.append(tok)
            if len(b.w) > 16:
                b.w = _compact(b.w)

    def op(self, eng, reads, writes, fn, pwrites=()):
        self._wait(eng, self._deps(eng, reads, writes, pwrites, False))
        inst = fn(self.E[eng])
        self.ccnt[eng] += 1
        tok = ("c_" + eng, self.csem[eng], self.ccnt[eng])
        inst.then_inc(self.csem[eng], 1)
        self._commit(tok, reads, writes, pwrites)
        return tok

    def dma(self, q, reads, writes, out, in_, pwrites=(), indirect=None, **kw):
        i = self.dcnt[q]
        slot = i % self.NDS
        self._wait(q, self._deps(q, reads, writes, pwrites, True) + [self.dtok[q][slot]])
        if indirect is None:
            inst = self.E[q].dma_start(out=out, in_=in_, **kw)
        else:
            inst = self.E[q].indirect_dma_start(out=out, in_=in_, **indirect)
        val = 16 * (i // self.NDS + 1)
        sem = self.dsem[q][slot]
        inst.then_inc(sem, 16)
        tok = ("d_%s%d" % (q, slot), sem, val)
        self.dtok[q][slot] = tok
        self.dcnt[q] += 1
        self._commit(tok, reads, writes, pwrites)
        return tok

    def barrier(self, engines=None):
        toks = []
        for e in self.COMPUTE:
            if self.ccnt[e]:
                toks.append(("c_" + e, self.csem[e], self.ccnt[e]))
        for q in self.dsem:
            toks.extend(t for t in self.dtok[q] if t is not None)
        for e in (engines or self.E):
            self._wait(e, toks)


def AP(t, off, dims):
    return bass.AP(t, off, [list(d) for d in dims])


def pdim(ap):
    return list(ap.ap[0])


def build_program(stages=99, dbg=()):
    nc = bass.Bass("TRN2", target_bir_lowering=False)
    es = ExitStack()
    with es:
        _build(nc, es, stages, dbg)
    return nc


def _build(nc, es, stages, dbg):
    def din(name, shape, dt=F32):
        return nc.dram_tensor(name, list(shape), dt, kind="ExternalInput").ap()

    def dscr(name, shape, dt):
        kind = "ExternalOutput" if name in dbg else "Internal"
        return nc.dram_tensor(name, list(shape), dt, kind=kind).ap()

    x_d = din("x", [S, D])
    cfm_d = din("c_fm", [128, 8])
    wada_d = din("w_ada", [D, 6 * D])
    bada_d = din("b_ada", [1, 6 * D])
    win_d = din("w_in", [D, 2208])
    wkr_d = din("w_kr", [D, 192])
    gq_d = din("g_q", [128, 3])
    gkv_d = din("g_kv", [128, 2])
    wuq_d = din("w_uq2", [384, 2, 768])
    wukvk_d = din("w_ukv_k", [256, 512])
    wukvv_d = din("w_ukv_v", [256, 512])
    cos_d = din("rope_cos", [32, S])
    sin_d = din("rope_sin", [32, S])
    toep_d = din("relb_toep", [8, 128, MW])
    mult_d = din("toep_mult", [128, MW])
    wout_d = din("w_out", [D, D])
    ln1g_d = din("ln1_g", [1, D])
    ln1b_d = din("ln1_b", [1, D])
    wr_d = din("w_router", [D, NE])
    wg_d = din("w_gate", [NE, D, DFF])
    wu_d = din("w_up", [NE, D, DFF])
    wd_d = din("w_down", [NE, DFF, D])
    ln2g_d = din("ln2_g", [1, D])
    ln2b_d = din("ln2_b", [1, D])
    ident_d = din("ident", [128, 128])
    triu_d = din("triu", [128, 128])
    iota_d = din("iota512", [128, 512])
    tokhl_d = din("tokhl", [128, NT, 2])
    out_d = nc.dram_tensor("out", [S, D], F32, kind="ExternalOutput").ap()

    mod_d = dscr("mod_s", [1, 6 * D], F32)
    qT_d = dscr("qT_s", [8, 96, S], BF16)
    kT_d = dscr("kT_s", [8, 96, S], BF16)
    Vm_d = dscr("Vm_s", [NT, 128, 768], BF16)
    dqT_d = dscr("dqT_s", [4, 128, S], BF16)
    dkT_d = dscr("dkT_s", [4, 128, S], BF16)
    Vd_d = dscr("Vd_s", [NT, 128, 768], BF16)
    attnT_d = dscr("attnT_s", [8, 128, S], BF16)
    x1_d = dscr("x1_s", [S, D], F32)
    h2_d = dscr("h2_s", [S, D], BF16)
    moe_d = dscr("moe_s", [S, D], F32)
    aff_d = dscr("aff_s", [128, NT * NE], F32)
    idx_d = dscr("idx_s", [128, NE * 4 * 8], F32)

    kb = KB(nc, es)
    B_mod, B_q, B_k, B_vm, B_dq, B_dk, B_vd = (Buf(n) for n in ("mod", "q", "k", "vm", "dq", "dk", "vd"))
    B_attn, B_x1, B_h2, B_moe = Buf("attn"), Buf("x1"), Buf("h2"), Buf("moe")

    ps = es.enter_context(nc.psum_tensor("ps", [128, 4096], F32))
    PB = [Buf("bank%d" % i, excl=True) for i in range(8)]
    bank_ctr = [0]

    nrot = [8]

    def bank():
        i = bank_ctr[0] % nrot[0]
        bank_ctr[0] += 1
        return ps[:, i * 512:(i + 1) * 512], PB[i]

    modfm = es.enter_context(nc.sbuf_tensor("modfm", [128, 16], F32))
    aff_all = es.enter_context(nc.sbuf_tensor("aff_all", [128, NT * NE], F32))
    B_modfm, B_aff = Buf("modfm"), Buf("aff")

    def bcast_row(dram_ap_row, n):
        return AP(dram_ap_row.tensor, dram_ap_row.offset, [[0, 128], [1, n]])

    with ExitStack() as s0:
        A = lambda n, sh, dt: s0.enter_context(nc.sbuf_tensor("p0_" + n, sh, dt))
        cfm = A("cfm", [128, 8], F32)
        sig = A("sig", [128, 8], F32)
        cond = A("cond", [128, 8], F32)
        bada = A("bada", [1, 6 * D], F32)
        modrow = A("modrow", [1, 6 * D], F32)
        wa = [A("wa%d" % i, [128, 8, 512], F32) for i in range(3)]
        Bc, Bcond, Bb, Bm = Buf(), Buf(), Buf(), Buf()
        Bwa = [Buf() for _ in range(3)]
        kb.dma("sp", [], [Bc], cfm[:], cfm_d[:, :])
        kb.dma("sp", [], [Bb], bada[:], bada_d[:, :])
        wada_v = wada_d.rearrange("(k p) n -> p k n", p=128)
        for j in range(2):
            kb.dma("sp", [], [Bwa[j]], wa[j][:], wada_v[:, :, j * 512:(j + 1) * 512])
        kb.op("act", [Bc], [Bcond], lambda e: e.activation(out=sig[:], in_=cfm[:], func=AF.Sigmoid))
        kb.op("dve", [Bc, Bcond], [Bcond], lambda e: e.tensor_tensor(out=cond[:], in0=cfm[:], in1=sig[:], op=ALU.mult))
        for j in range(12):
            if j + 2 < 12:
                kb.dma("sp", [], [Bwa[(j + 2) % 3]], wa[(j + 2) % 3][:], wada_v[:, :, (j + 2) * 512:(j + 3) * 512])
            bk, bb = bank()
            w = wa[j % 3]

            def mm(e, w=w, bk=bk):
                for k in range(8):
                    i = e.matmul(bk[0:1, :], cond[:, k:k + 1], w[:, k, :], start=(k == 0), stop=(k == 7))
                return i
            kb.op("pe", [Bcond, Bwa[j % 3]], [bb], mm)
            kb.op("dve", [bb, Bb], [], pwrites=[Bm], fn=lambda e, bk=bk, j=j: e.tensor_tensor(
                out=modrow[0:1, j * 512:(j + 1) * 512], in0=bk[0:1, :], in1=bada[0:1, j * 512:(j + 1) * 512], op=ALU.add))
        kb.dma("sp", [Bm], [B_mod], mod_d[:, :], modrow[:])
        onef = A("onef", [1, 1], F32)
        kb.op("pool", [], [Bc], lambda e: e.memset(onef[:], 1.0))
        bk, bb = bank()

        def mmT(e, bk=bk):
            for j in range(16):
                ins = e.matmul(bk[:, j:j + 1], modrow[0:1, j * 128:(j + 1) * 128], onef[0:1, 0:1], start=True, stop=True)
            return ins
        kb.op("pe", [Bm, Bc], [bb], mmT)
        kb.op("dve", [bb], [B_modfm], lambda e, bk=bk: e.tensor_copy(out=modfm[:], in_=bk[:, 0:16]))
        kb.op("dve", [B_modfm], [B_modfm], lambda e: e.tensor_scalar(
            out=modfm[:, 8:16], in0=modfm[:, 8:16], scalar1=1.0, scalar2=None, op0=ALU.add))
        kb.barrier()
    if stages <= 0:
        _finish(nc, kb, out_d, x_d)
        return

    with ExitStack() as s1:
        A = lambda n, sh, dt: s1.enter_context(nc.sbuf_tensor("p1_" + n, sh, dt))
        win = A("win", [128, 8, 2208], BF16)
        wkr = A("wkr", [128, 8, 192], BF16)
        wuq = A("wuq", [128, 3, 2, 768], BF16)
        wukvk = A("wukvk", [128, 2, 512], BF16)
        wukvv = A("wukvv", [128, 2, 512], BF16)
        ident = A("ident", [128, 128], F32)
        onesq = A("onesq", [128, 128], F32)
        oneskv = A("oneskv", [128, 128], F32)
        gq = A("gq", [128, 3], F32)
        gkv = A("gkv", [128, 2], F32)
        xg = [A("xg%d" % i, [128, 4, 1024], F32) for i in range(2)]
        hT = [A("hT%d" % i, [128, 8, 512], BF16) for i in range(2)]
        cT = A("cT", [128, 5, 512], F32)
        sq = A("sq", [128, 5, 512], F32)
        rstd = A("rstd", [128, 2, 512], F32)
        cn = [A("cn%d" % i, [128, 5, 512], BF16) for i in range(2)]
        cosr = [A("cosr%d" % i, [128, 512], F32) for i in range(2)]
        sinr = [A("sinr%d" % i, [128, 512], F32) for i in range(2)]
        tmpa = [A("tmpa%d" % i, [128, 512], F32) for i in range(2)]
        tmpb = [A("tmpb%d" % i, [128, 512], F32) for i in range(2)]
        NO = 6
        ob = [A("ob%d" % i, [128, 512], BF16) for i in range(NO)]
        vo = [A("vo%d" % i, [128, 768], BF16) for i in range(4)]
        Bw, Bid, Bones, Bg = Buf(), Buf(), Buf(), Buf()
        Bxg = [Buf(), Buf()]
        BhT = [Buf(), Buf()]
        BcT, Bsq, Brstd = [Buf() for _ in range(5)], [Buf() for _ in range(5)], [Buf(), Buf()]
        Bcn = [[Buf() for _ in range(5)] for _ in range(2)]
        Brope = [Buf(), Buf()]
        Bta, Btb = [Buf(), Buf()], [Buf(), Buf()]
        Bob = [Buf() for _ in range(NO)]
        Bvo = [Buf() for _ in range(4)]
        ob_ctr, vo_ctr, t_ctr = [0], [0], [0]

        SKIP = os.environ.get('P1SKIP', '')
        for k in range(0 if 'w' in SKIP else 8):
            kb.dma("pool", [], [], win[:, k, :], win_d[k * 128:(k + 1) * 128, :], pwrites=[Bw])
            kb.dma("pool", [], [], wkr[:, k, :], wkr_d[k * 128:(k + 1) * 128, :], pwrites=[Bw])
        for c in range(0 if 'u' in SKIP else 3):
            kb.dma("pool", [], [], wuq[:, c, :, :], wuq_d[c * 128:(c + 1) * 128, :, :], pwrites=[Bw])
        for c in range(0 if 'v' in SKIP else 2):
            kb.dma("pool", [], [], wukvk[:, c, :], wukvk_d[c * 128:(c + 1) * 128, :], pwrites=[Bw])
            kb.dma("pool", [], [], wukvv[:, c, :], wukvv_d[c * 128:(c + 1) * 128, :], pwrites=[Bw])
        kb.dma("sp", [], [Bid], ident[:], ident_d[:, :])
        kb.dma("sp", [], [], gq[:], gq_d[:, :], pwrites=[Bg])
        kb.dma("sp", [], [], gkv[:], gkv_d[:, :], pwrites=[Bg])
        kb.op("pool", [], [], lambda e: e.memset(onesq[:], 1.0 / 384.0), pwrites=[Bones])
        kb.op("pool", [], [], lambda e: e.memset(oneskv[:], 1.0 / 256.0), pwrites=[Bones])
        for i in range(4):
            kb.op("pool", [], [Bvo[i]], lambda e, i=i: e.memset(vo[i][:], 1.0))

        x_v = x_d.rearrange("(t p) d -> p t d", p=128)

        def load_x(g):
            kb.dma("sp", [], [Bxg[g % 2]], xg[g % 2][:], x_v[:, g * 4:(g + 1) * 4, :])
            kb.dma("sp", [], [Brope[g % 2]], cosr[g % 2][64:96, :], cos_d[:, g * 512:(g + 1) * 512])
            kb.dma("sp", [], [Brope[g % 2]], sinr[g % 2][64:96, :], sin_d[:, g * 512:(g + 1) * 512])

        def next_ob():
            i = ob_ctr[0] % NO
            ob_ctr[0] += 1
            return ob[i], Bob[i]

        def evac_copy(eng, src, bsrc, dst, bdst):
            if eng == "act":
                kb.op("act", [bsrc], [bdst], lambda e: e.copy(out=dst, in_=src))
            else:
                kb.op("dve", [bsrc], [bdst], lambda e: e.tensor_copy(out=dst, in_=src))

        def rope_rows(bm, bbm, br, bbr, g, dst, bdst, full):
            i = t_ctr[0] % 2
            t_ctr[0] += 1
            kb.op("dve", [bbm, Brope[g % 2]], [Bta[i]], lambda e: e.tensor_tensor(
                out=tmpa[i][64:96, :], in0=bm[64:96, :], in1=cosr[g % 2][64:96, :], op=ALU.mult))
            kb.op("dve", [bbr, Brope[g % 2]], [Btb[i]], lambda e: e.tensor_tensor(
                out=tmpb[i][64:96, :], in0=br[64:96, :], in1=sinr[g % 2][64:96, :], op=ALU.mult))
            kb.op("dve", [Bta[i], Btb[i]], [bdst] if full else [], lambda e: e.tensor_tensor(
                out=dst[64:96, :], in0=tmpa[i][64:96, :], in1=tmpb[i][64:96, :], op=ALU.add),
                pwrites=[] if full else [bdst])

        def vo_views(vt, bk):
            o = AP(vt, 0, [pdim(vt[:]), [192, 4], [128, 2], [1, 64]])
            i = AP(bk.tensor, bk.offset, [pdim(bk), [128, 4], [64, 2], [1, 64]])
            return o, i

        load_x(0)
        LIM = float(os.environ.get('P1LIM', '99'))
        SUB = float(os.environ.get('P1SUB', '99'))
        for g in range(NG if LIM >= 99 else 1):
            if g + 1 < NG:
                load_x(g + 1)
            X, BX = xg[g % 2], Bxg[g % 2]
            H, BH = hT[g % 2], BhT[g % 2]
            CN, BCN = cn[g % 2], Bcn[g % 2]
            cols = slice(g * 512, (g + 1) * 512)
            for k in range(8):
                bk, bb = bank()

                def tp(e, bk=bk, k=k):
                    for i in range(4):
                        ins = e.transpose(bk[:, i * 128:(i + 1) * 128], X[:, i, k * 128:(k + 1) * 128], ident[:])
                    return ins
                kb.op("pe", [BX, Bid], [bb], tp)
                kb.op("act", [bb, B_modfm], [BH] if k == 0 else [], lambda e, bk=bk, k=k: e.activation(
                    out=H[:, k, :], in_=bk, func=AF.Identity, scale=modfm[:, 8 + k:9 + k], bias=modfm[:, k:k + 1]),
                    pwrites=[] if k == 0 else [BH])
            if LIM < 1:
                break
            for c in range(5):
                bk, bb = bank()

                def mm(e, bk=bk, c=c):
                    for k in range(8):
                        ins = e.matmul(bk, win[:, k, c * 128:(c + 1) * 128], H[:, k, :], start=(k == 0), stop=(k == 7))
                    return ins
                kb.op("pe", [BH, Bw], [bb], mm)
                if SUB >= 0.1:
                    kb.op("act", [bb], [Bsq[c]], lambda e, bk=bk, c=c: e.activation(out=sq[:, c, :], in_=bk, func=AF.Square))
                if SUB >= 0.15:
                    kb.op("dve", [bb, Bsq[c]], [BcT[c]], lambda e, bk=bk, c=c: e.tensor_copy(out=cT[:, c, :], in_=bk))
            if SUB < 0.3:
                break
            for which, (cs, ones_t) in enumerate((((0, 1, 2), onesq), ((3, 4), oneskv))):
                bk, bb = bank()

                def mm(e, bk=bk, cs=cs, ones_t=ones_t):
                    for n, c in enumerate(cs):
                        ins = e.matmul(bk, ones_t[:], sq[:, c, :], start=(n == 0), stop=(n == len(cs) - 1))
                    return ins
                kb.op("pe", [Bsq[c] for c in cs] + [Bones], [bb], mm)
                kb.op("dve", [bb], [Brstd[which]], lambda e, bk=bk, which=which: e.tensor_scalar(
                    out=rstd[:, which, :], in0=bk, scalar1=EPS, scalar2=None, op0=ALU.add))
                if SUB < 0.5:
                    continue
                kb.op("act", [Brstd[which]], [Brstd[which]], lambda e, which=which: e.activation(
                    out=rstd[:, which, :], in_=rstd[:, which, :], func=AF.Sqrt))
                kb.op("dve", [Brstd[which]], [Brstd[which]], lambda e, which=which: e.reciprocal(
                    out=rstd[:, which, :], in_=rstd[:, which, :]))
                if SUB < 0.7:
                    continue
                for c in cs:
                    gsc = gq[:, c:c + 1] if which == 0 else gkv[:, c - 3:c - 2]
                    kb.op("dve", [BcT[c], Brstd[which], Bg], [BCN[c]], lambda e, c=c, gsc=gsc, which=which: e.scalar_tensor_tensor(
                        out=CN[:, c, :], in0=cT[:, c, :], scalar=gsc, in1=rstd[:, which, :], op0=ALU.mult, op1=ALU.mult))
            if LIM < 2:
                break
            for h in range(8):
                bm, bbm = bank()
                br, bbr = bank()

                def mm(e, h=h, bm=bm, br=br):
                    for which, bk in ((0, bm), (1, br)):
                        for c in range(3):
                            ins = e.matmul(bk[0:96, :], wuq[:, c, which, h * 96:(h + 1) * 96], CN[:, c, :],
                                           start=(c == 0), stop=(c == 2))
                    return ins
                kb.op("pe", [BCN[0], BCN[1], BCN[2], Bw], [bbm, bbr], mm)
                o, bo = next_ob()
                kb.op("act", [bbm], [bo], lambda e, o=o, bm=bm: e.copy(out=o[0:64, :], in_=bm[0:64, :]))
                rope_rows(bm, bbm, br, bbr, g, o, bo, False)
                kb.dma("sp", [bo], [], qT_d[h, :, cols], o[0:96, :], pwrites=[B_q])
            if LIM < 3:
                break
            for hp in range(4):
                bk, bb = bank()

                def mm(e, hp=hp, bk=bk):
                    for c in range(2):
                        ins = e.matmul(bk, wukvk[:, c, hp * 128:(hp + 1) * 128], CN[:, 3 + c, :], start=(c == 0), stop=(c == 1))
                    return ins
                kb.op("pe", [BCN[3], BCN[4], Bw], [bb], mm)
                o, bo = next_ob()
                evac_copy("act", bk, bb, o[:, :], bo)
                kb.dma("sp", [bo], [], kT_d[2 * hp, 0:64, cols], o[0:64, :], pwrites=[B_k])
                kb.dma("sp", [bo], [], kT_d[2 * hp + 1, 0:64, cols], o[64:128, :], pwrites=[B_k])
            if LIM < 4:
                break
            bm, bbm = bank()
            br, bbr = bank()

            def mm(e, bm=bm, br=br):
                for which, bk in ((0, bm), (1, br)):
                    for k in range(8):
                        ins = e.matmul(bk[0:96, :], wkr[:, k, which * 96:(which + 1) * 96], H[:, k, :],
                                       start=(k == 0), stop=(k == 7))
                return ins
            kb.op("pe", [BH, Bw], [bbm, bbr], mm)
            o, bo = next_ob()
            rope_rows(bm, bbm, br, bbr, g, o, bo, True)
            for h in range(8):
                kb.dma("sp", [bo], [], kT_d[h, 64:96, cols], o[64:96, :], pwrites=[B_k])
            if LIM < 5:
                break
            for i in range(4):
                bk, bb = bank()

                def mm(e, i=i, bk=bk):
                    for c in range(2):
                        ins = e.matmul(bk, CN[:, 3 + c, i * 128:(i + 1) * 128], wukvv[:, c, :], start=(c == 0), stop=(c == 1))
                    return ins
                kb.op("pe", [BCN[3], BCN[4], Bw], [bb], mm)
                vi = vo_ctr[0] % 4
                vo_ctr[0] += 1
                ov, iv = vo_views(vo[vi], bk)
                kb.op("act", [bb], [Bvo[vi]], lambda e, ov=ov, iv=iv: e.copy(out=ov, in_=iv))
                kb.dma("sp", [Bvo[vi]], [], Vm_d[g * 4 + i, :, :], vo[vi][:], pwrites=[B_vm])
            if LIM < 6:
                break
            for base, dst, bd in ((672, dqT_d, B_dq), (1184, dkT_d, B_dk)):
                for hp in range(4):
                    bk, bb = bank()

                    def mm(e, hp=hp, bk=bk, base=base):
                        for k in range(8):
                            ins = e.matmul(bk, win[:, k, base + hp * 128: base + (hp + 1) * 128], H[:, k, :],
                                           start=(k == 0), stop=(k == 7))
                        return ins
                    kb.op("pe", [BH, Bw], [bb], mm)
                    o, bo = next_ob()
                    evac_copy("act" if hp % 2 == 0 else "dve", bk, bb, o[:, :], bo)
                    kb.dma("sp", [bo], [], dst[hp, :, cols], o[:, :], pwrites=[bd])
            if LIM < 7:
                break
            for i in range(4):
                bk, bb = bank()

                def mm(e, i=i, bk=bk):
                    for k in range(8):
                        ins = e.matmul(bk, H[:, k, i * 128:(i + 1) * 128], win[:, k, 1696:2208], start=(k == 0), stop=(k == 7))
                    return ins
                kb.op("pe", [BH, Bw], [bb], mm)
                vi = vo_ctr[0] % 4
                vo_ctr[0] += 1
                ov, iv = vo_views(vo[vi], bk)
                kb.op("act", [bb], [Bvo[vi]], lambda e, ov=ov, iv=iv: e.copy(out=ov, in_=iv))
                kb.dma("sp", [Bvo[vi]], [], Vd_d[g * 4 + i, :, :], vo[vi][:], pwrites=[B_vd])
        kb.barrier()
    if stages <= 1:
        _finish(nc, kb, out_d, x_d)
        return

    def attention(tag, kT_src, qT_src, V_src, per_pair_kq, krows, scale, chunk0, dilated, Bk_src, Bq_src, Bv_src):
        with ExitStack() as sa:
            A = lambda n, sh, dt: sa.enter_context(nc.sbuf_tensor(tag + "_" + n, sh, dt))
            Vp = [A("Vp%d" % i, [128, 32, 192], BF16) for i in range(2)]
            Kt = [A("Kt%d" % i, [128, S], BF16) for i in range(2)]
            Qt = [A("Qt%d" % i, [128, S], BF16) for i in range(2)]
            pte = [A("pte%d" % i, [128, 1024], BF16) for i in range(3)]
            ptm = [A("ptm%d" % i, [128, 1024], BF16) for i in range(3)] if dilated else None
            ao = [A("ao%d" % i, [128, S], BF16) for i in range(2)]
            rec = [A("rec%d" % i, [128, 512], F32) for i in range(2)]
            BVp, BKt, BQt = [Buf(), Buf()], [Buf(), Buf()], [Buf(), Buf()]
            Bpte, Bptm = [Buf() for _ in range(3)], [Buf() for _ in range(3)]
            Bao, Brec = [Buf(), Buf()], [Buf(), Buf()]
            SB = [Buf("S%d" % i, excl=True) for i in range(3)]
            AB = [Buf("acc%d" % i, excl=True) for i in range(2)]
            Sap = [ps[:, i * 1024:(i + 1) * 1024] for i in range(3)]
            Aap = [ps[:, 3072 + i * 512: 3072 + (i + 1) * 512] for i in range(2)]
            Bzk = Buf()
            if per_pair_kq:
                KtB = [A("KtB%d" % i, [128, S], BF16) for i in range(2)]
                for i in range(2):
                    kb.op("dve", [], [], lambda e, i=i: e.memset(Kt[i][64:128, :], 0.0), pwrites=[Bzk])
                    kb.op("dve", [], [], lambda e, i=i: e.memset(KtB[i][0:64, :], 0.0), pwrites=[Bzk])
            if dilated:
                toep_st = A("toep", [128, MW], F32)
                multb = A("multb", [128, MW], BF16)
                master = [A("master%d" % i, [128, MW], BF16) for i in range(2)]
                Btoep, Bmult, Bmaster = Buf(), Buf(), [Buf(), Buf()]
                kb.dma("pool", [], [Bmult], multb[:], mult_d[:, :])

            def load_pair(hp):
                for j in range(4):
                    kb.dma("sp", [Bv_src], [], Vp[hp % 2][:, j * 8:(j + 1) * 8, :],
                           V_src[j * 8:(j + 1) * 8, :, hp * 192:(hp + 1) * 192].rearrange("t p c -> p t c"),
                           pwrites=[BVp[hp % 2]])

            def load_kq(u):
                if per_pair_kq:
                    kb.dma("sp", [Bk_src, Bzk], [BKt[u % 2]], Kt[u % 2][0:64, :], kT_src[u, 0:64, :])
                    kb.dma("sp", [Bk_src, Bzk], [], KtB[u % 2][64:128, :], kT_src[u, 64:128, :], pwrites=[BKt[u % 2]])
                    kb.dma("sp", [Bq_src], [BQt[u % 2]], Qt[u % 2][:, :], qT_src[u, :, :])
                else:
                    kb.dma("sp", [Bk_src], [BKt[u % 2]], Kt[u % 2][0:krows, :], kT_src[u, :, :])
                    kb.dma("sp", [Bq_src], [BQt[u % 2]], Qt[u % 2][0:krows, :], qT_src[u, :, :])

            def prep_mask(h):
                kb.dma("sp", [], [Btoep], toep_st[:], toep_d[h, :, :])
                kb.op("act", [Btoep], [Btoep], lambda e: e.activation(out=toep_st[:], in_=toep_st[:], func=AF.Exp))
                kb.op("dve", [Btoep, Bmult], [Bmaster[h % 2]], lambda e: e.tensor_tensor(
                    out=master[h % 2][:], in0=toep_st[:], in1=multb[:], op=ALU.mult))

            items = []
            for h in range(8):
                hp, hh = h // 2, h % 2
                for Q in range(8):
                    if dilated:
                        Ts = list(range(max(0, 4 * Q - 8), min(31, 4 * Q + 11) + 1))
                    else:
                        Ts = list(range(32))
                    groups = []
                    n = 0
                    while n < len(Ts):
                        if n + 1 < len(Ts):
                            groups.append([Ts[n + 1], Ts[n]])
                            n += 2
                        else:
                            groups.append([Ts[n]])
                            n += 1
                    for gi, gT in enumerate(groups):
                        items.append(dict(h=h, hp=hp, hh=hh, Q=Q, Ts=gT, first=(gi == 0), last=(gi == len(groups) - 1),
                                          hq=h * 8 + Q))
            N = len(items)
            for i, it in enumerate(items):
                it["i"] = i

            def kq_unit(it):
                return it["hp"] if per_pair_kq else it["h"]

            def rows_of(it):
                if per_pair_kq:
                    return it["hh"] * 64, it["hh"] * 64 + 64
                return 0, krows

            def emit_qk(it):
                i = it["i"]
                u = kq_unit(it)
                r0, r1 = rows_of(it)
                K_, Q_ = Kt[u % 2], Qt[u % 2]
                if per_pair_kq:
                    r0, r1 = 0, 128
                    if it["hh"] == 1:
                        K_ = KtB[u % 2]

                def f(e):
                    for j, T in enumerate(it["Ts"]):
                        ins = e.matmul(Sap[i % 3][:, j * 512:(j + 1) * 512], K_[r0:r1, T * 128:(T + 1) * 128],
                                       Q_[r0:r1, it["Q"] * 512:(it["Q"] + 1) * 512], start=True, stop=True)
                    return ins
                kb.op("pe", [BKt[u % 2], BQt[u % 2]], [SB[i % 3]], f)

            def emit_exp(it):
                i = it["i"]
                w = 512 * len(it["Ts"])
                kb.op("act", [SB[i % 3]], [Bpte[i % 3]], lambda e: e.activation(
                    out=pte[i % 3][:, 0:w], in_=Sap[i % 3][:, 0:w], func=AF.Exp, scale=scale))

            def emit_mask(it):
                i = it["i"]
                n = len(it["Ts"])
                h = it["h"]
                c0 = C0 - 128 * it["Ts"][0] + 512 * it["Q"]
                m = master[h % 2]
                in1 = AP(m, c0, [pdim(m[:]), [128, n], [1, 512]])
                o = AP(ptm[i % 3], 0, [pdim(ptm[i % 3][:]), [512, n], [1, 512]])
                i0 = AP(pte[i % 3], 0, [pdim(pte[i % 3][:]), [512, n], [1, 512]])
                kb.op("dve", [Bpte[i % 3], Bmaster[h % 2]], [Bptm[i % 3]], lambda e: e.tensor_tensor(
                    out=o, in0=i0, in1=in1, op=ALU.mult))

            def emit_pv(it):
                i = it["i"]
                P_, BP_ = (ptm[i % 3], Bptm[i % 3]) if dilated else (pte[i % 3], Bpte[i % 3])
                acc = Aap[it["hq"] % 2]
                V_ = Vp[it["hp"] % 2]
                vc = 64 * it["hh"]
                nT = len(it["Ts"])

                def f(e):
                    for j, T in enumerate(it["Ts"]):
                        ins = e.matmul(acc, V_[:, T, vc:vc + 128], P_[:, j * 512:(j + 1) * 512],
                                       start=(it["first"] and j == 0), stop=(it["last"] and j == nT - 1))
                    return ins
                if it["first"]:
                    kb.op("pe", [BP_, BVp[it["hp"] % 2]], [AB[it["hq"] % 2]], f)
                else:
                    kb.op("pe", [BP_, BVp[it["hp"] % 2]], [], f, pwrites=[AB[it["hq"] % 2]])

            def emit_fin(it):
                a = it["hq"] % 2
                acc = Aap[a]
                hh, hp, Q = it["hh"], it["hp"], it["Q"]
                nr = slice(64 * hh, 64 * hh + 64)
                dr = slice(64 * (1 - hh), 64 * (1 - hh) + 64)
                if dilated:
                    kb.op("act", [AB[a]], [Brec[a]], lambda e: e.activation(out=rec[a][nr, :], in_=acc[dr, :], func=AF.Ln))
                    kb.op("act", [Brec[a]], [Brec[a]], lambda e: e.activation(out=rec[a][nr, :], in_=rec[a][nr, :], func=AF.Exp, scale=-1.0))
                else:
                    kb.op("dve", [AB[a]], [Brec[a]], lambda e: e.reciprocal(out=rec[a][nr, :], in_=acc[dr, :]))
                first_of_pair = (hh == 0 and Q == 0)
                kb.op("dve", [AB[a], Brec[a]], [Bao[hp % 2]] if first_of_pair else [], lambda e: e.tensor_tensor(
                    out=ao[hp % 2][nr, Q * 512:(Q + 1) * 512], in0=acc[nr, :], in1=rec[a][nr, :], op=ALU.mult),
                    pwrites=[] if first_of_pair else [Bao[hp % 2]])
                if hh == 1 and Q == 7:
                    kb.dma("sp", [Bao[hp % 2]], [], attnT_d[chunk0 + hp, :, :], ao[hp % 2][:, :], pwrites=[B_attn])

            def unit_start(it):
                h = it["h"]
                if it["Q"] == 0 and it["first"]:
                    if h + 1 < 8:
                        if per_pair_kq:
                            if (h + 1) % 2 == 0:
                                load_kq((h + 1) // 2)
                        else:
                            load_kq(h + 1)
                        if (h + 1) % 2 == 0:
                            load_pair((h + 1) // 2)
                        if dilated:
                            prep_mask(h + 1)

            load_pair(0)
            load_kq(0)
            if dilated:
                prep_mask(0)
            emit_qk(items[0])
            emit_exp(items[0])
            if dilated:
                emit_mask(items[0])
            if N > 1:
                emit_qk(items[1])
            for i in range(N):
                it = items[i]
                unit_start(it)
                if i + 1 < N:
                    emit_exp(items[i + 1])
                    if dilated:
                        emit_mask(items[i + 1])
                if i + 2 < N:
                    emit_qk(items[i + 2])
                emit_pv(it)
                if it["last"]:
                    emit_fin(it)
            kb.barrier()

    run_dil = os.environ.get("SKIPDIL", "") == ""
    run_mla = os.environ.get("SKIPMLA", "") == ""
    if run_dil:
        attention("dil", dkT_d, dqT_d, Vd_d, True, 64, 64.0 ** -0.5, 4, True, B_dk, B_dq, B_vd)
    if run_mla:
        attention("mla", kT_d, qT_d, Vm_d, False, 96, 96.0 ** -0.5, 0, False, B_k, B_q, B_vm)
    if stages <= 3:
        _finish(nc, kb, out_d, x_d)
        return

    def bank2():
        if bank_ctr[0] % 2:
            bank_ctr[0] += 1
        i = bank_ctr[0] % 8
        bank_ctr[0] += 2
        return ps[:, i * 512:(i + 2) * 512], [PB[i], PB[i + 1]]

    class LNState:
        def __init__(self, A, tag, nb=2):
            self.junk = A(tag + "junk", [128, D], F32)
            self.st = [A(tag + "st%d" % i, [128, 8], F32) for i in range(nb)]
            self.Bjunk = Buf()
            self.Bst = [Buf() for _ in range(nb)]
            self.nb = nb
            self.n = 0

    def ln_stats(L, y, By):
        i = L.n % L.nb
        L.n += 1
        st, Bst = L.st[i], L.Bst[i]
        kb.op("dve", [], [Bst], lambda e: e.memset(st[:], 0.0))
        kb.op("act", [By, Bst], [L.Bjunk], lambda e: e.activation(out=L.junk[:], in_=y[:], func=AF.Identity, accum_out=st[:, 0:1]),
              pwrites=[Bst])
        kb.op("act", [By, Bst], [L.Bjunk], lambda e: e.activation(out=L.junk[:], in_=y[:], func=AF.Square, accum_out=st[:, 1:2]),
              pwrites=[Bst])
        return st, Bst

    def ln_apply(st, Bst, y, By, xh, Bxh):
        kb.op("dve", [Bst], [], lambda e: e.tensor_scalar(out=st[:, 2:3], in0=st[:, 0:1], scalar1=1.0 / D, scalar2=None, op0=ALU.mult), pwrites=[Bst])
        kb.op("dve", [Bst], [], lambda e: e.tensor_tensor(out=st[:, 3:4], in0=st[:, 2:3], in1=st[:, 2:3], op=ALU.mult), pwrites=[Bst])
        kb.op("dve", [Bst], [], lambda e: e.scalar_tensor_tensor(out=st[:, 4:5], in0=st[:, 1:2], scalar=1.0 / D, in1=st[:, 3:4],
                                                                   op0=ALU.mult, op1=ALU.subtract), pwrites=[Bst])
        kb.op("dve", [Bst], [], lambda e: e.tensor_scalar(out=st[:, 4:5], in0=st[:, 4:5], scalar1=EPS, scalar2=None, op0=ALU.add), pwrites=[Bst])
        kb.op("act", [Bst], [], lambda e: e.activation(out=st[:, 5:6], in_=st[:, 4:5], func=AF.Ln), pwrites=[Bst])
        kb.op("act", [Bst], [], lambda e: e.activation(out=st[:, 5:6], in_=st[:, 5:6], func=AF.Exp, scale=-0.5), pwrites=[Bst])
        kb.op("dve", [Bst], [], lambda e: e.scalar_tensor_tensor(out=st[:, 6:7], in0=st[:, 2:3], scalar=-1.0, in1=st[:, 5:6],
                                                                   op0=ALU.mult, op1=ALU.mult), pwrites=[Bst])
        kb.op("act", [By, Bst], [Bxh], lambda e: e.activation(out=xh[:], in_=y[:], func=AF.Identity, scale=st[:, 5:6], bias=st[:, 6:7]))

    with ExitStack() as s4:
        A = lambda n, sh, dt: s4.enter_context(nc.sbuf_tensor("p4_" + n, sh, dt))
        wout = A("wout", [128, 8, D], BF16)
        g1bc, lngbc, lnbbc, sc2bc, sh2bc = (A(n, [128, D], F32) for n in ("g1bc", "lngbc", "lnbbc", "sc2bc", "sh2bc"))
        wr = A("wr", [128, 8, NE], F32)
        ident = A("ident", [128, 128], F32)
        at = [A("at%d" % i, [128, 8, 512], BF16) for i in range(2)]
        xt = [A("xt%d" % i, [128, D], F32) for i in range(2)]
        t1 = [A("t1%d" % i, [128, D], F32) for i in range(2)]
        yy = [A("y%d" % i, [128, D], F32) for i in range(2)]
        xh = [A("xh%d" % i, [128, D], F32) for i in range(2)]
        x1 = [A("x1%d" % i, [128, D], F32) for i in range(2)]
        h2f = [A("h2f%d" % i, [128, D], F32) for i in range(2)]
        h2b = [A("h2b%d" % i, [128, D], BF16) for i in range(2)]
        h2T = [A("h2T%d" % i, [128, 8, 128], F32) for i in range(2)]
        sm = [A("sm%d" % i, [128, 8], F32) for i in range(2)]
        ee = [A("ee%d" % i, [128, NE], F32) for i in range(2)]
        L = LNState(A, "ln", 3)
        Bc4 = Buf()
        Bat, Bxt, Bt1, By, Bxh, Bx1, Bh2f, Bh2b, Bh2T, Bsm, Bee = ([Buf(), Buf()] for _ in range(11))
        kb.dma("sp", [B_mod], [], g1bc[:], bcast_row(mod_d[0:1, 2048:3072], D), pwrites=[Bc4])
        Bws = [Buf(), Buf()]
        for c in range(8):
            kb.dma("sp", [], [Bws[c % 2]], t1[c % 2][:], wout_d[c * 128:(c + 1) * 128, :])
            kb.op("dve", [Bws[c % 2], Bc4], [], lambda e, c=c: e.tensor_tensor(out=wout[:, c, :], in0=t1[c % 2][:], in1=g1bc[:], op=ALU.mult),
                  pwrites=[Bc4])
        kb.dma("sp", [B_mod], [], sh2bc[:], bcast_row(mod_d[0:1, 3072:4096], D), pwrites=[Bc4])
        kb.dma("sp", [B_mod], [], sc2bc[:], bcast_row(mod_d[0:1, 4096:5120], D), pwrites=[Bc4])
        kb.dma("sp", [], [], lngbc[:], bcast_row(ln1g_d[0:1, :], D), pwrites=[Bc4])
        kb.dma("sp", [], [], lnbbc[:], bcast_row(ln1b_d[0:1, :], D), pwrites=[Bc4])
        kb.dma("sp", [], [], wr[:], wr_d.rearrange("(k p) e -> p k e", p=128), pwrites=[Bc4])
        kb.dma("sp", [], [], ident[:], ident_d[:, :], pwrites=[Bc4])
        kb.op("dve", [Bc4], [], lambda e: e.tensor_scalar(out=sc2bc[:], in0=sc2bc[:], scalar1=1.0, scalar2=None, op0=ALU.add), pwrites=[Bc4])
        attn_v = attnT_d.rearrange("c p n -> p c n")
        x_t = x_d.rearrange("(t p) d -> t p d", p=128)
        x1_t = x1_d.rearrange("(t p) d -> t p d", p=128)
        h2_t = h2_d.rearrange("(t p) d -> t p d", p=128)
        stA = {}
        stC = {}

        mixb = {}

        def A_mix(t):
            g, i = t // 4, t % 4
            if i == 0:
                kb.dma("sp", [B_attn], [Bat[g % 2]], at[g % 2][:], attn_v[:, :, g * 512:(g + 1) * 512])
            b = t % 2
            kb.dma("sp", [], [Bxt[b]], xt[b][:], x_t[t])
            bk2, bb2 = bank2()

            def mm(e):
                for half in range(2):
                    for c in range(8):
                        ins = e.matmul(bk2[:, half * 512:(half + 1) * 512], at[g % 2][:, c, i * 128:(i + 1) * 128],
                                       wout[:, c, half * 512:(half + 1) * 512], start=(c == 0), stop=(c == 7))
                return ins
            kb.op("pe", [Bat[g % 2], Bc4], bb2, mm)
            mixb[t] = (bk2, bb2)

        def A_y(t):
            b = t % 2
            bk2, bb2 = mixb.pop(t)
            kb.op("dve", bb2 + [Bxt[b]] + Bws, [By[b]], lambda e: e.scalar_tensor_tensor(
                out=yy[b][:], in0=xt[b][:], scalar=ALPHA, in1=bk2, op0=ALU.mult, op1=ALU.add))

        def A_stats(t):
            b = t % 2
            stA[t] = ln_stats(L, yy[b], By[b])

        def B1_smalls(t):
            st, Bst = stA[t]
            kb.op("dve", [Bst], [], lambda e: e.tensor_scalar(out=st[:, 2:3], in0=st[:, 0:1], scalar1=1.0 / D, scalar2=None, op0=ALU.mult), pwrites=[Bst])
            kb.op("dve", [Bst], [], lambda e: e.tensor_tensor(out=st[:, 3:4], in0=st[:, 2:3], in1=st[:, 2:3], op=ALU.mult), pwrites=[Bst])
            kb.op("dve", [Bst], [], lambda e: e.scalar_tensor_tensor(out=st[:, 4:5], in0=st[:, 1:2], scalar=1.0 / D, in1=st[:, 3:4],
                                                                       op0=ALU.mult, op1=ALU.subtract), pwrites=[Bst])
            kb.op("dve", [Bst], [], lambda e: e.tensor_scalar(out=st[:, 4:5], in0=st[:, 4:5], scalar1=EPS, scalar2=None, op0=ALU.add), pwrites=[Bst])

        def B1_lnexp(t):
            st, Bst = stA[t]
            kb.op("act", [Bst], [], lambda e: e.activation(out=st[:, 5:6], in_=st[:, 4:5], func=AF.Ln), pwrites=[Bst])
            kb.op("act", [Bst], [], lambda e: e.activation(out=st[:, 5:6], in_=st[:, 5:6], func=AF.Exp, scale=-0.5), pwrites=[Bst])

        def B1_nmr(t):
            st, Bst = stA[t]
            kb.op("dve", [Bst], [], lambda e: e.scalar_tensor_tensor(out=st[:, 6:7], in0=st[:, 2:3], scalar=-1.0, in1=st[:, 5:6],
                                                                       op0=ALU.mult, op1=ALU.mult), pwrites=[Bst])

        def B1_xh(t):
            b = t % 2
            st, Bst = stA.pop(t)
            kb.op("act", [By[b], Bst], [Bxh[b]], lambda e: e.activation(out=xh[b][:], in_=yy[b][:], func=AF.Identity,
                                                                        scale=st[:, 5:6], bias=st[:, 6:7]))

        def B2_dve(t):
            b = t % 2
            kb.op("dve", [Bxh[b], Bc4], [Bx1[b]], lambda e: e.tensor_tensor(out=x1[b][:], in0=xh[b][:], in1=lngbc[:], op=ALU.mult))
            kb.op("dve", [Bx1[b], Bc4], [Bx1[b]], lambda e: e.tensor_tensor(out=x1[b][:], in0=x1[b][:], in1=lnbbc[:], op=ALU.add))
            kb.dma("sp", [Bx1[b]], [], x1_t[t], x1[b][:], pwrites=[B_x1])
            kb.op("dve", [Bx1[b], Bc4], [Bh2f[b]], lambda e: e.tensor_tensor(out=h2f[b][:], in0=x1[b][:], in1=sc2bc[:], op=ALU.mult))
            kb.op("dve", [Bh2f[b], Bc4], [Bh2f[b]], lambda e: e.tensor_tensor(out=h2f[b][:], in0=h2f[b][:], in1=sh2bc[:], op=ALU.add))

        def B2_cast(t):
            b = t % 2
            kb.op("act", [Bh2f[b]], [Bh2b[b]], lambda e: e.copy(out=h2b[b][:], in_=h2f[b][:]))
            kb.dma("sp", [Bh2b[b]], [], h2_t[t], h2b[b][:], pwrites=[B_h2])

        def B2_tp(t):
            b = t % 2
            bk2, bb2 = bank2()

            def tp(e):
                for k in range(8):
                    ins = e.transpose(bk2[:, k * 128:(k + 1) * 128], h2f[b][:, k * 128:(k + 1) * 128], ident[:])
                return ins
            kb.op("pe", [Bh2f[b], Bc4], bb2, tp)
            stC[t] = (bk2, bb2)

        lgb = {}

        def C_copy(t):
            b = t % 2
            bk2, bb2 = stC.pop(t)
            kb.op("act", bb2, [Bh2T[b]], lambda e: e.copy(out=h2T[b][:].rearrange("p k n -> p (k n)"), in_=bk2))

        def C_logits(t):
            b = t % 2
            bk, bb = bank()

            def lg(e):
                for k in range(8):
                    ins = e.matmul(bk[:, 0:NE], h2T[b][:, k, :], wr[:, k, :], start=(k == 0), stop=(k == 7))
                return ins
            kb.op("pe", [Bh2T[b], Bc4], [bb], lg)
            lgb[t] = (bk, bb)

        def C_max(t):
            b = t % 2
            bk, bb = lgb[t]
            kb.op("dve", [], [Bsm[b]], lambda e: e.memset(sm[b][:], 0.0))
            kb.op("dve", [bb], [], lambda e: e.reduce_max(out=sm[b][:, 0:1], in_=bk[:, 0:NE], axis=AX.X), pwrites=[Bsm[b]])
            kb.op("dve", [Bsm[b]], [], lambda e: e.tensor_scalar(out=sm[b][:, 1:2], in0=sm[b][:, 0:1], scalar1=-1.0, scalar2=None, op0=ALU.mult),
                  pwrites=[Bsm[b]])

        def C_exp(t):
            b = t % 2
            bk, bb = lgb.pop(t)
            kb.op("act", [bb, Bsm[b]], [Bee[b]], lambda e: e.activation(out=ee[b][:], in_=bk[:, 0:NE], func=AF.Exp, bias=sm[b][:, 1:2],
                                                                       accum_out=sm[b][:, 2:3]), pwrites=[Bsm[b]])

        def C_aff(t):
            b = t % 2
            kb.op("dve", [Bsm[b]], [], lambda e: e.reciprocal(out=sm[b][:, 3:4], in_=sm[b][:, 2:3]), pwrites=[Bsm[b]])
            kb.op("dve", [Bee[b], Bsm[b]], [], lambda e: e.tensor_scalar(out=aff_all[:, t * NE:(t + 1) * NE], in0=ee[b][:], scalar1=sm[b][:, 3:4],
                                                                        scalar2=None, op0=ALU.mult), pwrites=[B_aff])

        for step in range(NT + 3):
            a, b1, b2, c = step, step - 1, step - 2, step - 3
            ok = lambda t: 0 <= t < NT
            if ok(a):
                A_mix(a)
            if ok(c):
                C_copy(c)
            if ok(b2):
                B2_dve(b2)
            if ok(c):
                C_logits(c)
            if ok(a):
                A_y(a)
            if ok(b2):
                B2_cast(b2)
                B2_tp(b2)
            if ok(b1):
                B1_smalls(b1)
            if ok(a):
                A_stats(a)
            if ok(c):
                C_max(c)
            if ok(b1):
                B1_lnexp(b1)
            if ok(c):
                C_exp(c)
            if ok(b1):
                B1_nmr(b1)
            if ok(c):
                C_aff(c)
            if ok(b1):
                B1_xh(b1)
        if "aff_s" in dbg:
            kb.dma("sp", [B_aff], [Buf()], aff_d[:, :], aff_all[:])
        kb.barrier()
    if stages <= 4:
        _finish(nc, kb, out_d, x_d)
        return

    def moe_phase():
        with ExitStack() as s5:
            A = lambda n, sh, dt: s5.enter_context(nc.sbuf_tensor("p5_" + n, sh, dt))
            onesf = A("onesf", [128, 128], F32)
            onesb = A("onesb", [128, 128], BF16)
            triub = A("triub", [128, 128], BF16)
            identb = A("identb", [128, 128], BF16)
            iota = A("iota", [128, 512], F32)
            tokhl = A("tokhl", [128, NT, 2], F32)
            zero = A("zero", [128, D], F32)
            lo = A("lo", [128, NE], F32)
            tau = A("tau", [128, NE], F32)
            part = A("part", [128, NE], F32)
            gw = A("gw", [128, NE], F32)
            cmp = A("cmp", [128, NT * NE], F32)
            self_ = A("self", [128, NT * NE], F32)
            selb = A("selb", [128, NT * NE], BF16)
            posm = A("posm", [128, NT * NE], F32)
            r1 = A("r1", [128, NT * NE], F32)
            R = A("R", [128, NT * NE * 5], BF16)
            OH = [A("OH%d" % i, [128, 512], BF16) for i in range(4)]
            idxv = [A("idxv%d" % i, [128, 32], F32) for i in range(2)]
            idxf = [A("idxf%d" % i, [128, 4], F32) for i in range(2)]
            idxi = [A("idxi%d" % i, [128, 4], I32) for i in range(2)]
            val = [A("val%d" % i, [128, 4], F32) for i in range(2)]
            xin = [A("xin%d" % i, [128, 4, D], BF16) for i in range(2)]
            xinT = [A("xinT%d" % i, [128, 8, 512], BF16) for i in range(2)]
            wg = [A("wg%d" % i, [128, 8, 512], BF16) for i in range(3)]
            wu = [A("wu%d" % i, [128, 8, 512], BF16) for i in range(3)]
            wd = [A("wd%d" % i, [128, 6, D], BF16) for i in range(3)]
            sg = [A("sg%d" % i, [128, 512], F32) for i in range(2)]
            act = [A("act%d" % i, [128, 12, 512], BF16) for i in range(2)]
            ys = [A("ys%d" % i, [128, D], F32) for i in range(4)]
            mo = [A("mo%d" % i, [128, D], F32) for i in range(4)]
            Bc = Buf()
            Blo, Btau, Bpart, Bgw, Bcmp, Bsel, Bposm, BR = (Buf() for _ in range(8))
            BOH = [Buf() for _ in range(4)]
            Bidxv, Bidx, Bval = [Buf(), Buf()], [Buf(), Buf()], [Buf(), Buf()]
            Bxin, BxinT = [Buf(), Buf()], [Buf(), Buf()]
            Bwg, Bwu, Bwd = [Buf() for _ in range(3)], [Buf() for _ in range(3)], [Buf() for _ in range(3)]
            Bsg, Bact = [Buf(), Buf()], [Buf(), Buf()]
            Bys, Bmo = [Buf() for _ in range(4)], [Buf() for _ in range(4)]

            kb.op("pool", [], [], lambda e: e.memset(onesf[:], 1.0), pwrites=[Bc])
            kb.op("pool", [], [], lambda e: e.memset(onesb[:], 1.0), pwrites=[Bc])
            kb.op("pool", [], [], lambda e: e.memset(zero[:], 0.0), pwrites=[Bc])
            kb.op("pool", [], [Blo], lambda e: e.memset(lo[:], 0.0))
            kb.dma("pool", [], [], triub[:], triu_d[:, :], pwrites=[Bc])
            kb.dma("pool", [], [], identb[:], ident_d[:, :], pwrites=[Bc])
            kb.dma("sp", [], [], iota[:], iota_d[:, :], pwrites=[Bc])
            kb.dma("sp", [], [], tokhl[:], tokhl_d[:, :, :], pwrites=[Bc])
            moe_t = moe_d.rearrange("(t p) d -> t p d", p=128)
            for t in range(NT):
                kb.dma("sp", [Bc], [], moe_t[t], zero[:], pwrites=[B_moe])

            def issue_gu(n):
                if n >= NE * 3:
                    return
                e, fb = n // 3, n % 3
                b = n % 3
                kb.dma("pool", [], [Bwg[b]], wg[b][:], wg_d[e, :, fb * 512:(fb + 1) * 512].rearrange("(k p) f -> p k f", p=128))
                kb.dma("pool", [], [Bwu[b]], wu[b][:], wu_d[e, :, fb * 512:(fb + 1) * 512].rearrange("(k p) f -> p k f", p=128))

            def issue_d(m):
                if m >= NE * 2:
                    return
                e, half = m // 2, m % 2
                b = m % 3
                kb.dma("pool", [], [Bwd[b]], wd[b][:], wd_d[e, half * 768:(half + 1) * 768, :].rearrange("(k p) n -> p k n", p=128))

            issue_gu(0)
            issue_gu(1)
            issue_d(0)
            issue_d(1)
            issue_d(2)

            aff3 = AP(aff_all, 0, [pdim(aff_all[:]), [NE, NT], [1, NE]])
            cmp3 = AP(cmp, 0, [pdim(cmp[:]), [NE, NT], [1, NE]])
            cmpT = AP(cmp, 0, [pdim(cmp[:]), [1, NE], [NE, NT]])

            def bc16(tile_):
                return AP(tile_, 0, [pdim(tile_[:]), [0, NT], [1, NE]])

            for it in range(1, NBIS + 1):
                w = 2.0 ** (-it)
                kb.op("dve", [Blo], [Btau], lambda e: e.tensor_scalar(out=tau[:], in0=lo[:], scalar1=w, scalar2=None, op0=ALU.add))
                kb.op("dve", [B_aff, Btau], [Bcmp], lambda e: e.tensor_tensor(out=cmp3, in0=aff3, in1=bc16(tau), op=ALU.is_ge))
                kb.op("dve", [Bcmp], [Bpart], lambda e: e.tensor_reduce(out=part[:], in_=cmpT, axis=AX.X, op=ALU.add))
                bk, bb = bank()
                kb.op("pe", [Bpart, Bc], [bb], lambda e: e.matmul(bk[:, 0:NE], onesf[:], part[:], start=True, stop=True))
                kb.op("dve", [bb], [Bgw], lambda e: e.tensor_scalar(out=gw[:], in0=bk[:, 0:NE], scalar1=CAP - 0.5, scalar2=w,
                                                                  op0=ALU.is_ge, op1=ALU.mult))
                kb.op("dve", [Bgw, Blo], [Blo], lambda e: e.tensor_tensor(out=lo[:], in0=lo[:], in1=gw[:], op=ALU.add))
            self3 = AP(self_, 0, [pdim(self_[:]), [NE, NT], [1, NE]])
            kb.op("dve", [B_aff, Blo], [Bsel], lambda e: e.tensor_tensor(out=self3, in0=aff3, in1=bc16(lo), op=ALU.is_ge))
            kb.op("dve", [Bsel], [], lambda e: e.tensor_copy(out=selb[:], in_=self_[:]), pwrites=[Bsel])
            bk, bb = bank()

            def prefix(e):
                for t in range(NT):
                    o = bk[:, t * NE:(t + 1) * NE]
                    for tp in range(t):
                        e.matmul(o, onesb[:], selb[:, tp * NE:(tp + 1) * NE], start=(tp == 0), stop=False)
                    ins = e.matmul(o, triub[:], selb[:, t * NE:(t + 1) * NE], start=(t == 0), stop=True)
                return ins
            kb.op("pe", [Bsel, Bc], [bb], prefix)
            kb.op("dve", [bb, Bsel], [Bposm], lambda e: e.scalar_tensor_tensor(out=posm[:], in0=bk, scalar=1.0, in1=self_[:],
                                                                            op0=ALU.add, op1=ALU.mult))
            kb.op("dve", [Bposm], [Bposm], lambda e: e.tensor_scalar(out=posm[:], in0=posm[:], scalar1=-1.0, scalar2=None, op0=ALU.add))
            Rv = lambda j: AP(R, j, [pdim(R[:]), [5, NT * NE]])
            kb.op("dve", [Bc], [BR], lambda e: e.tensor_copy(
                out=AP(R, 0, [pdim(R[:]), [5 * NE, NT], [5, NE], [1, 2]]),
                in_=AP(tokhl, 0, [pdim(tokhl[:]), [2, NT], [0, NE], [1, 2]])))
            kb.op("dve", [B_aff], [], lambda e: e.tensor_copy(out=Rv(2), in_=aff_all[:]), pwrites=[BR])
            kb.op("dve", [B_aff, BR], [Bcmp], lambda e: e.tensor_tensor(out=r1[:], in0=aff_all[:], in1=Rv(2), op=ALU.subtract))
            kb.op("dve", [Bcmp], [], lambda e: e.tensor_copy(out=Rv(3), in_=r1[:]), pwrites=[BR])
            kb.op("dve", [Bcmp, BR], [Bcmp], lambda e: e.tensor_tensor(out=r1[:], in0=r1[:], in1=Rv(3), op=ALU.subtract))
            kb.op("dve", [Bcmp], [], lambda e: e.tensor_copy(out=Rv(4), in_=r1[:]), pwrites=[BR])

            h2rows = h2_d
            oh_ctr = [0]

            rstate = {}

            def route_tiles(e, t0, t1):
                for t in range(t0, t1):
                    oi = oh_ctr[0] % 4
                    oh_ctr[0] += 1
                    kb.op("dve", [Bposm, Bc], [BOH[oi]], lambda en: en.tensor_scalar(
                        out=OH[oi][:], in0=iota[:], scalar1=posm[:, t * NE + e:t * NE + e + 1], scalar2=None, op0=ALU.is_equal))

                    def mm(en):
                        for cj in range(4):
                            c0 = 3072 + (cj * NT + t) * 8
                            ins = en.matmul(ps[:, c0:c0 + 5], OH[oi][:, cj * 128:(cj + 1) * 128],
                                            R[:, (t * NE + e) * 5:(t * NE + e) * 5 + 5], start=True, stop=True)
                        return ins
                    if t == 0:
                        kb.op("pe", [BOH[oi], BR], [PB[6], PB[7]], mm)
                    else:
                        kb.op("pe", [BOH[oi], BR], [], mm, pwrites=[PB[6], PB[7]])

            def route_idx(e):
                b = e % 2
                kb.op("dve", [PB[6], PB[7]], [Bidxv[b]], lambda en: en.tensor_reduce(
                    out=AP(idxv[b], 0, [pdim(idxv[b][:]), [8, 4], [1, 5]]),
                    in_=AP(ps, 3072, [pdim(ps[:]), [NT * 8, 4], [1, 5], [8, NT]]),
                    axis=AX.X, op=ALU.add))
                v3 = lambda j: AP(idxv[b], j, [pdim(idxv[b][:]), [8, 4]])
                kb.op("dve", [Bidxv[b]], [Bidx[b]], lambda en: en.scalar_tensor_tensor(
                    out=idxf[b][:], in0=v3(0), scalar=128.0, in1=v3(1), op0=ALU.mult, op1=ALU.add))
                kb.op("dve", [Bidx[b]], [], lambda en: en.tensor_copy(out=idxi[b][:], in_=idxf[b][:]), pwrites=[Bidx[b]])
                kb.op("dve", [Bidxv[b]], [Bval[b]], lambda en: en.tensor_tensor(out=val[b][:], in0=v3(2), in1=v3(3), op=ALU.add))
                kb.op("dve", [Bidxv[b], Bval[b]], [Bval[b]], lambda en: en.tensor_tensor(out=val[b][:], in0=val[b][:], in1=v3(4), op=ALU.add))
                for cj in range(4):
                    kb.dma("pool", [Bidx[b], B_h2], [] if cj else [Bxin[b]], xin[b][:, cj, :], h2rows[:, :],
                           pwrites=[Bxin[b]] if cj else [],
                           indirect=dict(out_offset=None, in_offset=bass.IndirectOffsetOnAxis(ap=idxi[b][:, cj:cj + 1], axis=0)))

            def route_T(e):
                b = e % 2
                for k in range(8):
                    bk2, bb2 = bank()
                    bkb = bk2.bitcast(BF16)

                    def tp(en):
                        for cj in range(4):
                            ins = en.transpose(bkb[:, cj * 128:(cj + 1) * 128], xin[b][:, cj, k * 128:(k + 1) * 128], identb[:])
                        return ins
                    kb.op("pe", [Bxin[b], Bc], [bb2], tp)
                    kb.op("act", [bb2], [] if k else [BxinT[b]],
                          (lambda en: en.copy(out=xinT[b][:, k, :], in_=bkb[:, 0:512])),
                          pwrites=[BxinT[b]] if k else [])

            def route(e):
                route_tiles(e, 0, NT)
                route_idx(e)
                route_T(e)

            def tail_gather(e):
                b = e % 2
                for cj in range(4):
                    kb.dma("pool", [Bidx[b], B_moe], [Bmo[cj]], mo[cj][:, :], moe_d[:, :],
                           indirect=dict(out_offset=None, in_offset=bass.IndirectOffsetOnAxis(ap=idxi[b][:, cj:cj + 1], axis=0)))

            def tail_add_scatter(e):
                b = e % 2
                for cj in range(4):
                    kb.op("dve", [Bmo[cj], Bys[cj]], [Bmo[cj]], lambda en: en.tensor_tensor(out=mo[cj][:], in0=mo[cj][:], in1=ys[cj][:], op=ALU.add))
                for cj in range(4):
                    kb.dma("pool", [Bidx[b], Bmo[cj]], [], moe_d[:, :], mo[cj][:, :], pwrites=[B_moe],
                           indirect=dict(out_offset=bass.IndirectOffsetOnAxis(ap=idxi[b][:, cj:cj + 1], axis=0), in_offset=None))

            def compute(e):
                b = e % 2
                for fb in range(3):
                    n = e * 3 + fb
                    wb = n % 3
                    for j in range(4):
                        bg, bbg = bank()
                        bu, bbu = bank()

                        def mm(en):
                            for bk_, w_ in ((bg, wg[wb]), (bu, wu[wb])):
                                for k in range(8):
                                    ins = en.matmul(bk_, w_[:, k, j * 128:(j + 1) * 128], xinT[b][:, k, :], start=(k == 0), stop=(k == 7))
                            return ins
                        kb.op("pe", [BxinT[b], Bwg[wb], Bwu[wb]], [bbg, bbu], mm)
                        if e + 1 < NE:
                            if fb * 4 + j < 11:
                                route_tiles(e + 1, 3 * (fb * 4 + j), min(NT, 3 * (fb * 4 + j) + 3))
                            else:
                                route_idx(e + 1)
                        if fb == 0 and j == 2 and e > 0:
                            tail_add_scatter(e - 1)
                        si = (fb * 4 + j) % 2
                        kb.op("act", [bbg], [Bsg[si]], lambda en: en.activation(out=sg[si][:], in_=bg, func=AF.Silu))
                        fi = fb * 4 + j
                        kb.op("dve", [Bsg[si], bbu], [] if fi else [Bact[b]], lambda en: en.tensor_tensor(
                            out=act[b][:, fi, :], in0=sg[si][:], in1=bu, op=ALU.mult), pwrites=[Bact[b]] if fi else [])
                    issue_gu(n + 2)
                for cj in range(4):
                    yi = cj
                    for half in range(2):
                        bk, bb = bank()

                        def mm(en):
                            for fi in range(12):
                                m = e * 2 + fi // 6
                                ins = en.matmul(bk, act[b][:, fi, cj * 128:(cj + 1) * 128], wd[m % 3][:, fi % 6, half * 512:(half + 1) * 512],
                                                start=(fi == 0), stop=(fi == 11))
                            return ins
                        kb.op("pe", [Bact[b], Bwd[(e * 2) % 3], Bwd[(e * 2 + 1) % 3]], [bb], mm)
                        kb.op("act", [bb, Bval[b]], [] if half else [Bys[yi]], lambda en: en.activation(
                            out=ys[yi][:, half * 512:(half + 1) * 512], in_=bk, func=AF.Copy, scale=val[b][:, cj:cj + 1]),
                            pwrites=[Bys[yi]] if half else [])
                issue_d(e * 2 + 3)
                issue_d(e * 2 + 4)
                if e + 1 < NE:
                    route_T(e + 1)
                tail_gather(e)
                if e == NE - 1:
                    tail_add_scatter(e)

            nrot[0] = 6
            bank_ctr[0] = 0
            route(0)
            for e in range(int(os.environ.get("NEXP", str(NE)))):
                compute(e)
            kb.barrier()
            nrot[0] = 8

    if os.environ.get("SKIPMOE", "") == "":
        moe_phase()

    with ExitStack() as s6:
        A = lambda n, sh, dt: s6.enter_context(nc.sbuf_tensor("p6_" + n, sh, dt))
        g2bc, lngbc, lnbbc = (A(n, [128, D], F32) for n in ("g2bc", "lngbc", "lnbbc"))
        NB6 = 3
        xt = [A("xt%d" % i, [128, D], F32) for i in range(NB6)]
        mt = [A("mt%d" % i, [128, D], F32) for i in range(NB6)]
        yy = [A("y%d" % i, [128, D], F32) for i in range(NB6)]
        xh = [A("xh%d" % i, [128, D], F32) for i in range(NB6)]
        oo = [A("o%d" % i, [128, D], F32) for i in range(NB6)]
        L = LNState(A, "ln", 3)
        Bc6 = Buf()
        Bxt, Bmt, By, Bxh, Boo = ([Buf() for _ in range(NB6)] for _ in range(5))
        kb.dma("sp", [B_mod], [], g2bc[:], bcast_row(mod_d[0:1, 5120:6144], D), pwrites=[Bc6])
        kb.dma("sp", [], [], lngbc[:], bcast_row(ln2g_d[0:1, :], D), pwrites=[Bc6])
        kb.dma("sp", [], [], lnbbc[:], bcast_row(ln2b_d[0:1, :], D), pwrites=[Bc6])
        x1_t = x1_d.rearrange("(t p) d -> t p d", p=128)
        moe_t = moe_d.rearrange("(t p) d -> t p d", p=128)
        out_t = out_d.rearrange("(t p) d -> t p d", p=128)
        stA = {}

        def stageA6(t):
            b = t % NB6
            kb.dma("sp", [B_x1], [Bxt[b]], xt[b][:], x1_t[t])
            kb.dma("sp", [B_moe], [Bmt[b]], mt[b][:], moe_t[t])
            kb.op("dve", [Bmt[b], Bc6], [Bmt[b]], lambda e: e.tensor_tensor(out=mt[b][:], in0=mt[b][:], in1=g2bc[:], op=ALU.mult))
            kb.op("dve", [Bxt[b], Bmt[b]], [By[b]], lambda e: e.scalar_tensor_tensor(
                out=yy[b][:], in0=xt[b][:], scalar=ALPHA, in1=mt[b][:], op0=ALU.mult, op1=ALU.add))
            stA[t] = ln_stats(L, yy[b], By[b])

        def stageB6(t):
            b = t % NB6
            st, Bst = stA.pop(t)
            ln_apply(st, Bst, yy[b], By[b], xh[b], Bxh[b])

        def stageC6(t):
            b = t % NB6
            kb.op("dve", [Bxh[b], Bc6], [Boo[b]], lambda e: e.tensor_tensor(out=oo[b][:], in0=xh[b][:], in1=lngbc[:], op=ALU.mult))
            kb.op("dve", [Boo[b], Bc6], [Boo[b]], lambda e: e.tensor_tensor(out=oo[b][:], in0=oo[b][:], in1=lnbbc[:], op=ALU.add))
            kb.dma("sp", [Boo[b]], [Buf()], out_t[t], oo[b][:])

        for step in range(NT + 2):
            if 0 <= step - 2 < NT:
                stageC6(step - 2)
            if 0 <= step - 1 < NT:
                stageB6(step - 1)
            if step < NT:
                stageA6(step)
        kb.barrier()


def _finish(nc, kb, out_d, x_d):
    kb.barrier()
    tok = kb.dma("sp", [], [Buf()], out_d[0:128, :], x_d[0:128, :])
    kb._wait("sp", [tok])


def _t5_bucket(rel):
    half = 16
    ret = (rel > 0).astype(np.int32) * half
    n = np.abs(rel)
    large = 8 + (np.log(np.maximum(n, 1) / 8) / np.log(1024 / 8) * (half - 8)).astype(np.int32)
    large = np.minimum(large, half - 1)
    return ret + np.where(n < 8, n, large).astype(np.int32)


def _static_tables():
    a = np.arange(128)[:, None]
    c = np.arange(MW)[None, :]
    rel = a - c + C0
    mult = ((np.abs(rel) <= 64).astype(np.float32)
            + ((rel % 4 == 0) & (np.abs(rel) <= 256)).astype(np.float32)
            + ((rel % 16 == 0) & (np.abs(rel) <= 1024)).astype(np.float32))
    bucket = _t5_bucket(rel)
    inv = (10000.0 ** (-(np.arange(0, 32, 2, dtype=np.float32)) / np.float32(32))).astype(np.float32)
    ang = (np.arange(S, dtype=np.float32)[:, None] * inv[None, :]).astype(np.float32)
    cos = np.cos(ang.astype(np.float64)).astype(np.float32).T
    sin = np.sin(ang.astype(np.float64)).astype(np.float32).T
    rope_cos = np.ascontiguousarray(np.concatenate([cos, cos], axis=0))
    rope_sin = np.ascontiguousarray(np.concatenate([-sin, sin], axis=0))
    tokhl = np.zeros((128, NT, 2), np.float32)
    tokhl[:, :, 0] = np.arange(NT)[None, :]
    tokhl[:, :, 1] = np.arange(128)[:, None]
    return dict(
        toep_mult=mult.astype(np.float32), bucket=bucket,
        rope_cos=rope_cos, rope_sin=rope_sin,
        ident=np.eye(128, dtype=np.float32),
        triu=np.triu(np.ones((128, 128), np.float32), k=1),
        iota512=np.tile(np.arange(512, dtype=np.float32)[None, :], (128, 1)),
        tokhl=tokhl,
    )


def make_in_maps(inputs):
    f = lambda a: np.ascontiguousarray(np.asarray(a, dtype=np.float32))
    st = _static_tables()
    x = f(inputs["x"])
    c = f(inputs["c"])
    w_in = f(inputs["w_in"][0])
    kr = w_in[:, 640:672]
    w_kr = np.zeros((D, 192), np.float32)
    w_kr[:, 64:96] = kr
    w_kr[:, 96 + 64:96 + 80] = kr[:, 16:32]
    w_kr[:, 96 + 80:96 + 96] = kr[:, 0:16]
    w_uq = f(inputs["w_uq"][0]).reshape(384, 8, 96)
    w_uq_rot = w_uq.copy()
    w_uq_rot[:, :, 64:80] = w_uq[:, :, 80:96]
    w_uq_rot[:, :, 80:96] = w_uq[:, :, 64:80]
    w_uq2 = np.ascontiguousarray(np.stack([w_uq.reshape(384, 768), w_uq_rot.reshape(384, 768)], axis=1))
    w_ukv = f(inputs["w_ukv"][0]).reshape(256, 8, 128)
    rel_bias = f(inputs["rel_bias"])
    toep = np.ascontiguousarray(np.transpose(rel_bias[st["bucket"]], (2, 0, 1)))
    shared = dict(
        w_ada=f(inputs["w_ada"][0]), b_ada=f(inputs["b_ada"][0]).reshape(1, -1),
        w_in=w_in, w_kr=w_kr,
        g_q=np.ascontiguousarray(f(inputs["q_norm_g"][0]).reshape(3, 128).T),
        g_kv=np.ascontiguousarray(f(inputs["kv_norm_g"][0]).reshape(2, 128).T),
        w_uq2=w_uq2,
        w_ukv_k=np.ascontiguousarray(w_ukv[:, :, 0:64].reshape(256, 512)),
        w_ukv_v=np.ascontiguousarray(w_ukv[:, :, 64:128].reshape(256, 512)),
        rope_cos=st["rope_cos"], rope_sin=st["rope_sin"],
        relb_toep=toep, toep_mult=st["toep_mult"],
        w_out=f(inputs["w_out"][0]), ln1_g=f(inputs["ln1_g"][0]).reshape(1, -1), ln1_b=f(inputs["ln1_b"][0]).reshape(1, -1),
        w_router=f(inputs["w_router"][0]),
        w_gate=f(inputs["w_gate"][0]), w_up=f(inputs["w_up"][0]), w_down=f(inputs["w_down"][0]),
        ln2_g=f(inputs["ln2_g"][0]).reshape(1, -1), ln2_b=f(inputs["ln2_b"][0]).reshape(1, -1),
        ident=st["ident"], triu=st["triu"], iota512=st["iota512"], tokhl=st["tokhl"],
    )
    maps = []
    for b in range(x.shape[0]):
        m = dict(shared)
        m["x"] = x[b]
        m["c_fm"] = np.ascontiguousarray(c[b].reshape(8, 128).T)
        maps.append(m)
    return maps


def kernel(**inputs):
    maps = make_in_maps(inputs)
    nc = build_program()
    res = run_bass_kernel_spmd(nc, maps, core_ids=list(range(len(maps))))
    return np.stack([np.asarray(r["out"], dtype=np.float32) for r in res.results], axis=0)
```

```python
import math
import os
from contextlib import ExitStack

import numpy as np
import concourse.bass as bass
import concourse.mybir as mybir
from concourse.bass_utils import run_bass_kernel_spmd

F32 = mybir.dt.float32
BF16 = mybir.dt.bfloat16
I32 = mybir.dt.int32
AF = mybir.ActivationFunctionType
ALU = mybir.AluOpType
AX = mybir.AxisListType

D = 1024
S = 4096
NT = 32
NG = 8
ALPHA = 2.0 ** 0.25
EPS = 1e-6
NE = 16
CAP = 512
DFF = 1536
C0 = 1408
MW = 2944
NBIS = 26


class Buf:
    __slots__ = ("w", "r", "name", "excl")

    def __init__(self, name="", excl=False):
        self.excl = excl
        self.w = []
        self.r = []
        self.name = name


def _compact(toks):
    best = {}
    for t in toks:
        if t[0] not in best or best[t[0]][2] < t[2]:
            best[t[0]] = t
    return list(best.values())


class KB:
    COMPUTE = ("pe", "act", "dve", "pool")
    NDS = 12

    def __init__(self, nc, es):
        self.nc = nc
        self.E = {"pe": nc.tensor, "act": nc.scalar, "dve": nc.vector, "pool": nc.gpsimd, "sp": nc.sync}
        self.csem = {e: es.enter_context(nc.semaphore("c_" + e)) for e in self.COMPUTE}
        self.ccnt = {e: 0 for e in self.COMPUTE}
        self.dsem = {q: [es.enter_context(nc.semaphore("d_%s%d" % (q, i))) for i in range(self.NDS)]
                     for q in ("sp", "pool", "act")}
        self.dcnt = {q: 0 for q in self.dsem}
        self.dtok = {q: [None] * self.NDS for q in self.dsem}
        self.seen = {e: {} for e in self.E}
        self.nwait = 0

    def _wait(self, eng, toks):
        need = {}
        for t in toks:
            if t is None:
                continue
            key, sem, val = t
            if self.seen[eng].get(key, 0) >= val:
                continue
            if need.get(key, (None, 0))[1] < val:
                need[key] = (sem, val)
        for key, (sem, val) in need.items():
            self.E[eng].wait_ge(sem, val)
            self.seen[eng][key] = val
            self.nwait += 1

    def _deps(self, eng, reads, writes, pwrites, is_dma):
        own = None if is_dma else "c_" + eng
        toks = []
        for b in reads:
            for t in b.w:
                if not (eng == "pe" and t[0] == own):
                    toks.append(t)
            if b.excl:
                toks.extend(t for t in b.r if t[0] != own)
        for b in writes:
            toks.extend(t for t in b.w if t[0] != own)
            toks.extend(t for t in b.r if t[0] != own)
        for b in pwrites:
            toks.extend(t for t in b.r if t[0] != own)
        return toks

    def _commit(self, tok, reads, writes, pwrites):
        for b in reads:
            b.r.append(tok)
            if len(b.r) > 16:
                b.r = _compact(b.r)
        for b in writes:
            b.w = [tok]
            b.r = []
        for b in pwrites:
            b.w.append(tok)
            if len(b.w) > 16:
                b.w = _compact(b.w)

    def op(self, eng, reads, writes, fn, pwrites=()):
        self._wait(eng, self._deps(eng, reads, writes, pwrites, False))
        inst = fn(self.E[eng])
        self.ccnt[eng] += 1
        tok = ("c_" + eng, self.csem[eng], self.ccnt[eng])
        inst.then_inc(self.csem[eng], 1)
        self._commit(tok, reads, writes, pwrites)
        return tok

    def dma(self, q, reads, writes, out, in_, pwrites=(), indirect=None, **kw):
        i = self.dcnt[q]
        slot = i % self.NDS
        self._wait(q, self._deps(q, reads, writes, pwrites, True) + [self.dtok[q][slot]])
        if indirect is None:
            inst = self.E[q].dma_start(out=out, in_=in_, **kw)
        else:
            inst = self.E[q].indirect_dma_start(out=out, in_=in_, **indirect)
        val = 16 * (i // self.NDS + 1)
        sem = self.dsem[q][slot]
        inst.then_inc(sem, 16)
        tok = ("d_%s%d" % (q, slot), sem, val)
        self.dtok[q][slot] = tok
        self.dcnt[q] += 1
        self._commit(tok, reads, writes, pwrites)
        return tok

    def barrier(self, engines=None):
        toks = []
        for e in self.COMPUTE:
            if self.ccnt[e]:
                toks.append(("c_" + e, self.csem[e], self.ccnt[e]))
        for q in self.dsem:
            toks.extend(t for t in self.dtok[q] if t is not None)
        for e in (engines or self.E):
            self._wait(e, toks)


def AP(t, off, dims):
    return bass.AP(t, off, [list(d) for d in dims])


def pdim(ap):
    return list(ap.ap[0])


def build_program(stages=99, dbg=()):
    nc = bass.Bass("TRN2", target_bir_lowering=False)
    es = ExitStack()
    with es:
        _build(nc, es, stages, dbg)
    return nc


def _build(nc, es, stages, dbg):
    def din(name, shape, dt=F32):
        return nc.dram_tensor(name, list(shape), dt, kind="ExternalInput").ap()

    def dscr(name, shape, dt):
        kind = "ExternalOutput" if name in dbg else "Internal"
        return nc.dram_tensor(name, list(shape), dt, kind=kind).ap()

    x_d = din("x", [S, D])
    cfm_d = din("c_fm", [128, 8])
    wada_d = din("w_ada", [D, 6 * D])
    bada_d = din("b_ada", [1, 6 * D])
    win_d = din("w_in", [D, 2208])
    wkr_d = din("w_kr", [D, 192])
    gq_d = din("g_q", [128, 3])
    gkv_d = din("g_kv", [128, 2])
    wuq_d = din("w_uq2", [384, 2, 768])
    wukvk_d = din("w_ukv_k", [256, 512])
    wukvv_d = din("w_ukv_v", [256, 512])
    cos_d = din("rope_cos", [32, S])
    sin_d = din("rope_sin", [32, S])
    toep_d = din("relb_toep", [8, 128, MW])
    mult_d = din("toep_mult", [128, MW])
    wout_d = din("w_out", [D, D])
    ln1g_d = din("ln1_g", [1, D])
    ln1b_d = din("ln1_b", [1, D])
    wr_d = din("w_router", [D, NE])
    wg_d = din("w_gate", [NE, D, DFF])
    wu_d = din("w_up", [NE, D, DFF])
    wd_d = din("w_down", [NE, DFF, D])
    ln2g_d = din("ln2_g", [1, D])
    ln2b_d = din("ln2_b", [1, D])
    ident_d = din("ident", [128, 128])
    triu_d = din("triu", [128, 128])
    iota_d = din("iota512", [128, 512])
    tokhl_d = din("tokhl", [128, NT, 2])
    out_d = nc.dram_tensor("out", [S, D], F32, kind="ExternalOutput").ap()

    mod_d = dscr("mod_s", [1, 6 * D], F32)
    qT_d = dscr("qT_s", [8, 96, S], BF16)
    kT_d = dscr("kT_s", [8, 96, S], BF16)
    Vm_d = dscr("Vm_s", [NT, 128, 768], BF16)
    dqT_d = dscr("dqT_s", [4, 128, S], BF16)
    dkT_d = dscr("dkT_s", [4, 128, S], BF16)
    Vd_d = dscr("Vd_s", [NT, 128, 768], BF16)
    attnT_d = dscr("attnT_s", [8, 128, S], BF16)
    x1_d = dscr("x1_s", [S, D], F32)
    h2_d = dscr("h2_s", [S, D], BF16)
    moe_d = dscr("moe_s", [S, D], F32)
    aff_d = dscr("aff_s", [128, NT * NE], F32)
    idx_d = dscr("idx_s", [128, NE * 4 * 8], F32)

    kb = KB(nc, es)
    B_mod, B_q, B_k, B_vm, B_dq, B_dk, B_vd = (Buf(n) for n in ("mod", "q", "k", "vm", "dq", "dk", "vd"))
    B_attn, B_x1, B_h2, B_moe = Buf("attn"), Buf("x1"), Buf("h2"), Buf("moe")

    ps = es.enter_context(nc.psum_tensor("ps", [128, 4096], F32))
    PB = [Buf("bank%d" % i, excl=True) for i in range(8)]
    bank_ctr = [0]

    nrot = [8]

    def bank():
        i = bank_ctr[0] % nrot[0]
        bank_ctr[0] += 1
        return ps[:, i * 512:(i + 1) * 512], PB[i]

    modfm = es.enter_context(nc.sbuf_tensor("modfm", [128, 16], F32))
    aff_all = es.enter_context(nc.sbuf_tensor("aff_all", [128, NT * NE], F32))
    B_modfm, B_aff = Buf("modfm"), Buf("aff")

    def bcast_row(dram_ap_row, n):
        return AP(dram_ap_row.tensor, dram_ap_row.offset, [[0, 128], [1, n]])

    with ExitStack() as s0:
        A = lambda n, sh, dt: s0.enter_context(nc.sbuf_tensor("p0_" + n, sh, dt))
        cfm = A("cfm", [128, 8], F32)
        sig = A("sig", [128, 8], F32)
        cond = A("cond", [128, 8], F32)
        bada = A("bada", [1, 6 * D], F32)
        modrow = A("modrow", [1, 6 * D], F32)
        wa = [A("wa%d" % i, [128, 8, 512], F32) for i in range(3)]
        Bc, Bcond, Bb, Bm = Buf(), Buf(), Buf(), Buf()
        Bwa = [Buf() for _ in range(3)]
        kb.dma("sp", [], [Bc], cfm[:], cfm_d[:, :])
        kb.dma("sp", [], [Bb], bada[:], bada_d[:, :])
        wada_v = wada_d.rearrange("(k p) n -> p k n", p=128)
        for j in range(2):
            kb.dma("sp", [], [Bwa[j]], wa[j][:], wada_v[:, :, j * 512:(j + 1) * 512])
        kb.op("act", [Bc], [Bcond], lambda e: e.activation(out=sig[:], in_=cfm[:], func=AF.Sigmoid))
        kb.op("dve", [Bc, Bcond], [Bcond], lambda e: e.tensor_tensor(out=cond[:], in0=cfm[:], in1=sig[:], op=ALU.mult))
        for j in range(12):
            if j + 2 < 12:
                kb.dma("sp", [], [Bwa[(j + 2) % 3]], wa[(j + 2) % 3][:], wada_v[:, :, (j + 2) * 512:(j + 3) * 512])
            bk, bb = bank()
            w = wa[j % 3]

            def mm(e, w=w, bk=bk):
                for k in range(8):
                    i = e.matmul(bk[0:1, :], cond[:, k:k + 1], w[:, k, :], start=(k == 0), stop=(k == 7))
                return i
            kb.op("pe", [Bcond, Bwa[j % 3]], [bb], mm)
            kb.op("dve", [bb, Bb], [], pwrites=[Bm], fn=lambda e, bk=bk, j=j: e.tensor_tensor(
                out=modrow[0:1, j * 512:(j + 1) * 512], in0=bk[0:1, :], in1=bada[0:1, j * 512:(j + 1) * 512], op=ALU.add))
        kb.dma("sp", [Bm], [B_mod], mod_d[:, :], modrow[:])
        onef = A("onef", [1, 1], F32)
        kb.op("pool", [], [Bc], lambda e: e.memset(onef[:], 1.0))
        bk, bb = bank()

        def mmT(e, bk=bk):
            for j in range(16):
                ins = e.matmul(bk[:, j:j + 1], modrow[0:1, j * 128:(j + 1) * 128], onef[0:1, 0:1], start=True, stop=True)
            return ins
        kb.op("pe", [Bm, Bc], [bb], mmT)
        kb.op("dve", [bb], [B_modfm], lambda e, bk=bk: e.tensor_copy(out=modfm[:], in_=bk[:, 0:16]))
        kb.op("dve", [B_modfm], [B_modfm], lambda e: e.tensor_scalar(
            out=modfm[:, 8:16], in0=modfm[:, 8:16], scalar1=1.0, scalar2=None, op0=ALU.add))
        kb.barrier()
    if stages <= 0:
        _finish(nc, kb, out_d, x_d)
        return

    with ExitStack() as s1:
        A = lambda n, sh, dt: s1.enter_context(nc.sbuf_tensor("p1_" + n, sh, dt))
        win = A("win", [128, 8, 2208], BF16)
        wkr = A("wkr", [128, 8, 192], BF16)
        wuq = A("wuq", [128, 3, 2, 768], BF16)
        wukvk = A("wukvk", [128, 2, 512], BF16)
        wukvv = A("wukvv", [128, 2, 512], BF16)
        ident = A("ident", [128, 128], F32)
        onesq = A("onesq", [128, 128], F32)
        oneskv = A("oneskv", [128, 128], F32)
        gq = A("gq", [128, 3], F32)
        gkv = A("gkv", [128, 2], F32)
        xg = [A("xg%d" % i, [128, 4, 1024], F32) for i in range(2)]
        hT = [A("hT%d" % i, [128, 8, 512], BF16) for i in range(2)]
        cT = A("cT", [128, 5, 512], F32)
        sq = A("sq", [128, 5, 512], F32)
        rstd = A("rstd", [128, 2, 512], F32)
        cn = [A("cn%d" % i, [128, 5, 512], BF16) for i in range(2)]
        cosr = [A("cosr%d" % i, [128, 512], F32) for i in range(2)]
        sinr = [A("sinr%d" % i, [128, 512], F32) for i in range(2)]
        tmpa = [A("tmpa%d" % i, [128, 512], F32) for i in range(2)]
        tmpb = [A("tmpb%d" % i, [128, 512], F32) for i in range(2)]
        NO = 6
        ob = [A("ob%d" % i, [128, 512], BF16) for i in range(NO)]
        vo = [A("vo%d" % i, [128, 768], BF16) for i in range(4)]
        Bw, Bid, Bones, Bg = Buf(), Buf(), Buf(), Buf()
        Bxg = [Buf(), Buf()]
        BhT = [Buf(), Buf()]
        BcT, Bsq, Brstd = [Buf() for _ in range(5)], [Buf() for _ in range(5)], [Buf(), Buf()]
        Bcn = [[Buf() for _ in range(5)] for _ in range(2)]
        Brope = [Buf(), Buf()]
        Bta, Btb = [Buf(), Buf()], [Buf(), Buf()]
        Bob = [Buf() for _ in range(NO)]
        Bvo = [Buf() for _ in range(4)]
        ob_ctr, vo_ctr, t_ctr = [0], [0], [0]

        SKIP = os.environ.get('P1SKIP', '')
        for k in range(0 if 'w' in SKIP else 8):
            kb.dma("pool", [], [], win[:, k, :], win_d[k * 128:(k + 1) * 128, :], pwrites=[Bw])
            kb.dma("pool", [], [], wkr[:, k, :], wkr_d[k * 128:(k + 1) * 128, :], pwrites=[Bw])
        for c in range(0 if 'u' in SKIP else 3):
            kb.dma("pool", [], [], wuq[:, c, :, :], wuq_d[c * 128:(c + 1) * 128, :, :], pwrites=[Bw])
        for c in range(0 if 'v' in SKIP else 2):
            kb.dma("pool", [], [], wukvk[:, c, :], wukvk_d[c * 128:(c + 1) * 128, :], pwrites=[Bw])
            kb.dma("pool", [], [], wukvv[:, c, :], wukvv_d[c * 128:(c + 1) * 128, :], pwrites=[Bw])
        kb.dma("sp", [], [Bid], ident[:], ident_d[:, :])
        kb.dma("sp", [], [], gq[:], gq_d[:, :], pwrites=[Bg])
        kb.dma("sp", [], [], gkv[:], gkv_d[:, :], pwrites=[Bg])
        kb.op("pool", [], [], lambda e: e.memset(onesq[:], 1.0 / 384.0), pwrites=[Bones])
        kb.op("pool", [], [], lambda e: e.memset(oneskv[:], 1.0 / 256.0), pwrites=[Bones])
        for i in range(4):
            kb.op("pool", [], [Bvo[i]], lambda e, i=i: e.memset(vo[i][:], 1.0))

        x_v = x_d.rearrange("(t p) d -> p t d", p=128)

        def load_x(g):
            kb.dma("sp", [], [Bxg[g % 2]], xg[g % 2][:], x_v[:, g * 4:(g + 1) * 4, :])
            kb.dma("sp", [], [Brope[g % 2]], cosr[g % 2][64:96, :], cos_d[:, g * 512:(g + 1) * 512])
            kb.dma("sp", [], [Brope[g % 2]], sinr[g % 2][64:96, :], sin_d[:, g * 512:(g + 1) * 512])

        def next_ob():
            i = ob_ctr[0] % NO
            ob_ctr[0] += 1
            return ob[i], Bob[i]

        def evac_copy(eng, src, bsrc, dst, bdst):
            if eng == "act":
                kb.op("act", [bsrc], [bdst], lambda e: e.copy(out=dst, in_=src))
            else:
                kb.op("dve", [bsrc], [bdst], lambda e: e.tensor_copy(out=dst, in_=src))

        def rope_rows(bm, bbm, br, bbr, g, dst, bdst, full):
            i = t_ctr[0] % 2
            t_ctr[0] += 1
            kb.op("dve", [bbm, Brope[g % 2]], [Bta[i]], lambda e: e.tensor_tensor(
                out=tmpa[i][64:96, :], in0=bm[64:96, :], in1=cosr[g % 2][64:96, :], op=ALU.mult))
            kb.op("dve", [bbr, Brope[g % 2]], [Btb[i]], lambda e: e.tensor_tensor(
                out=tmpb[i][64:96, :], in0=br[64:96, :], in1=sinr[g % 2][64:96, :], op=ALU.mult))
            kb.op("dve", [Bta[i], Btb[i]], [bdst] if full else [], lambda e: e.tensor_tensor(
                out=dst[64:96, :], in0=tmpa[i][64:96, :], in1=tmpb[i][64:96, :], op=ALU.add),
                pwrites=[] if full else [bdst])

        def vo_views(vt, bk):
            o = AP(vt, 0, [pdim(vt[:]), [192, 4], [128, 2], [1, 64]])
            i = AP(bk.tensor, bk.offset, [pdim(bk), [128, 4], [64, 2], [1, 64]])
            return o, i

        load_x(0)
        LIM = float(os.environ.get('P1LIM', '99'))
        SUB = float(os.environ.get('P1SUB', '99'))
        for g in range(NG if LIM >= 99 else 1):
            if g + 1 < NG:
                load_x(g + 1)
            X, BX = xg[g % 2], Bxg[g % 2]
            H, BH = hT[g % 2], BhT[g % 2]
            CN, BCN = cn[g % 2], Bcn[g % 2]
            cols = slice(g * 512, (g + 1) * 512)
            for k in range(8):
                bk, bb = bank()

                def tp(e, bk=bk, k=k):
                    for i in range(4):
                        ins = e.transpose(bk[:, i * 128:(i + 1) * 128], X[:, i, k * 128:(k + 1) * 128], ident[:])
                    return ins
                kb.op("pe", [BX, Bid], [bb], tp)
                kb.op("act", [bb, B_modfm], [BH] if k == 0 else [], lambda e, bk=bk, k=k: e.activation(
                    out=H[:, k, :], in_=bk, func=AF.Identity, scale=modfm[:, 8 + k:9 + k], bias=modfm[:, k:k + 1]),
                    pwrites=[] if k == 0 else [BH])
            if LIM < 1:
                break
            for c in range(5):
                bk, bb = bank()

                def mm(e, bk=bk, c=c):
                    for k in range(8):
                        ins = e.matmul(bk, win[:, k, c * 128:(c + 1) * 128], H[:, k, :], start=(k == 0), stop=(k == 7))
                    return ins
                kb.op("pe", [BH, Bw], [bb], mm)
                if SUB >= 0.1:
                    kb.op("act", [bb], [Bsq[c]], lambda e, bk=bk, c=c: e.activation(out=sq[:, c, :], in_=bk, func=AF.Square))
                if SUB >= 0.15:
                    kb.op("dve", [bb, Bsq[c]], [BcT[c]], lambda e, bk=bk, c=c: e.tensor_copy(out=cT[:, c, :], in_=bk))
            if SUB < 0.3:
                break
            for which, (cs, ones_t) in enumerate((((0, 1, 2), onesq), ((3, 4), oneskv))):
                bk, bb = bank()

                def mm(e, bk=bk, cs=cs, ones_t=ones_t):
                    for n, c in enumerate(cs):
                        ins = e.matmul(bk, ones_t[:], sq[:, c, :], start=(n == 0), stop=(n == len(cs) - 1))
                    return ins
                kb.op("pe", [Bsq[c] for c in cs] + [Bones], [bb], mm)
                kb.op("dve", [bb], [Brstd[which]], lambda e, bk=bk, which=which: e.tensor_scalar(
                    out=rstd[:, which, :], in0=bk, scalar1=EPS, scalar2=None, op0=ALU.add))
                if SUB < 0.5:
                    continue
                kb.op("act", [Brstd[which]], [Brstd[which]], lambda e, which=which: e.activation(
                    out=rstd[:, which, :], in_=rstd[:, which, :], func=AF.Sqrt))
                kb.op("dve", [Brstd[which]], [Brstd[which]], lambda e, which=which: e.reciprocal(
                    out=rstd[:, which, :], in_=rstd[:, which, :]))
                if SUB < 0.7:
                    continue
                for c in cs:
                    gsc = gq[:, c:c + 1] if which == 0 else gkv[:, c - 3:c - 2]
                    kb.op("dve", [BcT[c], Brstd[which], Bg], [BCN[c]], lambda e, c=c, gsc=gsc, which=which: e.scalar_tensor_tensor(
                        out=CN[:, c, :], in0=cT[:, c, :], scalar=gsc, in1=rstd[:, which, :], op0=ALU.mult, op1=ALU.mult))
            if LIM < 6:
                break
            for base, dst, bd in ((672, dqT_d, B_dq), (1184, dkT_d, B_dk)):
                for hp in range(4):
                    bk, bb = bank()

                    def mm(e, hp=hp, bk=bk, base=base):
                        for k in range(8):
                            ins = e.matmul(bk, win[:, k, base + hp * 128: base + (hp + 1) * 128], H[:, k, :],
                                           start=(k == 0), stop=(k == 7))
                        return ins
                    kb.op("pe", [BH, Bw], [bb], mm)
                    o, bo = next_ob()
                    evac_copy("act" if hp % 2 == 0 else "dve", bk, bb, o[:, :], bo)
                    kb.dma("sp", [bo], [], dst[hp, :, cols], o[:, :], pwrites=[bd])
            if LIM < 7:
                break
            for i in range(4):
                bk, bb = bank()

                def mm(e, i=i, bk=bk):
                    for k in range(8):
                        ins = e.matmul(bk, H[:, k, i * 128:(i + 1) * 128], win[:, k, 1696:2208], start=(k == 0), stop=(k == 7))
                    return ins
                kb.op("pe", [BH, Bw], [bb], mm)
                vi = vo_ctr[0] % 4
                vo_ctr[0] += 1
                ov, iv = vo_views(vo[vi], bk)
                kb.op("act", [bb], [Bvo[vi]], lambda e, ov=ov, iv=iv: e.copy(out=ov, in_=iv))
                kb.dma("sp", [Bvo[vi]], [], Vd_d[g * 4 + i, :, :], vo[vi][:], pwrites=[B_vd])
            if LIM < 2:
                break
            for h in range(8):
                bm, bbm = bank()
                br, bbr = bank()

                def mm(e, h=h, bm=bm, br=br):
                    for which, bk in ((0, bm), (1, br)):
                        for c in range(3):
                            ins = e.matmul(bk[0:96, :], wuq[:, c, which, h * 96:(h + 1) * 96], CN[:, c, :],
                                           start=(c == 0), stop=(c == 2))
                    return ins
                kb.op("pe", [BCN[0], BCN[1], BCN[2], Bw], [bbm, bbr], mm)
                o, bo = next_ob()
                kb.op("act", [bbm], [bo], lambda e, o=o, bm=bm: e.copy(out=o[0:64, :], in_=bm[0:64, :]))
                rope_rows(bm, bbm, br, bbr, g, o, bo, False)
                kb.dma("sp", [bo], [], qT_d[h, :, cols], o[0:96, :], pwrites=[B_q])
            if LIM < 3:
                break
            for hp in range(4):
                bk, bb = bank()

                def mm(e, hp=hp, bk=bk):
                    for c in range(2):
                        ins = e.matmul(bk, wukvk[:, c, hp * 128:(hp + 1) * 128], CN[:, 3 + c, :], start=(c == 0), stop=(c == 1))
                    return ins
                kb.op("pe", [BCN[3], BCN[4], Bw], [bb], mm)
                o, bo = next_ob()
                evac_copy("act", bk, bb, o[:, :], bo)
                kb.dma("sp", [bo], [], kT_d[2 * hp, 0:64, cols], o[0:64, :], pwrites=[B_k])
                kb.dma("sp", [bo], [], kT_d[2 * hp + 1, 0:64, cols], o[64:128, :], pwrites=[B_k])
            if LIM < 4:
                break
            bm, bbm = bank()
            br, bbr = bank()

            def mm(e, bm=bm, br=br):
                for which, bk in ((0, bm), (1, br)):
                    for k in range(8):
                        ins = e.matmul(bk[0:96, :], wkr[:, k, which * 96:(which + 1) * 96], H[:, k, :],
                                       start=(k == 0), stop=(k == 7))
                return ins
            kb.op("pe", [BH, Bw], [bbm, bbr], mm)
            o, bo = next_ob()
            rope_rows(bm, bbm, br, bbr, g, o, bo, True)
            for h in range(8):
                kb.dma("sp", [bo], [], kT_d[h, 64:96, cols], o[64:96, :], pwrites=[B_k])
            if LIM < 5:
                break
            for i in range(4):
                bk, bb = bank()

                def mm(e, i=i, bk=bk):
                    for c in range(2):
                        ins = e.matmul(bk, CN[:, 3 + c, i * 128:(i + 1) * 128], wukvv[:, c, :], start=(c == 0), stop=(c == 1))
                    return ins
                kb.op("pe", [BCN[3], BCN[4], Bw], [bb], mm)
                vi = vo_ctr[0] % 4
                vo_ctr[0] += 1
                ov, iv = vo_views(vo[vi], bk)
                kb.op("act", [bb], [Bvo[vi]], lambda e, ov=ov, iv=iv: e.copy(out=ov, in_=iv))
                kb.dma("sp", [Bvo[vi]], [], Vm_d[g * 4 + i, :, :], vo[vi][:], pwrites=[B_vm])
        kb.barrier()
    if stages <= 1:
        _finish(nc, kb, out_d, x_d)
        return

    def attention(tag, kT_src, qT_src, V_src, per_pair_kq, krows, scale, chunk0, dilated, Bk_src, Bq_src, Bv_src):
        with ExitStack() as sa:
            A = lambda n, sh, dt: sa.enter_context(nc.sbuf_tensor(tag + "_" + n, sh, dt))
            Vp = [A("Vp%d" % i, [128, 32, 192], BF16) for i in range(2)]
            Kt = [A("Kt%d" % i, [128, S], BF16) for i in range(2)]
            Qt = [A("Qt%d" % i, [128, S], BF16) for i in range(2)]
            pte = [A("pte%d" % i, [128, 1024], BF16) for i in range(3)]
            ptm = [A("ptm%d" % i, [128, 1024], BF16) for i in range(3)] if dilated else None
            ao = [A("ao%d" % i, [128, S], BF16) for i in range(2)]
            rec = [A("rec%d" % i, [128, 512], F32) for i in range(2)]
            BVp, BKt, BQt = [Buf(), Buf()], [Buf(), Buf()], [Buf(), Buf()]
            Bpte, Bptm = [Buf() for _ in range(3)], [Buf() for _ in range(3)]
            Bao, Brec = [Buf(), Buf()], [Buf(), Buf()]
            SB = [Buf("S%d" % i, excl=True) for i in range(3)]
            AB = [Buf("acc%d" % i, excl=True) for i in range(2)]
            Sap = [ps[:, i * 1024:(i + 1) * 1024] for i in range(3)]
            Aap = [ps[:, 3072 + i * 512: 3072 + (i + 1) * 512] for i in range(2)]
            Bzk = Buf()
            if per_pair_kq:
                KtB = [A("KtB%d" % i, [128, S], BF16) for i in range(2)]
                for i in range(2):
                    kb.op("dve", [], [], lambda e, i=i: e.memset(Kt[i][64:128, :], 0.0), pwrites=[Bzk])
                    kb.op("dve", [], [], lambda e, i=i: e.memset(KtB[i][0:64, :], 0.0), pwrites=[Bzk])
            if dilated:
                toep_st = A("toep", [128, MW], F32)
                multb = A("multb", [128, MW], BF16)
                master = [A("master%d" % i, [128, MW], BF16) for i in range(2)]
                Btoep, Bmult, Bmaster = Buf(), Buf(), [Buf(), Buf()]
                kb.dma("pool", [], [Bmult], multb[:], mult_d[:, :])

            def load_pair(hp):
                for j in range(4):
                    kb.dma("sp", [Bv_src], [], Vp[hp % 2][:, j * 8:(j + 1) * 8, :],
                           V_src[j * 8:(j + 1) * 8, :, hp * 192:(hp + 1) * 192].rearrange("t p c -> p t c"),
                           pwrites=[BVp[hp % 2]])

            def load_kq(u):
                if per_pair_kq:
                    kb.dma("sp", [Bk_src, Bzk], [BKt[u % 2]], Kt[u % 2][0:64, :], kT_src[u, 0:64, :])
                    kb.dma("sp", [Bk_src, Bzk], [], KtB[u % 2][64:128, :], kT_src[u, 64:128, :], pwrites=[BKt[u % 2]])
                    kb.dma("sp", [Bq_src], [BQt[u % 2]], Qt[u % 2][:, :], qT_src[u, :, :])
                else:
                    kb.dma("sp", [Bk_src], [BKt[u % 2]], Kt[u % 2][0:krows, :], kT_src[u, :, :])
                    kb.dma("sp", [Bq_src], [BQt[u % 2]], Qt[u % 2][0:krows, :], qT_src[u, :, :])

            def prep_mask(h):
                kb.dma("sp", [], [Btoep], toep_st[:], toep_d[h, :, :])
                kb.op("act", [Btoep], [Btoep], lambda e: e.activation(out=toep_st[:], in_=toep_st[:], func=AF.Exp))
                kb.op("dve", [Btoep, Bmult], [Bmaster[h % 2]], lambda e: e.tensor_tensor(
                    out=master[h % 2][:], in0=toep_st[:], in1=multb[:], op=ALU.mult))

            items = []
            for h in range(8):
                hp, hh = h // 2, h % 2
                for Q in range(8):
                    if dilated:
                        Ts = list(range(max(0, 4 * Q - 8), min(31, 4 * Q + 11) + 1))
                    else:
                        Ts = list(range(32))
                    groups = []
                    n = 0
                    while n < len(Ts):
                        if n + 1 < len(Ts):
                            groups.append([Ts[n + 1], Ts[n]])
                            n += 2
                        else:
                            groups.append([Ts[n]])
                            n += 1
                    for gi, gT in enumerate(groups):
                        items.append(dict(h=h, hp=hp, hh=hh, Q=Q, Ts=gT, first=(gi == 0), last=(gi == len(groups) - 1),
                                          hq=h * 8 + Q))
            N = len(items)
            for i, it in enumerate(items):
                it["i"] = i

            def kq_unit(it):
                return it["hp"] if per_pair_kq else it["h"]

            def rows_of(it):
                if per_pair_kq:
                    return it["hh"] * 64, it["hh"] * 64 + 64
                return 0, krows

            def emit_qk(it):
                i = it["i"]
                u = kq_unit(it)
                r0, r1 = rows_of(it)
                K_, Q_ = Kt[u % 2], Qt[u % 2]
                if per_pair_kq:
                    r0, r1 = 0, 128
                    if it["hh"] == 1:
                        K_ = KtB[u % 2]

                def f(e):
                    for j, T in enumerate(it["Ts"]):
                        ins = e.matmul(Sap[i % 3][:, j * 512:(j + 1) * 512], K_[r0:r1, T * 128:(T + 1) * 128],
                                       Q_[r0:r1, it["Q"] * 512:(it["Q"] + 1) * 512], start=True, stop=True)
                    return ins
                kb.op("pe", [BKt[u % 2], BQt[u % 2]], [SB[i % 3]], f)

            def emit_exp(it):
                i = it["i"]
                w = 512 * len(it["Ts"])
                kb.op("act", [SB[i % 3]], [Bpte[i % 3]], lambda e: e.activation(
                    out=pte[i % 3][:, 0:w], in_=Sap[i % 3][:, 0:w], func=AF.Exp, scale=scale))

            def emit_mask(it):
                i = it["i"]
                n = len(it["Ts"])
                h = it["h"]
                c0 = C0 - 128 * it["Ts"][0] + 512 * it["Q"]
                m = master[h % 2]
                in1 = AP(m, c0, [pdim(m[:]), [128, n], [1, 512]])
                o = AP(ptm[i % 3], 0, [pdim(ptm[i % 3][:]), [512, n], [1, 512]])
                i0 = AP(pte[i % 3], 0, [pdim(pte[i % 3][:]), [512, n], [1, 512]])
                kb.op("dve", [Bpte[i % 3], Bmaster[h % 2]], [Bptm[i % 3]], lambda e: e.tensor_tensor(
                    out=o, in0=i0, in1=in1, op=ALU.mult))

            def emit_pv(it):
                i = it["i"]
                P_, BP_ = (ptm[i % 3], Bptm[i % 3]) if dilated else (pte[i % 3], Bpte[i % 3])
                acc = Aap[it["hq"] % 2]
                V_ = Vp[it["hp"] % 2]
                vc = 64 * it["hh"]
                nT = len(it["Ts"])

                def f(e):
                    for j, T in enumerate(it["Ts"]):
                        ins = e.matmul(acc, V_[:, T, vc:vc + 128], P_[:, j * 512:(j + 1) * 512],
                                       start=(it["first"] and j == 0), stop=(it["last"] and j == nT - 1))
                    return ins
                if it["first"]:
                    kb.op("pe", [BP_, BVp[it["hp"] % 2]], [AB[it["hq"] % 2]], f)
                else:
                    kb.op("pe", [BP_, BVp[it["hp"] % 2]], [], f, pwrites=[AB[it["hq"] % 2]])

            def emit_fin(it):
                a = it["hq"] % 2
                acc = Aap[a]
                hh, hp, Q = it["hh"], it["hp"], it["Q"]
                nr = slice(64 * hh, 64 * hh + 64)
                dr = slice(64 * (1 - hh), 64 * (1 - hh) + 64)
                if dilated:
                    kb.op("act", [AB[a]], [Brec[a]], lambda e: e.activation(out=rec[a][nr, :], in_=acc[dr, :], func=AF.Ln))
                    kb.op("act", [Brec[a]], [Brec[a]], lambda e: e.activation(out=rec[a][nr, :], in_=rec[a][nr, :], func=AF.Exp, scale=-1.0))
                else:
                    kb.op("dve", [AB[a]], [Brec[a]], lambda e: e.reciprocal(out=rec[a][nr, :], in_=acc[dr, :]))
                first_of_pair = (hh == 0 and Q == 0)
                kb.op("dve", [AB[a], Brec[a]], [Bao[hp % 2]] if first_of_pair else [], lambda e: e.tensor_tensor(
                    out=ao[hp % 2][nr, Q * 512:(Q + 1) * 512], in0=acc[nr, :], in1=rec[a][nr, :], op=ALU.mult),
                    pwrites=[] if first_of_pair else [Bao[hp % 2]])
                if hh == 1 and Q == 7:
                    kb.dma("sp", [Bao[hp % 2]], [], attnT_d[chunk0 + hp, :, :], ao[hp % 2][:, :], pwrites=[B_attn])

            def unit_start(it):
                h = it["h"]
                if it["Q"] == 0 and it["first"]:
                    if h + 1 < 8:
                        if per_pair_kq:
                            if (h + 1) % 2 == 0:
                                load_kq((h + 1) // 2)
                        else:
                            load_kq(h + 1)
                        if (h + 1) % 2 == 0:
                            load_pair((h + 1) // 2)
                        if dilated:
                            prep_mask(h + 1)

            load_pair(0)
            load_kq(0)
            if dilated:
                prep_mask(0)
            emit_qk(items[0])
            emit_exp(items[0])
            if dilated:
                emit_mask(items[0])
            if N > 1:
                emit_qk(items[1])
            pend = []
            for i in range(N):
                it = items[i]
                unit_start(it)
                if i + 1 < N:
                    emit_exp(items[i + 1])
                    if dilated:
                        emit_mask(items[i + 1])
                if i + 2 < N:
                    emit_qk(items[i + 2])
                emit_pv(it)
                if it["last"]:
                    pend.append((i, it))
                while pend and pend[0][0] + 3 <= i:
                    emit_fin(pend.pop(0)[1])
            while pend:
                emit_fin(pend.pop(0)[1])
            kb.barrier()

    run_dil = os.environ.get("SKIPDIL", "") == ""
    run_mla = os.environ.get("SKIPMLA", "") == ""
    if run_dil:
        attention("dil", dkT_d, dqT_d, Vd_d, True, 64, 64.0 ** -0.5, 4, True, B_dk, B_dq, B_vd)
    if run_mla:
        attention("mla", kT_d, qT_d, Vm_d, False, 96, 96.0 ** -0.5, 0, False, B_k, B_q, B_vm)
    if stages <= 3:
        _finish(nc, kb, out_d, x_d)
        return

    def bank2():
        if bank_ctr[0] % 2:
            bank_ctr[0] += 1
        i = bank_ctr[0] % 8
        bank_ctr[0] += 2
        return ps[:, i * 512:(i + 2) * 512], [PB[i], PB[i + 1]]

    class LNState:
        def __init__(self, A, tag, nb=2):
            self.junk = A(tag + "junk", [128, D], F32)
            self.st = [A(tag + "st%d" % i, [128, 8], F32) for i in range(nb)]
            self.Bjunk = Buf()
            self.Bst = [Buf() for _ in range(nb)]
            self.nb = nb
            self.n = 0

    def ln_stats(L, y, By):
        i = L.n % L.nb
        L.n += 1
        st, Bst = L.st[i], L.Bst[i]
        kb.op("dve", [], [Bst], lambda e: e.memset(st[:], 0.0))
        kb.op("act", [By, Bst], [L.Bjunk], lambda e: e.activation(out=L.junk[:], in_=y[:], func=AF.Identity, accum_out=st[:, 0:1]),
              pwrites=[Bst])
        kb.op("act", [By, Bst], [L.Bjunk], lambda e: e.activation(out=L.junk[:], in_=y[:], func=AF.Square, accum_out=st[:, 1:2]),
              pwrites=[Bst])
        return st, Bst

    def ln_apply(st, Bst, y, By, xh, Bxh):
        kb.op("dve", [Bst], [], lambda e: e.tensor_scalar(out=st[:, 2:3], in0=st[:, 0:1], scalar1=1.0 / D, scalar2=None, op0=ALU.mult), pwrites=[Bst])
        kb.op("dve", [Bst], [], lambda e: e.tensor_tensor(out=st[:, 3:4], in0=st[:, 2:3], in1=st[:, 2:3], op=ALU.mult), pwrites=[Bst])
        kb.op("dve", [Bst], [], lambda e: e.scalar_tensor_tensor(out=st[:, 4:5], in0=st[:, 1:2], scalar=1.0 / D, in1=st[:, 3:4],
                                                                   op0=ALU.mult, op1=ALU.subtract), pwrites=[Bst])
        kb.op("dve", [Bst], [], lambda e: e.tensor_scalar(out=st[:, 4:5], in0=st[:, 4:5], scalar1=EPS, scalar2=None, op0=ALU.add), pwrites=[Bst])
        kb.op("act", [Bst], [], lambda e: e.activation(out=st[:, 5:6], in_=st[:, 4:5], func=AF.Ln), pwrites=[Bst])
        kb.op("act", [Bst], [], lambda e: e.activation(out=st[:, 5:6], in_=st[:, 5:6], func=AF.Exp, scale=-0.5), pwrites=[Bst])
        kb.op("dve", [Bst], [], lambda e: e.scalar_tensor_tensor(out=st[:, 6:7], in0=st[:, 2:3], scalar=-1.0, in1=st[:, 5:6],
                                                                   op0=ALU.mult, op1=ALU.mult), pwrites=[Bst])
        kb.op("act", [By, Bst], [Bxh], lambda e: e.activation(out=xh[:], in_=y[:], func=AF.Identity, scale=st[:, 5:6], bias=st[:, 6:7]))

    with ExitStack() as s4:
        A = lambda n, sh, dt: s4.enter_context(nc.sbuf_tensor("p4_" + n, sh, dt))
        wout = A("wout", [128, 8, D], BF16)
        g1bc, lngbc, lnbbc, sc2bc, sh2bc = (A(n, [128, D], F32) for n in ("g1bc", "lngbc", "lnbbc", "sc2bc", "sh2bc"))
        wr = A("wr", [128, 8, NE], F32)
        ident = A("ident", [128, 128], F32)
        at = [A("at%d" % i, [128, 8, 512], BF16) for i in range(2)]
        xt = [A("xt%d" % i, [128, D], F32) for i in range(2)]
        t1 = [A("t1%d" % i, [128, D], F32) for i in range(2)]
        yy = [A("y%d" % i, [128, D], F32) for i in range(2)]
        xh = [A("xh%d" % i, [128, D], F32) for i in range(2)]
        x1 = [A("x1%d" % i, [128, D], F32) for i in range(2)]
        h2f = [A("h2f%d" % i, [128, D], F32) for i in range(2)]
        h2b = [A("h2b%d" % i, [128, D], BF16) for i in range(2)]
        h2T = [A("h2T%d" % i, [128, 8, 128], F32) for i in range(2)]
        sm = [A("sm%d" % i, [128, 8], F32) for i in range(2)]
        ee = [A("ee%d" % i, [128, NE], F32) for i in range(2)]
        L = LNState(A, "ln", 3)
        Bc4 = Buf()
        Bat, Bxt, Bt1, By, Bxh, Bx1, Bh2f, Bh2b, Bh2T, Bsm, Bee = ([Buf(), Buf()] for _ in range(11))
        kb.dma("sp", [B_mod], [], g1bc[:], bcast_row(mod_d[0:1, 2048:3072], D), pwrites=[Bc4])
        Bws = [Buf(), Buf()]
        for c in range(8):
            kb.dma("sp", [], [Bws[c % 2]], t1[c % 2][:], wout_d[c * 128:(c + 1) * 128, :])
            kb.op("dve", [Bws[c % 2], Bc4], [], lambda e, c=c: e.tensor_tensor(out=wout[:, c, :], in0=t1[c % 2][:], in1=g1bc[:], op=ALU.mult),
                  pwrites=[Bc4])
        kb.dma("sp", [B_mod], [], sh2bc[:], bcast_row(mod_d[0:1, 3072:4096], D), pwrites=[Bc4])
        kb.dma("sp", [B_mod], [], sc2bc[:], bcast_row(mod_d[0:1, 4096:5120], D), pwrites=[Bc4])
        kb.dma("sp", [], [], lngbc[:], bcast_row(ln1g_d[0:1, :], D), pwrites=[Bc4])
        kb.dma("sp", [], [], lnbbc[:], bcast_row(ln1b_d[0:1, :], D), pwrites=[Bc4])
        kb.dma("sp", [], [], wr[:], wr_d.rearrange("(k p) e -> p k e", p=128), pwrites=[Bc4])
        kb.dma("sp", [], [], ident[:], ident_d[:, :], pwrites=[Bc4])
        kb.op("dve", [Bc4], [], lambda e: e.tensor_scalar(out=sc2bc[:], in0=sc2bc[:], scalar1=1.0, scalar2=None, op0=ALU.add), pwrites=[Bc4])
        attn_v = attnT_d.rearrange("c p n -> p c n")
        x_t = x_d.rearrange("(t p) d -> t p d", p=128)
        x1_t = x1_d.rearrange("(t p) d -> t p d", p=128)
        h2_t = h2_d.rearrange("(t p) d -> t p d", p=128)
        stA = {}
        stC = {}

        mixb = {}

        def A_mix(t):
            g, i = t // 4, t % 4
            if i == 0:
                kb.dma("sp", [B_attn], [Bat[g % 2]], at[g % 2][:], attn_v[:, :, g * 512:(g + 1) * 512])
            b = t % 2
            kb.dma("sp", [], [Bxt[b]], xt[b][:], x_t[t])
            bk2, bb2 = bank2()

            def mm(e):
                for half in range(2):
                    for c in range(8):
                        ins = e.matmul(bk2[:, half * 512:(half + 1) * 512], at[g % 2][:, c, i * 128:(i + 1) * 128],
                                       wout[:, c, half * 512:(half + 1) * 512], start=(c == 0), stop=(c == 7))
                return ins
            kb.op("pe", [Bat[g % 2], Bc4], bb2, mm)
            mixb[t] = (bk2, bb2)

        def A_y(t):
            b = t % 2
            bk2, bb2 = mixb.pop(t)
            kb.op("dve", bb2 + [Bxt[b]] + Bws, [By[b]], lambda e: e.scalar_tensor_tensor(
                out=yy[b][:], in0=xt[b][:], scalar=ALPHA, in1=bk2, op0=ALU.mult, op1=ALU.add))

        def A_stats(t):
            b = t % 2
            stA[t] = ln_stats(L, yy[b], By[b])

        def B1_smalls(t):
            st, Bst = stA[t]
            kb.op("dve", [Bst], [], lambda e: e.tensor_scalar(out=st[:, 2:3], in0=st[:, 0:1], scalar1=1.0 / D, scalar2=None, op0=ALU.mult), pwrites=[Bst])
            kb.op("dve", [Bst], [], lambda e: e.tensor_tensor(out=st[:, 3:4], in0=st[:, 2:3], in1=st[:, 2:3], op=ALU.mult), pwrites=[Bst])
            kb.op("dve", [Bst], [], lambda e: e.scalar_tensor_tensor(out=st[:, 4:5], in0=st[:, 1:2], scalar=1.0 / D, in1=st[:, 3:4],
                                                                       op0=ALU.mult, op1=ALU.subtract), pwrites=[Bst])
            kb.op("dve", [Bst], [], lambda e: e.tensor_scalar(out=st[:, 4:5], in0=st[:, 4:5], scalar1=EPS, scalar2=None, op0=ALU.add), pwrites=[Bst])

        def B1_lnexp(t):
            st, Bst = stA[t]
            kb.op("act", [Bst], [], lambda e: e.activation(out=st[:, 5:6], in_=st[:, 4:5], func=AF.Ln), pwrites=[Bst])
            kb.op("act", [Bst], [], lambda e: e.activation(out=st[:, 5:6], in_=st[:, 5:6], func=AF.Exp, scale=-0.5), pwrites=[Bst])

        def B1_nmr(t):
            st, Bst = stA[t]
            kb.op("dve", [Bst], [], lambda e: e.scalar_tensor_tensor(out=st[:, 6:7], in0=st[:, 2:3], scalar=-1.0, in1=st[:, 5:6],
                                                                       op0=ALU.mult, op1=ALU.mult), pwrites=[Bst])

        def B1_xh(t):
            b = t % 2
            st, Bst = stA.pop(t)
            kb.op("act", [By[b], Bst], [Bxh[b]], lambda e: e.activation(out=xh[b][:], in_=yy[b][:], func=AF.Identity,
                                                                        scale=st[:, 5:6], bias=st[:, 6:7]))

        def B2_dve(t):
            b = t % 2
            kb.op("dve", [Bxh[b], Bc4], [Bx1[b]], lambda e: e.tensor_tensor(out=x1[b][:], in0=xh[b][:], in1=lngbc[:], op=ALU.mult))
            kb.op("dve", [Bx1[b], Bc4], [Bx1[b]], lambda e: e.tensor_tensor(out=x1[b][:], in0=x1[b][:], in1=lnbbc[:], op=ALU.add))
            kb.dma("sp", [Bx1[b]], [], x1_t[t], x1[b][:], pwrites=[B_x1])
            kb.op("dve", [Bx1[b], Bc4], [Bh2f[b]], lambda e: e.tensor_tensor(out=h2f[b][:], in0=x1[b][:], in1=sc2bc[:], op=ALU.mult))
            kb.op("dve", [Bh2f[b], Bc4], [Bh2f[b]], lambda e: e.tensor_tensor(out=h2f[b][:], in0=h2f[b][:], in1=sh2bc[:], op=ALU.add))

        def B2_cast(t):
            b = t % 2
            kb.op("act", [Bh2f[b]], [Bh2b[b]], lambda e: e.copy(out=h2b[b][:], in_=h2f[b][:]))
            kb.dma("sp", [Bh2b[b]], [], h2_t[t], h2b[b][:], pwrites=[B_h2])

        def B2_tp(t):
            b = t % 2
            bk2, bb2 = bank2()

            def tp(e):
                for k in range(8):
                    ins = e.transpose(bk2[:, k * 128:(k + 1) * 128], h2f[b][:, k * 128:(k + 1) * 128], ident[:])
                return ins
            kb.op("pe", [Bh2f[b], Bc4], bb2, tp)
            stC[t] = (bk2, bb2)

        lgb = {}

        def C_copy(t):
            b = t % 2
            bk2, bb2 = stC.pop(t)
            kb.op("act", bb2, [Bh2T[b]], lambda e: e.copy(out=h2T[b][:].rearrange("p k n -> p (k n)"), in_=bk2))

        def C_logits(t):
            b = t % 2
            bk, bb = bank()

            def lg(e):
                for k in range(8):
                    ins = e.matmul(bk[:, 0:NE], h2T[b][:, k, :], wr[:, k, :], start=(k == 0), stop=(k == 7))
                return ins
            kb.op("pe", [Bh2T[b], Bc4], [bb], lg)
            lgb[t] = (bk, bb)

        def C_max(t):
            b = t % 2
            bk, bb = lgb[t]
            kb.op("dve", [], [Bsm[b]], lambda e: e.memset(sm[b][:], 0.0))
            kb.op("dve", [bb], [], lambda e: e.reduce_max(out=sm[b][:, 0:1], in_=bk[:, 0:NE], axis=AX.X), pwrites=[Bsm[b]])
            kb.op("dve", [Bsm[b]], [], lambda e: e.tensor_scalar(out=sm[b][:, 1:2], in0=sm[b][:, 0:1], scalar1=-1.0, scalar2=None, op0=ALU.mult),
                  pwrites=[Bsm[b]])

        def C_exp(t):
            b = t % 2
            bk, bb = lgb.pop(t)
            kb.op("act", [bb, Bsm[b]], [Bee[b]], lambda e: e.activation(out=ee[b][:], in_=bk[:, 0:NE], func=AF.Exp, bias=sm[b][:, 1:2],
                                                                       accum_out=sm[b][:, 2:3]), pwrites=[Bsm[b]])

        def C_aff(t):
            b = t % 2
            kb.op("dve", [Bsm[b]], [], lambda e: e.reciprocal(out=sm[b][:, 3:4], in_=sm[b][:, 2:3]), pwrites=[Bsm[b]])
            kb.op("dve", [Bee[b], Bsm[b]], [], lambda e: e.tensor_scalar(out=aff_all[:, t * NE:(t + 1) * NE], in0=ee[b][:], scalar1=sm[b][:, 3:4],
                                                                        scalar2=None, op0=ALU.mult), pwrites=[B_aff])

        for step in range(NT + 3):
            a, b1, b2, c = step, step - 1, step - 2, step - 3
            ok = lambda t: 0 <= t < NT
            if ok(a):
                A_mix(a)
            if ok(c):
                C_copy(c)
            if ok(b2):
                B2_dve(b2)
            if ok(c):
                C_logits(c)
            if ok(a):
                A_y(a)
            if ok(b2):
                B2_cast(b2)
                B2_tp(b2)
            if ok(b1):
                B1_smalls(b1)
            if ok(a):
                A_stats(a)
            if ok(c):
                C_max(c)
            if ok(b1):
                B1_lnexp(b1)
            if ok(c):
                C_exp(c)
            if ok(b1):
                B1_nmr(b1)
            if ok(c):
                C_aff(c)
            if ok(b1):
                B1_xh(b1)
        if "aff_s" in dbg:
            kb.dma("sp", [B_aff], [Buf()], aff_d[:, :], aff_all[:])
        kb.barrier()
    if stages <= 4:
        _finish(nc, kb, out_d, x_d)
        return

    def moe_phase():
        with ExitStack() as s5:
            A = lambda n, sh, dt: s5.enter_context(nc.sbuf_tensor("p5_" + n, sh, dt))
            onesf = A("onesf", [128, 128], F32)
            onesb = A("onesb", [128, 128], BF16)
            triub = A("triub", [128, 128], BF16)
            identb = A("identb", [128, 128], BF16)
            iota = A("iota", [128, 512], F32)
            tokhl = A("tokhl", [128, NT, 2], F32)
            zero = A("zero", [128, D], F32)
            lo = A("lo", [128, NE], F32)
            tau = A("tau", [128, NE], F32)
            part = A("part", [128, NE], F32)
            gw = A("gw", [128, NE], F32)
            cmp = A("cmp", [128, NT * NE], F32)
            self_ = A("self", [128, NT * NE], F32)
            selb = A("selb", [128, NT * NE], BF16)
            posm = A("posm", [128, NT * NE], F32)
            r1 = A("r1", [128, NT * NE], F32)
            R = A("R", [128, NT * NE * 5], BF16)
            OH = [A("OH%d" % i, [128, 512], BF16) for i in range(4)]
            idxv = [A("idxv%d" % i, [128, 32], F32) for i in range(2)]
            idxf = [A("idxf%d" % i, [128, 4], F32) for i in range(2)]
            idxi = [A("idxi%d" % i, [128, 4], I32) for i in range(2)]
            val = [A("val%d" % i, [128, 4], F32) for i in range(2)]
            xin = [A("xin%d" % i, [128, 4, D], BF16) for i in range(2)]
            xinT = [A("xinT%d" % i, [128, 8, 512], BF16) for i in range(2)]
            wg = [A("wg%d" % i, [128, 8, 512], BF16) for i in range(3)]
            wu = [A("wu%d" % i, [128, 8, 512], BF16) for i in range(3)]
            wd = [A("wd%d" % i, [128, 6, D], BF16) for i in range(3)]
            sg = [A("sg%d" % i, [128, 512], F32) for i in range(2)]
            act = [A("act%d" % i, [128, 12, 512], BF16) for i in range(2)]
            ys = [A("ys%d" % i, [128, D], F32) for i in range(4)]
            mo = [A("mo%d" % i, [128, D], F32) for i in range(4)]
            Bc = Buf()
            Blo, Btau, Bpart, Bgw, Bcmp, Bsel, Bposm, BR = (Buf() for _ in range(8))
            BOH = [Buf() for _ in range(4)]
            Bidxv, Bidx, Bval = [Buf(), Buf()], [Buf(), Buf()], [Buf(), Buf()]
            Bxin, BxinT = [Buf(), Buf()], [Buf(), Buf()]
            Bwg, Bwu, Bwd = [Buf() for _ in range(3)], [Buf() for _ in range(3)], [Buf() for _ in range(3)]
            Bsg, Bact = [Buf(), Buf()], [Buf(), Buf()]
            Bys, Bmo = [Buf() for _ in range(4)], [Buf() for _ in range(4)]

            kb.op("pool", [], [], lambda e: e.memset(onesf[:], 1.0), pwrites=[Bc])
            kb.op("pool", [], [], lambda e: e.memset(onesb[:], 1.0), pwrites=[Bc])
            kb.op("pool", [], [], lambda e: e.memset(zero[:], 0.0), pwrites=[Bc])
            kb.op("pool", [], [Blo], lambda e: e.memset(lo[:], 0.0))
            kb.dma("pool", [], [], triub[:], triu_d[:, :], pwrites=[Bc])
            kb.dma("pool", [], [], identb[:], ident_d[:, :], pwrites=[Bc])
            kb.dma("sp", [], [], iota[:], iota_d[:, :], pwrites=[Bc])
            kb.dma("sp", [], [], tokhl[:], tokhl_d[:, :, :], pwrites=[Bc])
            moe_t = moe_d.rearrange("(t p) d -> t p d", p=128)
            for t in range(NT):
                kb.dma("sp", [Bc], [], moe_t[t], zero[:], pwrites=[B_moe])

            def issue_gu(n):
                if n >= NE * 3:
                    return
                e, fb = n // 3, n % 3
                b = n % 3
                kb.dma("pool", [], [Bwg[b]], wg[b][:], wg_d[e, :, fb * 512:(fb + 1) * 512].rearrange("(k p) f -> p k f", p=128))
                kb.dma("pool", [], [Bwu[b]], wu[b][:], wu_d[e, :, fb * 512:(fb + 1) * 512].rearrange("(k p) f -> p k f", p=128))

            def issue_d(m):
                if m >= NE * 2:
                    return
                e, half = m // 2, m % 2
                b = m % 3
                kb.dma("pool", [], [Bwd[b]], wd[b][:], wd_d[e, half * 768:(half + 1) * 768, :].rearrange("(k p) n -> p k n", p=128))

            issue_gu(0)
            issue_gu(1)
            issue_d(0)
            issue_d(1)
            issue_d(2)

            aff3 = AP(aff_all, 0, [pdim(aff_all[:]), [NE, NT], [1, NE]])
            cmp3 = AP(cmp, 0, [pdim(cmp[:]), [NE, NT], [1, NE]])
            cmpT = AP(cmp, 0, [pdim(cmp[:]), [1, NE], [NE, NT]])

            def bc16(tile_):
                return AP(tile_, 0, [pdim(tile_[:]), [0, NT], [1, NE]])

            for it in range(1, NBIS + 1):
                w = 2.0 ** (-it)
                kb.op("dve", [Blo], [Btau], lambda e: e.tensor_scalar(out=tau[:], in0=lo[:], scalar1=w, scalar2=None, op0=ALU.add))
                kb.op("dve", [B_aff, Btau], [Bcmp], lambda e: e.tensor_tensor(out=cmp3, in0=aff3, in1=bc16(tau), op=ALU.is_ge))
                kb.op("dve", [Bcmp], [Bpart], lambda e: e.tensor_reduce(out=part[:], in_=cmpT, axis=AX.X, op=ALU.add))
                bk, bb = bank()
                kb.op("pe", [Bpart, Bc], [bb], lambda e: e.matmul(bk[:, 0:NE], onesf[:], part[:], start=True, stop=True))
                kb.op("dve", [bb], [Bgw], lambda e: e.tensor_scalar(out=gw[:], in0=bk[:, 0:NE], scalar1=CAP - 0.5, scalar2=w,
                                                                  op0=ALU.is_ge, op1=ALU.mult))
                kb.op("dve", [Bgw, Blo], [Blo], lambda e: e.tensor_tensor(out=lo[:], in0=lo[:], in1=gw[:], op=ALU.add))
            self3 = AP(self_, 0, [pdim(self_[:]), [NE, NT], [1, NE]])
            kb.op("dve", [B_aff, Blo], [Bsel], lambda e: e.tensor_tensor(out=self3, in0=aff3, in1=bc16(lo), op=ALU.is_ge))
            kb.op("dve", [Bsel], [], lambda e: e.tensor_copy(out=selb[:], in_=self_[:]), pwrites=[Bsel])
            bk, bb = bank()

            def prefix(e):
                for t in range(NT):
                    o = bk[:, t * NE:(t + 1) * NE]
                    for tp in range(t):
                        e.matmul(o, onesb[:], selb[:, tp * NE:(tp + 1) * NE], start=(tp == 0), stop=False)
                    ins = e.matmul(o, triub[:], selb[:, t * NE:(t + 1) * NE], start=(t == 0), stop=True)
                return ins
            kb.op("pe", [Bsel, Bc], [bb], prefix)
            kb.op("dve", [bb, Bsel], [Bposm], lambda e: e.scalar_tensor_tensor(out=posm[:], in0=bk, scalar=1.0, in1=self_[:],
                                                                            op0=ALU.add, op1=ALU.mult))
            kb.op("dve", [Bposm], [Bposm], lambda e: e.tensor_scalar(out=posm[:], in0=posm[:], scalar1=-1.0, scalar2=None, op0=ALU.add))
            Rv = lambda j: AP(R, j, [pdim(R[:]), [5, NT * NE]])
            kb.op("dve", [Bc], [BR], lambda e: e.tensor_copy(
                out=AP(R, 0, [pdim(R[:]), [5 * NE, NT], [5, NE], [1, 2]]),
                in_=AP(tokhl, 0, [pdim(tokhl[:]), [2, NT], [0, NE], [1, 2]])))
            kb.op("dve", [B_aff], [], lambda e: e.tensor_copy(out=Rv(2), in_=aff_all[:]), pwrites=[BR])
            kb.op("dve", [B_aff, BR], [Bcmp], lambda e: e.tensor_tensor(out=r1[:], in0=aff_all[:], in1=Rv(2), op=ALU.subtract))
            kb.op("dve", [Bcmp], [], lambda e: e.tensor_copy(out=Rv(3), in_=r1[:]), pwrites=[BR])
            kb.op("dve", [Bcmp, BR], [Bcmp], lambda e: e.tensor_tensor(out=r1[:], in0=r1[:], in1=Rv(3), op=ALU.subtract))
            kb.op("dve", [Bcmp], [], lambda e: e.tensor_copy(out=Rv(4), in_=r1[:]), pwrites=[BR])

            h2rows = h2_d
            oh_ctr = [0]

            rstate = {}

            def route_tiles(e, t0, t1):
                for t in range(t0, t1):
                    oi = oh_ctr[0] % 4
                    oh_ctr[0] += 1
                    kb.op("dve", [Bposm, Bc], [BOH[oi]], lambda en: en.tensor_scalar(
                        out=OH[oi][:], in0=iota[:], scalar1=posm[:, t * NE + e:t * NE + e + 1], scalar2=None, op0=ALU.is_equal))

                    def mm(en):
                        for cj in range(4):
                            c0 = 3072 + (cj * NT + t) * 8
                            ins = en.matmul(ps[:, c0:c0 + 5], OH[oi][:, cj * 128:(cj + 1) * 128],
                                            R[:, (t * NE + e) * 5:(t * NE + e) * 5 + 5], start=True, stop=True)
                        return ins
                    if t == 0:
                        kb.op("pe", [BOH[oi], BR], [PB[6], PB[7]], mm)
                    else:
                        kb.op("pe", [BOH[oi], BR], [], mm, pwrites=[PB[6], PB[7]])

            def route_idx(e):
                b = e % 2
                kb.op("dve", [PB[6], PB[7]], [Bidxv[b]], lambda en: en.tensor_reduce(
                    out=AP(idxv[b], 0, [pdim(idxv[b][:]), [8, 4], [1, 5]]),
                    in_=AP(ps, 3072, [pdim(ps[:]), [NT * 8, 4], [1, 5], [8, NT]]),
                    axis=AX.X, op=ALU.add))
                v3 = lambda j: AP(idxv[b], j, [pdim(idxv[b][:]), [8, 4]])
                kb.op("dve", [Bidxv[b]], [Bidx[b]], lambda en: en.scalar_tensor_tensor(
                    out=idxf[b][:], in0=v3(0), scalar=128.0, in1=v3(1), op0=ALU.mult, op1=ALU.add))
                kb.op("dve", [Bidx[b]], [], lambda en: en.tensor_copy(out=idxi[b][:], in_=idxf[b][:]), pwrites=[Bidx[b]])
                kb.op("dve", [Bidxv[b]], [Bval[b]], lambda en: en.tensor_tensor(out=val[b][:], in0=v3(2), in1=v3(3), op=ALU.add))
                kb.op("dve", [Bidxv[b], Bval[b]], [Bval[b]], lambda en: en.tensor_tensor(out=val[b][:], in0=val[b][:], in1=v3(4), op=ALU.add))
                for cj in range(4):
                    kb.dma("pool", [Bidx[b], B_h2], [] if cj else [Bxin[b]], xin[b][:, cj, :], h2rows[:, :],
                           pwrites=[Bxin[b]] if cj else [],
                           indirect=dict(out_offset=None, in_offset=bass.IndirectOffsetOnAxis(ap=idxi[b][:, cj:cj + 1], axis=0)))

            def route_T(e):
                b = e % 2
                for k in range(8):
                    bk2, bb2 = bank()
                    bkb = bk2.bitcast(BF16)

                    def tp(en):
                        for cj in range(4):
                            ins = en.transpose(bkb[:, cj * 128:(cj + 1) * 128], xin[b][:, cj, k * 128:(k + 1) * 128], identb[:])
                        return ins
                    kb.op("pe", [Bxin[b], Bc], [bb2], tp)
                    kb.op("act", [bb2], [] if k else [BxinT[b]],
                          (lambda en: en.copy(out=xinT[b][:, k, :], in_=bkb[:, 0:512])),
                          pwrites=[BxinT[b]] if k else [])

            def route(e):
                route_tiles(e, 0, NT)
                route_idx(e)
                route_T(e)

            def tail_gather(e):
                b = e % 2
                for cj in range(4):
                    kb.dma("pool", [Bidx[b], B_moe], [Bmo[cj]], mo[cj][:, :], moe_d[:, :],
                           indirect=dict(out_offset=None, in_offset=bass.IndirectOffsetOnAxis(ap=idxi[b][:, cj:cj + 1], axis=0)))

            def tail_add_scatter(e, cjs=(0, 1, 2, 3)):
                b = e % 2
                for cj in cjs:
                    kb.op("dve", [Bmo[cj], Bys[cj]], [Bmo[cj]], lambda en: en.tensor_tensor(out=mo[cj][:], in0=mo[cj][:], in1=ys[cj][:], op=ALU.add))
                for cj in cjs:
                    kb.dma("pool", [Bidx[b], Bmo[cj]], [], moe_d[:, :], mo[cj][:, :], pwrites=[B_moe],
                           indirect=dict(out_offset=bass.IndirectOffsetOnAxis(ap=idxi[b][:, cj:cj + 1], axis=0), in_offset=None))

            def compute(e):
                b = e % 2
                for fb in range(3):
                    n = e * 3 + fb
                    wb = n % 3
                    for j in range(4):
                        bg, bbg = bank()
                        bu, bbu = bank()

                        def mm(en):
                            for bk_, w_ in ((bg, wg[wb]), (bu, wu[wb])):
                                for k in range(8):
                                    ins = en.matmul(bk_, w_[:, k, j * 128:(j + 1) * 128], xinT[b][:, k, :], start=(k == 0), stop=(k == 7))
                            return ins
                        kb.op("pe", [BxinT[b], Bwg[wb], Bwu[wb]], [bbg, bbu], mm)
                        if e + 1 < NE:
                            if fb * 4 + j < 11:
                                route_tiles(e + 1, 3 * (fb * 4 + j), min(NT, 3 * (fb * 4 + j) + 3))
                            else:
                                route_idx(e + 1)
                        if e > 0 and 2 <= fb * 4 + j < 6:
                            tail_add_scatter(e - 1, (fb * 4 + j - 2,))
                        si = (fb * 4 + j) % 2
                        kb.op("act", [bbg], [Bsg[si]], lambda en: en.activation(out=sg[si][:], in_=bg, func=AF.Silu))
                        fi = fb * 4 + j
                        kb.op("dve", [Bsg[si], bbu], [] if fi else [Bact[b]], lambda en: en.tensor_tensor(
                            out=act[b][:, fi, :], in0=sg[si][:], in1=bu, op=ALU.mult), pwrites=[Bact[b]] if fi else [])
                    issue_gu(n + 2)
                for cj in range(4):
                    yi = cj
                    for half in range(2):
                        bk, bb = bank()

                        def mm(en):
                            for fi in range(12):
                                m = e * 2 + fi // 6
                                ins = en.matmul(bk, act[b][:, fi, cj * 128:(cj + 1) * 128], wd[m % 3][:, fi % 6, half * 512:(half + 1) * 512],
                                                start=(fi == 0), stop=(fi == 11))
                            return ins
                        kb.op("pe", [Bact[b], Bwd[(e * 2) % 3], Bwd[(e * 2 + 1) % 3]], [bb], mm)
                        kb.op("act", [bb, Bval[b]], [] if half else [Bys[yi]], lambda en: en.activation(
                            out=ys[yi][:, half * 512:(half + 1) * 512], in_=bk, func=AF.Copy, scale=val[b][:, cj:cj + 1]),
                            pwrites=[Bys[yi]] if half else [])
                issue_d(e * 2 + 3)
                issue_d(e * 2 + 4)
                if e + 1 < NE:
                    route_T(e + 1)
                tail_gather(e)
                if e == NE - 1:
                    tail_add_scatter(e)

            nrot[0] = 6
            bank_ctr[0] = 0
            route(0)
            for e in range(int(os.environ.get("NEXP", str(NE)))):
                compute(e)
            kb.barrier()
            nrot[0] = 8

    if os.environ.get("SKIPMOE", "") == "":
        moe_phase()

    with ExitStack() as s6:
        A = lambda n, sh, dt: s6.enter_context(nc.sbuf_tensor("p6_" + n, sh, dt))
        g2bc, lngbc, lnbbc = (A(n, [128, D], F32) for n in ("g2bc", "lngbc", "lnbbc"))
        NB6 = 3
        xt = [A("xt%d" % i, [128, D], F32) for i in range(NB6)]
        mt = [A("mt%d" % i, [128, D], F32) for i in range(NB6)]
        yy = [A("y%d" % i, [128, D], F32) for i in range(NB6)]
        xh = [A("xh%d" % i, [128, D], F32) for i in range(NB6)]
        oo = [A("o%d" % i, [128, D], F32) for i in range(NB6)]
        L = LNState(A, "ln", 3)
        Bc6 = Buf()
        Bxt, Bmt, By, Bxh, Boo = ([Buf() for _ in range(NB6)] for _ in range(5))
        kb.dma("sp", [B_mod], [], g2bc[:], bcast_row(mod_d[0:1, 5120:6144], D), pwrites=[Bc6])
        kb.dma("sp", [], [], lngbc[:], bcast_row(ln2g_d[0:1, :], D), pwrites=[Bc6])
        kb.dma("sp", [], [], lnbbc[:], bcast_row(ln2b_d[0:1, :], D), pwrites=[Bc6])
        x1_t = x1_d.rearrange("(t p) d -> t p d", p=128)
        moe_t = moe_d.rearrange("(t p) d -> t p d", p=128)
        out_t = out_d.rearrange("(t p) d -> t p d", p=128)
        stA = {}

        def ln_smalls(st, Bst):
            kb.op("dve", [Bst], [], lambda e: e.tensor_scalar(out=st[:, 2:3], in0=st[:, 0:1], scalar1=1.0 / D, scalar2=None, op0=ALU.mult), pwrites=[Bst])
            kb.op("dve", [Bst], [], lambda e: e.tensor_tensor(out=st[:, 3:4], in0=st[:, 2:3], in1=st[:, 2:3], op=ALU.mult), pwrites=[Bst])
            kb.op("dve", [Bst], [], lambda e: e.scalar_tensor_tensor(out=st[:, 4:5], in0=st[:, 1:2], scalar=1.0 / D, in1=st[:, 3:4],
                                                                       op0=ALU.mult, op1=ALU.subtract), pwrites=[Bst])
            kb.op("dve", [Bst], [], lambda e: e.tensor_scalar(out=st[:, 4:5], in0=st[:, 4:5], scalar1=EPS, scalar2=None, op0=ALU.add), pwrites=[Bst])

        def A6_load(t):
            b = t % NB6
            kb.dma("sp", [B_x1], [Bxt[b]], xt[b][:], x1_t[t])
            kb.dma("sp", [B_moe], [Bmt[b]], mt[b][:], moe_t[t])

        def A6_dve(t):
            b = t % NB6
            kb.op("dve", [Bmt[b], Bc6], [Bmt[b]], lambda e: e.tensor_tensor(out=mt[b][:], in0=mt[b][:], in1=g2bc[:], op=ALU.mult))
            kb.op("dve", [Bxt[b], Bmt[b]], [By[b]], lambda e: e.scalar_tensor_tensor(
                out=yy[b][:], in0=xt[b][:], scalar=ALPHA, in1=mt[b][:], op0=ALU.mult, op1=ALU.add))

        def C6_dve(t):
            b = t % NB6
            kb.op("dve", [Bxh[b], Bc6], [Boo[b]], lambda e: e.tensor_tensor(out=oo[b][:], in0=xh[b][:], in1=lngbc[:], op=ALU.mult))
            kb.op("dve", [Boo[b], Bc6], [Boo[b]], lambda e: e.tensor_tensor(out=oo[b][:], in0=oo[b][:], in1=lnbbc[:], op=ALU.add))
            kb.dma("sp", [Boo[b]], [Buf()], out_t[t], oo[b][:])

        for step in range(NT + 2):
            a, b1, c = step, step - 1, step - 2
            ok = lambda t: 0 <= t < NT
            if ok(a):
                A6_load(a)
            if ok(c):
                C6_dve(c)
            if ok(a):
                A6_dve(a)
            if ok(b1):
                ln_smalls(*stA[b1])
            if ok(a):
                stA[a] = ln_stats(L, yy[a % NB6], By[a % NB6])
            if ok(b1):
                st, Bst = stA.pop(b1)
                bb_ = b1 % NB6
                kb.op("act", [Bst], [], lambda e: e.activation(out=st[:, 5:6], in_=st[:, 4:5], func=AF.Ln), pwrites=[Bst])
                kb.op("act", [Bst], [], lambda e: e.activation(out=st[:, 5:6], in_=st[:, 5:6], func=AF.Exp, scale=-0.5), pwrites=[Bst])
                kb.op("dve", [Bst], [], lambda e: e.scalar_tensor_tensor(out=st[:, 6:7], in0=st[:, 2:3], scalar=-1.0, in1=st[:, 5:6],
                                                                           op0=ALU.mult, op1=ALU.mult), pwrites=[Bst])
                kb.op("act", [By[bb_], Bst], [Bxh[bb_]], lambda e: e.activation(out=xh[bb_][:], in_=yy[bb_][:], func=AF.Identity,
                                                                              scale=st[:, 5:6], bias=st[:, 6:7]))
        kb.barrier()


def _finish(nc, kb, out_d, x_d):
    kb.barrier()
    tok = kb.dma("sp", [], [Buf()], out_d[0:128, :], x_d[0:128, :])
    kb._wait("sp", [tok])


def _t5_bucket(rel):
    half = 16
    ret = (rel > 0).astype(np.int32) * half
    n = np.abs(rel)
    large = 8 + (np.log(np.maximum(n, 1) / 8) / np.log(1024 / 8) * (half - 8)).astype(np.int32)
    large = np.minimum(large, half - 1)
    return ret + np.where(n < 8, n, large).astype(np.int32)


def _static_tables():
    a = np.arange(128)[:, None]
    c = np.arange(MW)[None, :]
    rel = a - c + C0
    mult = ((np.abs(rel) <= 64).astype(np.float32)
            + ((rel % 4 == 0) & (np.abs(rel) <= 256)).astype(np.float32)
            + ((rel % 16 == 0) & (np.abs(rel) <= 1024)).astype(np.float32))
    bucket = _t5_bucket(rel)
    inv = (10000.0 ** (-(np.arange(0, 32, 2, dtype=np.float32)) / np.float32(32))).astype(np.float32)
    ang = (np.arange(S, dtype=np.float32)[:, None] * inv[None, :]).astype(np.float32)
    cos = np.cos(ang.astype(np.float64)).astype(np.float32).T
    sin = np.sin(ang.astype(np.float64)).astype(np.float32).T
    rope_cos = np.ascontiguousarray(np.concatenate([cos, cos], axis=0))
    rope_sin = np.ascontiguousarray(np.concatenate([-sin, sin], axis=0))
    tokhl = np.zeros((128, NT, 2), np.float32)
    tokhl[:, :, 0] = np.arange(NT)[None, :]
    tokhl[:, :, 1] = np.arange(128)[:, None]
    return dict(
        toep_mult=mult.astype(np.float32), bucket=bucket,
        rope_cos=rope_cos, rope_sin=rope_sin,
        ident=np.eye(128, dtype=np.float32),
        triu=np.triu(np.ones((128, 128), np.float32), k=1),
        iota512=np.tile(np.arange(512, dtype=np.float32)[None, :], (128, 1)),
        tokhl=tokhl,
    )


def make_in_maps(inputs):
    f = lambda a: np.ascontiguousarray(np.asarray(a, dtype=np.float32))
    st = _static_tables()
    x = f(inputs["x"])
    c = f(inputs["c"])
    w_in = f(inputs["w_in"][0])
    kr = w_in[:, 640:672]
    w_kr = np.zeros((D, 192), np.float32)
    w_kr[:, 64:96] = kr
    w_kr[:, 96 + 64:96 + 80] = kr[:, 16:32]
    w_kr[:, 96 + 80:96 + 96] = kr[:, 0:16]
    w_uq = f(inputs["w_uq"][0]).reshape(384, 8, 96)
    w_uq_rot = w_uq.copy()
    w_uq_rot[:, :, 64:80] = w_uq[:, :, 80:96]
    w_uq_rot[:, :, 80:96] = w_uq[:, :, 64:80]
    w_uq2 = np.ascontiguousarray(np.stack([w_uq.reshape(384, 768), w_uq_rot.reshape(384, 768)], axis=1))
    w_ukv = f(inputs["w_ukv"][0]).reshape(256, 8, 128)
    rel_bias = f(inputs["rel_bias"])
    toep = np.ascontiguousarray(np.transpose(rel_bias[st["bucket"]], (2, 0, 1)))
    shared = dict(
        w_ada=f(inputs["w_ada"][0]), b_ada=f(inputs["b_ada"][0]).reshape(1, -1),
        w_in=w_in, w_kr=w_kr,
        g_q=np.ascontiguousarray(f(inputs["q_norm_g"][0]).reshape(3, 128).T),
        g_kv=np.ascontiguousarray(f(inputs["kv_norm_g"][0]).reshape(2, 128).T),
        w_uq2=w_uq2,
        w_ukv_k=np.ascontiguousarray(w_ukv[:, :, 0:64].reshape(256, 512)),
        w_ukv_v=np.ascontiguousarray(w_ukv[:, :, 64:128].reshape(256, 512)),
        rope_cos=st["rope_cos"], rope_sin=st["rope_sin"],
        relb_toep=toep, toep_mult=st["toep_mult"],
        w_out=f(inputs["w_out"][0]), ln1_g=f(inputs["ln1_g"][0]).reshape(1, -1), ln1_b=f(inputs["ln1_b"][0]).reshape(1, -1),
        w_router=f(inputs["w_router"][0]),
        w_gate=f(inputs["w_gate"][0]), w_up=f(inputs["w_up"][0]), w_down=f(inputs["w_down"][0]),
        ln2_g=f(inputs["ln2_g"][0]).reshape(1, -1), ln2_b=f(inputs["ln2_b"][0]).reshape(1, -1),
        ident=st["ident"], triu=st["triu"], iota512=st["iota512"], tokhl=st["tokhl"],
    )
    maps = []
    for b in range(x.shape[0]):
        m = dict(shared)
        m["x"] = x[b]
        m["c_fm"] = np.ascontiguousarray(c[b].reshape(8, 128).T)
        maps.append(m)
    return maps


def kernel(**inputs):
    maps = make_in_maps(inputs)
    nc = build_program()
    res = run_bass_kernel_spmd(nc, maps, core_ids=list(range(len(maps))))
    return np.stack([np.asarray(r["out"], dtype=np.float32) for r in res.results], axis=0)
```

```python
import math
import os
from contextlib import ExitStack

import numpy as np
import concourse.bass as bass
import concourse.mybir as mybir
from concourse.bass_utils import run_bass_kernel_spmd

F32 = mybir.dt.float32
BF16 = mybir.dt.bfloat16
I32 = mybir.dt.int32
AF = mybir.ActivationFunctionType
ALU = mybir.AluOpType
AX = mybir.AxisListType

D = 1024
S = 4096
NT = 32
NG = 8
ALPHA = 2.0 ** 0.25
EPS = 1e-6
NE = 16
CAP = 512
DFF = 1536
C0 = 1408
MW = 2944
NBIS = 26


class Buf:
    __slots__ = ("w", "r", "name", "excl")

    def __init__(self, name="", excl=False):
        self.excl = excl
        self.w = []
        self.r = []
        self.name = name


def _compact(toks):
    best = {}
    for t in toks:
        if t[0] not in best or best[t[0]][2] < t[2]:
            best[t[0]] = t
    return list(best.values())


class KB:
    COMPUTE = ("pe", "act", "dve", "pool")
    NDS = 12

    def __init__(self, nc, es):
        self.nc = nc
        self.E = {"pe": nc.tensor, "act": nc.scalar, "dve": nc.vector, "pool": nc.gpsimd, "sp": nc.sync}
        self.csem = {e: es.enter_context(nc.semaphore("c_" + e)) for e in self.COMPUTE}
        self.ccnt = {e: 0 for e in self.COMPUTE}
        self.dsem = {q: [es.enter_context(nc.semaphore("d_%s%d" % (q, i))) for i in range(self.NDS)]
                     for q in ("sp", "pool", "act")}
        self.dcnt = {q: 0 for q in self.dsem}
        self.dtok = {q: [None] * self.NDS for q in self.dsem}
        self.seen = {e: {} for e in self.E}
        self.nwait = 0

    def _wait(self, eng, toks):
        need = {}
        for t in toks:
            if t is None:
                continue
            key, sem, val = t
            if self.seen[eng].get(key, 0) >= val:
                continue
            if need.get(key, (None, 0))[1] < val:
                need[key] = (sem, val)
        for key, (sem, val) in need.items():
            self.E[eng].wait_ge(sem, val)
            self.seen[eng][key] = val
            self.nwait += 1

    def _deps(self, eng, reads, writes, pwrites, is_dma):
        own = None if is_dma else "c_" + eng
        toks = []
        for b in reads:
            for t in b.w:
                if not (eng == "pe" and t[0] == own):
                    toks.append(t)
            if b.excl:
                toks.extend(t for t in b.r if t[0] != own)
        pe_own = own if eng == "pe" else None
        for b in writes:
            toks.extend(t for t in b.w if t[0] != pe_own)
            toks.extend(t for t in b.r if t[0] != pe_own)
        for b in pwrites:
            toks.extend(t for t in b.r if t[0] != pe_own)
            toks.extend(t for t in b.w[:1] if t[0] != pe_own)
        return toks

    def _commit(self, tok, reads, writes, pwrites):
        for b in reads:
            b.r.append(tok)
            if len(b.r) > 16:
                b.r = _compact(b.r)
        for b in writes:
            b.w = [tok]
            b.r = []
        for b in pwrites:
            b.w.append(tok)
            if len(b.w) > 16:
                b.w = b.w[:1] + _compact(b.w[1:])

    def op(self, eng, reads, writes, fn, pwrites=()):
        self._wait(eng, self._deps(eng, reads, writes, pwrites, False))
        inst = fn(self.E[eng])
        self.ccnt[eng] += 1
        tok = ("c_" + eng, self.csem[eng], self.ccnt[eng])
        inst.then_inc(self.csem[eng], 1)
        self._commit(tok, reads, writes, pwrites)
        return tok

    def dma(self, q, reads, writes, out, in_, pwrites=(), indirect=None, **kw):
        i = self.dcnt[q]
        slot = i % self.NDS
        self._wait(q, self._deps(q, reads, writes, pwrites, True) + [self.dtok[q][slot]])
        if indirect is None:
            inst = self.E[q].dma_start(out=out, in_=in_, **kw)
        else:
            inst = self.E[q].indirect_dma_start(out=out, in_=in_, **indirect)
        val = 16 * (i // self.NDS + 1)
        sem = self.dsem[q][slot]
        inst.then_inc(sem, 16)
        tok = ("d_%s%d" % (q, slot), sem, val)
        self.dtok[q][slot] = tok
        self.dcnt[q] += 1
        self._commit(tok, reads, writes, pwrites)
        return tok

    def barrier(self, engines=None):
        toks = []
        for e in self.COMPUTE:
            if self.ccnt[e]:
                toks.append(("c_" + e, self.csem[e], self.ccnt[e]))
        for q in self.dsem:
            toks.extend(t for t in self.dtok[q] if t is not None)
        for e in (engines or self.E):
            self._wait(e, toks)


def AP(t, off, dims):
    return bass.AP(t, off, [list(d) for d in dims])


def pdim(ap):
    return list(ap.ap[0])


def build_program(stages=99, dbg=()):
    nc = bass.Bass("TRN2", target_bir_lowering=False)
    es = ExitStack()
    with es:
        _build(nc, es, stages, dbg)
    return nc


def _build(nc, es, stages, dbg):
    def din(name, shape, dt=F32):
        return nc.dram_tensor(name, list(shape), dt, kind="ExternalInput").ap()

    def dscr(name, shape, dt):
        kind = "ExternalOutput" if name in dbg else "Internal"
        return nc.dram_tensor(name, list(shape), dt, kind=kind).ap()

    x_d = din("x", [S, D])
    cfm_d = din("c_fm", [128, 8])
    wada_d = din("w_ada", [D, 6 * D])
    bada_d = din("b_ada", [1, 6 * D])
    win_d = din("w_in", [D, 2208])
    wkr_d = din("w_kr", [D, 192])
    gq_d = din("g_q", [128, 3])
    gkv_d = din("g_kv", [128, 2])
    wuq_d = din("w_uq2", [384, 2, 768])
    wukvk_d = din("w_ukv_k", [256, 512])
    wukvv_d = din("w_ukv_v", [256, 512])
    cos_d = din("rope_cos", [32, S])
    sin_d = din("rope_sin", [32, S])
    toep_d = din("relb_toep", [8, 128, MW])
    mult_d = din("toep_mult", [128, MW])
    wout_d = din("w_out", [D, D])
    ln1g_d = din("ln1_g", [1, D])
    ln1b_d = din("ln1_b", [1, D])
    wr_d = din("w_router", [D, NE])
    wg_d = din("w_gate", [NE, D, DFF])
    wu_d = din("w_up", [NE, D, DFF])
    wd_d = din("w_down", [NE, DFF, D])
    ln2g_d = din("ln2_g", [1, D])
    ln2b_d = din("ln2_b", [1, D])
    ident_d = din("ident", [128, 128])
    triu_d = din("triu", [128, 128])
    iota_d = din("iota512", [128, 512])
    tokhl_d = din("tokhl", [128, NT, 2])
    out_d = nc.dram_tensor("out", [S, D], F32, kind="ExternalOutput").ap()

    mod_d = dscr("mod_s", [1, 6 * D], F32)
    qT_d = dscr("qT_s", [8, 96, S], BF16)
    kT_d = dscr("kT_s", [8, 96, S], BF16)
    Vm_d = dscr("Vm_s", [NT, 128, 768], BF16)
    dqT_d = dscr("dqT_s", [4, 128, S], BF16)
    dkT_d = dscr("dkT_s", [4, 128, S], BF16)
    Vd_d = dscr("Vd_s", [NT, 128, 768], BF16)
    attnT_d = dscr("attnT_s", [8, 128, S], BF16)
    x1_d = dscr("x1_s", [S, D], F32)
    h2_d = dscr("h2_s", [S, D], BF16)
    moe_d = dscr("moe_s", [S, D], F32)
    aff_d = dscr("aff_s", [128, NT * NE], F32)
    idx_d = dscr("idx_s", [128, NE * 4 * 8], F32)

    kb = KB(nc, es)
    B_mod, B_q, B_k, B_vm, B_dq, B_dk, B_vd = (Buf(n) for n in ("mod", "q", "k", "vm", "dq", "dk", "vd"))
    B_attn, B_x1, B_h2, B_moe = Buf("attn"), Buf("x1"), Buf("h2"), Buf("moe")

    ps = es.enter_context(nc.psum_tensor("ps", [128, 4096], F32))
    PB = [Buf("bank%d" % i, excl=True) for i in range(8)]
    bank_ctr = [0]

    nrot = [8]

    def bank():
        i = bank_ctr[0] % nrot[0]
        bank_ctr[0] += 1
        return ps[:, i * 512:(i + 1) * 512], PB[i]

    modfm = es.enter_context(nc.sbuf_tensor("modfm", [128, 16], F32))
    aff_all = es.enter_context(nc.sbuf_tensor("aff_all", [128, NT * NE], F32))
    B_modfm, B_aff = Buf("modfm"), Buf("aff")

    def bcast_row(dram_ap_row, n):
        return AP(dram_ap_row.tensor, dram_ap_row.offset, [[0, 128], [1, n]])

    with ExitStack() as s0:
        A = lambda n, sh, dt: s0.enter_context(nc.sbuf_tensor("p0_" + n, sh, dt))
        cfm = A("cfm", [128, 8], F32)
        sig = A("sig", [128, 8], F32)
        cond = A("cond", [128, 8], F32)
        bada = A("bada", [1, 6 * D], F32)
        modrow = A("modrow", [1, 6 * D], F32)
        wa = [A("wa%d" % i, [128, 8, 512], F32) for i in range(3)]
        Bc, Bcond, Bb, Bm = Buf(), Buf(), Buf(), Buf()
        Bwa = [Buf() for _ in range(3)]
        kb.dma("sp", [], [Bc], cfm[:], cfm_d[:, :])
        kb.dma("sp", [], [Bb], bada[:], bada_d[:, :])
        wada_v = wada_d.rearrange("(k p) n -> p k n", p=128)
        for j in range(2):
            kb.dma("sp", [], [Bwa[j]], wa[j][:], wada_v[:, :, j * 512:(j + 1) * 512])
        kb.op("act", [Bc], [Bcond], lambda e: e.activation(out=sig[:], in_=cfm[:], func=AF.Sigmoid))
        kb.op("dve", [Bc, Bcond], [Bcond], lambda e: e.tensor_tensor(out=cond[:], in0=cfm[:], in1=sig[:], op=ALU.mult))
        for j in range(12):
            if j + 2 < 12:
                kb.dma("sp", [], [Bwa[(j + 2) % 3]], wa[(j + 2) % 3][:], wada_v[:, :, (j + 2) * 512:(j + 3) * 512])
            bk, bb = bank()
            w = wa[j % 3]

            def mm(e, w=w, bk=bk):
                for k in range(8):
                    i = e.matmul(bk[0:1, :], cond[:, k:k + 1], w[:, k, :], start=(k == 0), stop=(k == 7))
                return i
            kb.op("pe", [Bcond, Bwa[j % 3]], [bb], mm)
            kb.op("dve", [bb, Bb], [], pwrites=[Bm], fn=lambda e, bk=bk, j=j: e.tensor_tensor(
                out=modrow[0:1, j * 512:(j + 1) * 512], in0=bk[0:1, :], in1=bada[0:1, j * 512:(j + 1) * 512], op=ALU.add))
        kb.dma("sp", [Bm], [B_mod], mod_d[:, :], modrow[:])
        onef = A("onef", [1, 1], F32)
        kb.op("pool", [], [Bc], lambda e: e.memset(onef[:], 1.0))
        bk, bb = bank()

        def mmT(e, bk=bk):
            for j in range(16):
                ins = e.matmul(bk[:, j:j + 1], modrow[0:1, j * 128:(j + 1) * 128], onef[0:1, 0:1], start=True, stop=True)
            return ins
        kb.op("pe", [Bm, Bc], [bb], mmT)
        kb.op("dve", [bb], [B_modfm], lambda e, bk=bk: e.tensor_copy(out=modfm[:], in_=bk[:, 0:16]))
        kb.op("dve", [B_modfm], [B_modfm], lambda e: e.tensor_scalar(
            out=modfm[:, 8:16], in0=modfm[:, 8:16], scalar1=1.0, scalar2=None, op0=ALU.add))
        kb.barrier()
    if stages <= 0:
        _finish(nc, kb, out_d, x_d)
        return

    with ExitStack() as s1:
        A = lambda n, sh, dt: s1.enter_context(nc.sbuf_tensor("p1_" + n, sh, dt))
        win = A("win", [128, 8, 2208], BF16)
        wkr = A("wkr", [128, 8, 192], BF16)
        wuq = A("wuq", [128, 3, 2, 768], BF16)
        wukvk = A("wukvk", [128, 2, 512], BF16)
        wukvv = A("wukvv", [128, 2, 512], BF16)
        ident = A("ident", [128, 128], F32)
        onesq = A("onesq", [128, 128], F32)
        oneskv = A("oneskv", [128, 128], F32)
        gq = A("gq", [128, 3], F32)
        gkv = A("gkv", [128, 2], F32)
        xg = [A("xg%d" % i, [128, 4, 1024], F32) for i in range(2)]
        hT = [A("hT%d" % i, [128, 8, 512], BF16) for i in range(2)]
        cT = A("cT", [128, 5, 512], F32)
        sq = A("sq", [128, 5, 512], F32)
        rstd = A("rstd", [128, 2, 512], F32)
        cn = [A("cn%d" % i, [128, 5, 512], BF16) for i in range(2)]
        cosr = [A("cosr%d" % i, [128, 512], F32) for i in range(2)]
        sinr = [A("sinr%d" % i, [128, 512], F32) for i in range(2)]
        tmpa = [A("tmpa%d" % i, [128, 512], F32) for i in range(2)]
        tmpb = [A("tmpb%d" % i, [128, 512], F32) for i in range(2)]
        NO = 6
        ob = [A("ob%d" % i, [128, 512], BF16) for i in range(NO)]
        vo = [A("vo%d" % i, [128, 768], BF16) for i in range(4)]
        Bw, Bid, Bones, Bg = Buf(), Buf(), Buf(), Buf()
        Bxg = [Buf(), Buf()]
        BhT = [Buf(), Buf()]
        BcT, Bsq, Brstd = [Buf() for _ in range(5)], [Buf() for _ in range(5)], [Buf(), Buf()]
        Bcn = [[Buf() for _ in range(5)] for _ in range(2)]
        Brope = [Buf(), Buf()]
        Bta, Btb = [Buf(), Buf()], [Buf(), Buf()]
        Bob = [Buf() for _ in range(NO)]
        Bvo = [Buf() for _ in range(4)]
        ob_ctr, vo_ctr, t_ctr = [0], [0], [0]

        SKIP = os.environ.get('P1SKIP', '')
        for k in range(0 if 'w' in SKIP else 8):
            kb.dma("pool", [], [], win[:, k, :], win_d[k * 128:(k + 1) * 128, :], pwrites=[Bw])
            kb.dma("pool", [], [], wkr[:, k, :], wkr_d[k * 128:(k + 1) * 128, :], pwrites=[Bw])
        for c in range(0 if 'u' in SKIP else 3):
            kb.dma("pool", [], [], wuq[:, c, :, :], wuq_d[c * 128:(c + 1) * 128, :, :], pwrites=[Bw])
        for c in range(0 if 'v' in SKIP else 2):
            kb.dma("pool", [], [], wukvk[:, c, :], wukvk_d[c * 128:(c + 1) * 128, :], pwrites=[Bw])
            kb.dma("pool", [], [], wukvv[:, c, :], wukvv_d[c * 128:(c + 1) * 128, :], pwrites=[Bw])
        kb.dma("sp", [], [Bid], ident[:], ident_d[:, :])
        kb.dma("sp", [], [], gq[:], gq_d[:, :], pwrites=[Bg])
        kb.dma("sp", [], [], gkv[:], gkv_d[:, :], pwrites=[Bg])
        kb.op("pool", [], [], lambda e: e.memset(onesq[:], 1.0 / 384.0), pwrites=[Bones])
        kb.op("pool", [], [], lambda e: e.memset(oneskv[:], 1.0 / 256.0), pwrites=[Bones])
        for i in range(4):
            kb.op("pool", [], [Bvo[i]], lambda e, i=i: e.memset(vo[i][:], 1.0))

        x_v = x_d.rearrange("(t p) d -> p t d", p=128)

        def load_x(g):
            kb.dma("sp", [], [Bxg[g % 2]], xg[g % 2][:], x_v[:, g * 4:(g + 1) * 4, :])
            kb.dma("sp", [], [Brope[g % 2]], cosr[g % 2][64:96, :], cos_d[:, g * 512:(g + 1) * 512])
            kb.dma("sp", [], [Brope[g % 2]], sinr[g % 2][64:96, :], sin_d[:, g * 512:(g + 1) * 512])

        def next_ob():
            i = ob_ctr[0] % NO
            ob_ctr[0] += 1
            return ob[i], Bob[i]

        def evac_copy(eng, src, bsrc, dst, bdst):
            if eng == "act":
                kb.op("act", [bsrc], [bdst], lambda e: e.copy(out=dst, in_=src))
            else:
                kb.op("dve", [bsrc], [bdst], lambda e: e.tensor_copy(out=dst, in_=src))

        def rope_rows(bm, bbm, br, bbr, g, dst, bdst, full):
            i = t_ctr[0] % 2
            t_ctr[0] += 1
            kb.op("dve", [bbm, Brope[g % 2]], [Bta[i]], lambda e: e.tensor_tensor(
                out=tmpa[i][64:96, :], in0=bm[64:96, :], in1=cosr[g % 2][64:96, :], op=ALU.mult))
            kb.op("dve", [bbr, Brope[g % 2]], [Btb[i]], lambda e: e.tensor_tensor(
                out=tmpb[i][64:96, :], in0=br[64:96, :], in1=sinr[g % 2][64:96, :], op=ALU.mult))
            kb.op("dve", [Bta[i], Btb[i]], [bdst] if full else [], lambda e: e.tensor_tensor(
                out=dst[64:96, :], in0=tmpa[i][64:96, :], in1=tmpb[i][64:96, :], op=ALU.add),
                pwrites=[] if full else [bdst])

        def vo_views(vt, bk):
            o = AP(vt, 0, [pdim(vt[:]), [192, 4], [128, 2], [1, 64]])
            i = AP(bk.tensor, bk.offset, [pdim(bk), [128, 4], [64, 2], [1, 64]])
            return o, i

        load_x(0)
        LIM = float(os.environ.get('P1LIM', '99'))
        SUB = float(os.environ.get('P1SUB', '99'))
        for g in range(NG if LIM >= 99 else 1):
            if g + 1 < NG:
                load_x(g + 1)
            X, BX = xg[g % 2], Bxg[g % 2]
            H, BH = hT[g % 2], BhT[g % 2]
            CN, BCN = cn[g % 2], Bcn[g % 2]
            cols = slice(g * 512, (g + 1) * 512)
            for k in range(8):
                bk, bb = bank()

                def tp(e, bk=bk, k=k):
                    for i in range(4):
                        ins = e.transpose(bk[:, i * 128:(i + 1) * 128], X[:, i, k * 128:(k + 1) * 128], ident[:])
                    return ins
                kb.op("pe", [BX, Bid], [bb], tp)
                kb.op("act", [bb, B_modfm], [BH] if k == 0 else [], lambda e, bk=bk, k=k: e.activation(
                    out=H[:, k, :], in_=bk, func=AF.Identity, scale=modfm[:, 8 + k:9 + k], bias=modfm[:, k:k + 1]),
                    pwrites=[] if k == 0 else [BH])
            if LIM < 1:
                break
            for c in range(5):
                bk, bb = bank()

                def mm(e, bk=bk, c=c):
                    for k in range(8):
                        ins = e.matmul(bk, win[:, k, c * 128:(c + 1) * 128], H[:, k, :], start=(k == 0), stop=(k == 7))
                    return ins
                kb.op("pe", [BH, Bw], [bb], mm)
                if SUB >= 0.1:
                    kb.op("act", [bb], [Bsq[c]], lambda e, bk=bk, c=c: e.activation(out=sq[:, c, :], in_=bk, func=AF.Square))
                if SUB >= 0.15:
                    kb.op("dve", [bb, Bsq[c]], [BcT[c]], lambda e, bk=bk, c=c: e.tensor_copy(out=cT[:, c, :], in_=bk))
            if SUB < 0.3:
                break
            for which, (cs, ones_t) in enumerate((((0, 1, 2), onesq), ((3, 4), oneskv))):
                bk, bb = bank()

                def mm(e, bk=bk, cs=cs, ones_t=ones_t):
                    for n, c in enumerate(cs):
                        ins = e.matmul(bk, ones_t[:], sq[:, c, :], start=(n == 0), stop=(n == len(cs) - 1))
                    return ins
                kb.op("pe", [Bsq[c] for c in cs] + [Bones], [bb], mm)
                kb.op("dve", [bb], [Brstd[which]], lambda e, bk=bk, which=which: e.tensor_scalar(
                    out=rstd[:, which, :], in0=bk, scalar1=EPS, scalar2=None, op0=ALU.add))
                if SUB < 0.5:
                    continue
                kb.op("act", [Brstd[which]], [Brstd[which]], lambda e, which=which: e.activation(
                    out=rstd[:, which, :], in_=rstd[:, which, :], func=AF.Sqrt))
                kb.op("dve", [Brstd[which]], [Brstd[which]], lambda e, which=which: e.reciprocal(
                    out=rstd[:, which, :], in_=rstd[:, which, :]))
                if SUB < 0.7:
                    continue
                for c in cs:
                    gsc = gq[:, c:c + 1] if which == 0 else gkv[:, c - 3:c - 2]
                    kb.op("dve", [BcT[c], Brstd[which], Bg], [BCN[c]], lambda e, c=c, gsc=gsc, which=which: e.scalar_tensor_tensor(
                        out=CN[:, c, :], in0=cT[:, c, :], scalar=gsc, in1=rstd[:, which, :], op0=ALU.mult, op1=ALU.mult))
            if LIM < 6:
                break
            for base, dst, bd in ((672, dqT_d, B_dq), (1184, dkT_d, B_dk)):
                for hp in range(4):
                    bk, bb = bank()

                    def mm(e, hp=hp, bk=bk, base=base):
                        for k in range(8):
                            ins = e.matmul(bk, win[:, k, base + hp * 128: base + (hp + 1) * 128], H[:, k, :],
                                           start=(k == 0), stop=(k == 7))
                        return ins
                    kb.op("pe", [BH, Bw], [bb], mm)
                    o, bo = next_ob()
                    evac_copy("act" if hp % 2 == 0 else "dve", bk, bb, o[:, :], bo)
                    kb.dma("sp", [bo], [], dst[hp, :, cols], o[:, :], pwrites=[bd])
            if LIM < 7:
                break
            for i in range(4):
                bk, bb = bank()

                def mm(e, i=i, bk=bk):
                    for k in range(8):
                        ins = e.matmul(bk, H[:, k, i * 128:(i + 1) * 128], win[:, k, 1696:2208], start=(k == 0), stop=(k == 7))
                    return ins
                kb.op("pe", [BH, Bw], [bb], mm)
                vi = vo_ctr[0] % 4
                vo_ctr[0] += 1
                ov, iv = vo_views(vo[vi], bk)
                kb.op("act", [bb], [Bvo[vi]], lambda e, ov=ov, iv=iv: e.copy(out=ov, in_=iv))
                kb.dma("sp", [Bvo[vi]], [], Vd_d[g * 4 + i, :, :], vo[vi][:], pwrites=[B_vd])
            if LIM < 2:
                break
            for h in range(8):
                bm, bbm = bank()
                br, bbr = bank()

                def mm(e, h=h, bm=bm, br=br):
                    for which, bk in ((0, bm), (1, br)):
                        for c in range(3):
                            ins = e.matmul(bk[0:96, :], wuq[:, c, which, h * 96:(h + 1) * 96], CN[:, c, :],
                                           start=(c == 0), stop=(c == 2))
                    return ins
                kb.op("pe", [BCN[0], BCN[1], BCN[2], Bw], [bbm, bbr], mm)
                o, bo = next_ob()
                kb.op("act", [bbm], [bo], lambda e, o=o, bm=bm: e.copy(out=o[0:64, :], in_=bm[0:64, :]))
                rope_rows(bm, bbm, br, bbr, g, o, bo, False)
                kb.dma("sp", [bo], [], qT_d[h, :, cols], o[0:96, :], pwrites=[B_q])
            if LIM < 3:
                break
            for hp in range(4):
                bk, bb = bank()

                def mm(e, hp=hp, bk=bk):
                    for c in range(2):
                        ins = e.matmul(bk, wukvk[:, c, hp * 128:(hp + 1) * 128], CN[:, 3 + c, :], start=(c == 0), stop=(c == 1))
                    return ins
                kb.op("pe", [BCN[3], BCN[4], Bw], [bb], mm)
                o, bo = next_ob()
                evac_copy("act", bk, bb, o[:, :], bo)
                kb.dma("sp", [bo], [], kT_d[2 * hp, 0:64, cols], o[0:64, :], pwrites=[B_k])
                kb.dma("sp", [bo], [], kT_d[2 * hp + 1, 0:64, cols], o[64:128, :], pwrites=[B_k])
            if LIM < 4:
                break
            bm, bbm = bank()
            br, bbr = bank()

            def mm(e, bm=bm, br=br):
                for which, bk in ((0, bm), (1, br)):
                    for k in range(8):
                        ins = e.matmul(bk[0:96, :], wkr[:, k, which * 96:(which + 1) * 96], H[:, k, :],
                                       start=(k == 0), stop=(k == 7))
                return ins
            kb.op("pe", [BH, Bw], [bbm, bbr], mm)
            o, bo = next_ob()
            rope_rows(bm, bbm, br, bbr, g, o, bo, True)
            for h in range(8):
                kb.dma("sp", [bo], [], kT_d[h, 64:96, cols], o[64:96, :], pwrites=[B_k])
            if LIM < 5:
                break
            for i in range(4):
                bk, bb = bank()

                def mm(e, i=i, bk=bk):
                    for c in range(2):
                        ins = e.matmul(bk, CN[:, 3 + c, i * 128:(i + 1) * 128], wukvv[:, c, :], start=(c == 0), stop=(c == 1))
                    return ins
                kb.op("pe", [BCN[3], BCN[4], Bw], [bb], mm)
                vi = vo_ctr[0] % 4
                vo_ctr[0] += 1
                ov, iv = vo_views(vo[vi], bk)
                kb.op("act", [bb], [Bvo[vi]], lambda e, ov=ov, iv=iv: e.copy(out=ov, in_=iv))
                kb.dma("sp", [Bvo[vi]], [], Vm_d[g * 4 + i, :, :], vo[vi][:], pwrites=[B_vm])
        kb.barrier()
    if stages <= 1:
        _finish(nc, kb, out_d, x_d)
        return

    def attention(tag, kT_src, qT_src, V_src, per_pair_kq, krows, scale, chunk0, dilated, Bk_src, Bq_src, Bv_src):
        with ExitStack() as sa:
            A = lambda n, sh, dt: sa.enter_context(nc.sbuf_tensor(tag + "_" + n, sh, dt))
            Vp = [A("Vp%d" % i, [128, 32, 192], BF16) for i in range(2)]
            Kt = [A("Kt%d" % i, [128, S], BF16) for i in range(2)]
            Qt = [A("Qt%d" % i, [128, S], BF16) for i in range(2)]
            pte = [A("pte%d" % i, [128, 1024], BF16) for i in range(3)]
            ptm = [A("ptm%d" % i, [128, 1024], BF16) for i in range(3)] if dilated else None
            ao = [A("ao%d" % i, [128, S], BF16) for i in range(2)]
            rec = [A("rec%d" % i, [128, 512], F32) for i in range(2)]
            BVp, BKt, BQt = [Buf(), Buf()], [Buf(), Buf()], [Buf(), Buf()]
            Bpte, Bptm = [Buf() for _ in range(3)], [Buf() for _ in range(3)]
            Bao, Brec = [Buf(), Buf()], [Buf(), Buf()]
            SB = [Buf("S%d" % i, excl=True) for i in range(3)]
            AB = [Buf("acc%d" % i, excl=True) for i in range(2)]
            Sap = [ps[:, i * 1024:(i + 1) * 1024] for i in range(3)]
            Aap = [ps[:, 3072 + i * 512: 3072 + (i + 1) * 512] for i in range(2)]
            Bzk = Buf()
            if per_pair_kq:
                KtB = [A("KtB%d" % i, [128, S], BF16) for i in range(2)]
                for i in range(2):
                    kb.op("dve", [], [], lambda e, i=i: e.memset(Kt[i][64:128, :], 0.0), pwrites=[Bzk])
                    kb.op("dve", [], [], lambda e, i=i: e.memset(KtB[i][0:64, :], 0.0), pwrites=[Bzk])
            if dilated:
                toep_st = A("toep", [128, MW], F32)
                multb = A("multb", [128, MW], BF16)
                master = [A("master%d" % i, [128, MW], BF16) for i in range(2)]
                Btoep, Bmult, Bmaster = Buf(), Buf(), [Buf(), Buf()]
                kb.dma("pool", [], [Bmult], multb[:], mult_d[:, :])

            def load_pair(hp):
                for j in range(4):
                    kb.dma("sp", [Bv_src], [], Vp[hp % 2][:, j * 8:(j + 1) * 8, :],
                           V_src[j * 8:(j + 1) * 8, :, hp * 192:(hp + 1) * 192].rearrange("t p c -> p t c"),
                           pwrites=[BVp[hp % 2]])

            def load_kq(u):
                if per_pair_kq:
                    kb.dma("sp", [Bk_src, Bzk], [BKt[u % 2]], Kt[u % 2][0:64, :], kT_src[u, 0:64, :])
                    kb.dma("sp", [Bk_src, Bzk], [], KtB[u % 2][64:128, :], kT_src[u, 64:128, :], pwrites=[BKt[u % 2]])
                    kb.dma("sp", [Bq_src], [BQt[u % 2]], Qt[u % 2][:, :], qT_src[u, :, :])
                else:
                    kb.dma("sp", [Bk_src], [BKt[u % 2]], Kt[u % 2][0:krows, :], kT_src[u, :, :])
                    kb.dma("sp", [Bq_src], [BQt[u % 2]], Qt[u % 2][0:krows, :], qT_src[u, :, :])

            def prep_mask(h):
                kb.dma("sp", [], [Btoep], toep_st[:], toep_d[h, :, :])
                kb.op("act", [Btoep], [Btoep], lambda e: e.activation(out=toep_st[:], in_=toep_st[:], func=AF.Exp))
                kb.op("dve", [Btoep, Bmult], [Bmaster[h % 2]], lambda e: e.tensor_tensor(
                    out=master[h % 2][:], in0=toep_st[:], in1=multb[:], op=ALU.mult))

            items = []
            for h in range(8):
                hp, hh = h // 2, h % 2
                for Q in range(8):
                    if dilated:
                        Ts = list(range(max(0, 4 * Q - 8), min(31, 4 * Q + 11) + 1))
                    else:
                        Ts = list(range(32))
                    groups = []
                    n = 0
                    while n < len(Ts):
                        if n + 1 < len(Ts):
                            groups.append([Ts[n + 1], Ts[n]])
                            n += 2
                        else:
                            groups.append([Ts[n]])
                            n += 1
                    for gi, gT in enumerate(groups):
                        items.append(dict(h=h, hp=hp, hh=hh, Q=Q, Ts=gT, first=(gi == 0), last=(gi == len(groups) - 1),
                                          hq=h * 8 + Q))
            N = len(items)
            for i, it in enumerate(items):
                it["i"] = i

            def kq_unit(it):
                return it["hp"] if per_pair_kq else it["h"]

            def rows_of(it):
                if per_pair_kq:
                    return it["hh"] * 64, it["hh"] * 64 + 64
                return 0, krows

            def emit_qk(it):
                i = it["i"]
                u = kq_unit(it)
                r0, r1 = rows_of(it)
                K_, Q_ = Kt[u % 2], Qt[u % 2]
                if per_pair_kq:
                    r0, r1 = 0, 128
                    if it["hh"] == 1:
                        K_ = KtB[u % 2]

                def f(e):
                    for j, T in enumerate(it["Ts"]):
                        ins = e.matmul(Sap[i % 3][:, j * 512:(j + 1) * 512], K_[r0:r1, T * 128:(T + 1) * 128],
                                       Q_[r0:r1, it["Q"] * 512:(it["Q"] + 1) * 512], start=True, stop=True)
                    return ins
                kb.op("pe", [BKt[u % 2], BQt[u % 2]], [SB[i % 3]], f)

            def emit_exp(it):
                i = it["i"]
                w = 512 * len(it["Ts"])
                kb.op("act", [SB[i % 3]], [Bpte[i % 3]], lambda e: e.activation(
                    out=pte[i % 3][:, 0:w], in_=Sap[i % 3][:, 0:w], func=AF.Exp, scale=scale))

            def emit_mask(it):
                i = it["i"]
                n = len(it["Ts"])
                h = it["h"]
                c0 = C0 - 128 * it["Ts"][0] + 512 * it["Q"]
                m = master[h % 2]
                in1 = AP(m, c0, [pdim(m[:]), [128, n], [1, 512]])
                o = AP(ptm[i % 3], 0, [pdim(ptm[i % 3][:]), [512, n], [1, 512]])
                i0 = AP(pte[i % 3], 0, [pdim(pte[i % 3][:]), [512, n], [1, 512]])
                kb.op("dve", [Bpte[i % 3], Bmaster[h % 2]], [Bptm[i % 3]], lambda e: e.tensor_tensor(
                    out=o, in0=i0, in1=in1, op=ALU.mult))

            def emit_pv(it):
                i = it["i"]
                P_, BP_ = (ptm[i % 3], Bptm[i % 3]) if dilated else (pte[i % 3], Bpte[i % 3])
                acc = Aap[it["hq"] % 2]
                V_ = Vp[it["hp"] % 2]
                vc = 64 * it["hh"]
                nT = len(it["Ts"])

                def f(e):
                    for j, T in enumerate(it["Ts"]):
                        ins = e.matmul(acc, V_[:, T, vc:vc + 128], P_[:, j * 512:(j + 1) * 512],
                                       start=(it["first"] and j == 0), stop=(it["last"] and j == nT - 1))
                    return ins
                if it["first"]:
                    kb.op("pe", [BP_, BVp[it["hp"] % 2]], [AB[it["hq"] % 2]], f)
                else:
                    kb.op("pe", [BP_, BVp[it["hp"] % 2]], [], f, pwrites=[AB[it["hq"] % 2]])

            def emit_fin(it):
                a = it["hq"] % 2
                acc = Aap[a]
                hh, hp, Q = it["hh"], it["hp"], it["Q"]
                nr = slice(64 * hh, 64 * hh + 64)
                dr = slice(64 * (1 - hh), 64 * (1 - hh) + 64)
                if dilated:
                    kb.op("act", [AB[a]], [Brec[a]], lambda e: e.activation(out=rec[a][nr, :], in_=acc[dr, :], func=AF.Ln))
                    kb.op("act", [Brec[a]], [Brec[a]], lambda e: e.activation(out=rec[a][nr, :], in_=rec[a][nr, :], func=AF.Exp, scale=-1.0))
                else:
                    kb.op("dve", [AB[a]], [Brec[a]], lambda e: e.reciprocal(out=rec[a][nr, :], in_=acc[dr, :]))
                first_of_pair = (hh == 0 and Q == 0)
                kb.op("dve", [AB[a], Brec[a]], [Bao[hp % 2]] if first_of_pair else [], lambda e: e.tensor_tensor(
                    out=ao[hp % 2][nr, Q * 512:(Q + 1) * 512], in0=acc[nr, :], in1=rec[a][nr, :], op=ALU.mult),
                    pwrites=[] if first_of_pair else [Bao[hp % 2]])
                if hh == 1 and Q == 7:
                    kb.dma("sp", [Bao[hp % 2]], [], attnT_d[chunk0 + hp, :, :], ao[hp % 2][:, :], pwrites=[B_attn])

            def unit_start(it):
                h = it["h"]
                if it["Q"] == 0 and it["first"]:
                    if h + 1 < 8:
                        if per_pair_kq:
                            if (h + 1) % 2 == 0:
                                load_kq((h + 1) // 2)
                        else:
                            load_kq(h + 1)
                        if (h + 1) % 2 == 0:
                            load_pair((h + 1) // 2)
                        if dilated:
                            prep_mask(h + 1)

            load_pair(0)
            load_kq(0)
            if dilated:
                prep_mask(0)
            emit_qk(items[0])
            emit_exp(items[0])
            if dilated:
                emit_mask(items[0])
            if N > 1:
                emit_qk(items[1])
            pend = []
            for i in range(N):
                it = items[i]
                unit_start(it)
                if i + 1 < N:
                    emit_exp(items[i + 1])
                    if dilated:
                        emit_mask(items[i + 1])
                if i + 2 < N:
                    emit_qk(items[i + 2])
                emit_pv(it)
                if it["last"]:
                    pend.append((i, it))
                while pend and pend[0][0] + 3 <= i:
                    emit_fin(pend.pop(0)[1])
            while pend:
                emit_fin(pend.pop(0)[1])
            kb.barrier()

    run_dil = os.environ.get("SKIPDIL", "") == ""
    run_mla = os.environ.get("SKIPMLA", "") == ""
    if run_dil:
        attention("dil", dkT_d, dqT_d, Vd_d, True, 64, 64.0 ** -0.5, 4, True, B_dk, B_dq, B_vd)
    if run_mla:
        attention("mla", kT_d, qT_d, Vm_d, False, 96, 96.0 ** -0.5, 0, False, B_k, B_q, B_vm)
    if stages <= 3:
        _finish(nc, kb, out_d, x_d)
        return

    def bank2():
        if bank_ctr[0] % 2:
            bank_ctr[0] += 1
        i = bank_ctr[0] % 8
        bank_ctr[0] += 2
        return ps[:, i * 512:(i + 2) * 512], [PB[i], PB[i + 1]]

    class LNState:
        def __init__(self, A, tag, nb=2):
            self.junk = A(tag + "junk", [128, D], F32)
            self.st = [A(tag + "st%d" % i, [128, 8], F32) for i in range(nb)]
            self.Bjunk = Buf()
            self.Bst = [Buf() for _ in range(nb)]
            self.nb = nb
            self.n = 0

    def ln_stats(L, y, By):
        i = L.n % L.nb
        L.n += 1
        st, Bst = L.st[i], L.Bst[i]
        kb.op("dve", [], [Bst], lambda e: e.memset(st[:], 0.0))
        kb.op("act", [By, Bst], [L.Bjunk], lambda e: e.activation(out=L.junk[:], in_=y[:], func=AF.Identity, accum_out=st[:, 0:1]),
              pwrites=[Bst])
        kb.op("act", [By, Bst], [L.Bjunk], lambda e: e.activation(out=L.junk[:], in_=y[:], func=AF.Square, accum_out=st[:, 1:2]),
              pwrites=[Bst])
        return st, Bst

    def ln_apply(st, Bst, y, By, xh, Bxh):
        kb.op("dve", [Bst], [], lambda e: e.tensor_scalar(out=st[:, 2:3], in0=st[:, 0:1], scalar1=1.0 / D, scalar2=None, op0=ALU.mult), pwrites=[Bst])
        kb.op("dve", [Bst], [], lambda e: e.tensor_tensor(out=st[:, 3:4], in0=st[:, 2:3], in1=st[:, 2:3], op=ALU.mult), pwrites=[Bst])
        kb.op("dve", [Bst], [], lambda e: e.scalar_tensor_tensor(out=st[:, 4:5], in0=st[:, 1:2], scalar=1.0 / D, in1=st[:, 3:4],
                                                                   op0=ALU.mult, op1=ALU.subtract), pwrites=[Bst])
        kb.op("dve", [Bst], [], lambda e: e.tensor_scalar(out=st[:, 4:5], in0=st[:, 4:5], scalar1=EPS, scalar2=None, op0=ALU.add), pwrites=[Bst])
        kb.op("act", [Bst], [], lambda e: e.activation(out=st[:, 5:6], in_=st[:, 4:5], func=AF.Ln), pwrites=[Bst])
        kb.op("act", [Bst], [], lambda e: e.activation(out=st[:, 5:6], in_=st[:, 5:6], func=AF.Exp, scale=-0.5), pwrites=[Bst])
        kb.op("dve", [Bst], [], lambda e: e.scalar_tensor_tensor(out=st[:, 6:7], in0=st[:, 2:3], scalar=-1.0, in1=st[:, 5:6],
                                                                   op0=ALU.mult, op1=ALU.mult), pwrites=[Bst])
        kb.op("act", [By, Bst], [Bxh], lambda e: e.activation(out=xh[:], in_=y[:], func=AF.Identity, scale=st[:, 5:6], bias=st[:, 6:7]))

    with ExitStack() as s4:
        A = lambda n, sh, dt: s4.enter_context(nc.sbuf_tensor("p4_" + n, sh, dt))
        wout = A("wout", [128, 8, D], BF16)
        g1bc, lngbc, lnbbc, sc2bc, sh2bc = (A(n, [128, D], F32) for n in ("g1bc", "lngbc", "lnbbc", "sc2bc", "sh2bc"))
        wr = A("wr", [128, 8, NE], F32)
        ident = A("ident", [128, 128], F32)
        at = [A("at%d" % i, [128, 8, 512], BF16) for i in range(2)]
        xt = [A("xt%d" % i, [128, D], F32) for i in range(2)]
        t1 = [A("t1%d" % i, [128, D], F32) for i in range(2)]
        yy = [A("y%d" % i, [128, D], F32) for i in range(2)]
        xh = [A("xh%d" % i, [128, D], F32) for i in range(2)]
        x1 = [A("x1%d" % i, [128, D], F32) for i in range(2)]
        h2f = [A("h2f%d" % i, [128, D], F32) for i in range(2)]
        h2b = [A("h2b%d" % i, [128, D], BF16) for i in range(2)]
        h2T = [A("h2T%d" % i, [128, 8, 128], F32) for i in range(2)]
        sm = [A("sm%d" % i, [128, 8], F32) for i in range(2)]
        ee = [A("ee%d" % i, [128, NE], F32) for i in range(2)]
        L = LNState(A, "ln", 3)
        Bc4 = Buf()
        Bat, Bxt, Bt1, By, Bxh, Bx1, Bh2f, Bh2b, Bh2T, Bsm, Bee = ([Buf(), Buf()] for _ in range(11))
        kb.dma("sp", [B_mod], [], g1bc[:], bcast_row(mod_d[0:1, 2048:3072], D), pwrites=[Bc4])
        Bws = [Buf(), Buf()]
        for c in range(8):
            kb.dma("sp", [], [Bws[c % 2]], t1[c % 2][:], wout_d[c * 128:(c + 1) * 128, :])
            kb.op("dve", [Bws[c % 2], Bc4], [], lambda e, c=c: e.tensor_tensor(out=wout[:, c, :], in0=t1[c % 2][:], in1=g1bc[:], op=ALU.mult),
                  pwrites=[Bc4])
        kb.dma("sp", [B_mod], [], sh2bc[:], bcast_row(mod_d[0:1, 3072:4096], D), pwrites=[Bc4])
        kb.dma("sp", [B_mod], [], sc2bc[:], bcast_row(mod_d[0:1, 4096:5120], D), pwrites=[Bc4])
        kb.dma("sp", [], [], lngbc[:], bcast_row(ln1g_d[0:1, :], D), pwrites=[Bc4])
        kb.dma("sp", [], [], lnbbc[:], bcast_row(ln1b_d[0:1, :], D), pwrites=[Bc4])
        kb.dma("sp", [], [], wr[:], wr_d.rearrange("(k p) e -> p k e", p=128), pwrites=[Bc4])
        kb.dma("sp", [], [], ident[:], ident_d[:, :], pwrites=[Bc4])
        kb.op("dve", [Bc4], [], lambda e: e.tensor_scalar(out=sc2bc[:], in0=sc2bc[:], scalar1=1.0, scalar2=None, op0=ALU.add), pwrites=[Bc4])
        attn_v = attnT_d.rearrange("c p n -> p c n")
        x_t = x_d.rearrange("(t p) d -> t p d", p=128)
        x1_t = x1_d.rearrange("(t p) d -> t p d", p=128)
        h2_t = h2_d.rearrange("(t p) d -> t p d", p=128)
        stA = {}
        stC = {}

        mixb = {}

        def A_mix(t):
            g, i = t // 4, t % 4
            if i == 0:
                kb.dma("sp", [B_attn], [Bat[g % 2]], at[g % 2][:], attn_v[:, :, g * 512:(g + 1) * 512])
            b = t % 2
            kb.dma("sp", [], [Bxt[b]], xt[b][:], x_t[t])
            bk2, bb2 = bank2()

            def mm(e):
                for half in range(2):
                    for c in range(8):
                        ins = e.matmul(bk2[:, half * 512:(half + 1) * 512], at[g % 2][:, c, i * 128:(i + 1) * 128],
                                       wout[:, c, half * 512:(half + 1) * 512], start=(c == 0), stop=(c == 7))
                return ins
            kb.op("pe", [Bat[g % 2], Bc4], bb2, mm)
            mixb[t] = (bk2, bb2)

        def A_y(t):
            b = t % 2
            bk2, bb2 = mixb.pop(t)
            kb.op("dve", bb2 + [Bxt[b]] + Bws, [By[b]], lambda e: e.scalar_tensor_tensor(
                out=yy[b][:], in0=xt[b][:], scalar=ALPHA, in1=bk2, op0=ALU.mult, op1=ALU.add))

        def A_stats(t):
            b = t % 2
            stA[t] = ln_stats(L, yy[b], By[b])

        def B1_smalls(t):
            st, Bst = stA[t]
            kb.op("dve", [Bst], [], lambda e: e.tensor_scalar(out=st[:, 2:3], in0=st[:, 0:1], scalar1=1.0 / D, scalar2=None, op0=ALU.mult), pwrites=[Bst])
            kb.op("dve", [Bst], [], lambda e: e.tensor_tensor(out=st[:, 3:4], in0=st[:, 2:3], in1=st[:, 2:3], op=ALU.mult), pwrites=[Bst])
            kb.op("dve", [Bst], [], lambda e: e.scalar_tensor_tensor(out=st[:, 4:5], in0=st[:, 1:2], scalar=1.0 / D, in1=st[:, 3:4],
                                                                       op0=ALU.mult, op1=ALU.subtract), pwrites=[Bst])
            kb.op("dve", [Bst], [], lambda e: e.tensor_scalar(out=st[:, 4:5], in0=st[:, 4:5], scalar1=EPS, scalar2=None, op0=ALU.add), pwrites=[Bst])

        def B1_lnexp(t):
            st, Bst = stA[t]
            kb.op("act", [Bst], [], lambda e: e.activation(out=st[:, 5:6], in_=st[:, 4:5], func=AF.Ln), pwrites=[Bst])
            kb.op("act", [Bst], [], lambda e: e.activation(out=st[:, 5:6], in_=st[:, 5:6], func=AF.Exp, scale=-0.5), pwrites=[Bst])

        def B1_nmr(t):
            st, Bst = stA[t]
            kb.op("dve", [Bst], [], lambda e: e.scalar_tensor_tensor(out=st[:, 6:7], in0=st[:, 2:3], scalar=-1.0, in1=st[:, 5:6],
                                                                       op0=ALU.mult, op1=ALU.mult), pwrites=[Bst])

        def B1_xh(t):
            b = t % 2
            st, Bst = stA.pop(t)
            kb.op("act", [By[b], Bst], [Bxh[b]], lambda e: e.activation(out=xh[b][:], in_=yy[b][:], func=AF.Identity,
                                                                        scale=st[:, 5:6], bias=st[:, 6:7]))

        def B2_dve(t):
            b = t % 2
            kb.op("dve", [Bxh[b], Bc4], [Bx1[b]], lambda e: e.tensor_tensor(out=x1[b][:], in0=xh[b][:], in1=lngbc[:], op=ALU.mult))
            kb.op("dve", [Bx1[b], Bc4], [Bx1[b]], lambda e: e.tensor_tensor(out=x1[b][:], in0=x1[b][:], in1=lnbbc[:], op=ALU.add))
            kb.dma("sp", [Bx1[b]], [], x1_t[t], x1[b][:], pwrites=[B_x1])
            kb.op("dve", [Bx1[b], Bc4], [Bh2f[b]], lambda e: e.tensor_tensor(out=h2f[b][:], in0=x1[b][:], in1=sc2bc[:], op=ALU.mult))
            kb.op("dve", [Bh2f[b], Bc4], [Bh2f[b]], lambda e: e.tensor_tensor(out=h2f[b][:], in0=h2f[b][:], in1=sh2bc[:], op=ALU.add))

        def B2_cast(t):
            b = t % 2
            kb.op("act", [Bh2f[b]], [Bh2b[b]], lambda e: e.copy(out=h2b[b][:], in_=h2f[b][:]))
            kb.dma("sp", [Bh2b[b]], [], h2_t[t], h2b[b][:], pwrites=[B_h2])

        def B2_tp(t):
            b = t % 2
            bk2, bb2 = bank2()

            def tp(e):
                for k in range(8):
                    ins = e.transpose(bk2[:, k * 128:(k + 1) * 128], h2f[b][:, k * 128:(k + 1) * 128], ident[:])
                return ins
            kb.op("pe", [Bh2f[b], Bc4], bb2, tp)
            stC[t] = (bk2, bb2)

        lgb = {}

        def C_copy(t):
            b = t % 2
            bk2, bb2 = stC.pop(t)
            kb.op("act", bb2, [Bh2T[b]], lambda e: e.copy(out=h2T[b][:].rearrange("p k n -> p (k n)"), in_=bk2))

        def C_logits(t):
            b = t % 2
            bk, bb = bank()

            def lg(e):
                for k in range(8):
                    ins = e.matmul(bk[:, 0:NE], h2T[b][:, k, :], wr[:, k, :], start=(k == 0), stop=(k == 7))
                return ins
            kb.op("pe", [Bh2T[b], Bc4], [bb], lg)
            lgb[t] = (bk, bb)

        def C_max(t):
            b = t % 2
            bk, bb = lgb[t]
            kb.op("dve", [], [Bsm[b]], lambda e: e.memset(sm[b][:], 0.0))
            kb.op("dve", [bb], [], lambda e: e.reduce_max(out=sm[b][:, 0:1], in_=bk[:, 0:NE], axis=AX.X), pwrites=[Bsm[b]])
            kb.op("dve", [Bsm[b]], [], lambda e: e.tensor_scalar(out=sm[b][:, 1:2], in0=sm[b][:, 0:1], scalar1=-1.0, scalar2=None, op0=ALU.mult),
                  pwrites=[Bsm[b]])

        def C_exp(t):
            b = t % 2
            bk, bb = lgb.pop(t)
            kb.op("act", [bb, Bsm[b]], [Bee[b]], lambda e: e.activation(out=ee[b][:], in_=bk[:, 0:NE], func=AF.Exp, bias=sm[b][:, 1:2],
                                                                       accum_out=sm[b][:, 2:3]), pwrites=[Bsm[b]])

        def C_aff(t):
            b = t % 2
            kb.op("dve", [Bsm[b]], [], lambda e: e.reciprocal(out=sm[b][:, 3:4], in_=sm[b][:, 2:3]), pwrites=[Bsm[b]])
            kb.op("dve", [Bee[b], Bsm[b]], [], lambda e: e.tensor_scalar(out=aff_all[:, t * NE:(t + 1) * NE], in0=ee[b][:], scalar1=sm[b][:, 3:4],
                                                                        scalar2=None, op0=ALU.mult), pwrites=[B_aff])

        for step in range(NT + 3):
            a, b1, b2, c = step, step - 1, step - 2, step - 3
            ok = lambda t: 0 <= t < NT
            if ok(a):
                A_mix(a)
            if ok(c):
                C_copy(c)
            if ok(b2):
                B2_dve(b2)
            if ok(c):
                C_logits(c)
            if ok(a):
                A_y(a)
            if ok(b2):
                B2_cast(b2)
                B2_tp(b2)
            if ok(b1):
                B1_smalls(b1)
            if ok(a):
                A_stats(a)
            if ok(c):
                C_max(c)
            if ok(b1):
                B1_lnexp(b1)
            if ok(c):
                C_exp(c)
            if ok(b1):
                B1_nmr(b1)
            if ok(c):
                C_aff(c)
            if ok(b1):
                B1_xh(b1)
        if "aff_s" in dbg:
            kb.dma("sp", [B_aff], [Buf()], aff_d[:, :], aff_all[:])
        kb.barrier()
    if stages <= 4:
        _finish(nc, kb, out_d, x_d)
        return

    def moe_phase():
        with ExitStack() as s5:
            A = lambda n, sh, dt: s5.enter_context(nc.sbuf_tensor("p5_" + n, sh, dt))
            onesf = A("onesf", [128, 128], F32)
            onesb = A("onesb", [128, 128], BF16)
            triub = A("triub", [128, 128], BF16)
            identb = A("identb", [128, 128], BF16)
            iota = A("iota", [128, 512], F32)
            tokhl = A("tokhl", [128, NT, 2], F32)
            zero = A("zero", [128, D], F32)
            lo = A("lo", [128, NE], F32)
            tau = A("tau", [128, NE], F32)
            part = A("part", [128, NE], F32)
            gw = A("gw", [128, NE], F32)
            cmp = A("cmp", [128, NT * NE], F32)
            self_ = A("self", [128, NT * NE], F32)
            selb = A("selb", [128, NT * NE], BF16)
            posm = A("posm", [128, NT * NE], F32)
            r1 = A("r1", [128, NT * NE], F32)
            R = A("R", [128, NT * NE * 5], BF16)
            OH = [A("OH%d" % i, [128, 512], BF16) for i in range(4)]
            idxv = [A("idxv%d" % i, [128, 32], F32) for i in range(2)]
            idxf = [A("idxf%d" % i, [128, 4], F32) for i in range(2)]
            idxi = [A("idxi%d" % i, [128, 4], I32) for i in range(2)]
            val = [A("val%d" % i, [128, 4], F32) for i in range(2)]
            xin = [A("xin%d" % i, [128, 4, D], BF16) for i in range(2)]
            xinT = [A("xinT%d" % i, [128, 8, 512], BF16) for i in range(2)]
            wg = [A("wg%d" % i, [128, 8, 512], BF16) for i in range(3)]
            wu = [A("wu%d" % i, [128, 8, 512], BF16) for i in range(3)]
            wd = [A("wd%d" % i, [128, 6, D], BF16) for i in range(3)]
            sg = [A("sg%d" % i, [128, 512], F32) for i in range(2)]
            act = [A("act%d" % i, [128, 12, 512], BF16) for i in range(2)]
            ys = [A("ys%d" % i, [128, D], F32) for i in range(4)]
            mo = [A("mo%d" % i, [128, D], F32) for i in range(4)]
            Bc = Buf()
            Blo, Btau, Bpart, Bgw, Bcmp, Bsel, Bposm, BR = (Buf() for _ in range(8))
            BOH = [Buf() for _ in range(4)]
            Bidxv, Bidx, Bval = [Buf(), Buf()], [Buf(), Buf()], [Buf(), Buf()]
            Bxin, BxinT = [Buf(), Buf()], [Buf(), Buf()]
            Bwg, Bwu, Bwd = [Buf() for _ in range(3)], [Buf() for _ in range(3)], [Buf() for _ in range(3)]
            Bsg, Bact = [Buf(), Buf()], [Buf(), Buf()]
            Bys, Bmo = [Buf() for _ in range(4)], [Buf() for _ in range(4)]

            kb.op("pool", [], [], lambda e: e.memset(onesf[:], 1.0), pwrites=[Bc])
            kb.op("pool", [], [], lambda e: e.memset(onesb[:], 1.0), pwrites=[Bc])
            kb.op("pool", [], [], lambda e: e.memset(zero[:], 0.0), pwrites=[Bc])
            kb.op("pool", [], [Blo], lambda e: e.memset(lo[:], 0.0))
            kb.dma("pool", [], [], triub[:], triu_d[:, :], pwrites=[Bc])
            kb.dma("pool", [], [], identb[:], ident_d[:, :], pwrites=[Bc])
            kb.dma("sp", [], [], iota[:], iota_d[:, :], pwrites=[Bc])
            kb.dma("sp", [], [], tokhl[:], tokhl_d[:, :, :], pwrites=[Bc])
            moe_t = moe_d.rearrange("(t p) d -> t p d", p=128)
            for t in range(NT):
                kb.dma("sp", [Bc], [], moe_t[t], zero[:], pwrites=[B_moe])

            def issue_gu(n):
                if n >= NE * 3:
                    return
                e, fb = n // 3, n % 3
                b = n % 3
                kb.dma("pool", [], [Bwg[b]], wg[b][:], wg_d[e, :, fb * 512:(fb + 1) * 512].rearrange("(k p) f -> p k f", p=128))
                kb.dma("pool", [], [Bwu[b]], wu[b][:], wu_d[e, :, fb * 512:(fb + 1) * 512].rearrange("(k p) f -> p k f", p=128))

            def issue_d(m):
                if m >= NE * 2:
                    return
                e, half = m // 2, m % 2
                b = m % 3
                kb.dma("pool", [], [Bwd[b]], wd[b][:], wd_d[e, half * 768:(half + 1) * 768, :].rearrange("(k p) n -> p k n", p=128))

            issue_gu(0)
            issue_gu(1)
            issue_d(0)
            issue_d(1)
            issue_d(2)

            aff3 = AP(aff_all, 0, [pdim(aff_all[:]), [NE, NT], [1, NE]])
            cmp3 = AP(cmp, 0, [pdim(cmp[:]), [NE, NT], [1, NE]])
            cmpT = AP(cmp, 0, [pdim(cmp[:]), [1, NE], [NE, NT]])

            def bc16(tile_):
                return AP(tile_, 0, [pdim(tile_[:]), [0, NT], [1, NE]])

            for it in range(1, NBIS + 1):
                w = 2.0 ** (-it)
                kb.op("dve", [Blo], [Btau], lambda e: e.tensor_scalar(out=tau[:], in0=lo[:], scalar1=w, scalar2=None, op0=ALU.add))
                kb.op("dve", [B_aff, Btau], [Bcmp], lambda e: e.tensor_tensor(out=cmp3, in0=aff3, in1=bc16(tau), op=ALU.is_ge))
                kb.op("dve", [Bcmp], [Bpart], lambda e: e.tensor_reduce(out=part[:], in_=cmpT, axis=AX.X, op=ALU.add))
                bk, bb = bank()
                kb.op("pe", [Bpart, Bc], [bb], lambda e: e.matmul(bk[:, 0:NE], onesf[:], part[:], start=True, stop=True))
                kb.op("dve", [bb], [Bgw], lambda e: e.tensor_scalar(out=gw[:], in0=bk[:, 0:NE], scalar1=CAP - 0.5, scalar2=w,
                                                                  op0=ALU.is_ge, op1=ALU.mult))
                kb.op("dve", [Bgw, Blo], [Blo], lambda e: e.tensor_tensor(out=lo[:], in0=lo[:], in1=gw[:], op=ALU.add))
            self3 = AP(self_, 0, [pdim(self_[:]), [NE, NT], [1, NE]])
            kb.op("dve", [B_aff, Blo], [Bsel], lambda e: e.tensor_tensor(out=self3, in0=aff3, in1=bc16(lo), op=ALU.is_ge))
            kb.op("dve", [Bsel], [], lambda e: e.tensor_copy(out=selb[:], in_=self_[:]), pwrites=[Bsel])
            bk, bb = bank()

            def prefix(e):
                for t in range(NT):
                    o = bk[:, t * NE:(t + 1) * NE]
                    for tp in range(t):
                        e.matmul(o, onesb[:], selb[:, tp * NE:(tp + 1) * NE], start=(tp == 0), stop=False)
                    ins = e.matmul(o, triub[:], selb[:, t * NE:(t + 1) * NE], start=(t == 0), stop=True)
                return ins
            kb.op("pe", [Bsel, Bc], [bb], prefix)
            kb.op("dve", [bb, Bsel], [Bposm], lambda e: e.scalar_tensor_tensor(out=posm[:], in0=bk, scalar=1.0, in1=self_[:],
                                                                            op0=ALU.add, op1=ALU.mult))
            kb.op("dve", [Bposm], [Bposm], lambda e: e.tensor_scalar(out=posm[:], in0=posm[:], scalar1=-1.0, scalar2=None, op0=ALU.add))
            Rv = lambda j: AP(R, j, [pdim(R[:]), [5, NT * NE]])
            kb.op("dve", [Bc], [BR], lambda e: e.tensor_copy(
                out=AP(R, 0, [pdim(R[:]), [5 * NE, NT], [5, NE], [1, 2]]),
                in_=AP(tokhl, 0, [pdim(tokhl[:]), [2, NT], [0, NE], [1, 2]])))
            kb.op("dve", [B_aff], [], lambda e: e.tensor_copy(out=Rv(2), in_=aff_all[:]), pwrites=[BR])
            kb.op("dve", [B_aff, BR], [Bcmp], lambda e: e.tensor_tensor(out=r1[:], in0=aff_all[:], in1=Rv(2), op=ALU.subtract))
            kb.op("dve", [Bcmp], [], lambda e: e.tensor_copy(out=Rv(3), in_=r1[:]), pwrites=[BR])
            kb.op("dve", [Bcmp, BR], [Bcmp], lambda e: e.tensor_tensor(out=r1[:], in0=r1[:], in1=Rv(3), op=ALU.subtract))
            kb.op("dve", [Bcmp], [], lambda e: e.tensor_copy(out=Rv(4), in_=r1[:]), pwrites=[BR])

            h2rows = h2_d
            oh_ctr = [0]

            rstate = {}

            def route_tiles(e, t0, t1):
                for t in range(t0, t1):
                    oi = oh_ctr[0] % 4
                    oh_ctr[0] += 1
                    kb.op("dve", [Bposm, Bc], [BOH[oi]], lambda en: en.tensor_scalar(
                        out=OH[oi][:], in0=iota[:], scalar1=posm[:, t * NE + e:t * NE + e + 1], scalar2=None, op0=ALU.is_equal))

                    def mm(en):
                        for cj in range(4):
                            c0 = 3072 + (cj * NT + t) * 8
                            ins = en.matmul(ps[:, c0:c0 + 5], OH[oi][:, cj * 128:(cj + 1) * 128],
                                            R[:, (t * NE + e) * 5:(t * NE + e) * 5 + 5], start=True, stop=True)
                        return ins
                    if t == 0:
                        kb.op("pe", [BOH[oi], BR], [PB[6], PB[7]], mm)
                    else:
                        kb.op("pe", [BOH[oi], BR], [], mm, pwrites=[PB[6], PB[7]])

            def route_idx(e):
                b = e % 2
                kb.op("dve", [PB[6], PB[7]], [Bidxv[b]], lambda en: en.tensor_reduce(
                    out=AP(idxv[b], 0, [pdim(idxv[b][:]), [8, 4], [1, 5]]),
                    in_=AP(ps, 3072, [pdim(ps[:]), [NT * 8, 4], [1, 5], [8, NT]]),
                    axis=AX.X, op=ALU.add))
                v3 = lambda j: AP(idxv[b], j, [pdim(idxv[b][:]), [8, 4]])
                kb.op("dve", [Bidxv[b]], [Bidx[b]], lambda en: en.scalar_tensor_tensor(
                    out=idxf[b][:], in0=v3(0), scalar=128.0, in1=v3(1), op0=ALU.mult, op1=ALU.add))
                kb.op("dve", [Bidx[b]], [], lambda en: en.tensor_copy(out=idxi[b][:], in_=idxf[b][:]), pwrites=[Bidx[b]])
                kb.op("dve", [Bidxv[b]], [Bval[b]], lambda en: en.tensor_tensor(out=val[b][:], in0=v3(2), in1=v3(3), op=ALU.add))
                kb.op("dve", [Bidxv[b], Bval[b]], [Bval[b]], lambda en: en.tensor_tensor(out=val[b][:], in0=val[b][:], in1=v3(4), op=ALU.add))
                for cj in range(4):
                    kb.dma("pool", [Bidx[b], B_h2], [] if cj else [Bxin[b]], xin[b][:, cj, :], h2rows[:, :],
                           pwrites=[Bxin[b]] if cj else [],
                           indirect=dict(out_offset=None, in_offset=bass.IndirectOffsetOnAxis(ap=idxi[b][:, cj:cj + 1], axis=0)))

            def route_T(e):
                b = e % 2
                for k in range(8):
                    bk2, bb2 = bank()
                    bkb = bk2.bitcast(BF16)

                    def tp(en):
                        for cj in range(4):
                            ins = en.transpose(bkb[:, cj * 128:(cj + 1) * 128], xin[b][:, cj, k * 128:(k + 1) * 128], identb[:])
                        return ins
                    kb.op("pe", [Bxin[b], Bc], [bb2], tp)
                    kb.op("act", [bb2], [] if k else [BxinT[b]],
                          (lambda en: en.copy(out=xinT[b][:, k, :], in_=bkb[:, 0:512])),
                          pwrites=[BxinT[b]] if k else [])

            def route(e):
                route_tiles(e, 0, NT)
                route_idx(e)
                route_T(e)

            def tail_gather(e):
                b = e % 2
                for cj in range(4):
                    kb.dma("pool", [Bidx[b], B_moe], [Bmo[cj]], mo[cj][:, :], moe_d[:, :],
                           indirect=dict(out_offset=None, in_offset=bass.IndirectOffsetOnAxis(ap=idxi[b][:, cj:cj + 1], axis=0)))

            def tail_add_scatter(e, cjs=(0, 1, 2, 3)):
                b = e % 2
                for cj in cjs:
                    kb.op("dve", [Bmo[cj], Bys[cj]], [Bmo[cj]], lambda en: en.tensor_tensor(out=mo[cj][:], in0=mo[cj][:], in1=ys[cj][:], op=ALU.add))
                for cj in cjs:
                    kb.dma("pool", [Bidx[b], Bmo[cj]], [], moe_d[:, :], mo[cj][:, :], pwrites=[B_moe],
                           indirect=dict(out_offset=bass.IndirectOffsetOnAxis(ap=idxi[b][:, cj:cj + 1], axis=0), in_offset=None))

            def compute(e):
                b = e % 2
                for fb in range(3):
                    n = e * 3 + fb
                    wb = n % 3
                    for j in range(4):
                        bg, bbg = bank()
                        bu, bbu = bank()

                        def mm(en):
                            for bk_, w_ in ((bg, wg[wb]), (bu, wu[wb])):
                                for k in range(8):
                                    ins = en.matmul(bk_, w_[:, k, j * 128:(j + 1) * 128], xinT[b][:, k, :], start=(k == 0), stop=(k == 7))
                            return ins
                        kb.op("pe", [BxinT[b], Bwg[wb], Bwu[wb]], [bbg, bbu], mm)
                        if e + 1 < NE:
                            if fb * 4 + j < 11:
                                route_tiles(e + 1, 3 * (fb * 4 + j), min(NT, 3 * (fb * 4 + j) + 3))
                            else:
                                route_idx(e + 1)
                        if e > 0 and 2 <= fb * 4 + j < 6:
                            tail_add_scatter(e - 1, (fb * 4 + j - 2,))
                        si = (fb * 4 + j) % 2
                        kb.op("act", [bbg], [Bsg[si]], lambda en: en.activation(out=sg[si][:], in_=bg, func=AF.Silu))
                        fi = fb * 4 + j
                        kb.op("dve", [Bsg[si], bbu], [] if fi else [Bact[b]], lambda en: en.tensor_tensor(
                            out=act[b][:, fi, :], in0=sg[si][:], in1=bu, op=ALU.mult), pwrites=[Bact[b]] if fi else [])
                    issue_gu(n + 2)
                for cj in range(4):
                    yi = cj
                    for half in range(2):
                        bk, bb = bank()

                        def mm(en):
                            for fi in range(12):
                                m = e * 2 + fi // 6
                                ins = en.matmul(bk, act[b][:, fi, cj * 128:(cj + 1) * 128], wd[m % 3][:, fi % 6, half * 512:(half + 1) * 512],
                                                start=(fi == 0), stop=(fi == 11))
                            return ins
                        kb.op("pe", [Bact[b], Bwd[(e * 2) % 3], Bwd[(e * 2 + 1) % 3]], [bb], mm)
                        kb.op("act", [bb, Bval[b]], [] if half else [Bys[yi]], lambda en: en.activation(
                            out=ys[yi][:, half * 512:(half + 1) * 512], in_=bk, func=AF.Copy, scale=val[b][:, cj:cj + 1]),
                            pwrites=[Bys[yi]] if half else [])
                issue_d(e * 2 + 3)
                issue_d(e * 2 + 4)
                if e + 1 < NE:
                    route_T(e + 1)
                tail_gather(e)
                if e == NE - 1:
                    tail_add_scatter(e)

            nrot[0] = 6
            bank_ctr[0] = 0
            route(0)
            for e in range(int(os.environ.get("NEXP", str(NE)))):
                compute(e)
            kb.barrier()
            nrot[0] = 8

    if os.environ.get("SKIPMOE", "") == "":
        moe_phase()

    with ExitStack() as s6:
        A = lambda n, sh, dt: s6.enter_context(nc.sbuf_tensor("p6_" + n, sh, dt))
        g2bc, lngbc, lnbbc = (A(n, [128, D], F32) for n in ("g2bc", "lngbc", "lnbbc"))
        NB6 = 3
        xt = [A("xt%d" % i, [128, D], F32) for i in range(NB6)]
        mt = [A("mt%d" % i, [128, D], F32) for i in range(NB6)]
        yy = [A("y%d" % i, [128, D], F32) for i in range(NB6)]
        xh = [A("xh%d" % i, [128, D], F32) for i in range(NB6)]
        oo = [A("o%d" % i, [128, D], F32) for i in range(NB6)]
        L = LNState(A, "ln", 3)
        Bc6 = Buf()
        Bxt, Bmt, By, Bxh, Boo = ([Buf() for _ in range(NB6)] for _ in range(5))
        kb.dma("sp", [B_mod], [], g2bc[:], bcast_row(mod_d[0:1, 5120:6144], D), pwrites=[Bc6])
        kb.dma("sp", [], [], lngbc[:], bcast_row(ln2g_d[0:1, :], D), pwrites=[Bc6])
        kb.dma("sp", [], [], lnbbc[:], bcast_row(ln2b_d[0:1, :], D), pwrites=[Bc6])
        x1_t = x1_d.rearrange("(t p) d -> t p d", p=128)
        moe_t = moe_d.rearrange("(t p) d -> t p d", p=128)
        out_t = out_d.rearrange("(t p) d -> t p d", p=128)
        stA = {}

        def ln_smalls(st, Bst):
            kb.op("dve", [Bst], [], lambda e: e.tensor_scalar(out=st[:, 2:3], in0=st[:, 0:1], scalar1=1.0 / D, scalar2=None, op0=ALU.mult), pwrites=[Bst])
            kb.op("dve", [Bst], [], lambda e: e.tensor_tensor(out=st[:, 3:4], in0=st[:, 2:3], in1=st[:, 2:3], op=ALU.mult), pwrites=[Bst])
            kb.op("dve", [Bst], [], lambda e: e.scalar_tensor_tensor(out=st[:, 4:5], in0=st[:, 1:2], scalar=1.0 / D, in1=st[:, 3:4],
                                                                       op0=ALU.mult, op1=ALU.subtract), pwrites=[Bst])
            kb.op("dve", [Bst], [], lambda e: e.tensor_scalar(out=st[:, 4:5], in0=st[:, 4:5], scalar1=EPS, scalar2=None, op0=ALU.add), pwrites=[Bst])

        def A6_load(t):
            b = t % NB6
            kb.dma("sp", [B_x1], [Bxt[b]], xt[b][:], x1_t[t])
            kb.dma("sp", [B_moe], [Bmt[b]], mt[b][:], moe_t[t])

        def A6_dve(t):
            b = t % NB6
            kb.op("dve", [Bmt[b], Bc6], [Bmt[b]], lambda e: e.tensor_tensor(out=mt[b][:], in0=mt[b][:], in1=g2bc[:], op=ALU.mult))
            kb.op("dve", [Bxt[b], Bmt[b]], [By[b]], lambda e: e.scalar_tensor_tensor(
                out=yy[b][:], in0=xt[b][:], scalar=ALPHA, in1=mt[b][:], op0=ALU.mult, op1=ALU.add))

        def C6_dve(t):
            b = t % NB6
            kb.op("dve", [Bxh[b], Bc6], [Boo[b]], lambda e: e.tensor_tensor(out=oo[b][:], in0=xh[b][:], in1=lngbc[:], op=ALU.mult))
            kb.op("dve", [Boo[b], Bc6], [Boo[b]], lambda e: e.tensor_tensor(out=oo[b][:], in0=oo[b][:], in1=lnbbc[:], op=ALU.add))
            kb.dma("sp", [Boo[b]], [Buf()], out_t[t], oo[b][:])

        for step in range(NT + 2):
            a, b1, c = step, step - 1, step - 2
            ok = lambda t: 0 <= t < NT
            if ok(a):
                A6_load(a)
            if ok(c):
                C6_dve(c)
            if ok(a):
                A6_dve(a)
            if ok(b1):
                ln_smalls(*stA[b1])
            if ok(a):
                stA[a] = ln_stats(L, yy[a % NB6], By[a % NB6])
            if ok(b1):
                st, Bst = stA.pop(b1)
                bb_ = b1 % NB6
                kb.op("act", [Bst], [], lambda e: e.activation(out=st[:, 5:6], in_=st[:, 4:5], func=AF.Ln), pwrites=[Bst])
                kb.op("act", [Bst], [], lambda e: e.activation(out=st[:, 5:6], in_=st[:, 5:6], func=AF.Exp, scale=-0.5), pwrites=[Bst])
                kb.op("dve", [Bst], [], lambda e: e.scalar_tensor_tensor(out=st[:, 6:7], in0=st[:, 2:3], scalar=-1.0, in1=st[:, 5:6],
                                                                           op0=ALU.mult, op1=ALU.mult), pwrites=[Bst])
                kb.op("act", [By[bb_], Bst], [Bxh[bb_]], lambda e: e.activation(out=xh[bb_][:], in_=yy[bb_][:], func=AF.Identity,
                                                                              scale=st[:, 5:6], bias=st[:, 6:7]))
        kb.barrier()


def _finish(nc, kb, out_d, x_d):
    kb.barrier()
    tok = kb.dma("sp", [], [Buf()], out_d[0:128, :], x_d[0:128, :])
    kb._wait("sp", [tok])


def _t5_bucket(rel):
    half = 16
    ret = (rel > 0).astype(np.int32) * half
    n = np.abs(rel)
    large = 8 + (np.log(np.maximum(n, 1) / 8) / np.log(1024 / 8) * (half - 8)).astype(np.int32)
    large = np.minimum(large, half - 1)
    return ret + np.where(n < 8, n, large).astype(np.int32)


def _static_tables():
    a = np.arange(128)[:, None]
    c = np.arange(MW)[None, :]
    rel = a - c + C0
    mult = ((np.abs(rel) <= 64).astype(np.float32)
            + ((rel % 4 == 0) & (np.abs(rel) <= 256)).astype(np.float32)
            + ((rel % 16 == 0) & (np.abs(rel) <= 1024)).astype(np.float32))
    bucket = _t5_bucket(rel)
    inv = (10000.0 ** (-(np.arange(0, 32, 2, dtype=np.float32)) / np.float32(32))).astype(np.float32)
    ang = (np.arange(S, dtype=np.float32)[:, None] * inv[None, :]).astype(np.float32)
    cos = np.cos(ang.astype(np.float64)).astype(np.float32).T
    sin = np.sin(ang.astype(np.float64)).astype(np.float32).T
    rope_cos = np.ascontiguousarray(np.concatenate([cos, cos], axis=0))
    rope_sin = np.ascontiguousarray(np.concatenate([-sin, sin], axis=0))
    tokhl = np.zeros((128, NT, 2), np.float32)
    tokhl[:, :, 0] = np.arange(NT)[None, :]
    tokhl[:, :, 1] = np.arange(128)[:, None]
    return dict(
        toep_mult=mult.astype(np.float32), bucket=bucket,
        rope_cos=rope_cos, rope_sin=rope_sin,
        ident=np.eye(128, dtype=np.float32),
        triu=np.triu(np.ones((128, 128), np.float32), k=1),
        iota512=np.tile(np.arange(512, dtype=np.float32)[None, :], (128, 1)),
        tokhl=tokhl,
    )


def make_in_maps(inputs):
    f = lambda a: np.ascontiguousarray(np.asarray(a, dtype=np.float32))
    st = _static_tables()
    x = f(inputs["x"])
    c = f(inputs["c"])
    w_in = f(inputs["w_in"][0])
    kr = w_in[:, 640:672]
    w_kr = np.zeros((D, 192), np.float32)
    w_kr[:, 64:96] = kr
    w_kr[:, 96 + 64:96 + 80] = kr[:, 16:32]
    w_kr[:, 96 + 80:96 + 96] = kr[:, 0:16]
    w_uq = f(inputs["w_uq"][0]).reshape(384, 8, 96)
    w_uq_rot = w_uq.copy()
    w_uq_rot[:, :, 64:80] = w_uq[:, :, 80:96]
    w_uq_rot[:, :, 80:96] = w_uq[:, :, 64:80]
    w_uq2 = np.ascontiguousarray(np.stack([w_uq.reshape(384, 768), w_uq_rot.reshape(384, 768)], axis=1))
    w_ukv = f(inputs["w_ukv"][0]).reshape(256, 8, 128)
    rel_bias = f(inputs["rel_bias"])
    toep = np.ascontiguousarray(np.transpose(rel_bias[st["bucket"]], (2, 0, 1)))
    shared = dict(
        w_ada=f(inputs["w_ada"][0]), b_ada=f(inputs["b_ada"][0]).reshape(1, -1),
        w_in=w_in, w_kr=w_kr,
        g_q=np.ascontiguousarray(f(inputs["q_norm_g"][0]).reshape(3, 128).T),
        g_kv=np.ascontiguousarray(f(inputs["kv_norm_g"][0]).reshape(2, 128).T),
        w_uq2=w_uq2,
        w_ukv_k=np.ascontiguousarray(w_ukv[:, :, 0:64].reshape(256, 512)),
        w_ukv_v=np.ascontiguousarray(w_ukv[:, :, 64:128].reshape(256, 512)),
        rope_cos=st["rope_cos"], rope_sin=st["rope_sin"],
        relb_toep=toep, toep_mult=st["toep_mult"],
        w_out=f(inputs["w_out"][0]), ln1_g=f(inputs["ln1_g"][0]).reshape(1, -1), ln1_b=f(inputs["ln1_b"][0]).reshape(1, -1),
        w_router=f(inputs["w_router"][0]),
        w_gate=f(inputs["w_gate"][0]), w_up=f(inputs["w_up"][0]), w_down=f(inputs["w_down"][0]),
        ln2_g=f(inputs["ln2_g"][0]).reshape(1, -1), ln2_b=f(inputs["ln2_b"][0]).reshape(1, -1),
        ident=st["ident"], triu=st["triu"], iota512=st["iota512"], tokhl=st["tokhl"],
    )
    maps = []
    for b in range(x.shape[0]):
        m = dict(shared)
        m["x"] = x[b]
        m["c_fm"] = np.ascontiguousarray(c[b].reshape(8, 128).T)
        maps.append(m)
    return maps


def kernel(**inputs):
    maps = make_in_maps(inputs)
    nc = build_program()
    res = run_bass_kernel_spmd(nc, maps, core_ids=list(range(len(maps))))
    return np.stack([np.asarray(r["out"], dtype=np.float32) for r in res.results], axis=0)
```

```python
import math
import os
from contextlib import ExitStack

import numpy as np
import concourse.bass as bass
import concourse.mybir as mybir
from concourse.bass_utils import run_bass_kernel_spmd

F32 = mybir.dt.float32
BF16 = mybir.dt.bfloat16
I32 = mybir.dt.int32
AF = mybir.ActivationFunctionType
ALU = mybir.AluOpType
AX = mybir.AxisListType

D = 1024
S = 4096
NT = 32
NG = 8
ALPHA = 2.0 ** 0.25
EPS = 1e-6
NE = 16
CAP = 512
DFF = 1536
C0 = 1408
MW = 2944
NBIS = 26


class Buf:
    __slots__ = ("w", "r", "name", "excl")

    def __init__(self, name="", excl=False):
        self.excl = excl
        self.w = []
        self.r = []
        self.name = name


def _compact(toks):
    best = {}
    for t in toks:
        if t[0] not in best or best[t[0]][2] < t[2]:
            best[t[0]] = t
    return list(best.values())


class KB:
    COMPUTE = ("pe", "act", "dve", "pool")
    NDS = 12

    def __init__(self, nc, es):
        self.nc = nc
        self.E = {"pe": nc.tensor, "act": nc.scalar, "dve": nc.vector, "pool": nc.gpsimd, "sp": nc.sync}
        self.csem = {e: es.enter_context(nc.semaphore("c_" + e)) for e in self.COMPUTE}
        self.ccnt = {e: 0 for e in self.COMPUTE}
        self.dsem = {q: [es.enter_context(nc.semaphore("d_%s%d" % (q, i))) for i in range(self.NDS)]
                     for q in ("sp", "pool", "act")}
        self.dcnt = {q: 0 for q in self.dsem}
        self.dtok = {q: [None] * self.NDS for q in self.dsem}
        self.seen = {e: {} for e in self.E}
        self.nwait = 0

    def _wait(self, eng, toks):
        need = {}
        for t in toks:
            if t is None:
                continue
            key, sem, val = t
            if self.seen[eng].get(key, 0) >= val:
                continue
            if need.get(key, (None, 0))[1] < val:
                need[key] = (sem, val)
        for key, (sem, val) in need.items():
            self.E[eng].wait_ge(sem, val)
            self.seen[eng][key] = val
            self.nwait += 1

    def _deps(self, eng, reads, writes, pwrites, is_dma):
        own = None if is_dma else "c_" + eng
        toks = []
        for b in reads:
            for t in b.w:
                if not (eng == "pe" and t[0] == own):
                    toks.append(t)
            if b.excl:
                toks.extend(t for t in b.r if t[0] != own)
        pe_own = own if eng == "pe" else None
        for b in writes:
            toks.extend(t for t in b.w if t[0] != pe_own)
            toks.extend(t for t in b.r if t[0] != pe_own)
        for b in pwrites:
            toks.extend(t for t in b.r if t[0] != pe_own)
            toks.extend(t for t in b.w[:1] if t[0] != pe_own)
        return toks

    def _commit(self, tok, reads, writes, pwrites):
        for b in reads:
            b.r.append(tok)
            if len(b.r) > 16:
                b.r = _compact(b.r)
        for b in writes:
            b.w = [tok]
            b.r = []
        for b in pwrites:
            b.w.append(tok)
            if len(b.w) > 16:
                b.w = b.w[:1] + _compact(b.w[1:])

    def op(self, eng, reads, writes, fn, pwrites=()):
        self._wait(eng, self._deps(eng, reads, writes, pwrites, False))
        inst = fn(self.E[eng])
        self.ccnt[eng] += 1
        tok = ("c_" + eng, self.csem[eng], self.ccnt[eng])
        inst.then_inc(self.csem[eng], 1)
        self._commit(tok, reads, writes, pwrites)
        return tok

    def dma(self, q, reads, writes, out, in_, pwrites=(), indirect=None, **kw):
        i = self.dcnt[q]
        slot = i % self.NDS
        self._wait(q, self._deps(q, reads, writes, pwrites, True) + [self.dtok[q][slot]])
        if indirect is None:
            inst = self.E[q].dma_start(out=out, in_=in_, **kw)
        else:
            inst = self.E[q].indirect_dma_start(out=out, in_=in_, **indirect)
        val = 16 * (i // self.NDS + 1)
        sem = self.dsem[q][slot]
        inst.then_inc(sem, 16)
        tok = ("d_%s%d" % (q, slot), sem, val)
        self.dtok[q][slot] = tok
        self.dcnt[q] += 1
        self._commit(tok, reads, writes, pwrites)
        return tok

    def barrier(self, engines=None):
        toks = []
        for e in self.COMPUTE:
            if self.ccnt[e]:
                toks.append(("c_" + e, self.csem[e], self.ccnt[e]))
        for q in self.dsem:
            toks.extend(t for t in self.dtok[q] if t is not None)
        for e in (engines or self.E):
            self._wait(e, toks)


def AP(t, off, dims):
    return bass.AP(t, off, [list(d) for d in dims])


def pdim(ap):
    return list(ap.ap[0])


def build_program(stages=99, dbg=()):
    nc = bass.Bass("TRN2", target_bir_lowering=False)
    es = ExitStack()
    with es:
        _build(nc, es, stages, dbg)
    return nc


def _build(nc, es, stages, dbg):
    def din(name, shape, dt=F32):
        return nc.dram_tensor(name, list(shape), dt, kind="ExternalInput").ap()

    def dscr(name, shape, dt):
        kind = "ExternalOutput" if name in dbg else "Internal"
        return nc.dram_tensor(name, list(shape), dt, kind=kind).ap()

    x_d = din("x", [S, D])
    cfm_d = din("c_fm", [128, 8])
    wada_d = din("w_ada", [D, 6 * D])
    bada_d = din("b_ada", [1, 6 * D])
    win_d = din("w_in", [D, 2208])
    wkr_d = din("w_kr", [D, 192])
    gq_d = din("g_q", [128, 3])
    gkv_d = din("g_kv", [128, 2])
    wuq_d = din("w_uq2", [384, 2, 768])
    wukvk_d = din("w_ukv_k", [256, 512])
    wukvv_d = din("w_ukv_v", [256, 512])
    cos_d = din("rope_cos", [32, S])
    sin_d = din("rope_sin", [32, S])
    toep_d = din("relb_toep", [8, 128, MW])
    mult_d = din("toep_mult", [128, MW])
    wout_d = din("w_out", [D, D])
    ln1g_d = din("ln1_g", [1, D])
    ln1b_d = din("ln1_b", [1, D])
    wr_d = din("w_router", [D, NE])
    wg_d = din("w_gate", [NE, D, DFF])
    wu_d = din("w_up", [NE, D, DFF])
    wd_d = din("w_down", [NE, DFF, D])
    ln2g_d = din("ln2_g", [1, D])
    ln2b_d = din("ln2_b", [1, D])
    ident_d = din("ident", [128, 128])
    triu_d = din("triu", [128, 128])
    iota_d = din("iota512", [128, 512])
    tokhl_d = din("tokhl", [128, NT, 2])
    out_d = nc.dram_tensor("out", [S, D], F32, kind="ExternalOutput").ap()

    mod_d = dscr("mod_s", [1, 6 * D], F32)
    qT_d = dscr("qT_s", [8, 96, S], BF16)
    kT_d = dscr("kT_s", [8, 96, S], BF16)
    Vm_d = dscr("Vm_s", [NT, 128, 768], BF16)
    dqT_d = dscr("dqT_s", [4, 128, S], BF16)
    dkT_d = dscr("dkT_s", [4, 128, S], BF16)
    Vd_d = dscr("Vd_s", [NT, 128, 768], BF16)
    attnT_d = dscr("attnT_s", [8, 128, S], BF16)
    x1_d = dscr("x1_s", [S, D], F32)
    h2_d = dscr("h2_s", [S, D], BF16)
    moe_d = dscr("moe_s", [S, D], F32)
    aff_d = dscr("aff_s", [128, NT * NE], F32)
    idx_d = dscr("idx_s", [128, NE * 4 * 8], F32)

    kb = KB(nc, es)
    B_mod, B_q, B_k, B_vm, B_dq, B_dk, B_vd = (Buf(n) for n in ("mod", "q", "k", "vm", "dq", "dk", "vd"))
    B_attn, B_x1, B_h2, B_moe = Buf("attn"), Buf("x1"), Buf("h2"), Buf("moe")

    ps = es.enter_context(nc.psum_tensor("ps", [128, 4096], F32))
    PB = [Buf("bank%d" % i, excl=True) for i in range(8)]
    bank_ctr = [0]

    nrot = [8]

    def bank():
        i = bank_ctr[0] % nrot[0]
        bank_ctr[0] += 1
        return ps[:, i * 512:(i + 1) * 512], PB[i]

    modfm = es.enter_context(nc.sbuf_tensor("modfm", [128, 16], F32))
    aff_all = es.enter_context(nc.sbuf_tensor("aff_all", [128, NT * NE], F32))
    B_modfm, B_aff = Buf("modfm"), Buf("aff")

    def bcast_row(dram_ap_row, n):
        return AP(dram_ap_row.tensor, dram_ap_row.offset, [[0, 128], [1, n]])

    with ExitStack() as s0:
        A = lambda n, sh, dt: s0.enter_context(nc.sbuf_tensor("p0_" + n, sh, dt))
        cfm = A("cfm", [128, 8], F32)
        sig = A("sig", [128, 8], F32)
        cond = A("cond", [128, 8], F32)
        bada = A("bada", [1, 6 * D], F32)
        modrow = A("modrow", [1, 6 * D], F32)
        wa = [A("wa%d" % i, [128, 8, 512], F32) for i in range(3)]
        Bc, Bcond, Bb, Bm = Buf(), Buf(), Buf(), Buf()
        Bwa = [Buf() for _ in range(3)]
        kb.dma("sp", [], [Bc], cfm[:], cfm_d[:, :])
        kb.dma("sp", [], [Bb], bada[:], bada_d[:, :])
        wada_v = wada_d.rearrange("(k p) n -> p k n", p=128)
        for j in range(2):
            kb.dma("sp", [], [Bwa[j]], wa[j][:], wada_v[:, :, j * 512:(j + 1) * 512])
        kb.op("act", [Bc], [Bcond], lambda e: e.activation(out=sig[:], in_=cfm[:], func=AF.Sigmoid))
        kb.op("dve", [Bc, Bcond], [Bcond], lambda e: e.tensor_tensor(out=cond[:], in0=cfm[:], in1=sig[:], op=ALU.mult))
        for j in range(12):
            if j + 2 < 12:
                kb.dma("sp", [], [Bwa[(j + 2) % 3]], wa[(j + 2) % 3][:], wada_v[:, :, (j + 2) * 512:(j + 3) * 512])
            bk, bb = bank()
            w = wa[j % 3]

            def mm(e, w=w, bk=bk):
                for k in range(8):
                    i = e.matmul(bk[0:1, :], cond[:, k:k + 1], w[:, k, :], start=(k == 0), stop=(k == 7))
                return i
            kb.op("pe", [Bcond, Bwa[j % 3]], [bb], mm)
            kb.op("dve", [bb, Bb], [], pwrites=[Bm], fn=lambda e, bk=bk, j=j: e.tensor_tensor(
                out=modrow[0:1, j * 512:(j + 1) * 512], in0=bk[0:1, :], in1=bada[0:1, j * 512:(j + 1) * 512], op=ALU.add))
        kb.dma("sp", [Bm], [B_mod], mod_d[:, :], modrow[:])
        onef = A("onef", [1, 1], F32)
        kb.op("pool", [], [Bc], lambda e: e.memset(onef[:], 1.0))
        bk, bb = bank()

        def mmT(e, bk=bk):
            for j in range(16):
                ins = e.matmul(bk[:, j:j + 1], modrow[0:1, j * 128:(j + 1) * 128], onef[0:1, 0:1], start=True, stop=True)
            return ins
        kb.op("pe", [Bm, Bc], [bb], mmT)
        kb.op("dve", [bb], [B_modfm], lambda e, bk=bk: e.tensor_copy(out=modfm[:], in_=bk[:, 0:16]))
        kb.op("dve", [B_modfm], [B_modfm], lambda e: e.tensor_scalar(
            out=modfm[:, 8:16], in0=modfm[:, 8:16], scalar1=1.0, scalar2=None, op0=ALU.add))
        kb.barrier()
    if stages <= 0:
        _finish(nc, kb, out_d, x_d)
        return

    with ExitStack() as s1:
        A = lambda n, sh, dt: s1.enter_context(nc.sbuf_tensor("p1_" + n, sh, dt))
        win = A("win", [128, 8, 2208], BF16)
        wkr = A("wkr", [128, 8, 192], BF16)
        wuq = A("wuq", [128, 3, 2, 768], BF16)
        wukvk = A("wukvk", [128, 2, 512], BF16)
        wukvv = A("wukvv", [128, 2, 512], BF16)
        ident = A("ident", [128, 128], F32)
        onesq = A("onesq", [128, 128], F32)
        oneskv = A("oneskv", [128, 128], F32)
        gq = A("gq", [128, 3], F32)
        gkv = A("gkv", [128, 2], F32)
        xg = [A("xg%d" % i, [128, 4, 1024], F32) for i in range(2)]
        hT = [A("hT%d" % i, [128, 8, 512], BF16) for i in range(2)]
        cT = A("cT", [128, 5, 512], F32)
        sq = A("sq", [128, 5, 512], F32)
        rstd = A("rstd", [128, 2, 512], F32)
        cn = [A("cn%d" % i, [128, 5, 512], BF16) for i in range(2)]
        cosr = [A("cosr%d" % i, [128, 512], F32) for i in range(2)]
        sinr = [A("sinr%d" % i, [128, 512], F32) for i in range(2)]
        tmpa = [A("tmpa%d" % i, [128, 512], F32) for i in range(2)]
        tmpb = [A("tmpb%d" % i, [128, 512], F32) for i in range(2)]
        NO = 6
        ob = [A("ob%d" % i, [128, 512], BF16) for i in range(NO)]
        vo = [A("vo%d" % i, [128, 768], BF16) for i in range(4)]
        Bw, Bid, Bones, Bg = Buf(), Buf(), Buf(), Buf()
        Bxg = [Buf(), Buf()]
        BhT = [Buf(), Buf()]
        BcT, Bsq, Brstd = [Buf() for _ in range(5)], [Buf() for _ in range(5)], [Buf(), Buf()]
        Bcn = [[Buf() for _ in range(5)] for _ in range(2)]
        Brope = [Buf(), Buf()]
        Bta, Btb = [Buf(), Buf()], [Buf(), Buf()]
        Bob = [Buf() for _ in range(NO)]
        Bvo = [Buf() for _ in range(4)]
        ob_ctr, vo_ctr, t_ctr = [0], [0], [0]

        SKIP = os.environ.get('P1SKIP', '')
        for k in range(0 if 'w' in SKIP else 8):
            kb.dma("pool", [], [], win[:, k, :], win_d[k * 128:(k + 1) * 128, :], pwrites=[Bw])
            kb.dma("pool", [], [], wkr[:, k, :], wkr_d[k * 128:(k + 1) * 128, :], pwrites=[Bw])
        for c in range(0 if 'u' in SKIP else 3):
            kb.dma("pool", [], [], wuq[:, c, :, :], wuq_d[c * 128:(c + 1) * 128, :, :], pwrites=[Bw])
        for c in range(0 if 'v' in SKIP else 2):
            kb.dma("pool", [], [], wukvk[:, c, :], wukvk_d[c * 128:(c + 1) * 128, :], pwrites=[Bw])
            kb.dma("pool", [], [], wukvv[:, c, :], wukvv_d[c * 128:(c + 1) * 128, :], pwrites=[Bw])
        kb.dma("sp", [], [Bid], ident[:], ident_d[:, :])
        kb.dma("sp", [], [], gq[:], gq_d[:, :], pwrites=[Bg])
        kb.dma("sp", [], [], gkv[:], gkv_d[:, :], pwrites=[Bg])
        kb.op("pool", [], [], lambda e: e.memset(onesq[:], 1.0 / 384.0), pwrites=[Bones])
        kb.op("pool", [], [], lambda e: e.memset(oneskv[:], 1.0 / 256.0), pwrites=[Bones])
        for i in range(4):
            kb.op("pool", [], [Bvo[i]], lambda e, i=i: e.memset(vo[i][:], 1.0))

        x_v = x_d.rearrange("(t p) d -> p t d", p=128)

        def load_x(g):
            kb.dma("sp", [], [Bxg[g % 2]], xg[g % 2][:], x_v[:, g * 4:(g + 1) * 4, :])
            kb.dma("sp", [], [Brope[g % 2]], cosr[g % 2][64:96, :], cos_d[:, g * 512:(g + 1) * 512])
            kb.dma("sp", [], [Brope[g % 2]], sinr[g % 2][64:96, :], sin_d[:, g * 512:(g + 1) * 512])

        def next_ob():
            i = ob_ctr[0] % NO
            ob_ctr[0] += 1
            return ob[i], Bob[i]

        def evac_copy(eng, src, bsrc, dst, bdst):
            if eng == "act":
                kb.op("act", [bsrc], [bdst], lambda e: e.copy(out=dst, in_=src))
            else:
                kb.op("dve", [bsrc], [bdst], lambda e: e.tensor_copy(out=dst, in_=src))

        def rope_rows(bm, bbm, br, bbr, g, dst, bdst, full):
            i = t_ctr[0] % 2
            t_ctr[0] += 1
            kb.op("dve", [bbm, Brope[g % 2]], [Bta[i]], lambda e: e.tensor_tensor(
                out=tmpa[i][64:96, :], in0=bm[64:96, :], in1=cosr[g % 2][64:96, :], op=ALU.mult))
            kb.op("dve", [bbr, Brope[g % 2]], [Btb[i]], lambda e: e.tensor_tensor(
                out=tmpb[i][64:96, :], in0=br[64:96, :], in1=sinr[g % 2][64:96, :], op=ALU.mult))
            kb.op("dve", [Bta[i], Btb[i]], [bdst] if full else [], lambda e: e.tensor_tensor(
                out=dst[64:96, :], in0=tmpa[i][64:96, :], in1=tmpb[i][64:96, :], op=ALU.add),
                pwrites=[] if full else [bdst])

        def vo_views(vt, bk):
            o = AP(vt, 0, [pdim(vt[:]), [192, 4], [128, 2], [1, 64]])
            i = AP(bk.tensor, bk.offset, [pdim(bk), [128, 4], [64, 2], [1, 64]])
            return o, i

        load_x(0)
        LIM = float(os.environ.get('P1LIM', '99'))
        SUB = float(os.environ.get('P1SUB', '99'))
        for g in range(NG if LIM >= 99 else 1):
            if g + 1 < NG:
                load_x(g + 1)
            X, BX = xg[g % 2], Bxg[g % 2]
            H, BH = hT[g % 2], BhT[g % 2]
            CN, BCN = cn[g % 2], Bcn[g % 2]
            cols = slice(g * 512, (g + 1) * 512)
            for k in range(8):
                bk, bb = bank()

                def tp(e, bk=bk, k=k):
                    for i in range(4):
                        ins = e.transpose(bk[:, i * 128:(i + 1) * 128], X[:, i, k * 128:(k + 1) * 128], ident[:])
                    return ins
                kb.op("pe", [BX, Bid], [bb], tp)
                kb.op("act", [bb, B_modfm], [BH] if k == 0 else [], lambda e, bk=bk, k=k: e.activation(
                    out=H[:, k, :], in_=bk, func=AF.Identity, scale=modfm[:, 8 + k:9 + k], bias=modfm[:, k:k + 1]),
                    pwrites=[] if k == 0 else [BH])
            if LIM < 1:
                break
            for c in range(5):
                bk, bb = bank()

                def mm(e, bk=bk, c=c):
                    for k in range(8):
                        ins = e.matmul(bk, win[:, k, c * 128:(c + 1) * 128], H[:, k, :], start=(k == 0), stop=(k == 7))
                    return ins
                kb.op("pe", [BH, Bw], [bb], mm)
                if SUB >= 0.1:
                    kb.op("act", [bb], [Bsq[c]], lambda e, bk=bk, c=c: e.activation(out=sq[:, c, :], in_=bk, func=AF.Square))
                if SUB >= 0.15:
                    kb.op("dve", [bb, Bsq[c]], [BcT[c]], lambda e, bk=bk, c=c: e.tensor_copy(out=cT[:, c, :], in_=bk))
            if SUB < 0.3:
                break
            for which, (cs, ones_t) in enumerate((((0, 1, 2), onesq), ((3, 4), oneskv))):
                bk, bb = bank()

                def mm(e, bk=bk, cs=cs, ones_t=ones_t):
                    for n, c in enumerate(cs):
                        ins = e.matmul(bk, ones_t[:], sq[:, c, :], start=(n == 0), stop=(n == len(cs) - 1))
                    return ins
                kb.op("pe", [Bsq[c] for c in cs] + [Bones], [bb], mm)
                kb.op("dve", [bb], [Brstd[which]], lambda e, bk=bk, which=which: e.tensor_scalar(
                    out=rstd[:, which, :], in0=bk, scalar1=EPS, scalar2=None, op0=ALU.add))
                if SUB < 0.5:
                    continue
                kb.op("act", [Brstd[which]], [Brstd[which]], lambda e, which=which: e.activation(
                    out=rstd[:, which, :], in_=rstd[:, which, :], func=AF.Sqrt))
                kb.op("dve", [Brstd[which]], [Brstd[which]], lambda e, which=which: e.reciprocal(
                    out=rstd[:, which, :], in_=rstd[:, which, :]))
                if SUB < 0.7:
                    continue
                for c in cs:
                    gsc = gq[:, c:c + 1] if which == 0 else gkv[:, c - 3:c - 2]
                    kb.op("dve", [BcT[c], Brstd[which], Bg], [BCN[c]], lambda e, c=c, gsc=gsc, which=which: e.scalar_tensor_tensor(
                        out=CN[:, c, :], in0=cT[:, c, :], scalar=gsc, in1=rstd[:, which, :], op0=ALU.mult, op1=ALU.mult))
            if LIM < 6:
                break
            for base, dst, bd in ((672, dqT_d, B_dq), (1184, dkT_d, B_dk)):
                for hp in range(4):
                    bk, bb = bank()

                    def mm(e, hp=hp, bk=bk, base=base):
                        for k in range(8):
                            ins = e.matmul(bk, win[:, k, base + hp * 128: base + (hp + 1) * 128], H[:, k, :],
                                           start=(k == 0), stop=(k == 7))
                        return ins
                    kb.op("pe", [BH, Bw], [bb], mm)
                    o, bo = next_ob()
                    evac_copy("act" if hp % 2 == 0 else "dve", bk, bb, o[:, :], bo)
                    kb.dma("sp", [bo], [], dst[hp, :, cols], o[:, :], pwrites=[bd])
            if LIM < 7:
                break
            for i in range(4):
                bk, bb = bank()

                def mm(e, i=i, bk=bk):
                    for k in range(8):
                        ins = e.matmul(bk, H[:, k, i * 128:(i + 1) * 128], win[:, k, 1696:2208], start=(k == 0), stop=(k == 7))
                    return ins
                kb.op("pe", [BH, Bw], [bb], mm)
                vi = vo_ctr[0] % 4
                vo_ctr[0] += 1
                ov, iv = vo_views(vo[vi], bk)
                kb.op("act", [bb], [Bvo[vi]], lambda e, ov=ov, iv=iv: e.copy(out=ov, in_=iv))
                kb.dma("sp", [Bvo[vi]], [], Vd_d[g * 4 + i, :, :], vo[vi][:], pwrites=[B_vd])
            if LIM < 2:
                break
            for h in range(8):
                bm, bbm = bank()
                br, bbr = bank()

                def mm(e, h=h, bm=bm, br=br):
                    for which, bk in ((0, bm), (1, br)):
                        for c in range(3):
                            ins = e.matmul(bk[0:96, :], wuq[:, c, which, h * 96:(h + 1) * 96], CN[:, c, :],
                                           start=(c == 0), stop=(c == 2))
                    return ins
                kb.op("pe", [BCN[0], BCN[1], BCN[2], Bw], [bbm, bbr], mm)
                o, bo = next_ob()
                kb.op("act", [bbm], [bo], lambda e, o=o, bm=bm: e.copy(out=o[0:64, :], in_=bm[0:64, :]))
                rope_rows(bm, bbm, br, bbr, g, o, bo, False)
                kb.dma("sp", [bo], [], qT_d[h, :, cols], o[0:96, :], pwrites=[B_q])
            if LIM < 3:
                break
            for hp in range(4):
                bk, bb = bank()

                def mm(e, hp=hp, bk=bk):
                    for c in range(2):
                        ins = e.matmul(bk, wukvk[:, c, hp * 128:(hp + 1) * 128], CN[:, 3 + c, :], start=(c == 0), stop=(c == 1))
                    return ins
                kb.op("pe", [BCN[3], BCN[4], Bw], [bb], mm)
                o, bo = next_ob()
                evac_copy("act", bk, bb, o[:, :], bo)
                kb.dma("sp", [bo], [], kT_d[2 * hp, 0:64, cols], o[0:64, :], pwrites=[B_k])
                kb.dma("sp", [bo], [], kT_d[2 * hp + 1, 0:64, cols], o[64:128, :], pwrites=[B_k])
            if LIM < 4:
                break
            bm, bbm = bank()
            br, bbr = bank()

            def mm(e, bm=bm, br=br):
                for which, bk in ((0, bm), (1, br)):
                    for k in range(8):
                        ins = e.matmul(bk[0:96, :], wkr[:, k, which * 96:(which + 1) * 96], H[:, k, :],
                                       start=(k == 0), stop=(k == 7))
                return ins
            kb.op("pe", [BH, Bw], [bbm, bbr], mm)
            o, bo = next_ob()
            rope_rows(bm, bbm, br, bbr, g, o, bo, True)
            for h in range(8):
                kb.dma("sp", [bo], [], kT_d[h, 64:96, cols], o[64:96, :], pwrites=[B_k])
            if LIM < 5:
                break
            for i in range(4):
                bk, bb = bank()

                def mm(e, i=i, bk=bk):
                    for c in range(2):
                        ins = e.matmul(bk, CN[:, 3 + c, i * 128:(i + 1) * 128], wukvv[:, c, :], start=(c == 0), stop=(c == 1))
                    return ins
                kb.op("pe", [BCN[3], BCN[4], Bw], [bb], mm)
                vi = vo_ctr[0] % 4
                vo_ctr[0] += 1
                ov, iv = vo_views(vo[vi], bk)
                kb.op("act", [bb], [Bvo[vi]], lambda e, ov=ov, iv=iv: e.copy(out=ov, in_=iv))
                kb.dma("sp", [Bvo[vi]], [], Vm_d[g * 4 + i, :, :], vo[vi][:], pwrites=[B_vm])
        kb.barrier()
    if stages <= 1:
        _finish(nc, kb, out_d, x_d)
        return

    def attention(tag, kT_src, qT_src, V_src, per_pair_kq, krows, scale, chunk0, dilated, Bk_src, Bq_src, Bv_src):
        with ExitStack() as sa:
            A = lambda n, sh, dt: sa.enter_context(nc.sbuf_tensor(tag + "_" + n, sh, dt))
            Vp = [A("Vp%d" % i, [128, 32, 192], BF16) for i in range(2)]
            Kt = [A("Kt%d" % i, [128, S], BF16) for i in range(2)]
            Qt = [A("Qt%d" % i, [128, S], BF16) for i in range(2)]
            pte = [A("pte%d" % i, [128, 1024], BF16) for i in range(3)]
            ptm = [A("ptm%d" % i, [128, 1024], BF16) for i in range(3)] if dilated else None
            ao = [A("ao%d" % i, [128, S], BF16) for i in range(2)]
            rec = [A("rec%d" % i, [128, 512], F32) for i in range(2)]
            BVp, BKt, BQt = [Buf(), Buf()], [Buf(), Buf()], [Buf(), Buf()]
            Bpte, Bptm = [Buf() for _ in range(3)], [Buf() for _ in range(3)]
            Bao, Brec = [Buf(), Buf()], [Buf(), Buf()]
            SB = [Buf("S%d" % i, excl=True) for i in range(3)]
            AB = [Buf("acc%d" % i, excl=True) for i in range(2)]
            Sap = [ps[:, i * 1024:(i + 1) * 1024] for i in range(3)]
            Aap = [ps[:, 3072 + i * 512: 3072 + (i + 1) * 512] for i in range(2)]
            Bzk = Buf()
            if per_pair_kq:
                KtB = [A("KtB%d" % i, [128, S], BF16) for i in range(2)]
                for i in range(2):
                    kb.op("dve", [], [], lambda e, i=i: e.memset(Kt[i][64:128, :], 0.0), pwrites=[Bzk])
                    kb.op("dve", [], [], lambda e, i=i: e.memset(KtB[i][0:64, :], 0.0), pwrites=[Bzk])
            if dilated:
                toep_st = A("toep", [128, MW], F32)
                multb = A("multb", [128, MW], BF16)
                master = [A("master%d" % i, [128, MW], BF16) for i in range(2)]
                Btoep, Bmult, Bmaster = Buf(), Buf(), [Buf(), Buf()]
                kb.dma("pool", [], [Bmult], multb[:], mult_d[:, :])

            def load_pair(hp):
                for j in range(4):
                    kb.dma("sp", [Bv_src], [], Vp[hp % 2][:, j * 8:(j + 1) * 8, :],
                           V_src[j * 8:(j + 1) * 8, :, hp * 192:(hp + 1) * 192].rearrange("t p c -> p t c"),
                           pwrites=[BVp[hp % 2]])

            def load_kq(u):
                if per_pair_kq:
                    kb.dma("sp", [Bk_src, Bzk], [BKt[u % 2]], Kt[u % 2][0:64, :], kT_src[u, 0:64, :])
                    kb.dma("sp", [Bk_src, Bzk], [], KtB[u % 2][64:128, :], kT_src[u, 64:128, :], pwrites=[BKt[u % 2]])
                    kb.dma("sp", [Bq_src], [BQt[u % 2]], Qt[u % 2][:, :], qT_src[u, :, :])
                else:
                    kb.dma("sp", [Bk_src], [BKt[u % 2]], Kt[u % 2][0:krows, :], kT_src[u, :, :])
                    kb.dma("sp", [Bq_src], [BQt[u % 2]], Qt[u % 2][0:krows, :], qT_src[u, :, :])

            def prep_mask(h):
                kb.dma("sp", [], [Btoep], toep_st[:], toep_d[h, :, :])
                kb.op("act", [Btoep], [Btoep], lambda e: e.activation(out=toep_st[:], in_=toep_st[:], func=AF.Exp))
                kb.op("dve", [Btoep, Bmult], [Bmaster[h % 2]], lambda e: e.tensor_tensor(
                    out=master[h % 2][:], in0=toep_st[:], in1=multb[:], op=ALU.mult))

            items = []
            for h in range(8):
                hp, hh = h // 2, h % 2
                for Q in range(8):
                    if dilated:
                        Ts = list(range(max(0, 4 * Q - 8), min(31, 4 * Q + 11) + 1))
                    else:
                        Ts = list(range(32))
                    groups = []
                    n = 0
                    while n < len(Ts):
                        if n + 1 < len(Ts):
                            groups.append([Ts[n + 1], Ts[n]])
                            n += 2
                        else:
                            groups.append([Ts[n]])
                            n += 1
                    for gi, gT in enumerate(groups):
                        items.append(dict(h=h, hp=hp, hh=hh, Q=Q, Ts=gT, first=(gi == 0), last=(gi == len(groups) - 1),
                                          hq=h * 8 + Q))
            N = len(items)
            for i, it in enumerate(items):
                it["i"] = i

            def kq_unit(it):
                return it["hp"] if per_pair_kq else it["h"]

            def rows_of(it):
                if per_pair_kq:
                    return it["hh"] * 64, it["hh"] * 64 + 64
                return 0, krows

            def emit_qk(it):
                i = it["i"]
                u = kq_unit(it)
                r0, r1 = rows_of(it)
                K_, Q_ = Kt[u % 2], Qt[u % 2]
                if per_pair_kq:
                    r0, r1 = 0, 128
                    if it["hh"] == 1:
                        K_ = KtB[u % 2]

                def f(e):
                    for j, T in enumerate(it["Ts"]):
                        ins = e.matmul(Sap[i % 3][:, j * 512:(j + 1) * 512], K_[r0:r1, T * 128:(T + 1) * 128],
                                       Q_[r0:r1, it["Q"] * 512:(it["Q"] + 1) * 512], start=True, stop=True)
                    return ins
                kb.op("pe", [BKt[u % 2], BQt[u % 2]], [SB[i % 3]], f)

            def emit_exp(it):
                i = it["i"]
                w = 512 * len(it["Ts"])
                kb.op("act", [SB[i % 3]], [Bpte[i % 3]], lambda e: e.activation(
                    out=pte[i % 3][:, 0:w], in_=Sap[i % 3][:, 0:w], func=AF.Exp, scale=scale))

            def emit_mask(it):
                i = it["i"]
                n = len(it["Ts"])
                h = it["h"]
                c0 = C0 - 128 * it["Ts"][0] + 512 * it["Q"]
                m = master[h % 2]
                in1 = AP(m, c0, [pdim(m[:]), [128, n], [1, 512]])
                o = AP(ptm[i % 3], 0, [pdim(ptm[i % 3][:]), [512, n], [1, 512]])
                i0 = AP(pte[i % 3], 0, [pdim(pte[i % 3][:]), [512, n], [1, 512]])
                kb.op("dve", [Bpte[i % 3], Bmaster[h % 2]], [Bptm[i % 3]], lambda e: e.tensor_tensor(
                    out=o, in0=i0, in1=in1, op=ALU.mult))

            def emit_pv(it):
                i = it["i"]
                P_, BP_ = (ptm[i % 3], Bptm[i % 3]) if dilated else (pte[i % 3], Bpte[i % 3])
                acc = Aap[it["hq"] % 2]
                V_ = Vp[it["hp"] % 2]
                vc = 64 * it["hh"]
                nT = len(it["Ts"])

                def f(e):
                    for j, T in enumerate(it["Ts"]):
                        ins = e.matmul(acc, V_[:, T, vc:vc + 128], P_[:, j * 512:(j + 1) * 512],
                                       start=(it["first"] and j == 0), stop=(it["last"] and j == nT - 1))
                    return ins
                if it["first"]:
                    kb.op("pe", [BP_, BVp[it["hp"] % 2]], [AB[it["hq"] % 2]], f)
                else:
                    kb.op("pe", [BP_, BVp[it["hp"] % 2]], [], f, pwrites=[AB[it["hq"] % 2]])

            def emit_fin(it):
                a = it["hq"] % 2
                acc = Aap[a]
                hh, hp, Q = it["hh"], it["hp"], it["Q"]
                nr = slice(64 * hh, 64 * hh + 64)
                dr = slice(64 * (1 - hh), 64 * (1 - hh) + 64)
                if dilated:
                    kb.op("act", [AB[a]], [Brec[a]], lambda e: e.activation(out=rec[a][nr, :], in_=acc[dr, :], func=AF.Ln))
                    kb.op("act", [Brec[a]], [Brec[a]], lambda e: e.activation(out=rec[a][nr, :], in_=rec[a][nr, :], func=AF.Exp, scale=-1.0))
                else:
                    kb.op("dve", [AB[a]], [Brec[a]], lambda e: e.reciprocal(out=rec[a][nr, :], in_=acc[dr, :]))
                first_of_pair = (hh == 0 and Q == 0)
                kb.op("dve", [AB[a], Brec[a]], [Bao[hp % 2]] if first_of_pair else [], lambda e: e.tensor_tensor(
                    out=ao[hp % 2][nr, Q * 512:(Q + 1) * 512], in0=acc[nr, :], in1=rec[a][nr, :], op=ALU.mult),
                    pwrites=[] if first_of_pair else [Bao[hp % 2]])
                if hh == 1 and Q == 7:
                    kb.dma("sp", [Bao[hp % 2]], [], attnT_d[chunk0 + hp, :, :], ao[hp % 2][:, :], pwrites=[B_attn])

            def unit_start(it):
                h = it["h"]
                if it["Q"] == 0 and it["first"]:
                    if h + 1 < 8:
                        if per_pair_kq:
                            if (h + 1) % 2 == 0:
                                load_kq((h + 1) // 2)
                        else:
                            load_kq(h + 1)
                        if (h + 1) % 2 == 0:
                            load_pair((h + 1) // 2)
                        if dilated:
                            prep_mask(h + 1)

            load_pair(0)
            load_kq(0)
            if dilated:
                prep_mask(0)
            emit_qk(items[0])
            emit_exp(items[0])
            if dilated:
                emit_mask(items[0])
            if N > 1:
                emit_qk(items[1])
            pend = []
            for i in range(N):
                it = items[i]
                unit_start(it)
                if i + 1 < N:
                    emit_exp(items[i + 1])
                    if dilated:
                        emit_mask(items[i + 1])
                if i + 2 < N:
                    emit_qk(items[i + 2])
                emit_pv(it)
                if it["last"]:
                    pend.append((i, it))
                while pend and pend[0][0] + 3 <= i:
                    emit_fin(pend.pop(0)[1])
            while pend:
                emit_fin(pend.pop(0)[1])
            kb.barrier()

    run_dil = os.environ.get("SKIPDIL", "") == ""
    run_mla = os.environ.get("SKIPMLA", "") == ""
    if run_dil:
        attention("dil", dkT_d, dqT_d, Vd_d, True, 64, 64.0 ** -0.5, 4, True, B_dk, B_dq, B_vd)
    if run_mla:
        attention("mla", kT_d, qT_d, Vm_d, False, 96, 96.0 ** -0.5, 0, False, B_k, B_q, B_vm)
    if stages <= 3:
        _finish(nc, kb, out_d, x_d)
        return

    def bank2():
        if bank_ctr[0] % 2:
            bank_ctr[0] += 1
        i = bank_ctr[0] % 8
        bank_ctr[0] += 2
        return ps[:, i * 512:(i + 2) * 512], [PB[i], PB[i + 1]]

    class LNState:
        def __init__(self, A, tag, nb=2):
            self.junk = A(tag + "junk", [128, D], F32)
            self.st = [A(tag + "st%d" % i, [128, 8], F32) for i in range(nb)]
            self.Bjunk = Buf()
            self.Bst = [Buf() for _ in range(nb)]
            self.nb = nb
            self.n = 0
            self.eps = A(tag + "eps", [128, 1], F32)
            self.Beps = Buf()
            kb.op("dve", [], [self.Beps], lambda e: e.memset(self.eps[:], EPS))

    def ln_stats(L, y, By):
        i = L.n % L.nb
        L.n += 1
        st, Bst = L.st[i], L.Bst[i]
        kb.op("dve", [], [Bst], lambda e: e.memset(st[:], 0.0))
        kb.op("act", [By, Bst], [L.Bjunk], lambda e: e.activation(out=L.junk[:], in_=y[:], func=AF.Identity, scale=1.0 / D, accum_out=st[:, 0:1]),
              pwrites=[Bst])
        kb.op("act", [By, Bst], [L.Bjunk], lambda e: e.activation(out=L.junk[:], in_=y[:], func=AF.Square, scale=float(D) ** -0.5, accum_out=st[:, 1:2]),
              pwrites=[Bst])
        return st, Bst

    def ln_apply(st, Bst, y, By, xh, Bxh):
        kb.op("dve", [Bst], [], lambda e: e.tensor_scalar(out=st[:, 2:3], in0=st[:, 0:1], scalar1=1.0 / D, scalar2=None, op0=ALU.mult), pwrites=[Bst])
        kb.op("dve", [Bst], [], lambda e: e.tensor_tensor(out=st[:, 3:4], in0=st[:, 2:3], in1=st[:, 2:3], op=ALU.mult), pwrites=[Bst])
        kb.op("dve", [Bst], [], lambda e: e.scalar_tensor_tensor(out=st[:, 4:5], in0=st[:, 1:2], scalar=1.0 / D, in1=st[:, 3:4],
                                                                   op0=ALU.mult, op1=ALU.subtract), pwrites=[Bst])
        kb.op("dve", [Bst], [], lambda e: e.tensor_scalar(out=st[:, 4:5], in0=st[:, 4:5], scalar1=EPS, scalar2=None, op0=ALU.add), pwrites=[Bst])
        kb.op("act", [Bst], [], lambda e: e.activation(out=st[:, 5:6], in_=st[:, 4:5], func=AF.Ln), pwrites=[Bst])
        kb.op("act", [Bst], [], lambda e: e.activation(out=st[:, 5:6], in_=st[:, 5:6], func=AF.Exp, scale=-0.5), pwrites=[Bst])
        kb.op("dve", [Bst], [], lambda e: e.scalar_tensor_tensor(out=st[:, 6:7], in0=st[:, 2:3], scalar=-1.0, in1=st[:, 5:6],
                                                                   op0=ALU.mult, op1=ALU.mult), pwrites=[Bst])
        kb.op("act", [By, Bst], [Bxh], lambda e: e.activation(out=xh[:], in_=y[:], func=AF.Identity, scale=st[:, 5:6], bias=st[:, 6:7]))

    with ExitStack() as s4:
        A = lambda n, sh, dt: s4.enter_context(nc.sbuf_tensor("p4_" + n, sh, dt))
        wout = A("wout", [128, 8, D], BF16)
        g1bc, lngbc, lnbbc, sc2bc, sh2bc = (A(n, [128, D], F32) for n in ("g1bc", "lngbc", "lnbbc", "sc2bc", "sh2bc"))
        wr = A("wr", [128, 8, NE], F32)
        ident = A("ident", [128, 128], F32)
        at = [A("at%d" % i, [128, 8, 512], BF16) for i in range(2)]
        xt = [A("xt%d" % i, [128, D], F32) for i in range(2)]
        t1 = [A("t1%d" % i, [128, D], F32) for i in range(2)]
        yy = [A("y%d" % i, [128, D], F32) for i in range(2)]
        xh = [A("xh%d" % i, [128, D], F32) for i in range(2)]
        x1 = [A("x1%d" % i, [128, D], F32) for i in range(2)]
        h2f = [A("h2f%d" % i, [128, D], F32) for i in range(2)]
        h2b = [A("h2b%d" % i, [128, D], BF16) for i in range(2)]
        h2T = [A("h2T%d" % i, [128, 8, 128], F32) for i in range(2)]
        sm = [A("sm%d" % i, [128, 8], F32) for i in range(2)]
        ee = [A("ee%d" % i, [128, NE], F32) for i in range(2)]
        L = LNState(A, "ln", 3)
        Bc4 = Buf()
        Bat, Bxt, Bt1, By, Bxh, Bx1, Bh2f, Bh2b, Bh2T, Bsm, Bee = ([Buf(), Buf()] for _ in range(11))
        kb.dma("sp", [B_mod], [], g1bc[:], bcast_row(mod_d[0:1, 2048:3072], D), pwrites=[Bc4])
        Bws = [Buf(), Buf()]
        for c in range(8):
            kb.dma("sp", [], [Bws[c % 2]], t1[c % 2][:], wout_d[c * 128:(c + 1) * 128, :])
            kb.op("dve", [Bws[c % 2], Bc4], [], lambda e, c=c: e.tensor_tensor(out=wout[:, c, :], in0=t1[c % 2][:], in1=g1bc[:], op=ALU.mult),
                  pwrites=[Bc4])
        kb.dma("sp", [B_mod], [], sh2bc[:], bcast_row(mod_d[0:1, 3072:4096], D), pwrites=[Bc4])
        kb.dma("sp", [B_mod], [], sc2bc[:], bcast_row(mod_d[0:1, 4096:5120], D), pwrites=[Bc4])
        kb.dma("sp", [], [], lngbc[:], bcast_row(ln1g_d[0:1, :], D), pwrites=[Bc4])
        kb.dma("sp", [], [], lnbbc[:], bcast_row(ln1b_d[0:1, :], D), pwrites=[Bc4])
        kb.dma("sp", [], [], wr[:], wr_d.rearrange("(k p) e -> p k e", p=128), pwrites=[Bc4])
        kb.dma("sp", [], [], ident[:], ident_d[:, :], pwrites=[Bc4])
        kb.op("dve", [Bc4], [], lambda e: e.tensor_scalar(out=sc2bc[:], in0=sc2bc[:], scalar1=1.0, scalar2=None, op0=ALU.add), pwrites=[Bc4])
        attn_v = attnT_d.rearrange("c p n -> p c n")
        x_t = x_d.rearrange("(t p) d -> t p d", p=128)
        x1_t = x1_d.rearrange("(t p) d -> t p d", p=128)
        h2_t = h2_d.rearrange("(t p) d -> t p d", p=128)
        stA = {}
        stC = {}

        mixb = {}

        def A_mix(t):
            g, i = t // 4, t % 4
            if i == 0:
                kb.dma("sp", [B_attn], [Bat[g % 2]], at[g % 2][:], attn_v[:, :, g * 512:(g + 1) * 512])
            b = t % 2
            kb.dma("sp", [], [Bxt[b]], xt[b][:], x_t[t])
            bk2, bb2 = bank2()

            def mm(e):
                for half in range(2):
                    for c in range(8):
                        ins = e.matmul(bk2[:, half * 512:(half + 1) * 512], at[g % 2][:, c, i * 128:(i + 1) * 128],
                                       wout[:, c, half * 512:(half + 1) * 512], start=(c == 0), stop=(c == 7))
                return ins
            kb.op("pe", [Bat[g % 2], Bc4], bb2, mm)
            mixb[t] = (bk2, bb2)

        def A_y(t):
            b = t % 2
            bk2, bb2 = mixb.pop(t)
            kb.op("dve", bb2 + [Bxt[b]] + Bws, [By[b]], lambda e: e.scalar_tensor_tensor(
                out=yy[b][:], in0=xt[b][:], scalar=ALPHA, in1=bk2, op0=ALU.mult, op1=ALU.add))

        def A_stats(t):
            b = t % 2
            stA[t] = ln_stats(L, yy[b], By[b])

        def B1_smalls(t):
            st, Bst = stA[t]
            kb.op("dve", [Bst], [], lambda e: e.scalar_tensor_tensor(out=st[:, 4:5], in0=st[:, 0:1], scalar=st[:, 0:1], in1=st[:, 1:2],
                                                                       op0=ALU.mult, op1=ALU.subtract), pwrites=[Bst])

        def B1_lnexp(t):
            st, Bst = stA[t]
            kb.op("act", [Bst, L.Beps], [], lambda e: e.activation(out=st[:, 5:6], in_=st[:, 4:5], func=AF.Ln, scale=-1.0, bias=L.eps[:, 0:1]),
                  pwrites=[Bst])
            kb.op("act", [Bst], [], lambda e: e.activation(out=st[:, 5:6], in_=st[:, 5:6], func=AF.Exp, scale=-0.5), pwrites=[Bst])

        def B1_nmr(t):
            st, Bst = stA[t]
            kb.op("dve", [Bst], [], lambda e: e.scalar_tensor_tensor(out=st[:, 6:7], in0=st[:, 0:1], scalar=-1.0, in1=st[:, 5:6],
                                                                       op0=ALU.mult, op1=ALU.mult), pwrites=[Bst])

        def B1_xh(t):
            b = t % 2
            st, Bst = stA.pop(t)
            kb.op("act", [By[b], Bst], [Bxh[b]], lambda e: e.activation(out=xh[b][:], in_=yy[b][:], func=AF.Identity,
                                                                        scale=st[:, 5:6], bias=st[:, 6:7]))

        def B2_dve(t):
            b = t % 2
            kb.op("dve", [Bxh[b], Bc4], [Bx1[b]], lambda e: e.tensor_tensor(out=x1[b][:], in0=xh[b][:], in1=lngbc[:], op=ALU.mult))
            kb.op("dve", [Bx1[b], Bc4], [Bx1[b]], lambda e: e.tensor_tensor(out=x1[b][:], in0=x1[b][:], in1=lnbbc[:], op=ALU.add))
            kb.dma("sp", [Bx1[b]], [], x1_t[t], x1[b][:], pwrites=[B_x1])
            kb.op("dve", [Bx1[b], Bc4], [Bh2f[b]], lambda e: e.tensor_tensor(out=h2f[b][:], in0=x1[b][:], in1=sc2bc[:], op=ALU.mult))
            kb.op("dve", [Bh2f[b], Bc4], [Bh2f[b]], lambda e: e.tensor_tensor(out=h2f[b][:], in0=h2f[b][:], in1=sh2bc[:], op=ALU.add))

        def B2_cast(t):
            b = t % 2
            kb.op("act", [Bh2f[b]], [Bh2b[b]], lambda e: e.copy(out=h2b[b][:], in_=h2f[b][:]))
            kb.dma("sp", [Bh2b[b]], [], h2_t[t], h2b[b][:], pwrites=[B_h2])

        def B2_tp(t):
            b = t % 2
            bk2, bb2 = bank2()

            def tp(e):
                for k in range(8):
                    ins = e.transpose(bk2[:, k * 128:(k + 1) * 128], h2f[b][:, k * 128:(k + 1) * 128], ident[:])
                return ins
            kb.op("pe", [Bh2f[b], Bc4], bb2, tp)
            stC[t] = (bk2, bb2)

        lgb = {}

        def C_copy(t):
            b = t % 2
            bk2, bb2 = stC.pop(t)
            kb.op("act", bb2, [Bh2T[b]], lambda e: e.copy(out=h2T[b][:].rearrange("p k n -> p (k n)"), in_=bk2))

        def C_logits(t):
            b = t % 2
            bk, bb = bank()

            def lg(e):
                for k in range(8):
                    ins = e.matmul(bk[:, 0:NE], h2T[b][:, k, :], wr[:, k, :], start=(k == 0), stop=(k == 7))
                return ins
            kb.op("pe", [Bh2T[b], Bc4], [bb], lg)
            lgb[t] = (bk, bb)

        def C_max(t):
            b = t % 2
            bk, bb = lgb[t]
            kb.op("dve", [], [Bsm[b]], lambda e: e.memset(sm[b][:], 0.0))
            kb.op("dve", [bb], [], lambda e: e.reduce_max(out=sm[b][:, 0:1], in_=bk[:, 0:NE], axis=AX.X), pwrites=[Bsm[b]])
            kb.op("dve", [Bsm[b]], [], lambda e: e.tensor_scalar(out=sm[b][:, 1:2], in0=sm[b][:, 0:1], scalar1=-1.0, scalar2=None, op0=ALU.mult),
                  pwrites=[Bsm[b]])

        def C_exp(t):
            b = t % 2
            bk, bb = lgb.pop(t)
            kb.op("act", [bb, Bsm[b]], [Bee[b]], lambda e: e.activation(out=ee[b][:], in_=bk[:, 0:NE], func=AF.Exp, bias=sm[b][:, 1:2],
                                                                       accum_out=sm[b][:, 2:3]), pwrites=[Bsm[b]])

        def C_aff(t):
            b = t % 2
            kb.op("dve", [Bsm[b]], [], lambda e: e.reciprocal(out=sm[b][:, 3:4], in_=sm[b][:, 2:3]), pwrites=[Bsm[b]])
            kb.op("dve", [Bee[b], Bsm[b]], [], lambda e: e.tensor_scalar(out=aff_all[:, t * NE:(t + 1) * NE], in0=ee[b][:], scalar1=sm[b][:, 3:4],
                                                                        scalar2=None, op0=ALU.mult), pwrites=[B_aff])

        for step in range(NT + 3):
            a, b1, b2, c = step, step - 1, step - 2, step - 3
            ok = lambda t: 0 <= t < NT
            if ok(a):
                A_mix(a)
            if ok(c):
                C_copy(c)
            if ok(b2):
                B2_dve(b2)
            if ok(c):
                C_logits(c)
            if ok(a):
                A_y(a)
            if ok(b2):
                B2_cast(b2)
                B2_tp(b2)
            if ok(b1):
                B1_smalls(b1)
            if ok(a):
                A_stats(a)
            if ok(c):
                C_max(c)
            if ok(b1):
                B1_lnexp(b1)
            if ok(c):
                C_exp(c)
            if ok(b1):
                B1_nmr(b1)
            if ok(c):
                C_aff(c)
            if ok(b1):
                B1_xh(b1)
        if "aff_s" in dbg:
            kb.dma("sp", [B_aff], [Buf()], aff_d[:, :], aff_all[:])
        kb.barrier()
    if stages <= 4:
        _finish(nc, kb, out_d, x_d)
        return

    def moe_phase():
        with ExitStack() as s5:
            A = lambda n, sh, dt: s5.enter_context(nc.sbuf_tensor("p5_" + n, sh, dt))
            onesf = A("onesf", [128, 128], F32)
            onesb = A("onesb", [128, 128], BF16)
            triub = A("triub", [128, 128], BF16)
            identb = A("identb", [128, 128], BF16)
            iota = A("iota", [128, 512], F32)
            tokhl = A("tokhl", [128, NT, 2], F32)
            zero = A("zero", [128, D], F32)
            lo = A("lo", [128, NE], F32)
            tau = A("tau", [128, NE], F32)
            part = A("part", [128, NE], F32)
            gw = A("gw", [128, NE], F32)
            cmp = A("cmp", [128, NT * NE], F32)
            self_ = A("self", [128, NT * NE], F32)
            selb = A("selb", [128, NT * NE], BF16)
            posm = A("posm", [128, NT * NE], F32)
            r1 = A("r1", [128, NT * NE], F32)
            R = A("R", [128, NT * NE * 5], BF16)
            OH = [A("OH%d" % i, [128, 512], BF16) for i in range(4)]
            idxv = [A("idxv%d" % i, [128, 32], F32) for i in range(2)]
            idxf = [A("idxf%d" % i, [128, 4], F32) for i in range(2)]
            idxi = [A("idxi%d" % i, [128, 4], I32) for i in range(2)]
            val = [A("val%d" % i, [128, 4], F32) for i in range(2)]
            xin = [A("xin%d" % i, [128, 4, D], BF16) for i in range(2)]
            xinT = [A("xinT%d" % i, [128, 8, 512], BF16) for i in range(2)]
            wg = [A("wg%d" % i, [128, 8, 512], BF16) for i in range(3)]
            wu = [A("wu%d" % i, [128, 8, 512], BF16) for i in range(3)]
            wd = [A("wd%d" % i, [128, 6, D], BF16) for i in range(3)]
            sg = [A("sg%d" % i, [128, 512], F32) for i in range(2)]
            act = [A("act%d" % i, [128, 12, 512], BF16) for i in range(2)]
            ys = [A("ys%d" % i, [128, D], F32) for i in range(4)]
            mo = [A("mo%d" % i, [128, D], F32) for i in range(4)]
            Bc = Buf()
            Blo, Btau, Bpart, Bgw, Bcmp, Bsel, Bposm, BR = (Buf() for _ in range(8))
            BOH = [Buf() for _ in range(4)]
            Bidxv, Bidx, Bval = [Buf(), Buf()], [Buf(), Buf()], [Buf(), Buf()]
            Bxin, BxinT = [Buf(), Buf()], [Buf(), Buf()]
            Bwg, Bwu, Bwd = [Buf() for _ in range(3)], [Buf() for _ in range(3)], [Buf() for _ in range(3)]
            Bsg, Bact = [Buf(), Buf()], [Buf(), Buf()]
            Bys, Bmo = [Buf() for _ in range(4)], [Buf() for _ in range(4)]

            kb.op("pool", [], [], lambda e: e.memset(onesf[:], 1.0), pwrites=[Bc])
            kb.op("pool", [], [], lambda e: e.memset(onesb[:], 1.0), pwrites=[Bc])
            kb.op("pool", [], [], lambda e: e.memset(zero[:], 0.0), pwrites=[Bc])
            kb.op("pool", [], [Blo], lambda e: e.memset(lo[:], 0.0))
            kb.dma("pool", [], [], triub[:], triu_d[:, :], pwrites=[Bc])
            kb.dma("pool", [], [], identb[:], ident_d[:, :], pwrites=[Bc])
            kb.dma("sp", [], [], iota[:], iota_d[:, :], pwrites=[Bc])
            kb.dma("sp", [], [], tokhl[:], tokhl_d[:, :, :], pwrites=[Bc])
            moe_t = moe_d.rearrange("(t p) d -> t p d", p=128)
            for t in range(NT):
                kb.dma("sp", [Bc], [], moe_t[t], zero[:], pwrites=[B_moe])

            def issue_gu(n):
                if n >= NE * 3:
                    return
                e, fb = n // 3, n % 3
                b = n % 3
                kb.dma("pool", [], [Bwg[b]], wg[b][:], wg_d[e, :, fb * 512:(fb + 1) * 512].rearrange("(k p) f -> p k f", p=128))
                kb.dma("pool", [], [Bwu[b]], wu[b][:], wu_d[e, :, fb * 512:(fb + 1) * 512].rearrange("(k p) f -> p k f", p=128))

            def issue_d(m):
                if m >= NE * 2:
                    return
                e, half = m // 2, m % 2
                b = m % 3
                kb.dma("pool", [], [Bwd[b]], wd[b][:], wd_d[e, half * 768:(half + 1) * 768, :].rearrange("(k p) n -> p k n", p=128))

            issue_gu(0)
            issue_gu(1)
            issue_d(0)
            issue_d(1)
            issue_d(2)

            aff3 = AP(aff_all, 0, [pdim(aff_all[:]), [NE, NT], [1, NE]])
            cmp3 = AP(cmp, 0, [pdim(cmp[:]), [NE, NT], [1, NE]])
            cmpT = AP(cmp, 0, [pdim(cmp[:]), [1, NE], [NE, NT]])

            def bc16(tile_):
                return AP(tile_, 0, [pdim(tile_[:]), [0, NT], [1, NE]])

            for it in range(1, NBIS + 1):
                w = 2.0 ** (-it)
                kb.op("dve", [Blo], [Btau], lambda e: e.tensor_scalar(out=tau[:], in0=lo[:], scalar1=w, scalar2=None, op0=ALU.add))
                kb.op("dve", [B_aff, Btau], [Bcmp], lambda e: e.tensor_tensor(out=cmp3, in0=aff3, in1=bc16(tau), op=ALU.is_ge))
                kb.op("dve", [Bcmp], [Bpart], lambda e: e.tensor_reduce(out=part[:], in_=cmpT, axis=AX.X, op=ALU.add))
                bk, bb = bank()
                kb.op("pe", [Bpart, Bc], [bb], lambda e: e.matmul(bk[:, 0:NE], onesf[:], part[:], start=True, stop=True))
                kb.op("dve", [bb], [Bgw], lambda e: e.tensor_scalar(out=gw[:], in0=bk[:, 0:NE], scalar1=CAP - 0.5, scalar2=w,
                                                                  op0=ALU.is_ge, op1=ALU.mult))
                kb.op("dve", [Bgw, Blo], [Blo], lambda e: e.tensor_tensor(out=lo[:], in0=lo[:], in1=gw[:], op=ALU.add))
            self3 = AP(self_, 0, [pdim(self_[:]), [NE, NT], [1, NE]])
            kb.op("dve", [B_aff, Blo], [Bsel], lambda e: e.tensor_tensor(out=self3, in0=aff3, in1=bc16(lo), op=ALU.is_ge))
            kb.op("dve", [Bsel], [], lambda e: e.tensor_copy(out=selb[:], in_=self_[:]), pwrites=[Bsel])
            bk, bb = bank()

            def prefix(e):
                for t in range(NT):
                    o = bk[:, t * NE:(t + 1) * NE]
                    for tp in range(t):
                        e.matmul(o, onesb[:], selb[:, tp * NE:(tp + 1) * NE], start=(tp == 0), stop=False)
                    ins = e.matmul(o, triub[:], selb[:, t * NE:(t + 1) * NE], start=(t == 0), stop=True)
                return ins
            kb.op("pe", [Bsel, Bc], [bb], prefix)
            kb.op("dve", [bb, Bsel], [Bposm], lambda e: e.scalar_tensor_tensor(out=posm[:], in0=bk, scalar=1.0, in1=self_[:],
                                                                            op0=ALU.add, op1=ALU.mult))
            kb.op("dve", [Bposm], [Bposm], lambda e: e.tensor_scalar(out=posm[:], in0=posm[:], scalar1=-1.0, scalar2=None, op0=ALU.add))
            Rv = lambda j: AP(R, j, [pdim(R[:]), [5, NT * NE]])
            kb.op("dve", [Bc], [BR], lambda e: e.tensor_copy(
                out=AP(R, 0, [pdim(R[:]), [5 * NE, NT], [5, NE], [1, 2]]),
                in_=AP(tokhl, 0, [pdim(tokhl[:]), [2, NT], [0, NE], [1, 2]])))
            kb.op("dve", [B_aff], [], lambda e: e.tensor_copy(out=Rv(2), in_=aff_all[:]), pwrites=[BR])
            kb.op("dve", [B_aff, BR], [Bcmp], lambda e: e.tensor_tensor(out=r1[:], in0=aff_all[:], in1=Rv(2), op=ALU.subtract))
            kb.op("dve", [Bcmp], [], lambda e: e.tensor_copy(out=Rv(3), in_=r1[:]), pwrites=[BR])
            kb.op("dve", [Bcmp, BR], [Bcmp], lambda e: e.tensor_tensor(out=r1[:], in0=r1[:], in1=Rv(3), op=ALU.subtract))
            kb.op("dve", [Bcmp], [], lambda e: e.tensor_copy(out=Rv(4), in_=r1[:]), pwrites=[BR])

            h2rows = h2_d
            oh_ctr = [0]

            rstate = {}

            def route_tiles(e, t0, t1):
                for t in range(t0, t1):
                    oi = oh_ctr[0] % 4
                    oh_ctr[0] += 1
                    kb.op("dve", [Bposm, Bc], [BOH[oi]], lambda en: en.tensor_scalar(
                        out=OH[oi][:], in0=iota[:], scalar1=posm[:, t * NE + e:t * NE + e + 1], scalar2=None, op0=ALU.is_equal))

                    def mm(en):
                        for cj in range(4):
                            c0 = 3072 + (cj * NT + t) * 8
                            ins = en.matmul(ps[:, c0:c0 + 5], OH[oi][:, cj * 128:(cj + 1) * 128],
                                            R[:, (t * NE + e) * 5:(t * NE + e) * 5 + 5], start=True, stop=True)
                        return ins
                    if t == 0:
                        kb.op("pe", [BOH[oi], BR], [PB[6], PB[7]], mm)
                    else:
                        kb.op("pe", [BOH[oi], BR], [], mm, pwrites=[PB[6], PB[7]])

            def route_idx(e):
                b = e % 2
                kb.op("dve", [PB[6], PB[7]], [Bidxv[b]], lambda en: en.tensor_reduce(
                    out=AP(idxv[b], 0, [pdim(idxv[b][:]), [8, 4], [1, 5]]),
                    in_=AP(ps, 3072, [pdim(ps[:]), [NT * 8, 4], [1, 5], [8, NT]]),
                    axis=AX.X, op=ALU.add))
                v3 = lambda j: AP(idxv[b], j, [pdim(idxv[b][:]), [8, 4]])
                kb.op("dve", [Bidxv[b]], [Bidx[b]], lambda en: en.scalar_tensor_tensor(
                    out=idxf[b][:], in0=v3(0), scalar=128.0, in1=v3(1), op0=ALU.mult, op1=ALU.add))
                kb.op("dve", [Bidx[b]], [], lambda en: en.tensor_copy(out=idxi[b][:], in_=idxf[b][:]), pwrites=[Bidx[b]])
                kb.op("dve", [Bidxv[b]], [Bval[b]], lambda en: en.tensor_tensor(out=val[b][:], in0=v3(2), in1=v3(3), op=ALU.add))
                kb.op("dve", [Bidxv[b], Bval[b]], [Bval[b]], lambda en: en.tensor_tensor(out=val[b][:], in0=val[b][:], in1=v3(4), op=ALU.add))
                for cj in range(4):
                    kb.dma("pool", [Bidx[b], B_h2], [] if cj else [Bxin[b]], xin[b][:, cj, :], h2rows[:, :],
                           pwrites=[Bxin[b]] if cj else [],
                           indirect=dict(out_offset=None, in_offset=bass.IndirectOffsetOnAxis(ap=idxi[b][:, cj:cj + 1], axis=0)))

            def route_T(e):
                b = e % 2
                for k in range(8):
                    bk2, bb2 = bank()
                    bkb = bk2.bitcast(BF16)

                    def tp(en):
                        for cj in range(4):
                            ins = en.transpose(bkb[:, cj * 128:(cj + 1) * 128], xin[b][:, cj, k * 128:(k + 1) * 128], identb[:])
                        return ins
                    kb.op("pe", [Bxin[b], Bc], [bb2], tp)
                    kb.op("act", [bb2], [] if k else [BxinT[b]],
                          (lambda en: en.copy(out=xinT[b][:, k, :], in_=bkb[:, 0:512])),
                          pwrites=[BxinT[b]] if k else [])

            def route(e):
                route_tiles(e, 0, NT)
                route_idx(e)
                route_T(e)

            def tail_gather(e):
                b = e % 2
                for cj in range(4):
                    kb.dma("pool", [Bidx[b], B_moe], [Bmo[cj]], mo[cj][:, :], moe_d[:, :],
                           indirect=dict(out_offset=None, in_offset=bass.IndirectOffsetOnAxis(ap=idxi[b][:, cj:cj + 1], axis=0)))

            def tail_add_scatter(e, cjs=(0, 1, 2, 3)):
                b = e % 2
                for cj in cjs:
                    kb.op("dve", [Bmo[cj], Bys[cj]], [Bmo[cj]], lambda en: en.tensor_tensor(out=mo[cj][:], in0=mo[cj][:], in1=ys[cj][:], op=ALU.add))
                for cj in cjs:
                    kb.dma("pool", [Bidx[b], Bmo[cj]], [], moe_d[:, :], mo[cj][:, :], pwrites=[B_moe],
                           indirect=dict(out_offset=bass.IndirectOffsetOnAxis(ap=idxi[b][:, cj:cj + 1], axis=0), in_offset=None))

            def compute(e):
                b = e % 2
                for fb in range(3):
                    n = e * 3 + fb
                    wb = n % 3
                    for j in range(4):
                        bg, bbg = bank()
                        bu, bbu = bank()

                        def mm(en):
                            for bk_, w_ in ((bg, wg[wb]), (bu, wu[wb])):
                                for k in range(8):
                                    ins = en.matmul(bk_, w_[:, k, j * 128:(j + 1) * 128], xinT[b][:, k, :], start=(k == 0), stop=(k == 7))
                            return ins
                        kb.op("pe", [BxinT[b], Bwg[wb], Bwu[wb]], [bbg, bbu], mm)
                        if e + 1 < NE:
                            if fb * 4 + j < 11:
                                route_tiles(e + 1, 3 * (fb * 4 + j), min(NT, 3 * (fb * 4 + j) + 3))
                            else:
                                route_idx(e + 1)
                        if e > 0 and 2 <= fb * 4 + j < 6:
                            tail_add_scatter(e - 1, (fb * 4 + j - 2,))
                        si = (fb * 4 + j) % 2
                        kb.op("act", [bbg], [Bsg[si]], lambda en: en.activation(out=sg[si][:], in_=bg, func=AF.Silu))
                        fi = fb * 4 + j
                        kb.op("dve", [Bsg[si], bbu], [] if fi else [Bact[b]], lambda en: en.tensor_tensor(
                            out=act[b][:, fi, :], in0=sg[si][:], in1=bu, op=ALU.mult), pwrites=[Bact[b]] if fi else [])
                    issue_gu(n + 2)
                for cj in range(4):
                    yi = cj
                    for half in range(2):
                        bk, bb = bank()

                        def mm(en):
                            for fi in range(12):
                                m = e * 2 + fi // 6
                                ins = en.matmul(bk, act[b][:, fi, cj * 128:(cj + 1) * 128], wd[m % 3][:, fi % 6, half * 512:(half + 1) * 512],
                                                start=(fi == 0), stop=(fi == 11))
                            return ins
                        kb.op("pe", [Bact[b], Bwd[(e * 2) % 3], Bwd[(e * 2 + 1) % 3]], [bb], mm)
                        kb.op("act", [bb, Bval[b]], [] if half else [Bys[yi]], lambda en: en.activation(
                            out=ys[yi][:, half * 512:(half + 1) * 512], in_=bk, func=AF.Copy, scale=val[b][:, cj:cj + 1]),
                            pwrites=[Bys[yi]] if half else [])
                issue_d(e * 2 + 3)
                issue_d(e * 2 + 4)
                if e + 1 < NE:
                    route_T(e + 1)
                tail_gather(e)
                if e == NE - 1:
                    tail_add_scatter(e)

            nrot[0] = 6
            bank_ctr[0] = 0
            route(0)
            for e in range(int(os.environ.get("NEXP", str(NE)))):
                compute(e)
            kb.barrier()
            nrot[0] = 8

    if os.environ.get("SKIPMOE", "") == "":
        moe_phase()

    with ExitStack() as s6:
        A = lambda n, sh, dt: s6.enter_context(nc.sbuf_tensor("p6_" + n, sh, dt))
        g2bc, lngbc, lnbbc = (A(n, [128, D], F32) for n in ("g2bc", "lngbc", "lnbbc"))
        NB6 = 3
        xt = [A("xt%d" % i, [128, D], F32) for i in range(NB6)]
        mt = [A("mt%d" % i, [128, D], F32) for i in range(NB6)]
        yy = [A("y%d" % i, [128, D], F32) for i in range(NB6)]
        xh = [A("xh%d" % i, [128, D], F32) for i in range(NB6)]
        oo = [A("o%d" % i, [128, D], F32) for i in range(NB6)]
        L = LNState(A, "ln", 3)
        Bc6 = Buf()
        Bxt, Bmt, By, Bxh, Boo = ([Buf() for _ in range(NB6)] for _ in range(5))
        kb.dma("sp", [B_mod], [], g2bc[:], bcast_row(mod_d[0:1, 5120:6144], D), pwrites=[Bc6])
        kb.dma("sp", [], [], lngbc[:], bcast_row(ln2g_d[0:1, :], D), pwrites=[Bc6])
        kb.dma("sp", [], [], lnbbc[:], bcast_row(ln2b_d[0:1, :], D), pwrites=[Bc6])
        x1_t = x1_d.rearrange("(t p) d -> t p d", p=128)
        moe_t = moe_d.rearrange("(t p) d -> t p d", p=128)
        out_t = out_d.rearrange("(t p) d -> t p d", p=128)
        stA = {}

        def ln_smalls(st, Bst):
            kb.op("dve", [Bst], [], lambda e: e.scalar_tensor_tensor(out=st[:, 4:5], in0=st[:, 0:1], scalar=st[:, 0:1], in1=st[:, 1:2],
                                                                       op0=ALU.mult, op1=ALU.subtract), pwrites=[Bst])

        def A6_load(t):
            b = t % NB6
            kb.dma("sp", [B_x1], [Bxt[b]], xt[b][:], x1_t[t])
            kb.dma("sp", [B_moe], [Bmt[b]], mt[b][:], moe_t[t])

        def A6_dve(t):
            b = t % NB6
            kb.op("dve", [Bmt[b], Bc6], [Bmt[b]], lambda e: e.tensor_tensor(out=mt[b][:], in0=mt[b][:], in1=g2bc[:], op=ALU.mult))
            kb.op("dve", [Bxt[b], Bmt[b]], [By[b]], lambda e: e.scalar_tensor_tensor(
                out=yy[b][:], in0=xt[b][:], scalar=ALPHA, in1=mt[b][:], op0=ALU.mult, op1=ALU.add))

        def C6_dve(t):
            b = t % NB6
            kb.op("dve", [Bxh[b], Bc6], [Boo[b]], lambda e: e.tensor_tensor(out=oo[b][:], in0=xh[b][:], in1=lngbc[:], op=ALU.mult))
            kb.op("dve", [Boo[b], Bc6], [Boo[b]], lambda e: e.tensor_tensor(out=oo[b][:], in0=oo[b][:], in1=lnbbc[:], op=ALU.add))
            kb.dma("sp", [Boo[b]], [Buf()], out_t[t], oo[b][:])

        for step in range(NT + 2):
            a, b1, c = step, step - 1, step - 2
            ok = lambda t: 0 <= t < NT
            if ok(a):
                A6_load(a)
            if ok(c):
                C6_dve(c)
            if ok(a):
                A6_dve(a)
            if ok(b1):
                ln_smalls(*stA[b1])
            if ok(a):
                stA[a] = ln_stats(L, yy[a % NB6], By[a % NB6])
            if ok(b1):
                st, Bst = stA.pop(b1)
                bb_ = b1 % NB6
                kb.op("act", [Bst, L.Beps], [], lambda e: e.activation(out=st[:, 5:6], in_=st[:, 4:5], func=AF.Ln, scale=-1.0, bias=L.eps[:, 0:1]),
                      pwrites=[Bst])
                kb.op("act", [Bst], [], lambda e: e.activation(out=st[:, 5:6], in_=st[:, 5:6], func=AF.Exp, scale=-0.5), pwrites=[Bst])
                kb.op("dve", [Bst], [], lambda e: e.scalar_tensor_tensor(out=st[:, 6:7], in0=st[:, 0:1], scalar=-1.0, in1=st[:, 5:6],
                                                                           op0=ALU.mult, op1=ALU.mult), pwrites=[Bst])
                kb.op("act", [By[bb_], Bst], [Bxh[bb_]], lambda e: e.activation(out=xh[bb_][:], in_=yy[bb_][:], func=AF.Identity,
                                                                              scale=st[:, 5:6], bias=st[:, 6:7]))
        kb.barrier()


def _finish(nc, kb, out_d, x_d):
    kb.barrier()
    tok = kb.dma("sp", [], [Buf()], out_d[0:128, :], x_d[0:128, :])
    kb._wait("sp", [tok])


def _t5_bucket(rel):
    half = 16
    ret = (rel > 0).astype(np.int32) * half
    n = np.abs(rel)
    large = 8 + (np.log(np.maximum(n, 1) / 8) / np.log(1024 / 8) * (half - 8)).astype(np.int32)
    large = np.minimum(large, half - 1)
    return ret + np.where(n < 8, n, large).astype(np.int32)


def _static_tables():
    a = np.arange(128)[:, None]
    c = np.arange(MW)[None, :]
    rel = a - c + C0
    mult = ((np.abs(rel) <= 64).astype(np.float32)
            + ((rel % 4 == 0) & (np.abs(rel) <= 256)).astype(np.float32)
            + ((rel % 16 == 0) & (np.abs(rel) <= 1024)).astype(np.float32))
    bucket = _t5_bucket(rel)
    inv = (10000.0 ** (-(np.arange(0, 32, 2, dtype=np.float32)) / np.float32(32))).astype(np.float32)
    ang = (np.arange(S, dtype=np.float32)[:, None] * inv[None, :]).astype(np.float32)
    cos = np.cos(ang.astype(np.float64)).astype(np.float32).T
    sin = np.sin(ang.astype(np.float64)).astype(np.float32).T
    rope_cos = np.ascontiguousarray(np.concatenate([cos, cos], axis=0))
    rope_sin = np.ascontiguousarray(np.concatenate([-sin, sin], axis=0))
    tokhl = np.zeros((128, NT, 2), np.float32)
    tokhl[:, :, 0] = np.arange(NT)[None, :]
    tokhl[:, :, 1] = np.arange(128)[:, None]
    return dict(
        toep_mult=mult.astype(np.float32), bucket=bucket,
        rope_cos=rope_cos, rope_sin=rope_sin,
        ident=np.eye(128, dtype=np.float32),
        triu=np.triu(np.ones((128, 128), np.float32), k=1),
        iota512=np.tile(np.arange(512, dtype=np.float32)[None, :], (128, 1)),
        tokhl=tokhl,
    )


def make_in_maps(inputs):
    f = lambda a: np.ascontiguousarray(np.asarray(a, dtype=np.float32))
    st = _static_tables()
    x = f(inputs["x"])
    c = f(inputs["c"])
    w_in = f(inputs["w_in"][0])
    kr = w_in[:, 640:672]
    w_kr = np.zeros((D, 192), np.float32)
    w_kr[:, 64:96] = kr
    w_kr[:, 96 + 64:96 + 80] = kr[:, 16:32]
    w_kr[:, 96 + 80:96 + 96] = kr[:, 0:16]
    w_uq = f(inputs["w_uq"][0]).reshape(384, 8, 96)
    w_uq_rot = w_uq.copy()
    w_uq_rot[:, :, 64:80] = w_uq[:, :, 80:96]
    w_uq_rot[:, :, 80:96] = w_uq[:, :, 64:80]
    w_uq2 = np.ascontiguousarray(np.stack([w_uq.reshape(384, 768), w_uq_rot.reshape(384, 768)], axis=1))
    w_ukv = f(inputs["w_ukv"][0]).reshape(256, 8, 128)
    rel_bias = f(inputs["rel_bias"])
    toep = np.ascontiguousarray(np.transpose(rel_bias[st["bucket"]], (2, 0, 1)))
    shared = dict(
        w_ada=f(inputs["w_ada"][0]), b_ada=f(inputs["b_ada"][0]).reshape(1, -1),
        w_in=w_in, w_kr=w_kr,
        g_q=np.ascontiguousarray(f(inputs["q_norm_g"][0]).reshape(3, 128).T),
        g_kv=np.ascontiguousarray(f(inputs["kv_norm_g"][0]).reshape(2, 128).T),
        w_uq2=w_uq2,
        w_ukv_k=np.ascontiguousarray(w_ukv[:, :, 0:64].reshape(256, 512)),
        w_ukv_v=np.ascontiguousarray(w_ukv[:, :, 64:128].reshape(256, 512)),
        rope_cos=st["rope_cos"], rope_sin=st["rope_sin"],
        relb_toep=toep, toep_mult=st["toep_mult"],
        w_out=f(inputs["w_out"][0]), ln1_g=f(inputs["ln1_g"][0]).reshape(1, -1), ln1_b=f(inputs["ln1_b"][0]).reshape(1, -1),
        w_router=f(inputs["w_router"][0]),
        w_gate=f(inputs["w_gate"][0]), w_up=f(inputs["w_up"][0]), w_down=f(inputs["w_down"][0]),
        ln2_g=f(inputs["ln2_g"][0]).reshape(1, -1), ln2_b=f(inputs["ln2_b"][0]).reshape(1, -1),
        ident=st["ident"], triu=st["triu"], iota512=st["iota512"], tokhl=st["tokhl"],
    )
    maps = []
    for b in range(x.shape[0]):
        m = dict(shared)
        m["x"] = x[b]
        m["c_fm"] = np.ascontiguousarray(c[b].reshape(8, 128).T)
        maps.append(m)
    return maps


def kernel(**inputs):
    maps = make_in_maps(inputs)
    nc = build_program()
    res = run_bass_kernel_spmd(nc, maps, core_ids=list(range(len(maps))))
    return np.stack([np.asarray(r["out"], dtype=np.float32) for r in res.results], axis=0)
```
